# Optimizing a Trainium2 kernel written in Bass

```python
import jax, jax.numpy as jnp
from jax import lax
import numpy as np

D_MODEL = 1024
BATCH = 4
SEQ = 4096
DEPTH = 1

CTX_LEN = 256
GRID_W = 64
N_MOD = 6
EPS = 1e-6

GLA_HEADS = 4
GLA_DK = 64
GLA_DV = 128
GLA_LR = 16
GLA_TAU = 16.0
GLA_CHUNK = 64

ML_HEADS = 4
ML_DH = 128
ML_CHUNK = 64
CONV_K = 3

N_GROUPS = 4
EXP_PER_GROUP = 8
N_EXPERTS = N_GROUPS * EXP_PER_GROUP
TOP_K = 2
D_EXPERT = 512
MOE_BLOCK = 128

GLA_QK_W = GLA_HEADS * GLA_DK
GLA_V_W = GLA_HEADS * GLA_DV
ML_W = ML_HEADS * ML_DH
MIX_W = GLA_V_W + ML_W

IN_SPLITS = (GLA_QK_W, GLA_QK_W, GLA_V_W, GLA_V_W, 2 * GLA_LR, 2 * ML_W, ML_W, ML_W, 2 * ML_HEADS, 2 * ML_HEADS)
IN_W = sum(IN_SPLITS)

kernel_name = "hybrid_gla_mlstm_hmoe_ctxprefix_layer"


def rmsnorm(x, g):
    xf = x.astype(jnp.float32)
    y = xf * lax.rsqrt(jnp.mean(xf * xf, axis=-1, keepdims=True) + EPS)
    return (y * g.astype(jnp.float32)).astype(x.dtype)


def modulate(h, shift, scale):
    return h * (1 + scale) + shift


def to_heads(t, n_heads):
    b, l, w = t.shape
    return t.reshape(b, l, n_heads, w // n_heads).transpose(0, 2, 1, 3)


def from_heads(t):
    b, h, l, d = t.shape
    return t.transpose(0, 2, 1, 3).reshape(b, l, h * d)


def head_rmsnorm(t, g):
    y = t * lax.rsqrt(jnp.mean(t * t, axis=-1, keepdims=True) + EPS)
    return from_heads(y) * g.astype(jnp.float32)


def to_chunks(t, c):
    b, h, l = t.shape[:3]
    return jnp.moveaxis(t.reshape((b, h, l // c, c) + t.shape[3:]), 2, 0)


def from_chunks(t):
    n, b, h, c = t.shape[:4]
    return jnp.moveaxis(t, 0, 2).reshape((b, h, n * c) + t.shape[4:])


def flip_time(t):
    return jnp.flip(t, axis=2)


def gla_chunked(q, k, v, log_a, s0):
    mask = jnp.tril(jnp.ones((GLA_CHUNK, GLA_CHUNK), dtype=bool))

    def step(s, inp):
        qc, kc, vc, gc = inp
        b = jnp.cumsum(gc, axis=2)
        q_t = qc * jnp.exp(b)
        k_t = kc * jnp.exp(-b)
        att = jnp.where(mask, jnp.einsum('bhtd,bhsd->bhts', q_t, k_t), 0.0)
        o = jnp.einsum('bhts,bhsv->bhtv', att, vc) + jnp.einsum('bhtd,bhdv->bhtv', q_t, s)
        b_end = b[:, :, -1]
        s_new = jnp.exp(b_end)[..., None] * s + jnp.einsum(
            'bhsd,bhsv->bhdv', kc * jnp.exp(b_end[:, :, None] - b), vc)
        return s_new, o

    s_fin, o = lax.scan(step, s0, tuple(to_chunks(t, GLA_CHUNK) for t in (q, k, v, log_a)))
    return from_chunks(o), s_fin


def mlstm_chunked(q, k, v, ig, lf, state0):
    mask = jnp.tril(jnp.ones((ML_CHUNK, ML_CHUNK), dtype=bool))

    def step(state, inp):
        s, n, m = state
        qc, kc, vc, ic, fc = inp
        a = jnp.cumsum(fc, axis=-1)
        dmat = jnp.where(mask, a[..., :, None] - a[..., None, :] + ic[..., None, :], -jnp.inf)
        inter = a + m[..., None]
        m_t = jnp.maximum(inter, jnp.max(dmat, axis=-1))
        w_inter = jnp.exp(inter - m_t)
        qk = jnp.einsum('bhtd,bhsd->bhts', qc, kc) * jnp.exp(dmat - m_t[..., None])
        num = jnp.einsum('bhts,bhsv->bhtv', qk, vc) + w_inter[..., None] * jnp.einsum('bhtd,bhdv->bhtv', qc, s)
        den = jnp.sum(qk, axis=-1) + w_inter * jnp.einsum('bhtd,bhd->bht', qc, n)
        h = num / jnp.maximum(jnp.abs(den), jnp.exp(-m_t))[..., None]
        a_end = a[..., -1]
        g = a_end[..., None] - a + ic
        m_new = jnp.maximum(a_end + m, jnp.max(g, axis=-1))
        decay = jnp.exp(a_end + m - m_new)
        kw = kc * jnp.exp(g - m_new[..., None])[..., None]
        s_new = decay[..., None, None] * s + jnp.einsum('bhsd,bhsv->bhdv', kw, vc)
        n_new = decay[..., None] * n + jnp.sum(kw, axis=2)
        return (s_new, n_new, m_new), h

    state_fin, h = lax.scan(step, state0, tuple(to_chunks(t, ML_CHUNK) for t in (q, k, v, ig, lf)))
    return from_chunks(h), state_fin


def bidir(scan_fn, ctx_dirs, lat_dirs, init):
    outs_c, outs_l = [], []
    for direction in range(2):
        fl = flip_time if direction == 1 else (lambda t: t)
        oc, st = scan_fn(*[fl(t) for t in ctx_dirs[direction]], init)
        ol, _ = scan_fn(*[fl(t) for t in lat_dirs[direction]], st)
        outs_c.append(fl(oc))
        outs_l.append(fl(ol))
    return outs_c[0] + outs_c[1], outs_l[0] + outs_l[1]


def grid_conv(t, w, b, grid_w):
    bsz, l, ch = t.shape
    rows = l // grid_w
    img = t.reshape(bsz, rows, grid_w, ch)
    y = lax.conv_general_dilated(img, w.astype(t.dtype), (1, 1), 'SAME',
                                 dimension_numbers=('NHWC', 'HWIO', 'NHWC'), feature_group_count=ch)
    return y.reshape(bsz, l, ch) + b


def prepare_stream(z, grid_w, gla_up_w, gla_up_b, conv_w, conv_b, ml_i_b, ml_f_b):
    bsz, l, _ = z.shape
    zf = z.astype(jnp.float32)
    idx = np.cumsum(IN_SPLITS)[:-1].tolist()
    gq, gk, gv, gg, glr, mqk, mv, mo, mi, mf = jnp.split(zf, idx, axis=-1)
    gla_q = to_heads(gq, GLA_HEADS) * (GLA_DK ** -0.5)
    gla_k = to_heads(gk, GLA_HEADS)
    gla_v = to_heads(gv, GLA_HEADS)
    gate_logits = jnp.einsum('blrk,rkn->blrn', glr.reshape(bsz, l, 2, GLA_LR), gla_up_w) + gla_up_b
    log_a = jax.nn.log_sigmoid(gate_logits) / GLA_TAU
    gla_dirs = tuple((gla_q, gla_k, gla_v, to_heads(log_a[:, :, d], GLA_HEADS)) for d in range(2))
    qk = jax.nn.silu(grid_conv(mqk, conv_w, conv_b, grid_w))
    mq, mk = jnp.split(qk, 2, axis=-1)
    ml_q = to_heads(mq, ML_HEADS)
    ml_k = to_heads(mk, ML_HEADS) * (ML_DH ** -0.5)
    ml_v = to_heads(mv, ML_HEADS)
    ig = (mi.reshape(bsz, l, 2, ML_HEADS) + ml_i_b).transpose(2, 0, 3, 1)
    lf = jax.nn.log_sigmoid(mf.reshape(bsz, l, 2, ML_HEADS) + ml_f_b).transpose(2, 0, 3, 1)
    ml_dirs = tuple((ml_q, ml_k, ml_v, ig[d], lf[d]) for d in range(2))
    return gla_dirs, ml_dirs, gg, mo


def mixer_output(gla_h, ml_h, gg, mo, gla_norm_g, ml_norm_g, w_out, dtype):
    gla_o = head_rmsnorm(gla_h, gla_norm_g) * jax.nn.silu(gg)
    ml_o = jax.nn.sigmoid(mo) * head_rmsnorm(ml_h, ml_norm_g)
    return jnp.concatenate([gla_o, ml_o], axis=-1).astype(dtype) @ w_out


def hier_moe(h, rg_w, rg_b, re_w, re_b, e_w_in, e_w_out):
    bsz, l, d = h.shape
    t = bsz * l
    tok = h.reshape(t, d)
    g_logits = (tok @ rg_w + rg_b).astype(jnp.float32)
    p_group = jax.nn.softmax(g_logits, axis=-1)
    g_idx = jnp.argmax(g_logits, axis=-1)
    g_w = jnp.take_along_axis(p_group, g_idx[:, None], axis=-1)
    e_logits = (tok @ re_w + re_b).astype(jnp.float32).reshape(t, N_GROUPS, EXP_PER_GROUP)
    e_in_group = jnp.take_along_axis(e_logits, g_idx[:, None, None], axis=1)[:, 0]
    top_v, top_i = lax.top_k(e_in_group, TOP_K)
    weights = g_w * jax.nn.softmax(top_v, axis=-1)
    expert_ids = (g_idx[:, None] * EXP_PER_GROUP + top_i).reshape(-1)
    n_assign = t * TOP_K
    order = jnp.argsort(expert_ids)
    sorted_e = expert_ids[order]
    token_of = order // TOP_K
    counts = jnp.bincount(expert_ids, length=N_EXPERTS)
    padded = (counts + MOE_BLOCK - 1) // MOE_BLOCK * MOE_BLOCK
    pad_end = jnp.cumsum(padded)
    pad_start = pad_end - padded
    start = jnp.cumsum(counts) - counts
    dest = pad_start[sorted_e] + jnp.arange(n_assign) - start[sorted_e]
    n_blocks = (n_assign + MOE_BLOCK - 1) // MOE_BLOCK + N_EXPERTS
    buf = jnp.zeros((n_blocks * MOE_BLOCK, d), tok.dtype).at[dest].set(tok[token_of])
    block_e = jnp.minimum(jnp.searchsorted(pad_end, jnp.arange(n_blocks) * MOE_BLOCK, side='right'), N_EXPERTS - 1)

    def expert_block(args):
        xb, e = args
        gate, up = jnp.split(xb @ e_w_in[e], 2, axis=-1)
        return (jax.nn.silu(gate) * up) @ e_w_out[e]

    yb = lax.map(expert_block, (buf.reshape(n_blocks, MOE_BLOCK, d), block_e)).reshape(-1, d)
    y = yb[dest] * weights.reshape(-1)[order][:, None].astype(yb.dtype)
    out = jax.ops.segment_sum(y, token_of, num_segments=t)
    return out.reshape(bsz, l, d)


def hybrid_layer(x, ctx, c, c_ctx, ada_w, ada_b, norm1_g, w_in, gla_up_w, gla_up_b, gla_norm_g,
                 ml_conv_w, ml_conv_b, ml_i_b, ml_f_b, ml_norm_g, w_out, norm2_g,
                 rg_w, rg_b, re_w, re_b, e_w_in, e_w_out, update_ctx):
    bsz = x.shape[0]
    mod_x = (jax.nn.silu(c) @ ada_w + ada_b)[:, None, :]
    mod_c = (jax.nn.silu(c_ctx) @ ada_w + ada_b)[None, None, :]
    sh1x, sc1x, g1x, sh2x, sc2x, g2x = jnp.split(mod_x, N_MOD, axis=-1)
    sh1c, sc1c, g1c, sh2c, sc2c, g2c = jnp.split(mod_c, N_MOD, axis=-1)

    zx = modulate(rmsnorm(x, norm1_g), sh1x, sc1x) @ w_in
    zc = modulate(rmsnorm(ctx, norm1_g), sh1c, sc1c) @ w_in
    gla_c, ml_c, gg_c, mo_c = prepare_stream(zc, zc.shape[1], gla_up_w, gla_up_b, ml_conv_w, ml_conv_b, ml_i_b, ml_f_b)
    gla_x, ml_x, gg_x, mo_x = prepare_stream(zx, GRID_W, gla_up_w, gla_up_b, ml_conv_w, ml_conv_b, ml_i_b, ml_f_b)
    gla_init = jnp.zeros((bsz, GLA_HEADS, GLA_DK, GLA_DV), jnp.float32)
    ml_init = (jnp.zeros((bsz, ML_HEADS, ML_DH, ML_DH), jnp.float32),
               jnp.zeros((bsz, ML_HEADS, ML_DH), jnp.float32),
               jnp.zeros((bsz, ML_HEADS), jnp.float32))
    gla_hc, gla_hx = bidir(gla_chunked, gla_c, gla_x, gla_init)
    ml_hc, ml_hx = bidir(mlstm_chunked, ml_c, ml_x, ml_init)
    x = x + g1x * mixer_output(gla_hx, ml_hx, gg_x, mo_x, gla_norm_g, ml_norm_g, w_out, x.dtype)

    x = x + g2x * hier_moe(modulate(rmsnorm(x, norm2_g), sh2x, sc2x), rg_w, rg_b, re_w, re_b, e_w_in, e_w_out)

    if update_ctx:
        ctx = ctx + g1c * mixer_output(gla_hc, ml_hc, gg_c, mo_c, gla_norm_g, ml_norm_g, w_out, ctx.dtype)
        ctx = ctx + g2c * hier_moe(modulate(rmsnorm(ctx, norm2_g), sh2c, sc2c), rg_w, rg_b, re_w, re_b, e_w_in, e_w_out)
    return x, ctx


def setup_inputs(seed: int = 0) -> dict:
    key = jax.random.key(seed)
    ks = jax.random.split(key, 26)
    nrm = lambda k, shape, s: jax.random.normal(k, shape, jnp.float32) * s
    L = DEPTH
    return {
        "x": nrm(ks[0], (BATCH, SEQ, D_MODEL), 1.0),
        "c": nrm(ks[1], (BATCH, D_MODEL), 1.0),
        "ctx": nrm(ks[2], (BATCH, CTX_LEN, D_MODEL), 1.0),
        "c_ctx": nrm(ks[3], (D_MODEL,), 1.0),
        "ada_w": nrm(ks[4], (L, D_MODEL, N_MOD * D_MODEL), D_MODEL ** -0.5),
        "ada_b": nrm(ks[5], (L, N_MOD * D_MODEL), 0.02),
        "norm1_g": 1.0 + nrm(ks[6], (L, D_MODEL), 0.02),
        "w_in": nrm(ks[7], (L, D_MODEL, IN_W), D_MODEL ** -0.5),
        "gla_up_w": nrm(ks[8], (L, 2, GLA_LR, GLA_QK_W), GLA_LR ** -0.5),
        "gla_up_b": nrm(ks[9], (L, 2, GLA_QK_W), 0.1),
        "gla_norm_g": 1.0 + nrm(ks[10], (L, GLA_V_W), 0.02),
        "ml_conv_w": nrm(ks[11], (L, CONV_K, CONV_K, 1, 2 * ML_W), 1.0 / CONV_K),
        "ml_conv_b": nrm(ks[12], (L, 2 * ML_W), 0.02),
        "ml_i_b": nrm(ks[13], (L, 2, ML_HEADS), 0.1),
        "ml_f_b": 3.0 + nrm(ks[14], (L, 2, ML_HEADS), 0.5),
        "ml_norm_g": 1.0 + nrm(ks[15], (L, ML_W), 0.02),
        "w_out": nrm(ks[16], (L, MIX_W, D_MODEL), MIX_W ** -0.5),
        "norm2_g": 1.0 + nrm(ks[17], (L, D_MODEL), 0.02),
        "router_group_w": nrm(ks[18], (L, D_MODEL, N_GROUPS), D_MODEL ** -0.5),
        "router_group_b": nrm(ks[19], (L, N_GROUPS), 0.01),
        "router_expert_w": nrm(ks[20], (L, D_MODEL, N_EXPERTS), D_MODEL ** -0.5),
        "router_expert_b": nrm(ks[21], (L, N_EXPERTS), 0.01),
        "expert_w_in": nrm(ks[22], (L, N_EXPERTS, D_MODEL, 2 * D_EXPERT), D_MODEL ** -0.5),
        "expert_w_out": nrm(ks[23], (L, N_EXPERTS, D_EXPERT, D_MODEL), D_EXPERT ** -0.5),
        "final_norm_g": 1.0 + nrm(ks[24], (D_MODEL,), 0.02),
    }


def reference(x, c, ctx, c_ctx, ada_w, ada_b, norm1_g, w_in, gla_up_w, gla_up_b, gla_norm_g,
              ml_conv_w, ml_conv_b, ml_i_b, ml_f_b, ml_norm_g, w_out, norm2_g,
              router_group_w, router_group_b, router_expert_w, router_expert_b,
              expert_w_in, expert_w_out, final_norm_g):
    for layer in range(DEPTH):
        x, ctx = hybrid_layer(
            x, ctx, c, c_ctx, ada_w[layer], ada_b[layer], norm1_g[layer], w_in[layer],
            gla_up_w[layer], gla_up_b[layer], gla_norm_g[layer],
            ml_conv_w[layer], ml_conv_b[layer], ml_i_b[layer], ml_f_b[layer], ml_norm_g[layer],
            w_out[layer], norm2_g[layer],
            router_group_w[layer], router_group_b[layer], router_expert_w[layer], router_expert_b[layer],
            expert_w_in[layer], expert_w_out[layer], update_ctx=(layer < DEPTH - 1))
    return rmsnorm(x, final_norm_g)
```

```python
import numpy as np
import concourse.bass as bass
import concourse.mybir as mybir
from concourse.bass_utils import run_bass_kernel_spmd

F32 = mybir.dt.float32
BF16 = mybir.dt.bfloat16
AF = mybir.ActivationFunctionType
ALU = mybir.AluOpType
AX = mybir.AxisListType

D = 1024
SEQ = 4096
NLOC = 2048
CTX = 256
INW = 3632
NE = 32
DEXP = 512
EPS = 1e-6
NBLK = 9
C_GQ, C_GK, C_GV, C_GG, C_LR, C_MQ, C_MK, C_MV, C_MO, C_MI = 0, 256, 512, 1024, 1536, 1568, 2080, 2592, 3104, 3616

V_N1G, V_N2G, V_UPB, V_CONVB, V_CONVW, V_GNG, V_MNG = 0, 8, 16, 20, 28, 100, 104
NV = 108
R_GATEB, R_FNG, R_RB = 0, 16, 16 + 1024
R_N2G = 16 + 1024 + 36
NR = 16 + 1024 + 36 + 1024
I32 = mybir.dt.int32


class Tok:
    __slots__ = ("w", "r")

    def __init__(self):
        self.w = {}
        self.r = {}


class Sched:
    NDMA = 6

    def __init__(self, nc):
        self.nc = nc
        self.eng = {"pe": nc.tensor, "dve": nc.vector, "act": nc.scalar, "pool": nc.gpsimd, "sp": nc.sync}
        self.sem = {}
        self.cnt = {}
        self.waited = {k: {} for k in self.eng}
        self._cms = []
        for k in ["pe", "dve", "act", "pool"]:
            self._mk(k)
        self.dq = {}
        self.nq = {"sp": 8, "pool": 16, "act": 2}
        for q in ["sp", "pool", "act"]:
            names = []
            for i in range(self.nq[q]):
                n = "d%s%d" % (q, i)
                self._mk(n)
                names.append(n)
            self.dq[q] = [names, 0]
        self.ninstr = 0

    def _mk(self, k):
        cm = self.nc.semaphore("s_" + k)
        self.sem[k] = cm.__enter__()
        self._cms.append(cm)
        self.cnt[k] = 0

    def _wait(self, e, key, val):
        if self.waited[e].get(key, 0) >= val:
            return
        self.eng[e].wait_ge(self.sem[key], val)
        self.waited[e][key] = val

    def _deps(self, e, reads, writes):
        for t in reads:
            for k, v in t.w.items():
                if not (k == "pe" and e == "pe"):
                    self._wait(e, k, v)
        for t in writes:
            for k, v in t.w.items():
                if not (k == "pe" and e == "pe"):
                    self._wait(e, k, v)
            for k, v in t.r.items():
                if k != e:
                    self._wait(e, k, v)

    def _mark(self, key, val, reads, writes, wadd):
        for t in reads:
            if t.r.get(key, 0) < val:
                t.r[key] = val
        for t in writes:
            t.w = {key: val}
            t.r = {}
        for t in wadd:
            t.w[key] = val

    def op(self, e, fn, reads=(), writes=(), wadd=()):
        self._deps(e, reads, writes)
        for t in wadd:
            for k, v in t.r.items():
                if k != e:
                    self._wait(e, k, v)
        ins = fn(self.eng[e])
        self.cnt[e] += 1
        ins.then_inc(self.sem[e], 1)
        self._mark(e, self.cnt[e], reads, writes, wadd)
        self.ninstr += 1
        return ins

    def dma(self, q, out, in_, reads=(), writes=(), wadd=(), **kw):
        names, idx = self.dq[q]
        key = names[idx % len(names)]
        self.dq[q][1] = idx + 1
        self._wait(q, key, self.cnt[key])
        self._deps(q, reads, writes)
        for t in wadd:
            for k, v in t.r.items():
                self._wait(q, k, v)
        ins = self.eng[q].dma_start(out=out, in_=in_, **kw)
        self.cnt[key] += 16
        ins.then_inc(self.sem[key], 16)
        self._mark(key, self.cnt[key], reads, writes, wadd)
        self.ninstr += 1
        return ins

    def idma(self, out, out_off, in_, in_off, reads=(), writes=(), wadd=(), **kw):
        q = "pool"
        names, idx = self.dq[q]
        key = names[idx % len(names)]
        self.dq[q][1] = idx + 1
        self._wait(q, key, self.cnt[key])
        self._deps(q, reads, writes)
        for t in wadd:
            for k, v in t.r.items():
                self._wait(q, k, v)
        ins = self.eng[q].indirect_dma_start(out=out, out_offset=out_off, in_=in_, in_offset=in_off, **kw)
        self.cnt[key] += 16
        ins.then_inc(self.sem[key], 16)
        self._mark(key, self.cnt[key], reads, writes, wadd)
        self.ninstr += 1
        return ins

    def barrier(self):
        import os
        if os.environ.get('NOBAR'):
            return
        for e in self.eng:
            if e == 'pool' and os.environ.get('NOPOOLBAR'):
                continue
            for k in self.sem:
                if self.cnt[k] > 0 and k != e:
                    self._wait(e, k, self.cnt[k])

    def finish(self, toks, e="sp"):
        for t in toks:
            for k, v in t.w.items():
                self._wait(e, k, v)


class Ring:
    def __init__(self, items):
        self.items = items
        self.toks = [Tok() for _ in items]
        self.i = 0

    def next(self):
        j = self.i % len(self.items)
        self.i += 1
        return self.items[j], self.toks[j]


def build(dbg=0):
    import os
    dbg = int(os.environ.get("KSTOP", dbg))
    nc = bass.Bass("TRN2", target_bir_lowering=False)
    import os
    scratch_kind = "ExternalOutput" if (dbg or os.environ.get("SCR_EXT")) else "Internal"

    used_inputs = []

    def din(name, shape, dt=F32, need=0):
        if dbg and dbg < need:
            return None
        used_inputs.append(name)
        return nc.dram_tensor(name, list(shape), dt, kind="ExternalInput").ap()

    dump_toks = []

    def dump(name, ap, shape, toks, dt=F32):
        if not dbg:
            return
        dd = nc.dram_tensor("dbg_" + name, list(shape), dt, kind="ExternalOutput").ap()
        t = Tok()
        S.dma("sp", dd, ap, reads=toks, writes=[t])
        dump_toks.append(t)

    def dscr(name, shape, dt):
        return nc.dram_tensor(name, list(shape), dt, kind=scratch_kind).ap()

    x_d = din("x", [SEQ, D])
    ctx_d = din("ctx", [CTX, D])
    cvec_d = din("cvec", [128, 16])
    adaw_d = din("ada_w", [D, 6 * D])
    adab_d = din("ada_b", [1, 6 * D])
    vecs_d = din("vecs", [128, NV])
    rows_d = din("rows", [1, NR])
    win_d = din("w_in", [D, INW])
    upw_d = din("up_w", [2, 16, 256])
    wout_d = din("w_out", [D, D])
    rw_d = din("router_w", [D, 36])
    ewi_d = din("e_w_in", [NE, D, 2 * DEXP], need=8)
    ewo_d = din("e_w_out", [NE, DEXP, D], need=8)
    consts_d = din("consts", [128, 1024])
    out_d = nc.dram_tensor("out", [NLOC, D], F32, kind="ExternalOutput").ap()

    xnT_d = dscr("xnT_s", [NBLK, 128, 8 * 512], BF16)
    gqk_d = dscr("gqk_s", [NBLK, 128, 4 * 512], BF16)
    lr_d = dscr("lr_s", [NBLK, 16, 2 * 512], F32)
    sgg_d = dscr("sgg_s", [4, 128, 4 * 512], BF16)
    smo_d = dscr("smo_s", [4, 128, 4 * 512], BF16)
    mpre_d = dscr("mpre_s", [NBLK, 128, 8 * 512], BF16)
    gv_d = dscr("gv_s", [NBLK, 128, 4 * 512], BF16)
    mv_d = dscr("mv_s", [NBLK, 128, 4 * 512], BF16)
    gates_d = dscr("gates_s", [NBLK, 128, 4 * 16], F32)
    mqk_d = dscr("mqk_s", [NBLK, 128, 8 * 512], BF16)

    S = Sched(nc)
    es = []
    uid = [0]

    def sb(name, shape, dt):
        uid[0] += 1
        cm = nc.sbuf_tensor("sb%d_%s" % (uid[0], name), list(shape), dt)
        t = cm.__enter__()
        es.append(cm)
        return t

    def ps(name, shape, dt):
        uid[0] += 1
        cm = nc.psum_tensor("ps%d_%s" % (uid[0], name), list(shape), dt)
        t = cm.__enter__()
        es.append(cm)
        return t

    def release(n0):
        S.barrier()
        while len(es) > n0:
            es.pop().__exit__(None, None, None)

    consts = sb("consts", [128, 1024], F32)
    vecs = sb("vecs", [128, NV], F32)
    t_const = Tok()
    S.dma("sp", consts[:], consts_d[:, :], writes=[t_const])
    S.dma("sp", vecs[:], vecs_d[:, :], wadd=[t_const])
    ident_f = consts[:, 0:128]
    ones_f = consts[:, 384:512]
    cb = sb("constsb", [128, 512], BF16)
    t_cb = Tok()
    S.op("dve", lambda e: e.tensor_copy(out=cb[:], in_=consts[:, 0:512]), reads=[t_const], writes=[t_cb])
    ident_b = cb[:, 0:128]
    mask_b = [cb[:, 128:256], cb[:, 256:384]]
    ones_b = cb[:, 384:512]

    psA = [ps("psA%d" % i, [128, 512], F32) for i in range(6)]
    psB = [ps("psB%d" % i, [128, 1024], BF16) for i in range(2)]
    psA_ring = Ring(psA)
    psB_ring = Ring(psB)

    t_mod = Tok()
    A1 = sb("A1", [128, 4 * 8], F32)
    A2 = sb("A2", [128, 2 * 8], F32)
    t_A = Tok()
    g12 = sb("g12", [128, 4 * D], F32)
    t_g12 = Tok()
    idxi = sb("idxi", [128, 64 * 12], I32)
    Desti = sb("Desti", [128, 32], I32)
    Wt = sb("Wt", [128, 32], F32)
    t_idx, t_dest, t_Wt = Tok(), Tok(), Tok()

    n_keep = len(es)
    modx = sb("modx", [1, 6 * D], F32)
    modc = sb("modc", [1, 2 * D], F32)
    cvec = sb("cvec", [128, 16], F32)
    scv = sb("scv", [128, 16], F32)
    adab = sb("adab", [1, 6 * D], F32)
    t_cv, t_scv, t_adab = Tok(), Tok(), Tok()
    S.dma("sp", cvec[:], cvec_d[:, :], writes=[t_cv])
    S.dma("sp", adab[:], adab_d[:, :], writes=[t_adab])
    S.op("act", lambda e: e.activation(out=scv[:], in_=cvec[:], func=AF.Silu), reads=[t_cv], writes=[t_scv])
    adaw_v = adaw_d.rearrange("(p k) n -> p k n", k=8)
    wst = [sb("adaw%d" % i, [128, 8, 512], F32) for i in range(2)]
    wst_ring = Ring(wst)
    S.op("dve", lambda e: e.memset(modx[:], 0.0), writes=[t_mod])
    for blk in range(12):
        wt, wtok = wst_ring.next()
        S.dma("sp", wt[:], adaw_v[:, :, blk * 512:(blk + 1) * 512], writes=[wtok])
        for which in range(2):
            if which == 1 and blk >= 4:
                continue
            pt, ptok = psA_ring.next()
            for k in range(8):
                S.op("pe", lambda e, k=k, pt=pt, wt=wt, which=which: e.matmul(
                    pt[0:1, :], scv[:, which * 8 + k:which * 8 + k + 1], wt[:, k, :], start=(k == 0), stop=(k == 7)),
                    reads=[t_scv, wtok], writes=[ptok])
            dst = modx if which == 0 else modc
            S.op("dve", lambda e, pt=pt, dst=dst, blk=blk: e.tensor_tensor(
                out=dst[0:1, blk * 512:(blk + 1) * 512], in0=pt[0:1, :], in1=adab[0:1, blk * 512:(blk + 1) * 512], op=ALU.add),
                reads=[ptok, t_adab], wadd=[t_mod])
    colps, coltok = psA_ring.next()
    specs = [(modx, 1 * D), (modx, 0 * D), (modc, 1 * D), (modc, 0 * D), (modx, 4 * D), (modx, 3 * D)]
    first = True
    for si, (src, off) in enumerate(specs):
        for k in range(8):
            S.op("pe", lambda e, src=src, off=off, k=k, si=si: e.matmul(
                colps[:, si * 8 + k:si * 8 + k + 1], src[0:1, off + k * 128:off + (k + 1) * 128], ones_f[0:1, 0:1], start=True, stop=True),
                reads=[t_mod, t_const], writes=[coltok] if first else (), wadd=() if first else [coltok])
            first = False
    for (dst, c0, g0, s_sc, s_sh) in [(A1, 0, V_N1G, 0, 1), (A1, 16, V_N1G, 2, 3), (A2, 0, V_N2G, 4, 5)]:
        S.op("dve", lambda e, dst=dst, c0=c0, g0=g0, s_sc=s_sc: e.scalar_tensor_tensor(
            out=dst[:, c0:c0 + 8], in0=colps[:, s_sc * 8:s_sc * 8 + 8], scalar=1.0, in1=vecs[:, g0:g0 + 8], op0=ALU.add, op1=ALU.mult),
            reads=[coltok, t_const], wadd=[t_A])
        S.op("dve", lambda e, dst=dst, c0=c0, s_sh=s_sh: e.tensor_copy(out=dst[:, c0 + 8:c0 + 16], in_=colps[:, s_sh * 8:s_sh * 8 + 8]),
             reads=[coltok], wadd=[t_A])
    for gi, off in enumerate([2 * D, 5 * D]):
        for hf in range(2):
            pt, ptok = psA_ring.next()
            S.op("pe", lambda e, pt=pt, off=off, hf=hf: e.matmul(pt[:, :], ones_f[0:1, :], modx[0:1, off + hf * 512:off + (hf + 1) * 512], start=True, stop=True),
                 reads=[t_mod, t_const], writes=[ptok])
            S.op("act", lambda e, pt=pt, gi=gi, hf=hf: e.copy(out=g12[:, gi * D + hf * 512:gi * D + (hf + 1) * 512], in_=pt[:, :]),
                 reads=[ptok], wadd=[t_g12])
    n2gbc = sb("n2gbc", [128, D], F32)
    t_n2g = Tok()
    S.dma("sp", n2gbc[:], rows_d[0:1, R_N2G:R_N2G + D].partition_broadcast(128), writes=[t_n2g])
    for gi, off in ((2, 4 * D), (3, 3 * D)):
        for hf in range(2):
            pt, ptok = psA_ring.next()
            S.op("pe", lambda e, pt=pt, off=off, hf=hf: e.matmul(pt[:, :], ones_f[0:1, :], modx[0:1, off + hf * 512:off + (hf + 1) * 512], start=True, stop=True),
                 reads=[t_mod, t_const], writes=[ptok])
            dst = g12[:, gi * D + hf * 512:gi * D + (hf + 1) * 512]
            if gi == 2:
                S.op("dve", lambda e, pt=pt, dst=dst, hf=hf: e.scalar_tensor_tensor(out=dst, in0=pt[:, :], scalar=1.0, in1=n2gbc[:, hf * 512:(hf + 1) * 512], op0=ALU.add, op1=ALU.mult),
                     reads=[ptok, t_n2g], wadd=[t_g12])
            else:
                S.op("dve", lambda e, pt=pt, dst=dst: e.tensor_copy(out=dst, in_=pt[:, :]), reads=[ptok], wadd=[t_g12])
    dump("g12", g12[:, 0:2 * D], [128, 2 * D], [t_g12])
    dump("A1", A1[:], [128, 32], [t_A])
    dump("modx", modx[:], [1, 6 * D], [t_mod])
    if dbg == 1:
        S.finish(dump_toks)
        nc.used_inputs = used_inputs
        return nc
    release(n_keep)

    xt_ring = Ring([sb("xt%d" % i, [128, D], F32) for i in range(2)])
    xs_ring = Ring([sb("xs%d" % i, [128, D], BF16) for i in range(2)])
    junk = sb("junk", [128, D], F32)
    xsf_ring = Ring([sb("xsf%d" % i, [128, D], F32) for i in range(2)])
    t_junk = Tok()
    ss_ring = Ring([(sb("ssa%d" % i, [128, 1], F32), sb("ssb%d" % i, [128, 1], F32)) for i in range(4)])
    xnb_ring = Ring([sb("xnb%d" % i, [128, 8, 512], BF16) for i in range(2)])
    t_xnT = [Tok() for _ in range(NBLK)]
    import os
    for blk in range(int(os.environ.get('KLIM', NBLK))):
        ntile = 2 if blk == 0 else 4
        xnb, xnbtok = xnb_ring.next()
        xfirst = [True]

        def xw(tok=xnbtok, xfirst=xfirst):
            if xfirst[0]:
                xfirst[0] = False
                return dict(writes=[tok])
            return dict(wadd=[tok])
        for ti in range(ntile):
            if blk == 0:
                src = ctx_d[ti * 128:(ti + 1) * 128, :]
                acol = 16
            else:
                r0 = (blk - 1) * 512 + ti * 128
                src = x_d[r0:r0 + 128, :]
                acol = 0
            xt, xttok = xt_ring.next()
            S.dma("sp", xt[:], src, writes=[xttok])
            ss, sstok = ss_ring.next()
            S.op("act", lambda e, xt=xt, ss=ss: e.activation(out=junk[:], in_=xt[:], func=AF.Square, accum_out=ss[0][:, 0:1]),
                 reads=[xttok], writes=[t_junk, sstok])
            S.op("dve", lambda e, ss=ss: e.tensor_scalar(out=ss[1][:, 0:1], in0=ss[0][:, 0:1], scalar1=1.0 / D, scalar2=EPS, op0=ALU.mult, op1=ALU.add),
                 reads=[sstok], writes=[sstok])
            S.op("act", lambda e, ss=ss: e.activation(out=ss[0][:, 0:1], in_=ss[1][:, 0:1], func=AF.Ln), reads=[sstok], writes=[sstok])
            S.op("act", lambda e, ss=ss: e.activation(out=ss[1][:, 0:1], in_=ss[0][:, 0:1], func=AF.Exp, scale=-0.5), reads=[sstok], writes=[sstok])
            xs, xstok = xs_ring.next()
            xsf, xsftok = xsf_ring.next()
            S.op("dve", lambda e, xsf=xsf, xt=xt, ss=ss: e.tensor_scalar(out=xsf[:], in0=xt[:], scalar1=ss[1][:, 0:1], scalar2=None, op0=ALU.mult),
                 reads=[xttok, sstok], writes=[xsftok])
            S.op("dve", lambda e, xs=xs, xsf=xsf: e.tensor_copy(out=xs[:], in_=xsf[:]), reads=[xsftok], writes=[xstok])
            pb, pbtok = psB_ring.next()
            for k in range(8):
                S.op("pe", lambda e, pb=pb, xs=xs, k=k: e.transpose(pb[:, k * 128:(k + 1) * 128], xs[:, k * 128:(k + 1) * 128], ident_b),
                     reads=[xstok, t_cb], writes=[pbtok])
            for k in range(8):
                eng = "dve"
                if eng == "act":
                    S.op("act", lambda e, pb=pb, xnb=xnb, k=k, ti=ti, acol=acol: e.activation(
                        out=xnb[:, k, ti * 128:(ti + 1) * 128], in_=pb[:, k * 128:(k + 1) * 128], func=AF.Identity,
                        scale=A1[:, acol + k:acol + k + 1], bias=A1[:, acol + 8 + k:acol + 8 + k + 1]),
                        reads=[pbtok, t_A], **xw())
                else:
                    S.op("dve", lambda e, pb=pb, xnb=xnb, k=k, ti=ti, acol=acol: e.tensor_scalar(
                        out=xnb[:, k, ti * 128:(ti + 1) * 128], in0=pb[:, k * 128:(k + 1) * 128],
                        scalar1=A1[:, acol + k:acol + k + 1], scalar2=A1[:, acol + 8 + k:acol + 8 + k + 1], op0=ALU.mult, op1=ALU.add),
                        reads=[pbtok, t_A], **xw())
        S.dma("sp", xnT_d[blk], xnb[:].rearrange("p k n -> p (k n)"), reads=[xnbtok], writes=[t_xnT[blk]])
    release(n_keep)

    final_toks = list(t_xnT)
    if dbg == 2:
        S.finish(final_toks + dump_toks)
        nc.used_inputs = used_inputs
        return nc

    n_s2 = len(es)
    win_v = win_d.rearrange("(k p) n -> p k n", p=128)
    Wb = sb("Wb", [128, 8, INW], BF16)
    t_Wb = Tok()
    wstg = Ring([sb("wstg%d" % i, [128, 8, 227], F32) for i in range(2)])
    for pc in range(16):
        st, sttok = wstg.next()
        S.dma("sp", st[:], win_v[:, :, pc * 227:(pc + 1) * 227], writes=[sttok])
        S.op("dve", lambda e, st=st, pc=pc: e.tensor_copy(out=Wb[:, :, pc * 227:(pc + 1) * 227], in_=st[:]),
             reads=[sttok], **(dict(writes=[t_Wb]) if pc == 0 else dict(wadd=[t_Wb])))
    xin_ring = Ring([sb("xin%d" % i, [128, 8, 512], BF16) for i in range(2)])
    stg = {}
    for nm, shp, dt in [("gqk", [128, 4 * 512], BF16), ("sgg", [128, 4 * 512], BF16), ("smo", [128, 4 * 512], BF16),
                        ("mpre", [128, 8 * 512], BF16), ("gv", [128, 4 * 512], BF16), ("mv", [128, 4 * 512], BF16),
                        ("lr", [16, 2 * 512], F32), ("gates", [128, 64], F32)]:
        stg[nm] = Ring([sb("st_%s%d" % (nm, i), shp, dt) for i in range(2)])
    sig_ring = Ring([sb("sigt%d" % i, [128, 512], F32) for i in range(2)])
    t_gqk = [Tok() for _ in range(NBLK)]
    t_lr = [Tok() for _ in range(NBLK)]
    t_sgg = [Tok() for _ in range(4)]
    t_smo = [Tok() for _ in range(4)]
    t_mpre = [Tok() for _ in range(NBLK)]
    t_gv = [Tok() for _ in range(NBLK)]
    t_mv = [Tok() for _ in range(NBLK)]
    t_gates = [Tok() for _ in range(NBLK)]

    class Acc:
        def __init__(self, tok):
            self.tok = tok
            self.first = True

        def kw(self):
            if self.first:
                self.first = False
                return dict(writes=[self.tok])
            return dict(wadd=[self.tok])

    for blk in range(int(os.environ.get('KLIM2', NBLK))):
        N = 256 if blk == 0 else 512
        is_ctx, is_loc, is_far = blk == 0, 1 <= blk <= 4, blk >= 5
        xin, xintok = xin_ring.next()
        S.dma("sp", xin[:].rearrange("p k n -> p (k n)"), xnT_d[blk], reads=[t_xnT[blk]], writes=[xintok])
        cur = {nm: stg[nm].next() for nm in stg}
        acc = {nm: Acc(cur[nm][1]) for nm in stg}

        def cm_tile(col0, M, N=N, xin=xin, xintok=xintok):
            pt, ptok = psA_ring.next()
            for k in range(8):
                S.op("pe", lambda e, k=k, pt=pt: e.matmul(pt[0:M, 0:N], Wb[:, k, col0:col0 + M], xin[:, k, 0:N], start=(k == 0), stop=(k == 7)),
                     reads=[t_Wb, xintok], writes=[ptok])
            return pt, ptok

        def evac(nm, dst, pt, ptok, M, scale=None, N=N):
            if scale is None:
                S.op("dve", lambda e: e.tensor_copy(out=dst, in_=pt[0:M, 0:N]), reads=[ptok], **acc[nm].kw())
            else:
                S.op("dve", lambda e: e.tensor_scalar(out=dst, in0=pt[0:M, 0:N], scalar1=scale, scalar2=None, op0=ALU.mult),
                     reads=[ptok], **acc[nm].kw())

        def evac_sig(nm, dst, pt, ptok, silu, N=N):
            sg, sgtok = sig_ring.next()
            S.op("act", lambda e: e.activation(out=sg[:, 0:N], in_=pt[:, 0:N], func=AF.Exp, scale=-1.0), reads=[ptok], writes=[sgtok])
            S.op("dve", lambda e: e.tensor_scalar(out=sg[:, 0:N], in0=sg[:, 0:N], scalar1=1.0, scalar2=None, op0=ALU.add), reads=[sgtok], writes=[sgtok])
            S.op("dve", lambda e: e.reciprocal(out=sg[:, 0:N], in_=sg[:, 0:N]), reads=[sgtok], writes=[sgtok])
            if silu:
                S.op("dve", lambda e: e.tensor_tensor(out=dst, in0=pt[:, 0:N], in1=sg[:, 0:N], op=ALU.mult), reads=[ptok, sgtok], **acc[nm].kw())
            else:
                S.op("dve", lambda e: e.tensor_copy(out=dst, in_=sg[:, 0:N]), reads=[sgtok], **acc[nm].kw())

        gq_st, lr_st, sgg_st, smo_st = cur["gqk"][0], cur["lr"][0], cur["sgg"][0], cur["smo"][0]
        mp_st, gv_st, mv_st, ga_st = cur["mpre"][0], cur["gv"][0], cur["mv"][0], cur["gates"][0]
        for j in range(4):
            if j < 2 and not is_loc:
                continue
            col0 = C_GQ + j * 128 if j < 2 else C_GK + (j - 2) * 128
            pt, ptok = cm_tile(col0, 128)
            evac("gqk", gq_st[:, j * 512:j * 512 + N], pt, ptok, 128, scale=(0.125 if j < 2 else None))
        for dd in range(2):
            if is_far and dd == 0:
                continue
            pt, ptok = cm_tile(C_LR + dd * 16, 16)
            evac("lr", lr_st[0:16, dd * 512:dd * 512 + N], pt, ptok, 16)
        for j in range(8):
            if j < 4 and not (is_loc or blk == 5):
                continue
            col0 = C_MQ + j * 128 if j < 4 else C_MK + (j - 4) * 128
            pt, ptok = cm_tile(col0, 128)
            evac("mpre", mp_st[:, j * 512:j * 512 + N], pt, ptok, 128)
        if is_loc:
            for j in range(4):
                pt, ptok = cm_tile(C_GG + j * 128, 128)
                evac_sig("sgg", sgg_st[:, j * 512:(j + 1) * 512], pt, ptok, True)
            for j in range(4):
                pt, ptok = cm_tile(C_MO + j * 128, 128)
                evac_sig("smo", smo_st[:, j * 512:(j + 1) * 512], pt, ptok, False)
        for c in range(N // 128):
            for nm, col0, ncol, st_ in [("gv", C_GV, 512, gv_st), ("mv", C_MV, 512, mv_st), ("gates", C_MI, 16, ga_st)]:
                pt, ptok = psA_ring.next()
                for k in range(8):
                    S.op("pe", lambda e, k=k, pt=pt, c=c, col0=col0, ncol=ncol: e.matmul(
                        pt[:, 0:ncol], xin[:, k, c * 128:(c + 1) * 128], Wb[:, k, col0:col0 + ncol], start=(k == 0), stop=(k == 7)),
                        reads=[t_Wb, xintok], writes=[ptok])
                S.op("dve", lambda e, pt=pt, st_=st_, c=c, ncol=ncol: e.tensor_copy(out=st_[:, c * ncol:(c + 1) * ncol], in_=pt[:, 0:ncol]),
                     reads=[ptok], **acc[nm].kw())
        S.dma("sp", gqk_d[blk], gq_st[:], reads=[cur["gqk"][1]], writes=[t_gqk[blk]])
        S.dma("sp", lr_d[blk], lr_st[:], reads=[cur["lr"][1]], writes=[t_lr[blk]])
        S.dma("sp", mpre_d[blk], mp_st[:], reads=[cur["mpre"][1]], writes=[t_mpre[blk]])
        S.dma("sp", gv_d[blk], gv_st[:], reads=[cur["gv"][1]], writes=[t_gv[blk]])
        S.dma("sp", mv_d[blk], mv_st[:], reads=[cur["mv"][1]], writes=[t_mv[blk]])
        S.dma("sp", gates_d[blk], ga_st[:], reads=[cur["gates"][1]], writes=[t_gates[blk]])
        if is_loc:
            S.dma("sp", sgg_d[blk - 1], sgg_st[:], reads=[cur["sgg"][1]], writes=[t_sgg[blk - 1]])
            S.dma("sp", smo_d[blk - 1], smo_st[:], reads=[cur["smo"][1]], writes=[t_smo[blk - 1]])
    release(n_s2)
    if dbg == 3:
        S.finish(t_gqk + t_lr + t_sgg + t_smo + t_mpre + t_gv + t_mv + t_gates + dump_toks)
        nc.used_inputs = used_inputs
        return nc

    n_s3 = len(es)
    t_mqk = [Tok() for _ in range(NBLK)]
    Pbuf = sb("convP", [128, 66 * 64], BF16)
    accb = sb("convacc", [128, 64 * 64], F32)
    eb = sb("conve", [128, 64 * 64], F32)
    outb = sb("convout", [128, 64 * 64], BF16)
    tP, tacc, teb, toutb = Tok(), Tok(), Tok(), Tok()

    def conv_tile(j, R, Wd, srcs, dsts, taps_i):
        n_el = R * Wd
        S.op("dve", lambda e: e.memset(Pbuf[:, 0:Wd], 0.0), writes=[tP])
        S.op("dve", lambda e: e.memset(Pbuf[:, Wd + n_el:2 * Wd + n_el], 0.0), wadd=[tP])
        for (ap, tok, off, n) in srcs:
            S.dma("sp", Pbuf[:, Wd + off:Wd + off + n], ap, reads=[tok], wadd=[tP])
        wcol = lambda i, jj: vecs[:, V_CONVW + j * 9 + i * 3 + jj:V_CONVW + j * 9 + i * 3 + jj + 1]
        bcol = vecs[:, V_CONVB + j:V_CONVB + j + 1]
        S.op("dve", lambda e: e.tensor_scalar(out=accb[:, 0:n_el], in0=Pbuf[:, Wd:Wd + n_el], scalar1=wcol(1, 1), scalar2=bcol, op0=ALU.mult, op1=ALU.add),
             reads=[tP, t_const], writes=[tacc])
        P3 = Pbuf[:, 0:(R + 2) * Wd].rearrange("p (r c) -> p r c", c=Wd)
        A3 = accb[:, 0:n_el].rearrange("p (r c) -> p r c", c=Wd)
        for i in taps_i:
            for jj in range(3):
                if i == 1 and jj == 1:
                    continue
                oc0, oc1 = (1, Wd) if jj == 0 else ((0, Wd) if jj == 1 else (0, Wd - 1))
                ic0 = oc0 + jj - 1
                S.op("dve", lambda e, i=i, jj=jj, oc0=oc0, oc1=oc1, ic0=ic0: e.scalar_tensor_tensor(
                    out=A3[:, :, oc0:oc1], in0=P3[:, i:i + R, ic0:ic0 + (oc1 - oc0)], scalar=wcol(i, jj), in1=A3[:, :, oc0:oc1], op0=ALU.mult, op1=ALU.add),
                    reads=[tP, t_const, tacc], writes=[tacc])
        S.op("act", lambda e: e.activation(out=eb[:, 0:n_el], in_=accb[:, 0:n_el], func=AF.Exp, scale=-1.0), reads=[tacc], writes=[teb])
        S.op("dve", lambda e: e.tensor_scalar(out=eb[:, 0:n_el], in0=eb[:, 0:n_el], scalar1=1.0, scalar2=None, op0=ALU.add), reads=[teb], writes=[teb])
        S.op("dve", lambda e: e.reciprocal(out=eb[:, 0:n_el], in_=eb[:, 0:n_el]), reads=[teb], writes=[teb])
        sc = 1.0 if j < 4 else 128.0 ** -0.5
        S.op("dve", lambda e: e.scalar_tensor_tensor(out=outb[:, 0:n_el], in0=accb[:, 0:n_el], scalar=sc, in1=eb[:, 0:n_el], op0=ALU.mult, op1=ALU.mult),
             reads=[tacc, teb], writes=[toutb])
        for (ap, tok, off, n) in dsts:
            S.dma("sp", ap, outb[:, off:off + n], reads=[toutb], wadd=[tok])

    Ppad = sb("convPp", [128, 2 + 66 * 66], BF16)
    cstg = sb("convstg", [128, 64 * 64], BF16)
    accp = sb("convaccp", [128, 64 * 66], F32)
    ebp = sb("convebp", [128, 64 * 66], F32)
    dw_ring = Ring([sb("convdw%d" % i, [128, 9, 128], BF16) for i in range(2)])
    tPp, tcstg, taccp, tebp = Tok(), Tok(), Tok(), Tok()
    last_R = [None]

    def conv_tile_pe(j, R, srcs, dsts):
        n_el, npad = R * 64, R * 66
        first = True
        for (ap, tok, off, n) in srcs:
            S.dma("sp", cstg[:, off:off + n], ap, reads=[tok], **(dict(writes=[tcstg]) if first else dict(wadd=[tcstg])))
            first = False
        if last_R[0] != R:
            S.op("dve", lambda e: e.memset(Ppad[:], 0.0), writes=[tPp])
            last_R[0] = R
        P3 = Ppad[:, 1:1 + (R + 2) * 66].rearrange("p (r c) -> p r c", c=66)
        S.op("dve", lambda e: e.tensor_copy(out=P3[:, 1:R + 1, 1:65], in_=cstg[:, 0:n_el].rearrange("p (r c) -> p r c", c=64)),
             reads=[tcstg], writes=[tPp])
        dw, dwtok = dw_ring.next()
        for t in range(9):
            S.op("dve", lambda e, t=t: e.tensor_scalar(out=dw[:, t, :], in0=ident_b, scalar1=vecs[:, V_CONVW + j * 9 + t:V_CONVW + j * 9 + t + 1], scalar2=None, op0=ALU.mult),
                 reads=[t_cb, t_const], **(dict(writes=[dwtok]) if t == 0 else dict(wadd=[dwtok])))
        bcol = vecs[:, V_CONVB + j:V_CONVB + j + 1]
        firstc = True
        for q0 in range(0, npad, 512):
            N = min(512, npad - q0)
            pt, ptok = psA_ring.next()
            for t in range(9):
                i, jj = t // 3, t % 3
                o = q0 + i * 66 + jj
                S.op("pe", lambda e, pt=pt, t=t, o=o, N=N: e.matmul(pt[:, 0:N], dw[:, t, :], Ppad[:, o:o + N], start=(t == 0), stop=(t == 8)),
                     reads=[dwtok, tPp], writes=[ptok])
            S.op("dve", lambda e, pt=pt, q0=q0, N=N: e.tensor_scalar(out=accp[:, q0:q0 + N], in0=pt[:, 0:N], scalar1=bcol, scalar2=None, op0=ALU.add),
                 reads=[ptok, t_const], **(dict(writes=[taccp]) if firstc else dict(wadd=[taccp])))
            firstc = False
        S.op("act", lambda e: e.activation(out=ebp[:, 0:npad], in_=accp[:, 0:npad], func=AF.Exp, scale=-1.0), reads=[taccp], writes=[tebp])
        S.op("dve", lambda e: e.tensor_scalar(out=ebp[:, 0:npad], in0=ebp[:, 0:npad], scalar1=1.0, scalar2=None, op0=ALU.add), reads=[tebp], writes=[tebp])
        S.op("dve", lambda e: e.reciprocal(out=ebp[:, 0:npad], in_=ebp[:, 0:npad]), reads=[tebp], writes=[tebp])
        sc = 1.0 if j < 4 else 128.0 ** -0.5
        A3 = accp[:, 0:npad].rearrange("p (r c) -> p r c", c=66)[:, :, 1:65]
        E3 = ebp[:, 0:npad].rearrange("p (r c) -> p r c", c=66)[:, :, 1:65]
        S.op("dve", lambda e: e.scalar_tensor_tensor(out=outb[:, 0:n_el].rearrange("p (r c) -> p r c", c=64), in0=A3, scalar=sc, in1=E3, op0=ALU.mult, op1=ALU.mult),
             reads=[taccp, tebp], writes=[toutb])
        for (ap, tok, off, n) in dsts:
            S.dma("sp", ap, outb[:, off:off + n], reads=[toutb], wadd=[tok])

    for j in range(8):
        isq = j < 4
        nb = 4 if isq else 8
        R = 33 if isq else 64
        srcs = [(mpre_d[1 + b][:, j * 512:(j + 1) * 512], t_mpre[1 + b], b * 512, 512) for b in range(nb)]
        if isq:
            srcs.append((mpre_d[5][:, j * 512:j * 512 + 64], t_mpre[5], 2048, 64))
        dsts = [(mqk_d[1 + b][:, j * 512:(j + 1) * 512], t_mqk[1 + b], b * 512, 512) for b in range(nb)]
        conv_tile_pe(j, R, srcs, dsts)
    for j in range(4, 8):
        conv_tile(j, 1, 256, [(mpre_d[0][:, j * 512:j * 512 + 256], t_mpre[0], 0, 256)],
                  [(mqk_d[0][:, j * 512:j * 512 + 256], t_mqk[0], 0, 256)], (1,))
    release(n_s3)
    if dbg == 4:
        S.finish(t_mqk + dump_toks)
        nc.used_inputs = used_inputs
        return nc

    mixT_d = dscr("mixT_s", [4, 128, 8 * 512], BF16)
    t_mix = [Tok() for _ in range(4)]

    def finalize(OT, t_OT, gate_d, t_gate, gain_col0, koff):
        sq_ring = Ring([sb("fsq%d" % i, [128, 512], BF16) for i in range(2)])
        ms_ring = Ring([sb("fms%d" % i, [128, 512], F32) for i in range(2)])
        y_ring = Ring([sb("fy%d" % i, [128, 512], F32) for i in range(2)])
        gate_ring = Ring([sb("fgate%d" % i, [128, 4 * 512], BF16) for i in range(2)])
        mst_ring = Ring([sb("fmst%d" % i, [128, 4 * 512], BF16) for i in range(2)])
        for lb in range(4):
            gt, gttok = gate_ring.next()
            S.dma("sp", gt[:], gate_d[lb], reads=[t_gate[lb]], writes=[gttok])
            mst, msttok = mst_ring.next()
            for h in range(4):
                O = OT[:, h, lb * 512:(lb + 1) * 512]
                sq, sqtok = sq_ring.next()
                S.op("dve", lambda e, sq=sq, O=O: e.tensor_tensor(out=sq[:], in0=O, in1=O, op=ALU.mult), reads=[t_OT], writes=[sqtok])
                pt, ptok = psA_ring.next()
                S.op("pe", lambda e, pt=pt, sq=sq: e.matmul(pt[:, :], ones_b, sq[:], start=True, stop=True), reads=[sqtok, t_cb], writes=[ptok])
                ms, mstok = ms_ring.next()
                S.op("dve", lambda e, ms=ms, pt=pt: e.tensor_scalar(out=ms[:], in0=pt[:, :], scalar1=1.0 / 128, scalar2=EPS, op0=ALU.mult, op1=ALU.add),
                     reads=[ptok], writes=[mstok])
                S.op("act", lambda e, ms=ms: e.activation(out=ms[:], in_=ms[:], func=AF.Ln), reads=[mstok], writes=[mstok])
                S.op("act", lambda e, ms=ms: e.activation(out=ms[:], in_=ms[:], func=AF.Exp, scale=-0.5), reads=[mstok], writes=[mstok])
                y, ytok = y_ring.next()
                S.op("dve", lambda e, y=y, O=O, ms=ms, h=h: e.scalar_tensor_tensor(
                    out=y[:], in0=O, scalar=vecs[:, gain_col0 + h:gain_col0 + h + 1], in1=ms[:], op0=ALU.mult, op1=ALU.mult),
                    reads=[t_OT, mstok, t_const], writes=[ytok])
                S.op("dve", lambda e, y=y, gt=gt, mst=mst, h=h: e.tensor_tensor(
                    out=mst[:, h * 512:(h + 1) * 512], in0=y[:], in1=gt[:, h * 512:(h + 1) * 512], op=ALU.mult),
                    reads=[ytok, gttok], **(dict(writes=[msttok]) if h == 0 else dict(wadd=[msttok])))
            S.dma("sp", mixT_d[lb][:, koff * 512:(koff + 4) * 512], mst[:], reads=[msttok], wadd=[t_mix[lb]])

    n_s4 = len(es)
    OT = sb("OT", [128, 4, NLOC], F32)
    t_OT = Tok()
    upw_sb = sb("upw", [128, 2, 256], F32)
    t_upw = Tok()
    S.op("dve", lambda e: e.memset(upw_sb[:], 0.0), writes=[t_upw])
    S.dma("sp", upw_sb[0:16], upw_d.rearrange("d r n -> r d n"), writes=[t_upw])
    bmask = consts[:, 512:768]
    m2x = sb("m2x", [128, 2, 256], BF16)
    t_m2x = Tok()
    for d_ in range(2):
        for hh in range(2):
            S.op("dve", lambda e, d_=d_, hh=hh: e.tensor_copy(out=m2x[:, d_, hh * 128:(hh + 1) * 128], in_=mask_b[d_]),
                 reads=[t_cb], wadd=[t_m2x])
    Tst = [[sb("T%d%d" % (p, d_), [128, 256], F32) for d_ in range(2)] for p in range(2)]
    Sbt = [[sb("Sb%d%d" % (p, d_), [128, 256], BF16) for d_ in range(2)] for p in range(2)]
    ecol = [[sb("ec%d%d" % (p, d_), [128, 1], F32) for d_ in range(2)] for p in range(2)]
    t_T = [[Tok() for _ in range(2)] for _ in range(2)]
    t_Sb = [[Tok() for _ in range(2)] for _ in range(2)]
    t_ec = [[Tok() for _ in range(2)] for _ in range(2)]
    for p in range(2):
        for d_ in range(2):
            S.op("dve", lambda e, p=p, d_=d_: e.memset(Tst[p][d_][:], 0.0), writes=[t_T[p][d_]])
            S.op("dve", lambda e, p=p, d_=d_: e.memset(Sbt[p][d_][:], 0.0), writes=[t_Sb[p][d_]])
            S.op("dve", lambda e, p=p, d_=d_: e.memset(ecol[p][d_][:], 1.0), writes=[t_ec[p][d_]])
    gq_ring = Ring([sb("gqkb%d" % i, [128, 4 * 512], BF16) for i in range(2)])
    lr_ring = Ring([sb("lrb%d" % i, [128, 2 * 512], F32) for i in range(2)])
    for i_ in range(2):
        S.op("dve", lambda e, i_=i_: e.memset(lr_ring.items[i_][:], 0.0), writes=[lr_ring.toks[i_]])
    gv_ring = Ring([sb("gvb%d" % i, [128, 4 * 512], BF16) for i in range(2)])
    L_ring = Ring([sb("gL%d" % i, [128, 512], F32) for i in range(2)])
    C_ring = Ring([sb("gC%d" % i, [128, 512], F32) for i in range(2)])
    C2_ring = Ring([sb("gC2%d" % i, [128, 512], F32) for i in range(2)])
    eb_ring = Ring([sb("geb%d" % i, [128, 512], F32) for i in range(4)])
    enb_ring = Ring([sb("genb%d" % i, [128, 512], F32) for i in range(2)])
    qt_ring = Ring([sb("gqt%d" % i, [128, 512], BF16) for i in range(4)])
    kt_ring = Ring([sb("gkt%d" % i, [128, 512], BF16) for i in range(4)])
    kth_ring = Ring([sb("gkth%d" % i, [128, 512], BF16) for i in range(8)])
    ktok_ring = Ring([sb("gktok%d" % i, [128, 128], BF16) for i in range(4)])
    attm_ring = Ring([sb("gattm%d" % i, [128, 256], BF16) for i in range(4)])
    o_written = [False] * NBLK

    def gla_block(blk, d, full):
        N = 256 if blk == 0 else 512
        nch = N // 128
        gq, gqtok = gq_ring.next()
        S.dma("sp", gq[:], gqk_d[blk], reads=[t_gqk[blk]], writes=[gqtok])
        lrb, lrtok = lr_ring.next()
        S.dma("sp", lrb[0:16], lr_d[blk], reads=[t_lr[blk]], writes=[lrtok])
        gvb, gvtok = gv_ring.next()
        S.dma("sp", gvb[:], gv_d[blk], reads=[t_gv[blk]], writes=[gvtok])
        prep = []
        for p in range(2):
            pt, ptok = psA_ring.next()
            S.op("pe", lambda e, pt=pt, p=p: e.matmul(pt[:, 0:N], upw_sb[:, d, p * 128:(p + 1) * 128], lrb[:, d * 512:d * 512 + N], start=True, stop=True),
                 reads=[t_upw, lrtok], writes=[ptok])
            L, Ltok = L_ring.next()
            S.op("dve", lambda e, pt=pt, L=L, p=p: e.tensor_scalar(out=L[:, 0:N], in0=pt[:, 0:N], scalar1=vecs[:, V_UPB + d * 2 + p:V_UPB + d * 2 + p + 1], scalar2=None, op0=ALU.add),
                 reads=[ptok, t_const], writes=[Ltok])
            S.op("act", lambda e, L=L: e.activation(out=L[:, 0:N], in_=L[:, 0:N], func=AF.Exp, scale=-1.0), reads=[Ltok], writes=[Ltok])
            S.op("dve", lambda e, L=L: e.tensor_scalar(out=L[:, 0:N], in0=L[:, 0:N], scalar1=1.0, scalar2=None, op0=ALU.add), reads=[Ltok], writes=[Ltok])
            S.op("act", lambda e, L=L: e.activation(out=L[:, 0:N], in_=L[:, 0:N], func=AF.Ln), reads=[Ltok], writes=[Ltok])
            Cm, Ctok = C_ring.next()
            for c in range(nch):
                S.op("dve", lambda e, Cm=Cm, L=L, c=c: e.tensor_tensor_scan(
                    out=Cm[:, c * 128:(c + 1) * 128], data0=ones_f[:, 0:128], data1=L[:, c * 128:(c + 1) * 128], initial=0.0, op0=ALU.mult, op1=ALU.add),
                    reads=[Ltok, t_const], **(dict(writes=[Ctok]) if c == 0 else dict(wadd=[Ctok])))
            if d == 1:
                C2, C2tok = C2_ring.next()
                for c in range(nch):
                    S.op("dve", lambda e, Cm=Cm, C2=C2, c=c: e.tensor_scalar(
                        out=C2[:, c * 128:(c + 1) * 128], in0=Cm[:, c * 128:(c + 1) * 128], scalar1=-1.0, scalar2=Cm[:, c * 128 + 127:c * 128 + 128], op0=ALU.mult, op1=ALU.add),
                        reads=[Ctok], **(dict(writes=[C2tok]) if c == 0 else dict(wadd=[C2tok])))
                S.op("dve", lambda e, C2=C2, L=L: e.tensor_tensor(out=C2[:, 0:N], in0=C2[:, 0:N], in1=L[:, 0:N], op=ALU.add), reads=[C2tok, Ltok], writes=[C2tok])
                Cm, Ctok = C2, C2tok
            eb_, ebtok = eb_ring.next()
            S.op("act", lambda e, eb_=eb_, Cm=Cm: e.activation(out=eb_[:, 0:N], in_=Cm[:, 0:N], func=AF.Exp, scale=-1.0 / 16), reads=[Ctok], writes=[ebtok])
            enb, enbtok = enb_ring.next()
            S.op("act", lambda e, enb=enb, Cm=Cm: e.activation(out=enb[:, 0:N], in_=Cm[:, 0:N], func=AF.Exp, scale=1.0 / 16), reads=[Ctok], writes=[enbtok])
            kt, kttok = kt_ring.next()
            S.op("dve", lambda e, kt=kt, enb=enb, p=p: e.tensor_tensor(out=kt[:, 0:N], in0=gq[:, (2 + p) * 512:(2 + p) * 512 + N], in1=enb[:, 0:N], op=ALU.mult),
                 reads=[gqtok, enbtok], writes=[kttok])
            qt, qttok = None, None
            kth = [None, None]
            if full:
                for h in range(2):
                    kh, khtok = kth_ring.next()
                    S.op("dve", lambda e, kh=kh, enb=enb, p=p, h=h: e.scalar_tensor_tensor(
                        out=kh[:, 0:N], in0=gq[:, (2 + p) * 512:(2 + p) * 512 + N], scalar=consts[:, 768 + h:769 + h], in1=enb[:, 0:N], op0=ALU.mult, op1=ALU.mult),
                        reads=[gqtok, enbtok, t_const], writes=[khtok])
                    kth[h] = (kh, khtok)
                qt, qttok = qt_ring.next()
                S.op("dve", lambda e, qt=qt, eb_=eb_, p=p: e.tensor_tensor(out=qt[:, 0:N], in0=gq[:, p * 512:p * 512 + N], in1=eb_[:, 0:N], op=ALU.mult),
                     reads=[gqtok, ebtok], writes=[qttok])
            prep.append((eb_, ebtok, kt, kttok, qt, qttok, kth))
        order = list(range(nch)) if d == 0 else list(range(nch - 1, -1, -1))
        for c in order:
            cs = slice(c * 128, (c + 1) * 128)
            ecc = c * 128 + (127 if d == 0 else 0)
            st = [dict() for _ in range(2)]
            for p in range(2):
                eb_, ebtok, kt, kttok, qt, qttok, kth = prep[p]
                pb, pbtok = psB_ring.next()
                S.op("pe", lambda e, pb=pb, kt=kt: e.transpose(pb[:, 0:128], kt[:, cs], ident_b), reads=[kttok, t_cb], writes=[pbtok])
                st[p]["pb"] = (pb, pbtok)
                if full:
                    pa, patok = psA_ring.next()
                    for h in range(2):
                        S.op("pe", lambda e, pa=pa, h=h, kth=kth, qt=qt: e.matmul(pa[:, h * 128:(h + 1) * 128], kth[h][0][:, cs], qt[:, cs], start=True, stop=True),
                             reads=[kth[h][1], qttok], writes=[patok])
                    st[p]["pa"] = (pa, patok)
            for p in range(2):
                pb, pbtok = st[p]["pb"]
                ktk, ktktok = ktok_ring.next()
                S.op("dve", lambda e, ktk=ktk, pb=pb: e.tensor_copy(out=ktk[:], in_=pb[:, 0:128]), reads=[pbtok], writes=[ktktok])
                st[p]["ktk"] = (ktk, ktktok)
                if full:
                    pa, patok = st[p]["pa"]
                    am, amtok = attm_ring.next()
                    S.op("dve", lambda e, am=am, pa=pa: e.tensor_tensor(out=am[:], in0=pa[:, 0:256], in1=m2x[:, d, :], op=ALU.mult),
                         reads=[patok, t_m2x], writes=[amtok])
                    st[p]["am"] = (am, amtok)
            for p in range(2):
                ktk, ktktok = st[p]["ktk"]
                pd, pdtok = psA_ring.next()
                S.op("pe", lambda e, pd=pd, ktk=ktk, p=p: e.matmul(pd[:, 0:256], ktk[:], gvb[:, c * 512 + p * 256:c * 512 + (p + 1) * 256], start=True, stop=True),
                     reads=[ktktok, gvtok], writes=[pdtok])
                st[p]["pd"] = (pd, pdtok)
            if full:
                for p in range(2):
                    eb_, ebtok, kt, kttok, qt, qttok, kth = prep[p]
                    am, amtok = st[p]["am"]
                    po, potok = psA_ring.next()
                    for h in range(2):
                        hd = p * 2 + h
                        S.op("pe", lambda e, po=po, h=h, hd=hd, am=am: e.matmul(
                            po[:, h * 128:(h + 1) * 128], gvb[:, c * 512 + hd * 128:c * 512 + (hd + 1) * 128], am[:, h * 128:(h + 1) * 128], start=True, stop=False),
                            reads=[gvtok, amtok], writes=[potok])
                        S.op("pe", lambda e, po=po, h=h, qt=qt, p=p: e.matmul(
                            po[:, h * 128:(h + 1) * 128], Sbt[p][d][:, h * 128:(h + 1) * 128], qt[:, cs], start=False, stop=True),
                            reads=[t_Sb[p][d], qttok], writes=[potok])
                    st[p]["po"] = (po, potok)
            for p in range(2):
                eb_, ebtok = prep[p][0], prep[p][1]
                pd, pdtok = st[p]["pd"]
                S.op("dve", lambda e, pd=pd, p=p: e.scalar_tensor_tensor(
                    out=Tst[p][d][:], in0=Tst[p][d][:], scalar=ecol[p][d][:, 0:1], in1=pd[:, 0:256], op0=ALU.mult, op1=ALU.add),
                    reads=[pdtok, t_ec[p][d], t_T[p][d]], writes=[t_T[p][d]])
                S.op("dve", lambda e, p=p, eb_=eb_: e.scalar_tensor_tensor(
                    out=Sbt[p][d][:], in0=Tst[p][d][:], scalar=eb_[:, ecc:ecc + 1], in1=bmask, op0=ALU.mult, op1=ALU.mult),
                    reads=[t_T[p][d], ebtok, t_const], writes=[t_Sb[p][d]])
                S.op("dve", lambda e, p=p, eb_=eb_: e.tensor_copy(out=ecol[p][d][:], in_=eb_[:, ecc:ecc + 1]),
                     reads=[ebtok], writes=[t_ec[p][d]])
            if full:
                for p in range(2):
                    po, potok = st[p]["po"]
                    tok0 = (blk - 1) * 512 + c * 128
                    Odst = OT[:, p * 2:p * 2 + 2, tok0:tok0 + 128]
                    po3 = po[:, 0:256].rearrange("p (h t) -> p h t", h=2)
                    if not o_written[blk]:
                        S.op("dve", lambda e, Odst=Odst, po3=po3: e.tensor_copy(out=Odst, in_=po3), reads=[potok], wadd=[t_OT])
                    else:
                        S.op("dve", lambda e, Odst=Odst, po3=po3: e.tensor_tensor(out=Odst, in0=po3, in1=Odst, op=ALU.add), reads=[potok, t_OT], wadd=[t_OT])
        if full:
            o_written[blk] = True

    s4m = int(os.environ.get("S4MODE", 9))
    if s4m == 10:
        gla_block(8, 1, False)
    elif s4m == 11:
        gla_block(0, 0, False)
    else:
        gla_block(0, 1, False)
    if 10 > s4m >= 1:
        for blk in (8, 7, 6, 5):
            gla_block(blk, 1, False)
        gla_block(0, 0, False)
    if 10 > s4m >= 2:
        for i in range(4 if s4m >= 3 else 1):
            gla_block(1 + i, 0, True)
            gla_block(4 - i, 1, True)
    if 10 > s4m >= 4:
        finalize(OT, t_OT, sgg_d, t_sgg, V_GNG, 0)
    dump("OTg", OT[:, 0, :], [128, NLOC], [t_OT])
    release(n_s4)
    if dbg == 5:
        S.finish(t_mix + dump_toks)
        nc.used_inputs = used_inputs
        return nc

    n_s5 = len(es)
    HT = sb("HT", [128, 4, NLOC], F32)
    t_HT = Tok()
    gateb4 = sb("gateb4", [128, 64], F32)
    t_gb4 = Tok()
    for c in range(4):
        S.dma("sp", gateb4[:, c * 16:(c + 1) * 16], rows_d[0:1, R_GATEB:R_GATEB + 16].partition_broadcast(128), wadd=[t_gb4])
    maskf = [consts[:, 128:256], consts[:, 256:384]]
    T4 = [sb("T4_%d" % d_, [128, 4, 256], F32) for d_ in range(2)]
    Sb4 = [sb("Sb4_%d" % d_, [128, 4, 256], BF16) for d_ in range(2)]
    t_T4 = [Tok() for _ in range(2)]
    t_Sb4 = [Tok() for _ in range(2)]
    ec_one = sb("econe", [128, 4], F32)
    t_econe = Tok()
    S.op("dve", lambda e: e.memset(ec_one[:], 1.0), writes=[t_econe])
    prev_ec = [(ec_one, t_econe), (ec_one, t_econe)]
    for d_ in range(2):
        S.op("dve", lambda e, d_=d_: e.memset(T4[d_][:], 0.0), writes=[t_T4[d_]])
        S.op("dve", lambda e, d_=d_: e.memset(Sb4[d_][:], 0.0), writes=[t_Sb4[d_]])
    mq_ring = Ring([sb("mqkb%d" % i, [128, 8 * 512], BF16) for i in range(2)])
    mvb_ring = Ring([sb("mvb%d" % i, [128, 4 * 512], BF16) for i in range(2)])
    ga_ring = Ring([sb("gab%d" % i, [128, 64], F32) for i in range(2)])
    gbb_ring = Ring([sb("gbb%d" % i, [128, 64], F32) for i in range(2)])
    Lf_ring = Ring([sb("Lf%d" % i, [128, 16], F32) for i in range(2)])
    es_ring = Ring([sb("es%d" % i, [128, 16], F32) for i in range(2)])
    lfbc_ring = Ring([sb("lfbc%d" % i, [128, 4, 128], F32) for i in range(2)])
    flo_ring = Ring([sb("flo%d" % i, [128, 4, 128], F32) for i in range(2)])
    ecn_ring = Ring([sb("ecn%d" % i, [128, 4], F32) for i in range(12)])
    vext_ring = Ring([sb("vext%d" % i, [128, 4, 256], BF16) for i in range(2)])
    mktok_ring = Ring([sb("mktok%d" % i, [128, 512], BF16) for i in range(2)])
    mam_ring = Ring([sb("mam%d" % i, [128, 512], BF16) for i in range(2)])
    mask4 = sb("mask4", [128, 2, 512], BF16)
    t_mask4 = Tok()
    for d_ in range(2):
        for hh in range(4):
            S.op("dve", lambda e, d_=d_, hh=hh: e.tensor_copy(out=mask4[:, d_, hh * 128:(hh + 1) * 128], in_=mask_b[d_]), reads=[t_cb], wadd=[t_mask4])
    tP = [[t_] * 4 for t_ in [Tok() for _ in range(6)]]
    dd_ring = Ring([sb("mdd%d" % i, [128, 4, 128], F32) for i in range(2)])
    ht_ring = Ring([sb("mht%d" % i, [128, 4, 128], F32) for i in range(2)])
    h_written = [False] * NBLK

    def ml_block(blk, d, full):
        N = 256 if blk == 0 else 512
        nch = N // 128
        mq, mqtok = mq_ring.next()
        S.dma("sp", mq[:], mqk_d[blk], reads=[t_mqk[blk]], writes=[mqtok])
        mvb, mvtok = mvb_ring.next()
        S.dma("sp", mvb[:], mv_d[blk], reads=[t_mv[blk]], writes=[mvtok])
        ga, gatok = ga_ring.next()
        S.dma("sp", ga[:], gates_d[blk], reads=[t_gates[blk]], writes=[gatok])
        gbb, gbtok = gbb_ring.next()
        S.op("dve", lambda e: e.tensor_tensor(out=gbb[:], in0=ga[:], in1=gateb4[:], op=ALU.add), reads=[gatok, t_gb4], writes=[gbtok])
        gb3 = gbb[:].rearrange("p (c g) -> p c g", g=16)
        Lf, Lftok = Lf_ring.next()
        Lf3 = Lf[:].rearrange("p (c h) -> p c h", h=4)
        S.op("act", lambda e: e.activation(out=Lf3[:, 0:nch, :], in_=gb3[:, 0:nch, 8 + d * 4:12 + d * 4], func=AF.Exp, scale=-1.0), reads=[gbtok], writes=[Lftok])
        S.op("dve", lambda e: e.tensor_scalar(out=Lf[:, 0:nch * 4], in0=Lf[:, 0:nch * 4], scalar1=1.0, scalar2=None, op0=ALU.add), reads=[Lftok], writes=[Lftok])
        S.op("act", lambda e: e.activation(out=Lf[:, 0:nch * 4], in_=Lf[:, 0:nch * 4], func=AF.Ln), reads=[Lftok], writes=[Lftok])
        pt, ptok = psA[0], tP[0][0]
        S.op("pe", lambda e: e.matmul(pt[:, 0:nch * 4], maskf[d], Lf[:, 0:nch * 4], start=True, stop=True), reads=[Lftok, t_const], writes=[ptok])
        es_, estok = es_ring.next()
        es3 = es_[:].rearrange("p (c h) -> p c h", h=4)
        pt3 = pt[:, 0:16].rearrange("p (c h) -> p c h", h=4)
        S.op("dve", lambda e: e.tensor_tensor(out=es3[:, 0:nch, :], in0=pt3[:, 0:nch, :], in1=gb3[:, 0:nch, d * 4:d * 4 + 4], op=ALU.add),
             reads=[ptok, gbtok], writes=[estok])
        S.op("act", lambda e: e.activation(out=es_[:, 0:nch * 4], in_=es_[:, 0:nch * 4], func=AF.Exp), reads=[estok], writes=[estok])
        order = list(range(nch)) if d == 0 else list(range(nch - 1, -1, -1))
        endcol = 127 if d == 0 else 0
        for c in order:
            lf4, lf4tok = lfbc_ring.next()
            S.op("dve", lambda e, lf4=lf4: e.tensor_copy(out=lf4[:], in_=Lf[:, c * 4:c * 4 + 4].unsqueeze(2).to_broadcast([128, 4, 128])),
                 reads=[Lftok], writes=[lf4tok])
            vx4, vx4tok = vext_ring.next()
            es_bc = es_[:, c * 4:c * 4 + 4].unsqueeze(2).to_broadcast([128, 4, 128])
            S.op("dve", lambda e, vx4=vx4, es_bc=es_bc: e.tensor_tensor(
                out=vx4[:, :, 0:128], in0=mvb[:, c * 512:(c + 1) * 512].rearrange("p (h n) -> p h n", h=4), in1=es_bc, op=ALU.mult),
                reads=[mvtok, estok], writes=[vx4tok])
            S.op("dve", lambda e, vx4=vx4, es_bc=es_bc: e.tensor_copy(out=vx4[:, :, 128:256], in_=es_bc), reads=[estok], wadd=[vx4tok])
            kTs = [mq[:, (4 + h) * 512 + c * 128:(4 + h) * 512 + (c + 1) * 128] for h in range(4)]
            qTs = [mq[:, h * 512 + c * 128:h * 512 + (c + 1) * 128] for h in range(4)]
            pb, pbtok = psB_ring.next()
            for h in range(4):
                S.op("pe", lambda e, h=h, lf4=lf4: e.matmul(psA[0][:, h * 128:(h + 1) * 128], lf4[:, h, :], maskf[d], start=True, stop=True),
                     reads=[lf4tok, t_const], writes=[tP[0][0]])
                S.op("pe", lambda e, h=h, pb=pb: e.transpose(pb[:, h * 128:(h + 1) * 128], kTs[h], ident_b), reads=[mqtok, t_cb], writes=[pbtok])
            ecn4, ecn4tok = ecn_ring.next()
            S.op("act", lambda e, ecn4=ecn4: e.activation(
                out=ecn4[:, 0:4].unsqueeze(2), in_=psA[0][:, :].rearrange("p (h n) -> p h n", h=4)[:, :, endcol:endcol + 1], func=AF.Exp, scale=-1.0),
                reads=[tP[0][0]], writes=[ecn4tok])
            flo4, flo4tok = None, None
            if full:
                flo4, flo4tok = flo_ring.next()
                S.op("act", lambda e, flo4=flo4: e.activation(out=flo4[:].rearrange("p h n -> p (h n)"), in_=psA[0][:, :], func=AF.Exp), reads=[tP[0][0]], writes=[flo4tok])
            ktk4, ktk4tok = mktok_ring.next()
            S.op("dve", lambda e, ktk4=ktk4, pb=pb: e.tensor_copy(out=ktk4[:], in_=pb[:, 0:512]), reads=[pbtok], writes=[ktk4tok])
            if full:
                for h in range(4):
                    S.op("pe", lambda e, h=h: e.matmul(psA[1][:, h * 128:(h + 1) * 128], kTs[h], qTs[h], start=True, stop=True), reads=[mqtok], writes=[tP[1][0]])
                am4, am4tok = mam_ring.next()
                S.op("dve", lambda e, am4=am4: e.tensor_tensor(out=am4[:], in0=psA[1][:, :], in1=mask4[:, d, :], op=ALU.mult),
                     reads=[tP[1][0], t_mask4], writes=[am4tok])
                for h in range(4):
                    bk, o0 = 2 + h // 2, (h % 2) * 256
                    for half in range(2):
                        hc = slice(half * 128, (half + 1) * 128)
                        oc = slice(o0 + half * 128, o0 + (half + 1) * 128)
                        S.op("pe", lambda e, h=h, bk=bk, oc=oc, hc=hc, am4=am4, vx4=vx4: e.matmul(psA[bk][:, oc], vx4[:, h, hc], am4[:, h * 128:(h + 1) * 128], start=True, stop=False),
                             reads=[vx4tok, am4tok], writes=[tP[bk][0]])
                        S.op("pe", lambda e, h=h, bk=bk, oc=oc, hc=hc: e.matmul(psA[bk][:, oc], Sb4[d][:, h, hc], qTs[h], start=False, stop=True),
                             reads=[t_Sb4[d], mqtok], writes=[tP[bk][0]])
            for h in range(4):
                bk, o0 = 4 + h // 2, (h % 2) * 256
                S.op("pe", lambda e, h=h, bk=bk, o0=o0, ktk4=ktk4, vx4=vx4: e.matmul(psA[bk][:, o0:o0 + 256], ktk4[:, h * 128:(h + 1) * 128], vx4[:, h, :], start=True, stop=True),
                     reads=[ktk4tok, vx4tok], writes=[tP[bk][0]])
            pec, pectok = prev_ec[d]
            for h in range(4):
                bk, o0 = 4 + h // 2, (h % 2) * 256
                S.op("dve", lambda e, h=h, bk=bk, o0=o0, pec=pec: e.scalar_tensor_tensor(
                    out=T4[d][:, h, :], in0=T4[d][:, h, :], scalar=pec[:, h:h + 1], in1=psA[bk][:, o0:o0 + 256], op0=ALU.mult, op1=ALU.add),
                    reads=[tP[bk][0], pectok, t_T4[d]], writes=[t_T4[d]])
            S.op("dve", lambda e, ecn4=ecn4: e.tensor_tensor(out=Sb4[d][:], in0=T4[d][:], in1=ecn4[:, 0:4].unsqueeze(2).to_broadcast([128, 4, 256]), op=ALU.mult),
                 reads=[t_T4[d], ecn4tok], writes=[t_Sb4[d]])
            prev_ec[d] = (ecn4, ecn4tok)
            if full:
                dd4, dd4tok = dd_ring.next()
                for bk in (2, 3):
                    hs2 = slice((bk - 2) * 2, (bk - 2) * 2 + 2)
                    den = psA[bk][:, :].rearrange("p (h x n) -> p h x n", h=2, x=2)[:, :, 1, :]
                    S.op("dve", lambda e, dd4=dd4, den=den, hs2=hs2, flo4=flo4: e.scalar_tensor_tensor(
                        out=dd4[:, hs2, :], in0=den, scalar=-1.0, in1=flo4[:, hs2, :], op0=ALU.mult, op1=ALU.max),
                        reads=[tP[bk][0], flo4tok], **(dict(writes=[dd4tok]) if bk == 2 else dict(wadd=[dd4tok])))
                for bk in (2, 3):
                    hs2 = slice((bk - 2) * 2, (bk - 2) * 2 + 2)
                    den = psA[bk][:, :].rearrange("p (h x n) -> p h x n", h=2, x=2)[:, :, 1, :]
                    S.op("dve", lambda e, dd4=dd4, den=den, hs2=hs2: e.tensor_tensor(out=dd4[:, hs2, :], in0=den, in1=dd4[:, hs2, :], op=ALU.max),
                         reads=[tP[bk][0], dd4tok], wadd=[dd4tok])
                S.op("dve", lambda e, dd4=dd4: e.reciprocal(out=dd4[:], in_=dd4[:]), reads=[dd4tok], writes=[dd4tok])
                tok0 = (blk - 1) * 512 + c * 128
                if not h_written[blk]:
                    for bk in (2, 3):
                        hs2 = slice((bk - 2) * 2, (bk - 2) * 2 + 2)
                        num = psA[bk][:, :].rearrange("p (h x n) -> p h x n", h=2, x=2)[:, :, 0, :]
                        S.op("dve", lambda e, num=num, hs2=hs2, dd4=dd4: e.tensor_tensor(out=HT[:, hs2, tok0:tok0 + 128], in0=num, in1=dd4[:, hs2, :], op=ALU.mult),
                             reads=[tP[bk][0], dd4tok], wadd=[t_HT])
                else:
                    ht4, ht4tok = ht_ring.next()
                    for bk in (2, 3):
                        hs2 = slice((bk - 2) * 2, (bk - 2) * 2 + 2)
                        num = psA[bk][:, :].rearrange("p (h x n) -> p h x n", h=2, x=2)[:, :, 0, :]
                        S.op("dve", lambda e, num=num, hs2=hs2, dd4=dd4, ht4=ht4: e.tensor_tensor(out=ht4[:, hs2, :], in0=num, in1=dd4[:, hs2, :], op=ALU.mult),
                             reads=[tP[bk][0], dd4tok], **(dict(writes=[ht4tok]) if bk == 2 else dict(wadd=[ht4tok])))
                    S.op("dve", lambda e, ht4=ht4: e.tensor_tensor(out=HT[:, :, tok0:tok0 + 128], in0=HT[:, :, tok0:tok0 + 128], in1=ht4[:], op=ALU.add),
                         reads=[ht4tok, t_HT], wadd=[t_HT])
        if full:
            h_written[blk] = True

    s5m = int(os.environ.get("S5MODE", 9))
    ml_block(0, 1, False)
    if s5m >= 1:
        for blk in (8, 7, 6, 5):
            ml_block(blk, 1, False)
        ml_block(0, 0, False)
    if s5m >= 2:
        for i in range(4 if s5m >= 3 else 1):
            ml_block(1 + i, 0, True)
            ml_block(4 - i, 1, True)
    S.barrier()
    if s5m >= 4:
        finalize(HT, t_HT, smo_d, t_smo, V_MNG, 4)
    dump("HTm", HT[:, 0, :], [128, NLOC], [t_HT])
    release(n_s5)
    if dbg == 6:
        S.finish(t_mix + dump_toks)
        nc.used_inputs = used_inputs
        return nc

    n_s6 = len(es)
    x1_d = dscr("x1_s", [16, 128, D], F32)
    t_x1 = Tok()
    wout_v = wout_d.rearrange("(k p) n -> p k n", p=128)
    woutb = sb("woutb", [128, 8, D], BF16)
    t_wout = Tok()
    wo_stg = Ring([sb("wostg%d" % i, [128, 8, 256], F32) for i in range(2)])
    for pc in range(4):
        st, sttok = wo_stg.next()
        S.dma("sp", st[:], wout_v[:, :, pc * 256:(pc + 1) * 256], writes=[sttok])
        S.op("dve", lambda e, st=st, pc=pc: e.tensor_copy(out=woutb[:, :, pc * 256:(pc + 1) * 256], in_=st[:]), reads=[sttok], wadd=[t_wout])
    rw_sb = sb("rw", [128, 8, 36], F32)
    t_rw = Tok()
    S.dma("sp", rw_sb[:], rw_d.rearrange("(k p) n -> p k n", p=128), writes=[t_rw])
    rb_bc = sb("rbbc", [128, 36], F32)
    S.dma("sp", rb_bc[:], rows_d[0:1, R_RB:R_RB + 36].partition_broadcast(128), wadd=[t_rw])
    mixb_ring = Ring([sb("mixb%d" % i, [128, 8 * 512], BF16) for i in range(2)])
    xt6_ring = Ring([sb("x6t%d" % i, [128, D], F32) for i in range(2)])
    x1_ring = Ring([sb("x1t%d" % i, [128, D], F32) for i in range(2)])
    tmp6 = sb("tmp6", [128, D], F32)
    t_tmp6 = Tok()
    jk6 = sb("jk6", [128, D], F32)
    t_jk6 = Tok()
    s6_ring = Ring([(sb("s6a%d" % i, [128, 1], F32), sb("s6b%d" % i, [128, 1], F32)) for i in range(2)])
    h2s_ring = Ring([sb("h2s%d" % i, [128, D], F32) for i in range(2)])
    h2Tf_ring = Ring([sb("h2Tf%d" % i, [128, 8, 128], F32) for i in range(2)])
    Sel = sb("Sel", [128, 16, 2, 32], F32)
    t_Sel = Tok()
    h2tok = sb("h2tok", [128, 16, D], BF16)
    t_h2tok = Tok()
    LG = sb("LGall", [128, 16, 36], F32)
    t_LG = Tok()
    mixb, mixbtok = None, None
    for ti in range(16):
        lb, tt = ti // 4, ti % 4
        if tt == 0:
            mixb, mixbtok = mixb_ring.next()
            S.dma("sp", mixb[:], mixT_d[lb], reads=[t_mix[lb]], writes=[mixbtok])
        xt, xttok = xt6_ring.next()
        S.dma("sp", xt[:], x_d[ti * 128:(ti + 1) * 128, :], writes=[xttok])
        x1, x1tok = x1_ring.next()
        for half in range(2):
            po, potok = psA_ring.next()
            for k in range(8):
                S.op("pe", lambda e, po=po, k=k, half=half, mixb=mixb, tt=tt: e.matmul(
                    po[:, :], mixb[:, k * 512 + tt * 128:k * 512 + (tt + 1) * 128], woutb[:, k, half * 512:(half + 1) * 512], start=(k == 0), stop=(k == 7)),
                    reads=[mixbtok, t_wout], writes=[potok])
            hs_ = slice(half * 512, (half + 1) * 512)
            S.op("dve", lambda e, po=po, hs_=hs_: e.tensor_tensor(out=tmp6[:, hs_], in0=po[:, :], in1=g12[:, hs_], op=ALU.mult),
                 reads=[potok, t_g12], **(dict(writes=[t_tmp6]) if half == 0 else dict(wadd=[t_tmp6])))
            S.op("dve", lambda e, x1=x1, xt=xt, hs_=hs_: e.tensor_tensor(out=x1[:, hs_], in0=tmp6[:, hs_], in1=xt[:, hs_], op=ALU.add),
                 reads=[t_tmp6, xttok], **(dict(writes=[x1tok]) if half == 0 else dict(wadd=[x1tok])))
        S.dma("sp", x1_d[ti], x1[:], reads=[x1tok], wadd=[t_x1])
        ss, sstok = s6_ring.next()
        S.op("act", lambda e, x1=x1, ss=ss: e.activation(out=jk6[:], in_=x1[:], func=AF.Square, accum_out=ss[0][:, 0:1]), reads=[x1tok], writes=[t_jk6, sstok])
        S.op("dve", lambda e, ss=ss: e.tensor_scalar(out=ss[1][:, 0:1], in0=ss[0][:, 0:1], scalar1=1.0 / D, scalar2=EPS, op0=ALU.mult, op1=ALU.add), reads=[sstok], writes=[sstok])
        S.op("act", lambda e, ss=ss: e.activation(out=ss[0][:, 0:1], in_=ss[1][:, 0:1], func=AF.Ln), reads=[sstok], writes=[sstok])
        S.op("act", lambda e, ss=ss: e.activation(out=ss[1][:, 0:1], in_=ss[0][:, 0:1], func=AF.Exp, scale=-0.5), reads=[sstok], writes=[sstok])
        h2s, h2stok = h2s_ring.next()
        S.op("dve", lambda e, h2s=h2s, x1=x1, ss=ss: e.tensor_scalar(out=h2s[:], in0=x1[:], scalar1=ss[1][:, 0:1], scalar2=None, op0=ALU.mult),
             reads=[x1tok, sstok], writes=[h2stok])
        h2Tf, h2Tftok = h2Tf_ring.next()
        for g in range(2):
            pT, pTtok = psA_ring.next()
            for kk in range(4):
                k = g * 4 + kk
                S.op("pe", lambda e, pT=pT, kk=kk, k=k, h2s=h2s: e.transpose(pT[:, kk * 128:(kk + 1) * 128], h2s[:, k * 128:(k + 1) * 128], ident_f),
                     reads=[h2stok, t_const], writes=[pTtok])
            for kk in range(4):
                k = g * 4 + kk
                S.op("dve", lambda e, pT=pT, kk=kk, k=k, h2Tf=h2Tf: e.tensor_scalar(
                    out=h2Tf[:, k, :], in0=pT[:, kk * 128:(kk + 1) * 128], scalar1=A2[:, k:k + 1], scalar2=A2[:, 8 + k:9 + k], op0=ALU.mult, op1=ALU.add),
                    reads=[pTtok, t_A], **(dict(writes=[h2Tftok]) if k == 0 else dict(wadd=[h2Tftok])))
        pr, prtok = psA_ring.next()
        for k in range(8):
            S.op("pe", lambda e, pr=pr, k=k, h2Tf=h2Tf: e.matmul(pr[:, 0:36], h2Tf[:, k, :], rw_sb[:, k, :], start=(k == 0), stop=(k == 7)),
                 reads=[h2Tftok, t_rw], writes=[prtok])
        S.op("dve", lambda e, pr=pr, ti=ti: e.tensor_tensor(out=LG[:, ti, :], in0=pr[:, 0:36], in1=rb_bc[:], op=ALU.add), reads=[prtok, t_rw], wadd=[t_LG])
        S.op("dve", lambda e, h2s=h2s: e.tensor_tensor(out=tmp6[:], in0=h2s[:], in1=g12[:, 2 * D:3 * D], op=ALU.mult), reads=[h2stok, t_g12, t_tmp6], writes=[t_tmp6])
        S.op("dve", lambda e, ti=ti: e.tensor_tensor(out=h2tok[:, ti, :], in0=tmp6[:], in1=g12[:, 3 * D:4 * D], op=ALU.add), reads=[t_tmp6, t_g12], wadd=[t_h2tok])
    RT = sb("RTb", [128, 16 * 80], F32)
    t_RT = Tok()

    def V(c0, n):
        return RT[:, c0:c0 + 16 * n].rearrange("p (t n) -> p t n", n=n)

    def bc(ap2, n):
        return ap2.unsqueeze(2).to_broadcast([128, 16, n])
    G = LG[:, :, 0:4]
    E4 = LG[:, :, 4:36].rearrange("p t (g i) -> p t g i", g=4)
    gmax, gs, gw = RT[:, 0:16], RT[:, 16:32], RT[:, 32:48]
    m1, m2, w1, w2, w1g, w2g = RT[:, 48:64], RT[:, 64:80], RT[:, 80:96], RT[:, 96:112], RT[:, 112:128], RT[:, 128:144]
    goh, gex = V(144, 4), V(208, 4)
    eg, eq1, eg2, eq2, tmp8 = V(272, 8), V(400, 8), V(528, 8), V(656, 8), V(784, 8)

    def rop(fn, eng="dve", extra=()):
        S.op(eng, fn, reads=[t_RT, t_LG] + list(extra), writes=[t_RT])
    S.op("dve", lambda e: e.tensor_reduce(out=gmax, in_=G, axis=AX.X, op=ALU.max), reads=[t_LG], writes=[t_RT])
    rop(lambda e: e.tensor_tensor(out=goh, in0=G, in1=bc(gmax, 4), op=ALU.is_equal))
    rop(lambda e: e.tensor_tensor(out=gex, in0=G, in1=bc(gmax, 4), op=ALU.subtract))
    rop(lambda e: e.activation(out=RT[:, 208:272], in_=RT[:, 208:272], func=AF.Exp), eng="act")
    rop(lambda e: e.tensor_reduce(out=gs, in_=gex, axis=AX.X, op=ALU.add))
    rop(lambda e: e.reciprocal(out=gw, in_=gs))
    rop(lambda e: e.tensor_tensor(out=eg, in0=E4[:, :, 0, :], in1=bc(goh[:, :, 0], 8), op=ALU.mult))
    for g in range(1, 4):
        rop(lambda e, g=g: e.tensor_tensor(out=tmp8, in0=E4[:, :, g, :], in1=bc(goh[:, :, g], 8), op=ALU.mult))
        rop(lambda e: e.tensor_tensor(out=eg, in0=eg, in1=tmp8, op=ALU.add))
    rop(lambda e: e.tensor_reduce(out=m1, in_=eg, axis=AX.X, op=ALU.max))
    rop(lambda e: e.tensor_tensor(out=eq1, in0=eg, in1=bc(m1, 8), op=ALU.is_equal))
    rop(lambda e: e.scalar_tensor_tensor(out=RT[:, 528:656], in0=RT[:, 400:528], scalar=-1e30, in1=RT[:, 272:400], op0=ALU.mult, op1=ALU.add))
    rop(lambda e: e.tensor_reduce(out=m2, in_=eg2, axis=AX.X, op=ALU.max))
    rop(lambda e: e.tensor_tensor(out=eq2, in0=eg2, in1=bc(m2, 8), op=ALU.is_equal))
    rop(lambda e: e.tensor_tensor(out=w1, in0=m2, in1=m1, op=ALU.subtract))
    rop(lambda e: e.activation(out=w1, in_=w1, func=AF.Exp), eng="act")
    rop(lambda e: e.tensor_scalar(out=w1, in0=w1, scalar1=1.0, scalar2=None, op0=ALU.add))
    rop(lambda e: e.reciprocal(out=w1, in_=w1))
    rop(lambda e: e.tensor_scalar(out=w2, in0=w1, scalar1=-1.0, scalar2=1.0, op0=ALU.mult, op1=ALU.add))
    rop(lambda e: e.tensor_tensor(out=w1g, in0=w1, in1=gw, op=ALU.mult))
    rop(lambda e: e.tensor_tensor(out=w2g, in0=w2, in1=gw, op=ALU.mult))
    for g in range(4):
        S.op("dve", lambda e, g=g: e.tensor_tensor(out=Sel[:, :, 0, g * 8:(g + 1) * 8], in0=eq1, in1=bc(goh[:, :, g], 8), op=ALU.mult), reads=[t_RT], wadd=[t_Sel])
        S.op("dve", lambda e, g=g: e.tensor_tensor(out=Sel[:, :, 1, g * 8:(g + 1) * 8], in0=eq2, in1=bc(goh[:, :, g], 8), op=ALU.mult), reads=[t_RT], wadd=[t_Sel])
    Wt3 = Wt[:].rearrange("p (t k) -> p t k", k=2)
    S.op("dve", lambda e: e.tensor_copy(out=Wt3[:, :, 0], in_=w1g), reads=[t_RT], wadd=[t_Wt])
    S.op("dve", lambda e: e.tensor_copy(out=Wt3[:, :, 1], in_=w2g), reads=[t_RT], wadd=[t_Wt])
    Wselb = sb("Wselb", [128, 16, 32], BF16)
    t_wsel = Tok()
    S.op("dve", lambda e: e.tensor_tensor(out=Wselb[:], in0=Sel[:, :, 0, :], in1=Sel[:, :, 1, :], op=ALU.add), reads=[t_Sel], writes=[t_wsel])
    stri_b = sb("strib", [128, 128], BF16)
    t_stri = Tok()
    S.op("dve", lambda e: e.tensor_tensor(out=stri_b[:], in0=mask_b[0], in1=ident_b, op=ALU.subtract), reads=[t_cb], writes=[t_stri])
    rs = sb("rsm", [128, 512], F32)
    t_rs = Tok()
    cntf, nbf, padded, pad_end, pad_start = rs[:, 0:32], rs[:, 32:64], rs[:, 64:96], rs[:, 96:128], rs[:, 128:160]
    bef, be1024, be512 = rs[:, 192:256], rs[:, 256:320], rs[:, 320:384]
    pc, pctok = psA_ring.next()
    for ti in range(16):
        S.op("pe", lambda e, ti=ti: e.matmul(pc[:, 0:32], ones_b, Wselb[:, ti, :], start=(ti == 0), stop=(ti == 15)), reads=[t_wsel, t_cb], writes=[pctok])
    S.op("dve", lambda e: e.tensor_copy(out=cntf, in_=pc[:, 0:32]), reads=[pctok], writes=[t_rs])
    S.op("dve", lambda e: e.memset(nbf, 0.0), reads=[t_rs], writes=[t_rs])
    for j in range(16):
        S.op("dve", lambda e, j=j: e.scalar_tensor_tensor(out=nbf, in0=cntf, scalar=128.0 * j, in1=nbf, op0=ALU.is_gt, op1=ALU.add), reads=[t_rs], writes=[t_rs])
    S.op("dve", lambda e: e.tensor_scalar(out=padded, in0=nbf, scalar1=128.0, scalar2=None, op0=ALU.mult), reads=[t_rs], writes=[t_rs])
    S.op("dve", lambda e: e.tensor_tensor_scan(out=pad_end, data0=ones_f[:, 0:32], data1=padded, initial=0.0, op0=ALU.mult, op1=ALU.add), reads=[t_rs, t_const], writes=[t_rs])
    S.op("dve", lambda e: e.tensor_tensor(out=pad_start, in0=pad_end, in1=padded, op=ALU.subtract), reads=[t_rs], writes=[t_rs])
    DestF = sb("DestF", [128, 32], F32)
    t_destf = Tok()
    dt_ring = Ring([sb("dtt%d" % i, [128, 64], F32) for i in range(2)])
    for ti in range(16):
        pC, pCtok = psA_ring.next()
        for t2 in range(ti):
            S.op("pe", lambda e, pC=pC, t2=t2: e.matmul(pC[:, 0:32], ones_b, Wselb[:, t2, :], start=(t2 == 0), stop=False), reads=[t_wsel, t_cb], writes=[pCtok])
        S.op("pe", lambda e, pC=pC, ti=ti: e.matmul(pC[:, 0:32], stri_b[:], Wselb[:, ti, :], start=(ti == 0), stop=True), reads=[t_wsel, t_stri], writes=[pCtok])
        dtt, dtok = dt_ring.next()
        S.op("dve", lambda e, pC=pC, dtt=dtt: e.tensor_tensor(out=dtt[:, 0:32], in0=pC[:, 0:32], in1=pad_start, op=ALU.add), reads=[pCtok, t_rs], writes=[dtok])
        for k in range(2):
            S.op("dve", lambda e, dtt=dtt, ti=ti, k=k: e.tensor_tensor(out=dtt[:, 32:64], in0=dtt[:, 0:32], in1=Sel[:, ti, k, :], op=ALU.mult), reads=[dtok, t_Sel], writes=[dtok])
            S.op("dve", lambda e, dtt=dtt, ti=ti, k=k: e.tensor_reduce(out=DestF[:, ti * 2 + k:ti * 2 + k + 1], in_=dtt[:, 32:64], axis=AX.X, op=ALU.add),
                 reads=[dtok], wadd=[t_destf])
    S.op("dve", lambda e: e.tensor_copy(out=Desti[:], in_=DestF[:]), reads=[t_destf], writes=[t_dest])
    buf_d = dscr("moebuf_s", [8192, D], BF16)
    ybuf_d = dscr("moey_s", [8192, D], F32)
    t_buf = Tok()
    for ti in range(16):
        for k in range(2):
            S.idma(buf_d[:, :], bass.IndirectOffsetOnAxis(ap=Desti[:, ti * 2 + k:ti * 2 + k + 1], axis=0), h2tok[:, ti, :], None,
                   reads=[t_h2tok, t_dest], wadd=[t_buf])
    S.op("dve", lambda e: e.memset(bef, 0.0), reads=[t_rs], writes=[t_rs])
    TH = consts[:, 782:846]
    for ex in range(32):
        S.op("dve", lambda e, ex=ex: e.scalar_tensor_tensor(out=bef, in0=TH, scalar=rs[:, 96 + ex:97 + ex], in1=bef, op0=ALU.is_ge, op1=ALU.add), reads=[t_rs, t_const], writes=[t_rs])
    S.op("dve", lambda e: e.tensor_scalar(out=bef, in0=bef, scalar1=31.0, scalar2=None, op0=ALU.min), reads=[t_rs], writes=[t_rs])
    S.op("dve", lambda e: e.tensor_scalar(out=be1024, in0=bef, scalar1=1024.0, scalar2=None, op0=ALU.mult), reads=[t_rs], writes=[t_rs])
    S.op("dve", lambda e: e.tensor_scalar(out=be512, in0=bef, scalar1=512.0, scalar2=None, op0=ALU.mult), reads=[t_rs], writes=[t_rs])
    S.op("dve", lambda e: e.tensor_scalar(out=rs[:, 384:448], in0=TH, scalar1=rs[:, 127:128], scalar2=None, op0=ALU.is_ge), reads=[t_rs, t_const], writes=[t_rs])
    S.op("dve", lambda e: e.scalar_tensor_tensor(out=be1024, in0=rs[:, 384:448], scalar=1.0e6, in1=be1024, op0=ALU.mult, op1=ALU.add), reads=[t_rs], writes=[t_rs])
    S.op("dve", lambda e: e.scalar_tensor_tensor(out=be512, in0=rs[:, 384:448], scalar=1.0e6, in1=be512, op0=ALU.mult, op1=ALU.add), reads=[t_rs], writes=[t_rs])
    idxf = sb("idxf", [128, 64 * 12], F32)
    t_idxf = Tok()
    for b in range(64):
        S.op("dve", lambda e, b=b: e.tensor_scalar(out=idxf[:, b * 12:b * 12 + 8], in0=consts[:, 770:778], scalar1=rs[:, 256 + b:257 + b], scalar2=None, op0=ALU.add),
             reads=[t_rs, t_const], wadd=[t_idxf])
        S.op("dve", lambda e, b=b: e.tensor_scalar(out=idxf[:, b * 12 + 8:b * 12 + 12], in0=consts[:, 770:774], scalar1=rs[:, 320 + b:321 + b], scalar2=None, op0=ALU.add),
             reads=[t_rs, t_const], wadd=[t_idxf])
    S.op("dve", lambda e: e.tensor_copy(out=idxi[:], in_=idxf[:]), reads=[t_idxf], writes=[t_idx])
    dump("DestF", DestF[:], [128, 32], [t_destf])
    dump("rs", rs[:], [128, 512], [t_rs])
    release(n_s6)
    if dbg == 7:
        S.finish([t_x1, t_buf, t_idx] + dump_toks)
        nc.used_inputs = used_inputs
        return nc

    n_s7 = len(es)
    bc_reg = nc.gpsimd.to_reg(NE * D - 1)
    ewi_rows = ewi_d.rearrange("e r n -> (e r) n")
    ewo_rows = ewo_d.rearrange("e r n -> (e r) n")
    wib_ring = Ring([sb("wib%d" % i, [128, 8, 2 * DEXP], BF16) for i in range(2)])
    wob_ring = Ring([sb("wob%d" % i, [128, 4, D], BF16) for i in range(2)])
    wif_ring = Ring([sb("wif%d" % i, [128, 8, 2 * DEXP], F32) for i in range(2)])
    wof_ring = Ring([sb("wof%d" % i, [128, 4, D], F32) for i in range(2)])
    xb_ring = Ring([sb("xbr%d" % i, [128, D], BF16) for i in range(2)])
    xbT_ring = Ring([sb("xbT%d" % i, [128, 8, 128], BF16) for i in range(2)])
    sil_ring = Ring([sb("sil%d" % i, [128, 512], F32) for i in range(2)])
    hT_ring = Ring([sb("hT%d" % i, [128, 4, 128], BF16) for i in range(2)])
    ysb_ring = Ring([sb("ysb%d" % i, [128, D], F32) for i in range(2)])
    t_ybuf = Tok()
    for b in range(int(os.environ.get("BLIM", 64))):
        wif, wiftok = wif_ring.next()
        wof, woftok = wof_ring.next()
        for k in range(8):
            S.idma(wif[:, k, :], None, ewi_rows[:, :], bass.IndirectOffsetOnAxis(ap=idxi[:, b * 12 + k:b * 12 + k + 1], axis=0),
                   reads=[t_idx], bounds_check=bc_reg, oob_is_err=False, **(dict(writes=[wiftok]) if k == 0 else dict(wadd=[wiftok])))
        for j in range(4):
            S.idma(wof[:, j, :], None, ewo_rows[:, :], bass.IndirectOffsetOnAxis(ap=idxi[:, b * 12 + 8 + j:b * 12 + 9 + j], axis=0),
                   reads=[t_idx], bounds_check=bc_reg, oob_is_err=False, **(dict(writes=[woftok]) if j == 0 else dict(wadd=[woftok])))
        wib, wibtok = wib_ring.next()
        wob, wobtok = wob_ring.next()
        for hf in range(2):
            S.op("dve", lambda e, wib=wib, wif=wif, hf=hf: e.tensor_copy(out=wib[:, hf * 4:(hf + 1) * 4, :], in_=wif[:, hf * 4:(hf + 1) * 4, :]),
                 reads=[wiftok], **(dict(writes=[wibtok]) if hf == 0 else dict(wadd=[wibtok])))
        S.op("dve", lambda e, wob=wob, wof=wof: e.tensor_copy(out=wob[:], in_=wof[:]), reads=[woftok], writes=[wobtok])
        xb, xbtok = xb_ring.next()
        S.dma("sp", xb[:], buf_d[b * 128:(b + 1) * 128, :], reads=[t_buf], writes=[xbtok])
        pb, pbtok = psB_ring.next()
        for k in range(8):
            S.op("pe", lambda e, pb=pb, xb=xb, k=k: e.transpose(pb[:, k * 128:(k + 1) * 128], xb[:, k * 128:(k + 1) * 128], ident_b), reads=[xbtok, t_cb], writes=[pbtok])
        xbT, xbTtok = xbT_ring.next()
        S.op("dve", lambda e, xbT=xbT, pb=pb: e.tensor_copy(out=xbT[:].rearrange("p k n -> p (k n)"), in_=pb[:, :]), reads=[pbtok], writes=[xbTtok])
        pg, pgtok = psA_ring.next()
        pu, putok = psA_ring.next()
        for (pp, pptok, c0) in ((pg, pgtok, 0), (pu, putok, DEXP)):
            for j in range(4):
                for k in range(8):
                    S.op("pe", lambda e, pp=pp, j=j, k=k, c0=c0, wib=wib, xbT=xbT: e.matmul(
                        pp[:, j * 128:(j + 1) * 128], wib[:, k, c0 + j * 128:c0 + (j + 1) * 128], xbT[:, k, :], start=(k == 0), stop=(k == 7)),
                        reads=[wibtok, xbTtok], writes=[pptok])
        sil, siltok = sil_ring.next()
        S.op("act", lambda e, sil=sil, pg=pg: e.activation(out=sil[:], in_=pg[:, :], func=AF.Silu), reads=[pgtok], writes=[siltok])
        hT, hTtok = hT_ring.next()
        S.op("dve", lambda e, hT=hT, sil=sil, pu=pu: e.tensor_tensor(out=hT[:].rearrange("p j n -> p (j n)"), in0=pu[:, :], in1=sil[:], op=ALU.mult),
             reads=[putok, siltok], writes=[hTtok])
        ysb, ysbtok = ysb_ring.next()
        for half in range(2):
            py, pytok = psA_ring.next()
            for j in range(4):
                S.op("pe", lambda e, py=py, j=j, hT=hT, wob=wob, half=half: e.matmul(
                    py[:, :], hT[:, j, :], wob[:, j, half * 512:(half + 1) * 512], start=(j == 0), stop=(j == 3)), reads=[hTtok, wobtok], writes=[pytok])
            S.op("dve", lambda e, py=py, ysb=ysb, half=half: e.tensor_copy(out=ysb[:, half * 512:(half + 1) * 512], in_=py[:, :]),
                 reads=[pytok], **(dict(writes=[ysbtok]) if half == 0 else dict(wadd=[ysbtok])))
        S.dma("sp", ybuf_d[b * 128:(b + 1) * 128, :], ysb[:], reads=[ysbtok], wadd=[t_ybuf])
    release(n_s7)

    fng = sb("fng", [128, D], F32)
    t_fng = Tok()
    S.dma("sp", fng[:], rows_d[0:1, R_FNG:R_FNG + D].partition_broadcast(128), writes=[t_fng])
    fj = sb("fjunk", [128, D], F32)
    t_fj = Tok()
    fs_ring = Ring([(sb("fsa%d" % i, [128, 1], F32), sb("fsb%d" % i, [128, 1], F32)) for i in range(4)])
    y1_ring = Ring([sb("y1g%d" % i, [128, D], F32) for i in range(2)])
    y2_ring = Ring([sb("y2g%d" % i, [128, D], F32) for i in range(2)])
    x1l_ring = Ring([sb("x1l%d" % i, [128, D], F32) for i in range(2)])
    t_out = Tok()
    for ti in range(16):
        y1, y1tok = y1_ring.next()
        y2, y2tok = y2_ring.next()
        S.idma(y1[:, :], None, ybuf_d[:, :], bass.IndirectOffsetOnAxis(ap=Desti[:, ti * 2:ti * 2 + 1], axis=0), reads=[t_dest, t_ybuf], writes=[y1tok])
        S.idma(y2[:, :], None, ybuf_d[:, :], bass.IndirectOffsetOnAxis(ap=Desti[:, ti * 2 + 1:ti * 2 + 2], axis=0), reads=[t_dest, t_ybuf], writes=[y2tok])
        xl, xltok = x1l_ring.next()
        S.dma("sp", xl[:], x1_d[ti], reads=[t_x1], writes=[xltok])
        S.op("dve", lambda e, y1=y1, ti=ti: e.tensor_scalar(out=y1[:], in0=y1[:], scalar1=Wt[:, ti * 2:ti * 2 + 1], scalar2=None, op0=ALU.mult), reads=[y1tok, t_Wt], writes=[y1tok])
        S.op("dve", lambda e, y1=y1, y2=y2, ti=ti: e.scalar_tensor_tensor(out=y1[:], in0=y2[:], scalar=Wt[:, ti * 2 + 1:ti * 2 + 2], in1=y1[:], op0=ALU.mult, op1=ALU.add),
             reads=[y1tok, y2tok, t_Wt], writes=[y1tok])
        S.op("dve", lambda e, y1=y1: e.tensor_tensor(out=y1[:], in0=y1[:], in1=g12[:, D:2 * D], op=ALU.mult), reads=[y1tok, t_g12], writes=[y1tok])
        S.op("dve", lambda e, y1=y1, xl=xl: e.tensor_tensor(out=xl[:], in0=y1[:], in1=xl[:], op=ALU.add), reads=[y1tok, xltok], writes=[xltok])
        fs, fstok = fs_ring.next()
        S.op("act", lambda e, xl=xl, fs=fs: e.activation(out=fj[:], in_=xl[:], func=AF.Square, accum_out=fs[0][:, 0:1]), reads=[xltok], writes=[t_fj, fstok])
        S.op("dve", lambda e, fs=fs: e.tensor_scalar(out=fs[1][:, 0:1], in0=fs[0][:, 0:1], scalar1=1.0 / D, scalar2=EPS, op0=ALU.mult, op1=ALU.add), reads=[fstok], writes=[fstok])
        S.op("act", lambda e, fs=fs: e.activation(out=fs[0][:, 0:1], in_=fs[1][:, 0:1], func=AF.Ln), reads=[fstok], writes=[fstok])
        S.op("act", lambda e, fs=fs: e.activation(out=fs[1][:, 0:1], in_=fs[0][:, 0:1], func=AF.Exp, scale=-0.5), reads=[fstok], writes=[fstok])
        S.op("dve", lambda e, xl=xl, fs=fs: e.scalar_tensor_tensor(out=xl[:], in0=xl[:], scalar=fs[1][:, 0:1], in1=fng[:], op0=ALU.mult, op1=ALU.mult),
             reads=[fstok, t_fng, xltok], writes=[xltok])
        S.dma("sp", out_d[ti * 128:(ti + 1) * 128, :], xl[:], reads=[xltok], wadd=[t_out])
    S.finish([t_out] + dump_toks)
    nc.used_inputs = used_inputs
    return nc


def _host_inputs(inp):
    f = lambda a: np.ascontiguousarray(np.asarray(a, dtype=np.float32))
    x, c, ctx, c_ctx = f(inp["x"]), f(inp["c"]), f(inp["ctx"]), f(inp["c_ctx"])
    ada_w, ada_b = f(inp["ada_w"])[0], f(inp["ada_b"])[0]
    w_in = f(inp["w_in"])[0]
    up_w, up_b = f(inp["gla_up_w"])[0], f(inp["gla_up_b"])[0]
    conv_w, conv_b = f(inp["ml_conv_w"])[0], f(inp["ml_conv_b"])[0]
    i_b, f_b = f(inp["ml_i_b"])[0], f(inp["ml_f_b"])[0]
    consts = np.zeros((128, 1024), np.float32)
    consts[0:64, 512:640] = 1.0
    consts[64:128, 640:768] = 1.0
    consts[0:64, 768] = 1.0
    consts[64:128, 769] = 1.0
    consts[:, 770:782] = (np.arange(12)[None, :] % 8) * 128 + np.arange(128)[:, None]
    consts[:, 782:846] = np.arange(64)[None, :] * 128.0
    consts[:, 0:128] = np.eye(128)
    consts[:, 128:256] = np.triu(np.ones((128, 128)))
    consts[:, 256:384] = np.tril(np.ones((128, 128)))
    consts[:, 384:512] = 1.0
    router_w = np.concatenate([f(inp["router_group_w"])[0], f(inp["router_expert_w"])[0]], axis=1)
    shared = {
        "ada_w": ada_w, "ada_b": ada_b[None, :], "w_out": f(inp["w_out"])[0], "router_w": np.ascontiguousarray(router_w),
        "e_w_in": f(inp["expert_w_in"])[0], "e_w_out": f(inp["expert_w_out"])[0], "consts": consts,
    }
    maps = []
    for core in range(8):
        b, flip = core // 2, core % 2
        xs, cs = x[b], ctx[b]
        win, uw, ub, cw, ib, fb = w_in, up_w, up_b, conv_w, i_b, f_b
        if flip:
            xs, cs = xs[::-1], cs[::-1]
            win = win.copy()
            win[:, C_LR:C_LR + 16], win[:, C_LR + 16:C_LR + 32] = w_in[:, C_LR + 16:C_LR + 32], w_in[:, C_LR:C_LR + 16]
            win[:, C_MI:C_MI + 4], win[:, C_MI + 4:C_MI + 8] = w_in[:, C_MI + 4:C_MI + 8], w_in[:, C_MI:C_MI + 4]
            win[:, C_MI + 8:C_MI + 12], win[:, C_MI + 12:C_MI + 16] = w_in[:, C_MI + 12:C_MI + 16], w_in[:, C_MI + 8:C_MI + 12]
            uw, ub, ib, fb = uw[::-1], ub[::-1], ib[::-1], fb[::-1]
            cw = cw[::-1, ::-1]
        vecs = np.zeros((128, NV), np.float32)
        vecs[:, V_N1G:V_N1G + 8] = f(inp["norm1_g"])[0].reshape(8, 128).T
        vecs[:, V_N2G:V_N2G + 8] = f(inp["norm2_g"])[0].reshape(8, 128).T
        vecs[:, V_UPB:V_UPB + 4] = ub.reshape(2, 2, 128).transpose(2, 0, 1).reshape(128, 4)
        vecs[:, V_CONVB:V_CONVB + 8] = conv_b.reshape(8, 128).T
        vecs[:, V_CONVW:V_CONVW + 72] = cw.reshape(9, 8, 128).transpose(2, 1, 0).reshape(128, 72)
        vecs[:, V_GNG:V_GNG + 4] = f(inp["gla_norm_g"])[0].reshape(4, 128).T
        vecs[:, V_MNG:V_MNG + 4] = f(inp["ml_norm_g"])[0].reshape(4, 128).T
        rows = np.zeros((1, NR), np.float32)
        rows[0, R_GATEB:R_GATEB + 8] = ib.reshape(8)
        rows[0, R_GATEB + 8:R_GATEB + 16] = fb.reshape(8)
        rows[0, R_FNG:R_FNG + 1024] = f(inp["final_norm_g"])
        rows[0, R_RB:R_RB + 4] = f(inp["router_group_b"])[0]
        rows[0, R_RB + 4:R_RB + 36] = f(inp["router_expert_b"])[0]
        rows[0, R_N2G:R_N2G + 1024] = f(inp["norm2_g"])[0]
        cvec = np.concatenate([c[b].reshape(128, 8), c_ctx.reshape(128, 8)], axis=1)
        m = dict(shared)
        m.update({
            "x": np.ascontiguousarray(xs), "ctx": np.ascontiguousarray(cs), "cvec": np.ascontiguousarray(cvec),
            "vecs": vecs, "rows": rows, "w_in": np.ascontiguousarray(win), "up_w": np.ascontiguousarray(uw),
        })
        maps.append(m)
    return maps


def kernel(**inputs):
    maps = _host_inputs(inputs)
    nc = build()
    maps = [{k: m[k] for k in nc.used_inputs} for m in maps]
    res = run_bass_kernel_spmd(nc, maps, core_ids=list(range(8)))
    out = np.zeros((4, SEQ, D), np.float32)
    for core in range(8):
        b, flip = core // 2, core % 2
        o = res.results[core]["out"]
        if flip:
            out[b, NLOC:] = o[::-1]
        else:
            out[b, :NLOC] = o
    return out
```

```python
import numpy as np
import concourse.bass as bass
import concourse.mybir as mybir
from concourse.bass_utils import run_bass_kernel_spmd

F32 = mybir.dt.float32
BF16 = mybir.dt.bfloat16
AF = mybir.ActivationFunctionType
ALU = mybir.AluOpType
AX = mybir.AxisListType

D = 1024
SEQ = 4096
NLOC = 2048
CTX = 256
INW = 3632
NE = 32
DEXP = 512
EPS = 1e-6
NBLK = 9
C_GQ, C_GK, C_GV, C_GG, C_LR, C_MQ, C_MK, C_MV, C_MO, C_MI = 0, 256, 512, 1024, 1536, 1568, 2080, 2592, 3104, 3616

V_N1G, V_N2G, V_UPB, V_CONVB, V_CONVW, V_GNG, V_MNG = 0, 8, 16, 20, 28, 100, 104
NV = 108
R_GATEB, R_FNG, R_RB = 0, 16, 16 + 1024
R_N2G = 16 + 1024 + 36
NR = 16 + 1024 + 36 + 1024
I32 = mybir.dt.int32


class Tok:
    __slots__ = ("w", "r")

    def __init__(self):
        self.w = {}
        self.r = {}


class Sched:
    NDMA = 6

    def __init__(self, nc):
        self.nc = nc
        self.eng = {"pe": nc.tensor, "dve": nc.vector, "act": nc.scalar, "pool": nc.gpsimd, "sp": nc.sync}
        self.sem = {}
        self.cnt = {}
        self.waited = {k: {} for k in self.eng}
        self._cms = []
        for k in ["pe", "dve", "act", "pool"]:
            self._mk(k)
        self.dq = {}
        self.nq = {"sp": 8, "pool": 16, "act": 2}
        for q in ["sp", "pool", "act"]:
            names = []
            for i in range(self.nq[q]):
                n = "d%s%d" % (q, i)
                self._mk(n)
                names.append(n)
            self.dq[q] = [names, 0]
        self.ninstr = 0

    def _mk(self, k):
        cm = self.nc.semaphore("s_" + k)
        self.sem[k] = cm.__enter__()
        self._cms.append(cm)
        self.cnt[k] = 0

    def _wait(self, e, key, val):
        if self.waited[e].get(key, 0) >= val:
            return
        self.eng[e].wait_ge(self.sem[key], val)
        self.waited[e][key] = val

    def _deps(self, e, reads, writes):
        for t in reads:
            for k, v in t.w.items():
                if not (k == "pe" and e == "pe"):
                    self._wait(e, k, v)
        for t in writes:
            for k, v in t.w.items():
                if not (k == "pe" and e == "pe"):
                    self._wait(e, k, v)
            for k, v in t.r.items():
                if k != e:
                    self._wait(e, k, v)

    def _mark(self, key, val, reads, writes, wadd):
        for t in reads:
            if t.r.get(key, 0) < val:
                t.r[key] = val
        for t in writes:
            t.w = {key: val}
            t.r = {}
        for t in wadd:
            t.w[key] = val

    def op(self, e, fn, reads=(), writes=(), wadd=()):
        self._deps(e, reads, writes)
        for t in wadd:
            for k, v in t.r.items():
                if k != e:
                    self._wait(e, k, v)
        ins = fn(self.eng[e])
        self.cnt[e] += 1
        ins.then_inc(self.sem[e], 1)
        self._mark(e, self.cnt[e], reads, writes, wadd)
        self.ninstr += 1
        return ins

    def dma(self, q, out, in_, reads=(), writes=(), wadd=(), **kw):
        names, idx = self.dq[q]
        key = names[idx % len(names)]
        self.dq[q][1] = idx + 1
        self._wait(q, key, self.cnt[key])
        self._deps(q, reads, writes)
        for t in wadd:
            for k, v in t.r.items():
                self._wait(q, k, v)
        ins = self.eng[q].dma_start(out=out, in_=in_, **kw)
        self.cnt[key] += 16
        ins.then_inc(self.sem[key], 16)
        self._mark(key, self.cnt[key], reads, writes, wadd)
        self.ninstr += 1
        return ins

    def idma(self, out, out_off, in_, in_off, reads=(), writes=(), wadd=(), **kw):
        q = "pool"
        names, idx = self.dq[q]
        key = names[idx % len(names)]
        self.dq[q][1] = idx + 1
        self._wait(q, key, self.cnt[key])
        self._deps(q, reads, writes)
        for t in wadd:
            for k, v in t.r.items():
                self._wait(q, k, v)
        ins = self.eng[q].indirect_dma_start(out=out, out_offset=out_off, in_=in_, in_offset=in_off, **kw)
        self.cnt[key] += 16
        ins.then_inc(self.sem[key], 16)
        self._mark(key, self.cnt[key], reads, writes, wadd)
        self.ninstr += 1
        return ins

    def barrier(self):
        import os
        if os.environ.get('NOBAR'):
            return
        for e in self.eng:
            if e == 'pool' and os.environ.get('NOPOOLBAR'):
                continue
            for k in self.sem:
                if self.cnt[k] > 0 and k != e:
                    self._wait(e, k, self.cnt[k])

    def finish(self, toks, e="sp"):
        for t in toks:
            for k, v in t.w.items():
                self._wait(e, k, v)


class Ring:
    def __init__(self, items):
        self.items = items
        self.toks = [Tok() for _ in items]
        self.i = 0

    def next(self):
        j = self.i % len(self.items)
        self.i += 1
        return self.items[j], self.toks[j]


def build(dbg=0):
    import os
    dbg = int(os.environ.get("KSTOP", dbg))
    nc = bass.Bass("TRN2", target_bir_lowering=False)
    import os
    scratch_kind = "ExternalOutput" if (dbg or os.environ.get("SCR_EXT")) else "Internal"

    used_inputs = []

    def din(name, shape, dt=F32, need=0):
        if dbg and dbg < need:
            return None
        used_inputs.append(name)
        return nc.dram_tensor(name, list(shape), dt, kind="ExternalInput").ap()

    dump_toks = []

    def dump(name, ap, shape, toks, dt=F32):
        if not dbg:
            return
        dd = nc.dram_tensor("dbg_" + name, list(shape), dt, kind="ExternalOutput").ap()
        t = Tok()
        S.dma("sp", dd, ap, reads=toks, writes=[t])
        dump_toks.append(t)

    def dscr(name, shape, dt):
        return nc.dram_tensor(name, list(shape), dt, kind=scratch_kind).ap()

    x_d = din("x", [SEQ, D])
    ctx_d = din("ctx", [CTX, D])
    cvec_d = din("cvec", [128, 16])
    adaw_d = din("ada_w", [D, 6 * D])
    adab_d = din("ada_b", [1, 6 * D])
    vecs_d = din("vecs", [128, NV])
    rows_d = din("rows", [1, NR])
    win_d = din("w_in", [D, INW])
    upw_d = din("up_w", [2, 16, 256])
    wout_d = din("w_out", [D, D])
    rw_d = din("router_w", [D, 36])
    ewi_d = din("e_w_in", [NE, D, 2 * DEXP], need=8)
    ewo_d = din("e_w_out", [NE, DEXP, D], need=8)
    consts_d = din("consts", [128, 1024])
    out_d = nc.dram_tensor("out", [NLOC, D], F32, kind="ExternalOutput").ap()

    xnT_d = dscr("xnT_s", [NBLK, 128, 8 * 512], BF16)
    gqk_d = dscr("gqk_s", [NBLK, 128, 4 * 512], BF16)
    lr_d = dscr("lr_s", [NBLK, 16, 2 * 512], F32)
    sgg_d = dscr("sgg_s", [4, 128, 4 * 512], BF16)
    smo_d = dscr("smo_s", [4, 128, 4 * 512], BF16)
    mpre_d = dscr("mpre_s", [NBLK, 128, 8 * 512], BF16)
    gv_d = dscr("gv_s", [NBLK, 128, 4 * 512], BF16)
    mv_d = dscr("mv_s", [NBLK, 128, 4 * 512], BF16)
    gates_d = dscr("gates_s", [NBLK, 128, 4 * 16], F32)
    mqk_d = dscr("mqk_s", [NBLK, 128, 8 * 512], BF16)

    S = Sched(nc)
    es = []
    uid = [0]

    def sb(name, shape, dt):
        uid[0] += 1
        cm = nc.sbuf_tensor("sb%d_%s" % (uid[0], name), list(shape), dt)
        t = cm.__enter__()
        es.append(cm)
        return t

    def ps(name, shape, dt):
        uid[0] += 1
        cm = nc.psum_tensor("ps%d_%s" % (uid[0], name), list(shape), dt)
        t = cm.__enter__()
        es.append(cm)
        return t

    def release(n0):
        S.barrier()
        while len(es) > n0:
            es.pop().__exit__(None, None, None)

    consts = sb("consts", [128, 1024], F32)
    vecs = sb("vecs", [128, NV], F32)
    t_const = Tok()
    S.dma("sp", consts[:], consts_d[:, :], writes=[t_const])
    S.dma("sp", vecs[:], vecs_d[:, :], wadd=[t_const])
    ident_f = consts[:, 0:128]
    ones_f = consts[:, 384:512]
    cb = sb("constsb", [128, 512], BF16)
    t_cb = Tok()
    S.op("dve", lambda e: e.tensor_copy(out=cb[:], in_=consts[:, 0:512]), reads=[t_const], writes=[t_cb])
    ident_b = cb[:, 0:128]
    mask_b = [cb[:, 128:256], cb[:, 256:384]]
    ones_b = cb[:, 384:512]

    psA = [ps("psA%d" % i, [128, 512], F32) for i in range(6)]
    psB = [ps("psB%d" % i, [128, 1024], BF16) for i in range(2)]
    psA_ring = Ring(psA)
    psB_ring = Ring(psB)

    t_mod = Tok()
    A1 = sb("A1", [128, 4 * 8], F32)
    A2 = sb("A2", [128, 2 * 8], F32)
    t_A = Tok()
    g12 = sb("g12", [128, 4 * D], F32)
    t_g12 = Tok()
    idxi = sb("idxi", [128, 64 * 12], I32)
    Desti = sb("Desti", [128, 32], I32)
    Wt = sb("Wt", [128, 32], F32)
    t_idx, t_dest, t_Wt = Tok(), Tok(), Tok()

    n_keep = len(es)
    modx = sb("modx", [1, 6 * D], F32)
    modc = sb("modc", [1, 2 * D], F32)
    cvec = sb("cvec", [128, 16], F32)
    scv = sb("scv", [128, 16], F32)
    adab = sb("adab", [1, 6 * D], F32)
    t_cv, t_scv, t_adab = Tok(), Tok(), Tok()
    S.dma("sp", cvec[:], cvec_d[:, :], writes=[t_cv])
    S.dma("sp", adab[:], adab_d[:, :], writes=[t_adab])
    S.op("act", lambda e: e.activation(out=scv[:], in_=cvec[:], func=AF.Silu), reads=[t_cv], writes=[t_scv])
    adaw_v = adaw_d.rearrange("(p k) n -> p k n", k=8)
    wst = [sb("adaw%d" % i, [128, 8, 512], F32) for i in range(2)]
    wst_ring = Ring(wst)
    S.op("dve", lambda e: e.memset(modx[:], 0.0), writes=[t_mod])
    for blk in range(12):
        wt, wtok = wst_ring.next()
        S.dma("sp", wt[:], adaw_v[:, :, blk * 512:(blk + 1) * 512], writes=[wtok])
        for which in range(2):
            if which == 1 and blk >= 4:
                continue
            pt, ptok = psA_ring.next()
            for k in range(8):
                S.op("pe", lambda e, k=k, pt=pt, wt=wt, which=which: e.matmul(
                    pt[0:1, :], scv[:, which * 8 + k:which * 8 + k + 1], wt[:, k, :], start=(k == 0), stop=(k == 7)),
                    reads=[t_scv, wtok], writes=[ptok])
            dst = modx if which == 0 else modc
            S.op("dve", lambda e, pt=pt, dst=dst, blk=blk: e.tensor_tensor(
                out=dst[0:1, blk * 512:(blk + 1) * 512], in0=pt[0:1, :], in1=adab[0:1, blk * 512:(blk + 1) * 512], op=ALU.add),
                reads=[ptok, t_adab], wadd=[t_mod])
    colps, coltok = psA_ring.next()
    specs = [(modx, 1 * D), (modx, 0 * D), (modc, 1 * D), (modc, 0 * D), (modx, 4 * D), (modx, 3 * D)]
    first = True
    for si, (src, off) in enumerate(specs):
        for k in range(8):
            S.op("pe", lambda e, src=src, off=off, k=k, si=si: e.matmul(
                colps[:, si * 8 + k:si * 8 + k + 1], src[0:1, off + k * 128:off + (k + 1) * 128], ones_f[0:1, 0:1], start=True, stop=True),
                reads=[t_mod, t_const], writes=[coltok] if first else (), wadd=() if first else [coltok])
            first = False
    for (dst, c0, g0, s_sc, s_sh) in [(A1, 0, V_N1G, 0, 1), (A1, 16, V_N1G, 2, 3), (A2, 0, V_N2G, 4, 5)]:
        S.op("dve", lambda e, dst=dst, c0=c0, g0=g0, s_sc=s_sc: e.scalar_tensor_tensor(
            out=dst[:, c0:c0 + 8], in0=colps[:, s_sc * 8:s_sc * 8 + 8], scalar=1.0, in1=vecs[:, g0:g0 + 8], op0=ALU.add, op1=ALU.mult),
            reads=[coltok, t_const], wadd=[t_A])
        S.op("dve", lambda e, dst=dst, c0=c0, s_sh=s_sh: e.tensor_copy(out=dst[:, c0 + 8:c0 + 16], in_=colps[:, s_sh * 8:s_sh * 8 + 8]),
             reads=[coltok], wadd=[t_A])
    for gi, off in enumerate([2 * D, 5 * D]):
        for hf in range(2):
            pt, ptok = psA_ring.next()
            S.op("pe", lambda e, pt=pt, off=off, hf=hf: e.matmul(pt[:, :], ones_f[0:1, :], modx[0:1, off + hf * 512:off + (hf + 1) * 512], start=True, stop=True),
                 reads=[t_mod, t_const], writes=[ptok])
            S.op("act", lambda e, pt=pt, gi=gi, hf=hf: e.copy(out=g12[:, gi * D + hf * 512:gi * D + (hf + 1) * 512], in_=pt[:, :]),
                 reads=[ptok], wadd=[t_g12])
    n2gbc = sb("n2gbc", [128, D], F32)
    t_n2g = Tok()
    S.dma("sp", n2gbc[:], rows_d[0:1, R_N2G:R_N2G + D].partition_broadcast(128), writes=[t_n2g])
    for gi, off in ((2, 4 * D), (3, 3 * D)):
        for hf in range(2):
            pt, ptok = psA_ring.next()
            S.op("pe", lambda e, pt=pt, off=off, hf=hf: e.matmul(pt[:, :], ones_f[0:1, :], modx[0:1, off + hf * 512:off + (hf + 1) * 512], start=True, stop=True),
                 reads=[t_mod, t_const], writes=[ptok])
            dst = g12[:, gi * D + hf * 512:gi * D + (hf + 1) * 512]
            if gi == 2:
                S.op("dve", lambda e, pt=pt, dst=dst, hf=hf: e.scalar_tensor_tensor(out=dst, in0=pt[:, :], scalar=1.0, in1=n2gbc[:, hf * 512:(hf + 1) * 512], op0=ALU.add, op1=ALU.mult),
                     reads=[ptok, t_n2g], wadd=[t_g12])
            else:
                S.op("dve", lambda e, pt=pt, dst=dst: e.tensor_copy(out=dst, in_=pt[:, :]), reads=[ptok], wadd=[t_g12])
    dump("g12", g12[:, 0:2 * D], [128, 2 * D], [t_g12])
    dump("A1", A1[:], [128, 32], [t_A])
    dump("modx", modx[:], [1, 6 * D], [t_mod])
    if dbg == 1:
        S.finish(dump_toks)
        nc.used_inputs = used_inputs
        return nc
    release(n_keep)

    xt_ring = Ring([sb("xt%d" % i, [128, D], F32) for i in range(2)])
    xs_ring = Ring([sb("xs%d" % i, [128, D], BF16) for i in range(2)])
    junk = sb("junk", [128, D], F32)
    xsf_ring = Ring([sb("xsf%d" % i, [128, D], F32) for i in range(2)])
    t_junk = Tok()
    ss_ring = Ring([(sb("ssa%d" % i, [128, 1], F32), sb("ssb%d" % i, [128, 1], F32)) for i in range(4)])
    xnb_ring = Ring([sb("xnb%d" % i, [128, 8, 512], BF16) for i in range(2)])
    t_xnT = [Tok() for _ in range(NBLK)]
    import os
    for blk in range(int(os.environ.get('KLIM', NBLK))):
        ntile = 2 if blk == 0 else 4
        xnb, xnbtok = xnb_ring.next()
        xfirst = [True]

        def xw(tok=xnbtok, xfirst=xfirst):
            if xfirst[0]:
                xfirst[0] = False
                return dict(writes=[tok])
            return dict(wadd=[tok])
        for ti in range(ntile):
            if blk == 0:
                src = ctx_d[ti * 128:(ti + 1) * 128, :]
                acol = 16
            else:
                r0 = (blk - 1) * 512 + ti * 128
                src = x_d[r0:r0 + 128, :]
                acol = 0
            xt, xttok = xt_ring.next()
            S.dma("sp", xt[:], src, writes=[xttok])
            ss, sstok = ss_ring.next()
            S.op("act", lambda e, xt=xt, ss=ss: e.activation(out=junk[:], in_=xt[:], func=AF.Square, accum_out=ss[0][:, 0:1]),
                 reads=[xttok], writes=[t_junk, sstok])
            S.op("dve", lambda e, ss=ss: e.tensor_scalar(out=ss[1][:, 0:1], in0=ss[0][:, 0:1], scalar1=1.0 / D, scalar2=EPS, op0=ALU.mult, op1=ALU.add),
                 reads=[sstok], writes=[sstok])
            S.op("act", lambda e, ss=ss: e.activation(out=ss[0][:, 0:1], in_=ss[1][:, 0:1], func=AF.Ln), reads=[sstok], writes=[sstok])
            S.op("act", lambda e, ss=ss: e.activation(out=ss[1][:, 0:1], in_=ss[0][:, 0:1], func=AF.Exp, scale=-0.5), reads=[sstok], writes=[sstok])
            xs, xstok = xs_ring.next()
            S.op("dve", lambda e, xs=xs, xt=xt, ss=ss: e.tensor_scalar(out=xs[:], in0=xt[:], scalar1=ss[1][:, 0:1], scalar2=None, op0=ALU.mult),
                 reads=[xttok, sstok], writes=[xstok])
            pb, pbtok = psB_ring.next()
            for k in range(8):
                S.op("pe", lambda e, pb=pb, xs=xs, k=k: e.transpose(pb[:, k * 128:(k + 1) * 128], xs[:, k * 128:(k + 1) * 128], ident_b),
                     reads=[xstok, t_cb], writes=[pbtok])
            for k in range(8):
                eng = "dve"
                if eng == "act":
                    S.op("act", lambda e, pb=pb, xnb=xnb, k=k, ti=ti, acol=acol: e.activation(
                        out=xnb[:, k, ti * 128:(ti + 1) * 128], in_=pb[:, k * 128:(k + 1) * 128], func=AF.Identity,
                        scale=A1[:, acol + k:acol + k + 1], bias=A1[:, acol + 8 + k:acol + 8 + k + 1]),
                        reads=[pbtok, t_A], **xw())
                else:
                    S.op("dve", lambda e, pb=pb, xnb=xnb, k=k, ti=ti, acol=acol: e.tensor_scalar(
                        out=xnb[:, k, ti * 128:(ti + 1) * 128], in0=pb[:, k * 128:(k + 1) * 128],
                        scalar1=A1[:, acol + k:acol + k + 1], scalar2=A1[:, acol + 8 + k:acol + 8 + k + 1], op0=ALU.mult, op1=ALU.add),
                        reads=[pbtok, t_A], **xw())
        S.dma("sp", xnT_d[blk], xnb[:].rearrange("p k n -> p (k n)"), reads=[xnbtok], writes=[t_xnT[blk]])
    release(n_keep)

    final_toks = list(t_xnT)
    if dbg == 2:
        S.finish(final_toks + dump_toks)
        nc.used_inputs = used_inputs
        return nc

    n_s2 = len(es)
    win_v = win_d.rearrange("(k p) n -> p k n", p=128)
    Wb = sb("Wb", [128, 8, INW], BF16)
    t_Wb = Tok()
    wstg = Ring([sb("wstg%d" % i, [128, 8, 227], F32) for i in range(2)])
    for pc in range(16):
        st, sttok = wstg.next()
        S.dma("sp", st[:], win_v[:, :, pc * 227:(pc + 1) * 227], writes=[sttok])
        S.op("dve", lambda e, st=st, pc=pc: e.tensor_copy(out=Wb[:, :, pc * 227:(pc + 1) * 227], in_=st[:]),
             reads=[sttok], **(dict(writes=[t_Wb]) if pc == 0 else dict(wadd=[t_Wb])))
    xin_ring = Ring([sb("xin%d" % i, [128, 8, 512], BF16) for i in range(2)])
    stg = {}
    for nm, shp, dt in [("gqk", [128, 4 * 512], BF16), ("sgg", [128, 4 * 512], BF16), ("smo", [128, 4 * 512], BF16),
                        ("mpre", [128, 8 * 512], BF16), ("gv", [128, 4 * 512], BF16), ("mv", [128, 4 * 512], BF16),
                        ("lr", [16, 2 * 512], F32), ("gates", [128, 64], F32)]:
        stg[nm] = Ring([sb("st_%s%d" % (nm, i), shp, dt) for i in range(2)])
    sig_ring = Ring([sb("sigt%d" % i, [128, 512], F32) for i in range(2)])
    t_gqk = [Tok() for _ in range(NBLK)]
    t_lr = [Tok() for _ in range(NBLK)]
    t_sgg = [Tok() for _ in range(4)]
    t_smo = [Tok() for _ in range(4)]
    t_mpre = [Tok() for _ in range(NBLK)]
    t_gv = [Tok() for _ in range(NBLK)]
    t_mv = [Tok() for _ in range(NBLK)]
    t_gates = [Tok() for _ in range(NBLK)]

    class Acc:
        def __init__(self, tok):
            self.tok = tok
            self.first = True

        def kw(self):
            if self.first:
                self.first = False
                return dict(writes=[self.tok])
            return dict(wadd=[self.tok])

    for blk in range(int(os.environ.get('KLIM2', NBLK))):
        N = 256 if blk == 0 else 512
        is_ctx, is_loc, is_far = blk == 0, 1 <= blk <= 4, blk >= 5
        xin, xintok = xin_ring.next()
        S.dma("sp", xin[:].rearrange("p k n -> p (k n)"), xnT_d[blk], reads=[t_xnT[blk]], writes=[xintok])
        cur = {nm: stg[nm].next() for nm in stg}
        acc = {nm: Acc(cur[nm][1]) for nm in stg}

        def cm_tile(col0, M, N=N, xin=xin, xintok=xintok):
            pt, ptok = psA_ring.next()
            for k in range(8):
                S.op("pe", lambda e, k=k, pt=pt: e.matmul(pt[0:M, 0:N], Wb[:, k, col0:col0 + M], xin[:, k, 0:N], start=(k == 0), stop=(k == 7)),
                     reads=[t_Wb, xintok], writes=[ptok])
            return pt, ptok

        def evac(nm, dst, pt, ptok, M, scale=None, N=N):
            if scale is None:
                S.op("dve", lambda e: e.tensor_copy(out=dst, in_=pt[0:M, 0:N]), reads=[ptok], **acc[nm].kw())
            else:
                S.op("dve", lambda e: e.tensor_scalar(out=dst, in0=pt[0:M, 0:N], scalar1=scale, scalar2=None, op0=ALU.mult),
                     reads=[ptok], **acc[nm].kw())

        def evac_sig(nm, dst, pt, ptok, silu, N=N):
            sg, sgtok = sig_ring.next()
            S.op("act", lambda e: e.activation(out=sg[:, 0:N], in_=pt[:, 0:N], func=AF.Exp, scale=-1.0), reads=[ptok], writes=[sgtok])
            S.op("dve", lambda e: e.tensor_scalar(out=sg[:, 0:N], in0=sg[:, 0:N], scalar1=1.0, scalar2=None, op0=ALU.add), reads=[sgtok], writes=[sgtok])
            S.op("dve", lambda e: e.reciprocal(out=sg[:, 0:N], in_=sg[:, 0:N]), reads=[sgtok], writes=[sgtok])
            if silu:
                S.op("dve", lambda e: e.tensor_tensor(out=dst, in0=pt[:, 0:N], in1=sg[:, 0:N], op=ALU.mult), reads=[ptok, sgtok], **acc[nm].kw())
            else:
                S.op("dve", lambda e: e.tensor_copy(out=dst, in_=sg[:, 0:N]), reads=[sgtok], **acc[nm].kw())

        gq_st, lr_st, sgg_st, smo_st = cur["gqk"][0], cur["lr"][0], cur["sgg"][0], cur["smo"][0]
        mp_st, gv_st, mv_st, ga_st = cur["mpre"][0], cur["gv"][0], cur["mv"][0], cur["gates"][0]
        for j in range(4):
            if j < 2 and not is_loc:
                continue
            col0 = C_GQ + j * 128 if j < 2 else C_GK + (j - 2) * 128
            pt, ptok = cm_tile(col0, 128)
            evac("gqk", gq_st[:, j * 512:j * 512 + N], pt, ptok, 128, scale=(0.125 if j < 2 else None))
        for dd in range(2):
            if is_far and dd == 0:
                continue
            pt, ptok = cm_tile(C_LR + dd * 16, 16)
            evac("lr", lr_st[0:16, dd * 512:dd * 512 + N], pt, ptok, 16)
        for j in range(8):
            if j < 4 and not (is_loc or blk == 5):
                continue
            col0 = C_MQ + j * 128 if j < 4 else C_MK + (j - 4) * 128
            pt, ptok = cm_tile(col0, 128)
            evac("mpre", mp_st[:, j * 512:j * 512 + N], pt, ptok, 128)
        if is_loc:
            for j in range(4):
                pt, ptok = cm_tile(C_GG + j * 128, 128)
                evac_sig("sgg", sgg_st[:, j * 512:(j + 1) * 512], pt, ptok, True)
            for j in range(4):
                pt, ptok = cm_tile(C_MO + j * 128, 128)
                evac_sig("smo", smo_st[:, j * 512:(j + 1) * 512], pt, ptok, False)
        for c in range(N // 128):
            for nm, col0, ncol, st_ in [("gv", C_GV, 512, gv_st), ("mv", C_MV, 512, mv_st), ("gates", C_MI, 16, ga_st)]:
                pt, ptok = psA_ring.next()
                for k in range(8):
                    S.op("pe", lambda e, k=k, pt=pt, c=c, col0=col0, ncol=ncol: e.matmul(
                        pt[:, 0:ncol], xin[:, k, c * 128:(c + 1) * 128], Wb[:, k, col0:col0 + ncol], start=(k == 0), stop=(k == 7)),
                        reads=[t_Wb, xintok], writes=[ptok])
                S.op("dve", lambda e, pt=pt, st_=st_, c=c, ncol=ncol: e.tensor_copy(out=st_[:, c * ncol:(c + 1) * ncol], in_=pt[:, 0:ncol]),
                     reads=[ptok], **acc[nm].kw())
        S.dma("sp", gqk_d[blk], gq_st[:], reads=[cur["gqk"][1]], writes=[t_gqk[blk]])
        S.dma("sp", lr_d[blk], lr_st[:], reads=[cur["lr"][1]], writes=[t_lr[blk]])
        S.dma("sp", mpre_d[blk], mp_st[:], reads=[cur["mpre"][1]], writes=[t_mpre[blk]])
        S.dma("sp", gv_d[blk], gv_st[:], reads=[cur["gv"][1]], writes=[t_gv[blk]])
        S.dma("sp", mv_d[blk], mv_st[:], reads=[cur["mv"][1]], writes=[t_mv[blk]])
        S.dma("sp", gates_d[blk], ga_st[:], reads=[cur["gates"][1]], writes=[t_gates[blk]])
        if is_loc:
            S.dma("sp", sgg_d[blk - 1], sgg_st[:], reads=[cur["sgg"][1]], writes=[t_sgg[blk - 1]])
            S.dma("sp", smo_d[blk - 1], smo_st[:], reads=[cur["smo"][1]], writes=[t_smo[blk - 1]])
    release(n_s2)
    if dbg == 3:
        S.finish(t_gqk + t_lr + t_sgg + t_smo + t_mpre + t_gv + t_mv + t_gates + dump_toks)
        nc.used_inputs = used_inputs
        return nc

    n_s3 = len(es)
    t_mqk = [Tok() for _ in range(NBLK)]
    Pbuf = sb("convP", [128, 66 * 64], BF16)
    accb = sb("convacc", [128, 64 * 64], F32)
    eb = sb("conve", [128, 64 * 64], F32)
    outb = sb("convout", [128, 64 * 64], BF16)
    tP, tacc, teb, toutb = Tok(), Tok(), Tok(), Tok()

    def conv_tile(j, R, Wd, srcs, dsts, taps_i):
        n_el = R * Wd
        S.op("dve", lambda e: e.memset(Pbuf[:, 0:Wd], 0.0), writes=[tP])
        S.op("dve", lambda e: e.memset(Pbuf[:, Wd + n_el:2 * Wd + n_el], 0.0), wadd=[tP])
        for (ap, tok, off, n) in srcs:
            S.dma("sp", Pbuf[:, Wd + off:Wd + off + n], ap, reads=[tok], wadd=[tP])
        wcol = lambda i, jj: vecs[:, V_CONVW + j * 9 + i * 3 + jj:V_CONVW + j * 9 + i * 3 + jj + 1]
        bcol = vecs[:, V_CONVB + j:V_CONVB + j + 1]
        S.op("dve", lambda e: e.tensor_scalar(out=accb[:, 0:n_el], in0=Pbuf[:, Wd:Wd + n_el], scalar1=wcol(1, 1), scalar2=bcol, op0=ALU.mult, op1=ALU.add),
             reads=[tP, t_const], writes=[tacc])
        P3 = Pbuf[:, 0:(R + 2) * Wd].rearrange("p (r c) -> p r c", c=Wd)
        A3 = accb[:, 0:n_el].rearrange("p (r c) -> p r c", c=Wd)
        for i in taps_i:
            for jj in range(3):
                if i == 1 and jj == 1:
                    continue
                oc0, oc1 = (1, Wd) if jj == 0 else ((0, Wd) if jj == 1 else (0, Wd - 1))
                ic0 = oc0 + jj - 1
                S.op("dve", lambda e, i=i, jj=jj, oc0=oc0, oc1=oc1, ic0=ic0: e.scalar_tensor_tensor(
                    out=A3[:, :, oc0:oc1], in0=P3[:, i:i + R, ic0:ic0 + (oc1 - oc0)], scalar=wcol(i, jj), in1=A3[:, :, oc0:oc1], op0=ALU.mult, op1=ALU.add),
                    reads=[tP, t_const, tacc], writes=[tacc])
        S.op("act", lambda e: e.activation(out=eb[:, 0:n_el], in_=accb[:, 0:n_el], func=AF.Exp, scale=-1.0), reads=[tacc], writes=[teb])
        S.op("dve", lambda e: e.tensor_scalar(out=eb[:, 0:n_el], in0=eb[:, 0:n_el], scalar1=1.0, scalar2=None, op0=ALU.add), reads=[teb], writes=[teb])
        S.op("dve", lambda e: e.reciprocal(out=eb[:, 0:n_el], in_=eb[:, 0:n_el]), reads=[teb], writes=[teb])
        sc = 1.0 if j < 4 else 128.0 ** -0.5
        S.op("dve", lambda e: e.scalar_tensor_tensor(out=outb[:, 0:n_el], in0=accb[:, 0:n_el], scalar=sc, in1=eb[:, 0:n_el], op0=ALU.mult, op1=ALU.mult),
             reads=[tacc, teb], writes=[toutb])
        for (ap, tok, off, n) in dsts:
            S.dma("sp", ap, outb[:, off:off + n], reads=[toutb], wadd=[tok])

    Ppad = sb("convPp", [128, 2 + 66 * 66], BF16)
    cstg = sb("convstg", [128, 64 * 64], BF16)
    accp = sb("convaccp", [128, 64 * 66], F32)
    ebp = sb("convebp", [128, 64 * 66], F32)
    dw_ring = Ring([sb("convdw%d" % i, [128, 9, 128], BF16) for i in range(2)])
    tPp, tcstg, taccp, tebp = Tok(), Tok(), Tok(), Tok()
    last_R = [None]

    def conv_tile_pe(j, R, srcs, dsts):
        n_el, npad = R * 64, R * 66
        first = True
        for (ap, tok, off, n) in srcs:
            S.dma("sp", cstg[:, off:off + n], ap, reads=[tok], **(dict(writes=[tcstg]) if first else dict(wadd=[tcstg])))
            first = False
        if last_R[0] != R:
            S.op("dve", lambda e: e.memset(Ppad[:], 0.0), writes=[tPp])
            last_R[0] = R
        P3 = Ppad[:, 1:1 + (R + 2) * 66].rearrange("p (r c) -> p r c", c=66)
        S.op("dve", lambda e: e.tensor_copy(out=P3[:, 1:R + 1, 1:65], in_=cstg[:, 0:n_el].rearrange("p (r c) -> p r c", c=64)),
             reads=[tcstg], writes=[tPp])
        dw, dwtok = dw_ring.next()
        for t in range(9):
            S.op("dve", lambda e, t=t: e.tensor_scalar(out=dw[:, t, :], in0=ident_b, scalar1=vecs[:, V_CONVW + j * 9 + t:V_CONVW + j * 9 + t + 1], scalar2=None, op0=ALU.mult),
                 reads=[t_cb, t_const], **(dict(writes=[dwtok]) if t == 0 else dict(wadd=[dwtok])))
        bcol = vecs[:, V_CONVB + j:V_CONVB + j + 1]
        firstc = True
        for q0 in range(0, npad, 512):
            N = min(512, npad - q0)
            pt, ptok = psA_ring.next()
            for t in range(9):
                i, jj = t // 3, t % 3
                o = q0 + i * 66 + jj
                S.op("pe", lambda e, pt=pt, t=t, o=o, N=N: e.matmul(pt[:, 0:N], dw[:, t, :], Ppad[:, o:o + N], start=(t == 0), stop=(t == 8)),
                     reads=[dwtok, tPp], writes=[ptok])
            S.op("dve", lambda e, pt=pt, q0=q0, N=N: e.tensor_scalar(out=accp[:, q0:q0 + N], in0=pt[:, 0:N], scalar1=bcol, scalar2=None, op0=ALU.add),
                 reads=[ptok, t_const], **(dict(writes=[taccp]) if firstc else dict(wadd=[taccp])))
            firstc = False
        S.op("act", lambda e: e.activation(out=ebp[:, 0:npad], in_=accp[:, 0:npad], func=AF.Silu), reads=[taccp], writes=[tebp])
        sc = 1.0 if j < 4 else 128.0 ** -0.5
        E3 = ebp[:, 0:npad].rearrange("p (r c) -> p r c", c=66)[:, :, 1:65]
        S.op("dve", lambda e: e.tensor_scalar(out=outb[:, 0:n_el].rearrange("p (r c) -> p r c", c=64), in0=E3, scalar1=sc, scalar2=None, op0=ALU.mult),
             reads=[tebp], writes=[toutb])
        for (ap, tok, off, n) in dsts:
            S.dma("sp", ap, outb[:, off:off + n], reads=[toutb], wadd=[tok])

    for j in range(8):
        isq = j < 4
        nb = 4 if isq else 8
        R = 33 if isq else 64
        srcs = [(mpre_d[1 + b][:, j * 512:(j + 1) * 512], t_mpre[1 + b], b * 512, 512) for b in range(nb)]
        if isq:
            srcs.append((mpre_d[5][:, j * 512:j * 512 + 64], t_mpre[5], 2048, 64))
        dsts = [(mqk_d[1 + b][:, j * 512:(j + 1) * 512], t_mqk[1 + b], b * 512, 512) for b in range(nb)]
        conv_tile_pe(j, R, srcs, dsts)
    for j in range(4, 8):
        conv_tile(j, 1, 256, [(mpre_d[0][:, j * 512:j * 512 + 256], t_mpre[0], 0, 256)],
                  [(mqk_d[0][:, j * 512:j * 512 + 256], t_mqk[0], 0, 256)], (1,))
    release(n_s3)
    if dbg == 4:
        S.finish(t_mqk + dump_toks)
        nc.used_inputs = used_inputs
        return nc

    mixT_d = dscr("mixT_s", [4, 128, 8 * 512], BF16)
    t_mix = [Tok() for _ in range(4)]

    def finalize(OT, t_OT, gate_d, t_gate, gain_col0, koff):
        sq_ring = Ring([sb("fsq%d" % i, [128, 512], BF16) for i in range(2)])
        ms_ring = Ring([sb("fms%d" % i, [128, 512], F32) for i in range(2)])
        y_ring = Ring([sb("fy%d" % i, [128, 512], F32) for i in range(2)])
        gate_ring = Ring([sb("fgate%d" % i, [128, 4 * 512], BF16) for i in range(2)])
        mst_ring = Ring([sb("fmst%d" % i, [128, 4 * 512], BF16) for i in range(2)])
        for lb in range(4):
            gt, gttok = gate_ring.next()
            S.dma("sp", gt[:], gate_d[lb], reads=[t_gate[lb]], writes=[gttok])
            mst, msttok = mst_ring.next()
            for h in range(4):
                O = OT[:, h, lb * 512:(lb + 1) * 512]
                sq, sqtok = sq_ring.next()
                S.op("dve", lambda e, sq=sq, O=O: e.tensor_tensor(out=sq[:], in0=O, in1=O, op=ALU.mult), reads=[t_OT], writes=[sqtok])
                pt, ptok = psA_ring.next()
                S.op("pe", lambda e, pt=pt, sq=sq: e.matmul(pt[:, :], ones_b, sq[:], start=True, stop=True), reads=[sqtok, t_cb], writes=[ptok])
                ms, mstok = ms_ring.next()
                S.op("dve", lambda e, ms=ms, pt=pt: e.tensor_scalar(out=ms[:], in0=pt[:, :], scalar1=1.0 / 128, scalar2=EPS, op0=ALU.mult, op1=ALU.add),
                     reads=[ptok], writes=[mstok])
                S.op("act", lambda e, ms=ms: e.activation(out=ms[:], in_=ms[:], func=AF.Ln), reads=[mstok], writes=[mstok])
                S.op("act", lambda e, ms=ms: e.activation(out=ms[:], in_=ms[:], func=AF.Exp, scale=-0.5), reads=[mstok], writes=[mstok])
                y, ytok = y_ring.next()
                S.op("dve", lambda e, y=y, O=O, ms=ms, h=h: e.scalar_tensor_tensor(
                    out=y[:], in0=O, scalar=vecs[:, gain_col0 + h:gain_col0 + h + 1], in1=ms[:], op0=ALU.mult, op1=ALU.mult),
                    reads=[t_OT, mstok, t_const], writes=[ytok])
                S.op("dve", lambda e, y=y, gt=gt, mst=mst, h=h: e.tensor_tensor(
                    out=mst[:, h * 512:(h + 1) * 512], in0=y[:], in1=gt[:, h * 512:(h + 1) * 512], op=ALU.mult),
                    reads=[ytok, gttok], **(dict(writes=[msttok]) if h == 0 else dict(wadd=[msttok])))
            S.dma("sp", mixT_d[lb][:, koff * 512:(koff + 4) * 512], mst[:], reads=[msttok], wadd=[t_mix[lb]])

    n_s4 = len(es)
    OT = sb("OT", [128, 4, NLOC], F32)
    t_OT = Tok()
    upw_sb = sb("upw", [128, 2, 256], F32)
    t_upw = Tok()
    S.op("dve", lambda e: e.memset(upw_sb[:], 0.0), writes=[t_upw])
    S.dma("sp", upw_sb[0:16], upw_d.rearrange("d r n -> r d n"), writes=[t_upw])
    bmask = consts[:, 512:768]
    m2x = sb("m2x", [128, 2, 256], BF16)
    t_m2x = Tok()
    for d_ in range(2):
        for hh in range(2):
            S.op("dve", lambda e, d_=d_, hh=hh: e.tensor_copy(out=m2x[:, d_, hh * 128:(hh + 1) * 128], in_=mask_b[d_]),
                 reads=[t_cb], wadd=[t_m2x])
    Tst = [[sb("T%d%d" % (p, d_), [128, 256], F32) for d_ in range(2)] for p in range(2)]
    Sbt = [[sb("Sb%d%d" % (p, d_), [128, 256], BF16) for d_ in range(2)] for p in range(2)]
    ecol = [[sb("ec%d%d" % (p, d_), [128, 1], F32) for d_ in range(2)] for p in range(2)]
    t_T = [[Tok() for _ in range(2)] for _ in range(2)]
    t_Sb = [[Tok() for _ in range(2)] for _ in range(2)]
    t_ec = [[Tok() for _ in range(2)] for _ in range(2)]
    for p in range(2):
        for d_ in range(2):
            S.op("dve", lambda e, p=p, d_=d_: e.memset(Tst[p][d_][:], 0.0), writes=[t_T[p][d_]])
            S.op("dve", lambda e, p=p, d_=d_: e.memset(Sbt[p][d_][:], 0.0), writes=[t_Sb[p][d_]])
            S.op("dve", lambda e, p=p, d_=d_: e.memset(ecol[p][d_][:], 1.0), writes=[t_ec[p][d_]])
    gq_ring = Ring([sb("gqkb%d" % i, [128, 4 * 512], BF16) for i in range(2)])
    lr_ring = Ring([sb("lrb%d" % i, [128, 2 * 512], F32) for i in range(2)])
    for i_ in range(2):
        S.op("dve", lambda e, i_=i_: e.memset(lr_ring.items[i_][:], 0.0), writes=[lr_ring.toks[i_]])
    gv_ring = Ring([sb("gvb%d" % i, [128, 4 * 512], BF16) for i in range(2)])
    L_ring = Ring([sb("gL%d" % i, [128, 512], F32) for i in range(2)])
    C_ring = Ring([sb("gC%d" % i, [128, 512], F32) for i in range(2)])
    C2_ring = Ring([sb("gC2%d" % i, [128, 512], F32) for i in range(2)])
    eb_ring = Ring([sb("geb%d" % i, [128, 512], F32) for i in range(4)])
    enb_ring = Ring([sb("genb%d" % i, [128, 512], F32) for i in range(2)])
    qt_ring = Ring([sb("gqt%d" % i, [128, 512], BF16) for i in range(4)])
    kt_ring = Ring([sb("gkt%d" % i, [128, 512], BF16) for i in range(4)])
    kth_ring = Ring([sb("gkth%d" % i, [128, 512], BF16) for i in range(8)])
    ktok_ring = Ring([sb("gktok%d" % i, [128, 128], BF16) for i in range(4)])
    attm_ring = Ring([sb("gattm%d" % i, [128, 256], BF16) for i in range(4)])
    o_written = [False] * NBLK

    def gla_block(blk, d, full):
        N = 256 if blk == 0 else 512
        nch = N // 128
        gq, gqtok = gq_ring.next()
        S.dma("sp", gq[:], gqk_d[blk], reads=[t_gqk[blk]], writes=[gqtok])
        lrb, lrtok = lr_ring.next()
        S.dma("sp", lrb[0:16], lr_d[blk], reads=[t_lr[blk]], writes=[lrtok])
        gvb, gvtok = gv_ring.next()
        S.dma("sp", gvb[:], gv_d[blk], reads=[t_gv[blk]], writes=[gvtok])
        prep = []
        for p in range(2):
            pt, ptok = psA_ring.next()
            S.op("pe", lambda e, pt=pt, p=p: e.matmul(pt[:, 0:N], upw_sb[:, d, p * 128:(p + 1) * 128], lrb[:, d * 512:d * 512 + N], start=True, stop=True),
                 reads=[t_upw, lrtok], writes=[ptok])
            L, Ltok = L_ring.next()
            S.op("dve", lambda e, pt=pt, L=L, p=p: e.tensor_scalar(out=L[:, 0:N], in0=pt[:, 0:N], scalar1=vecs[:, V_UPB + d * 2 + p:V_UPB + d * 2 + p + 1], scalar2=None, op0=ALU.add),
                 reads=[ptok, t_const], writes=[Ltok])
            S.op("act", lambda e, L=L: e.activation(out=L[:, 0:N], in_=L[:, 0:N], func=AF.Exp, scale=-1.0), reads=[Ltok], writes=[Ltok])
            S.op("dve", lambda e, L=L: e.tensor_scalar(out=L[:, 0:N], in0=L[:, 0:N], scalar1=1.0, scalar2=None, op0=ALU.add), reads=[Ltok], writes=[Ltok])
            S.op("act", lambda e, L=L: e.activation(out=L[:, 0:N], in_=L[:, 0:N], func=AF.Ln), reads=[Ltok], writes=[Ltok])
            Cm, Ctok = C_ring.next()
            for c in range(nch):
                S.op("dve", lambda e, Cm=Cm, L=L, c=c: e.tensor_tensor_scan(
                    out=Cm[:, c * 128:(c + 1) * 128], data0=ones_f[:, 0:128], data1=L[:, c * 128:(c + 1) * 128], initial=0.0, op0=ALU.mult, op1=ALU.add),
                    reads=[Ltok, t_const], **(dict(writes=[Ctok]) if c == 0 else dict(wadd=[Ctok])))
            if d == 1:
                C2, C2tok = C2_ring.next()
                for c in range(nch):
                    S.op("dve", lambda e, Cm=Cm, C2=C2, c=c: e.tensor_scalar(
                        out=C2[:, c * 128:(c + 1) * 128], in0=Cm[:, c * 128:(c + 1) * 128], scalar1=-1.0, scalar2=Cm[:, c * 128 + 127:c * 128 + 128], op0=ALU.mult, op1=ALU.add),
                        reads=[Ctok], **(dict(writes=[C2tok]) if c == 0 else dict(wadd=[C2tok])))
                S.op("dve", lambda e, C2=C2, L=L: e.tensor_tensor(out=C2[:, 0:N], in0=C2[:, 0:N], in1=L[:, 0:N], op=ALU.add), reads=[C2tok, Ltok], writes=[C2tok])
                Cm, Ctok = C2, C2tok
            eb_, ebtok = eb_ring.next()
            S.op("act", lambda e, eb_=eb_, Cm=Cm: e.activation(out=eb_[:, 0:N], in_=Cm[:, 0:N], func=AF.Exp, scale=-1.0 / 16), reads=[Ctok], writes=[ebtok])
            enb, enbtok = enb_ring.next()
            S.op("act", lambda e, enb=enb, Cm=Cm: e.activation(out=enb[:, 0:N], in_=Cm[:, 0:N], func=AF.Exp, scale=1.0 / 16), reads=[Ctok], writes=[enbtok])
            kt, kttok = kt_ring.next()
            S.op("dve", lambda e, kt=kt, enb=enb, p=p: e.tensor_tensor(out=kt[:, 0:N], in0=gq[:, (2 + p) * 512:(2 + p) * 512 + N], in1=enb[:, 0:N], op=ALU.mult),
                 reads=[gqtok, enbtok], writes=[kttok])
            qt, qttok = None, None
            kth = [None, None]
            if full:
                for h in range(2):
                    kh, khtok = kth_ring.next()
                    S.op("dve", lambda e, kh=kh, enb=enb, p=p, h=h: e.scalar_tensor_tensor(
                        out=kh[:, 0:N], in0=gq[:, (2 + p) * 512:(2 + p) * 512 + N], scalar=consts[:, 768 + h:769 + h], in1=enb[:, 0:N], op0=ALU.mult, op1=ALU.mult),
                        reads=[gqtok, enbtok, t_const], writes=[khtok])
                    kth[h] = (kh, khtok)
                qt, qttok = qt_ring.next()
                S.op("dve", lambda e, qt=qt, eb_=eb_, p=p: e.tensor_tensor(out=qt[:, 0:N], in0=gq[:, p * 512:p * 512 + N], in1=eb_[:, 0:N], op=ALU.mult),
                     reads=[gqtok, ebtok], writes=[qttok])
            prep.append((eb_, ebtok, kt, kttok, qt, qttok, kth))
        order = list(range(nch)) if d == 0 else list(range(nch - 1, -1, -1))
        for c in order:
            cs = slice(c * 128, (c + 1) * 128)
            ecc = c * 128 + (127 if d == 0 else 0)
            st = [dict() for _ in range(2)]
            for p in range(2):
                eb_, ebtok, kt, kttok, qt, qttok, kth = prep[p]
                pb, pbtok = psB_ring.next()
                S.op("pe", lambda e, pb=pb, kt=kt: e.transpose(pb[:, 0:128], kt[:, cs], ident_b), reads=[kttok, t_cb], writes=[pbtok])
                st[p]["pb"] = (pb, pbtok)
                if full:
                    pa, patok = psA_ring.next()
                    for h in range(2):
                        S.op("pe", lambda e, pa=pa, h=h, kth=kth, qt=qt: e.matmul(pa[:, h * 128:(h + 1) * 128], kth[h][0][:, cs], qt[:, cs], start=True, stop=True),
                             reads=[kth[h][1], qttok], writes=[patok])
                    st[p]["pa"] = (pa, patok)
            for p in range(2):
                pb, pbtok = st[p]["pb"]
                ktk, ktktok = ktok_ring.next()
                S.op("dve", lambda e, ktk=ktk, pb=pb: e.tensor_copy(out=ktk[:], in_=pb[:, 0:128]), reads=[pbtok], writes=[ktktok])
                st[p]["ktk"] = (ktk, ktktok)
                if full:
                    pa, patok = st[p]["pa"]
                    am, amtok = attm_ring.next()
                    S.op("dve", lambda e, am=am, pa=pa: e.tensor_tensor(out=am[:], in0=pa[:, 0:256], in1=m2x[:, d, :], op=ALU.mult),
                         reads=[patok, t_m2x], writes=[amtok])
                    st[p]["am"] = (am, amtok)
            for p in range(2):
                ktk, ktktok = st[p]["ktk"]
                pd, pdtok = psA_ring.next()
                S.op("pe", lambda e, pd=pd, ktk=ktk, p=p: e.matmul(pd[:, 0:256], ktk[:], gvb[:, c * 512 + p * 256:c * 512 + (p + 1) * 256], start=True, stop=True),
                     reads=[ktktok, gvtok], writes=[pdtok])
                st[p]["pd"] = (pd, pdtok)
            if full:
                for p in range(2):
                    eb_, ebtok, kt, kttok, qt, qttok, kth = prep[p]
                    am, amtok = st[p]["am"]
                    po, potok = psA_ring.next()
                    for h in range(2):
                        hd = p * 2 + h
                        S.op("pe", lambda e, po=po, h=h, hd=hd, am=am: e.matmul(
                            po[:, h * 128:(h + 1) * 128], gvb[:, c * 512 + hd * 128:c * 512 + (hd + 1) * 128], am[:, h * 128:(h + 1) * 128], start=True, stop=False),
                            reads=[gvtok, amtok], writes=[potok])
                        S.op("pe", lambda e, po=po, h=h, qt=qt, p=p: e.matmul(
                            po[:, h * 128:(h + 1) * 128], Sbt[p][d][:, h * 128:(h + 1) * 128], qt[:, cs], start=False, stop=True),
                            reads=[t_Sb[p][d], qttok], writes=[potok])
                    st[p]["po"] = (po, potok)
            for p in range(2):
                eb_, ebtok = prep[p][0], prep[p][1]
                pd, pdtok = st[p]["pd"]
                S.op("dve", lambda e, pd=pd, p=p: e.scalar_tensor_tensor(
                    out=Tst[p][d][:], in0=Tst[p][d][:], scalar=ecol[p][d][:, 0:1], in1=pd[:, 0:256], op0=ALU.mult, op1=ALU.add),
                    reads=[pdtok, t_ec[p][d], t_T[p][d]], writes=[t_T[p][d]])
                S.op("dve", lambda e, p=p, eb_=eb_: e.scalar_tensor_tensor(
                    out=Sbt[p][d][:], in0=Tst[p][d][:], scalar=eb_[:, ecc:ecc + 1], in1=bmask, op0=ALU.mult, op1=ALU.mult),
                    reads=[t_T[p][d], ebtok, t_const], writes=[t_Sb[p][d]])
                S.op("dve", lambda e, p=p, eb_=eb_: e.tensor_copy(out=ecol[p][d][:], in_=eb_[:, ecc:ecc + 1]),
                     reads=[ebtok], writes=[t_ec[p][d]])
            if full:
                for p in range(2):
                    po, potok = st[p]["po"]
                    tok0 = (blk - 1) * 512 + c * 128
                    Odst = OT[:, p * 2:p * 2 + 2, tok0:tok0 + 128]
                    po3 = po[:, 0:256].rearrange("p (h t) -> p h t", h=2)
                    if not o_written[blk]:
                        S.op("dve", lambda e, Odst=Odst, po3=po3: e.tensor_copy(out=Odst, in_=po3), reads=[potok], wadd=[t_OT])
                    else:
                        S.op("dve", lambda e, Odst=Odst, po3=po3: e.tensor_tensor(out=Odst, in0=po3, in1=Odst, op=ALU.add), reads=[potok, t_OT], wadd=[t_OT])
        if full:
            o_written[blk] = True

    s4m = int(os.environ.get("S4MODE", 9))
    if s4m == 10:
        gla_block(8, 1, False)
    elif s4m == 11:
        gla_block(0, 0, False)
    else:
        gla_block(0, 1, False)
    if 10 > s4m >= 1:
        for blk in (8, 7, 6, 5):
            gla_block(blk, 1, False)
        gla_block(0, 0, False)
    if 10 > s4m >= 2:
        for i in range(4 if s4m >= 3 else 1):
            gla_block(1 + i, 0, True)
            gla_block(4 - i, 1, True)
    if 10 > s4m >= 4:
        finalize(OT, t_OT, sgg_d, t_sgg, V_GNG, 0)
    dump("OTg", OT[:, 0, :], [128, NLOC], [t_OT])
    release(n_s4)
    if dbg == 5:
        S.finish(t_mix + dump_toks)
        nc.used_inputs = used_inputs
        return nc

    n_s5 = len(es)
    HT = sb("HT", [128, 4, NLOC], F32)
    t_HT = Tok()
    gateb4 = sb("gateb4", [128, 64], F32)
    t_gb4 = Tok()
    for c in range(4):
        S.dma("sp", gateb4[:, c * 16:(c + 1) * 16], rows_d[0:1, R_GATEB:R_GATEB + 16].partition_broadcast(128), wadd=[t_gb4])
    maskf = [consts[:, 128:256], consts[:, 256:384]]
    T4 = [sb("T4_%d" % d_, [128, 4, 256], F32) for d_ in range(2)]
    Sb4 = [sb("Sb4_%d" % d_, [128, 4, 256], BF16) for d_ in range(2)]
    t_T4 = [Tok() for _ in range(2)]
    t_Sb4 = [Tok() for _ in range(2)]
    ec_one = sb("econe", [128, 4], F32)
    t_econe = Tok()
    S.op("dve", lambda e: e.memset(ec_one[:], 1.0), writes=[t_econe])
    prev_ec = [(ec_one, t_econe), (ec_one, t_econe)]
    for d_ in range(2):
        S.op("dve", lambda e, d_=d_: e.memset(T4[d_][:], 0.0), writes=[t_T4[d_]])
        S.op("dve", lambda e, d_=d_: e.memset(Sb4[d_][:], 0.0), writes=[t_Sb4[d_]])
    mq_ring = Ring([sb("mqkb%d" % i, [128, 8 * 512], BF16) for i in range(2)])
    mvb_ring = Ring([sb("mvb%d" % i, [128, 4 * 512], BF16) for i in range(2)])
    ga_ring = Ring([sb("gab%d" % i, [128, 64], F32) for i in range(2)])
    gbb_ring = Ring([sb("gbb%d" % i, [128, 64], F32) for i in range(2)])
    Lf_ring = Ring([sb("Lf%d" % i, [128, 16], F32) for i in range(2)])
    es_ring = Ring([sb("es%d" % i, [128, 16], F32) for i in range(2)])
    lfbc_ring = Ring([sb("lfbc%d" % i, [128, 4, 128], F32) for i in range(2)])
    flo_ring = Ring([sb("flo%d" % i, [128, 4, 128], F32) for i in range(2)])
    ecn_ring = Ring([sb("ecn%d" % i, [128, 4], F32) for i in range(12)])
    vext_ring = Ring([sb("vext%d" % i, [128, 4, 256], BF16) for i in range(2)])
    mktok_ring = Ring([sb("mktok%d" % i, [128, 512], BF16) for i in range(2)])
    mam_ring = Ring([sb("mam%d" % i, [128, 512], BF16) for i in range(2)])
    mask4 = sb("mask4", [128, 2, 512], BF16)
    t_mask4 = Tok()
    for d_ in range(2):
        for hh in range(4):
            S.op("dve", lambda e, d_=d_, hh=hh: e.tensor_copy(out=mask4[:, d_, hh * 128:(hh + 1) * 128], in_=mask_b[d_]), reads=[t_cb], wadd=[t_mask4])
    tP = [[t_] * 4 for t_ in [Tok() for _ in range(6)]]
    dd_ring = Ring([sb("mdd%d" % i, [128, 4, 128], F32) for i in range(2)])
    ht_ring = Ring([sb("mht%d" % i, [128, 4, 128], F32) for i in range(2)])
    h_written = [False] * NBLK

    def ml_block(blk, d, full):
        N = 256 if blk == 0 else 512
        nch = N // 128
        mq, mqtok = mq_ring.next()
        S.dma("sp", mq[:], mqk_d[blk], reads=[t_mqk[blk]], writes=[mqtok])
        mvb, mvtok = mvb_ring.next()
        S.dma("sp", mvb[:], mv_d[blk], reads=[t_mv[blk]], writes=[mvtok])
        ga, gatok = ga_ring.next()
        S.dma("sp", ga[:], gates_d[blk], reads=[t_gates[blk]], writes=[gatok])
        gbb, gbtok = gbb_ring.next()
        S.op("dve", lambda e: e.tensor_tensor(out=gbb[:], in0=ga[:], in1=gateb4[:], op=ALU.add), reads=[gatok, t_gb4], writes=[gbtok])
        gb3 = gbb[:].rearrange("p (c g) -> p c g", g=16)
        Lf, Lftok = Lf_ring.next()
        Lf3 = Lf[:].rearrange("p (c h) -> p c h", h=4)
        S.op("act", lambda e: e.activation(out=Lf3[:, 0:nch, :], in_=gb3[:, 0:nch, 8 + d * 4:12 + d * 4], func=AF.Exp, scale=-1.0), reads=[gbtok], writes=[Lftok])
        S.op("dve", lambda e: e.tensor_scalar(out=Lf[:, 0:nch * 4], in0=Lf[:, 0:nch * 4], scalar1=1.0, scalar2=None, op0=ALU.add), reads=[Lftok], writes=[Lftok])
        S.op("act", lambda e: e.activation(out=Lf[:, 0:nch * 4], in_=Lf[:, 0:nch * 4], func=AF.Ln), reads=[Lftok], writes=[Lftok])
        pt, ptok = psA[0], tP[0][0]
        S.op("pe", lambda e: e.matmul(pt[:, 0:nch * 4], maskf[d], Lf[:, 0:nch * 4], start=True, stop=True), reads=[Lftok, t_const], writes=[ptok])
        es_, estok = es_ring.next()
        es3 = es_[:].rearrange("p (c h) -> p c h", h=4)
        pt3 = pt[:, 0:16].rearrange("p (c h) -> p c h", h=4)
        S.op("dve", lambda e: e.tensor_tensor(out=es3[:, 0:nch, :], in0=pt3[:, 0:nch, :], in1=gb3[:, 0:nch, d * 4:d * 4 + 4], op=ALU.add),
             reads=[ptok, gbtok], writes=[estok])
        S.op("act", lambda e: e.activation(out=es_[:, 0:nch * 4], in_=es_[:, 0:nch * 4], func=AF.Exp), reads=[estok], writes=[estok])
        order = list(range(nch)) if d == 0 else list(range(nch - 1, -1, -1))
        endcol = 127 if d == 0 else 0
        for c in order:
            lf4, lf4tok = lfbc_ring.next()
            S.op("dve", lambda e, lf4=lf4: e.tensor_copy(out=lf4[:], in_=Lf[:, c * 4:c * 4 + 4].unsqueeze(2).to_broadcast([128, 4, 128])),
                 reads=[Lftok], writes=[lf4tok])
            vx4, vx4tok = vext_ring.next()
            es_bc = es_[:, c * 4:c * 4 + 4].unsqueeze(2).to_broadcast([128, 4, 128])
            S.op("dve", lambda e, vx4=vx4, es_bc=es_bc: e.tensor_tensor(
                out=vx4[:, :, 0:128], in0=mvb[:, c * 512:(c + 1) * 512].rearrange("p (h n) -> p h n", h=4), in1=es_bc, op=ALU.mult),
                reads=[mvtok, estok], writes=[vx4tok])
            S.op("dve", lambda e, vx4=vx4, es_bc=es_bc: e.tensor_copy(out=vx4[:, :, 128:256], in_=es_bc), reads=[estok], wadd=[vx4tok])
            kTs = [mq[:, (4 + h) * 512 + c * 128:(4 + h) * 512 + (c + 1) * 128] for h in range(4)]
            qTs = [mq[:, h * 512 + c * 128:h * 512 + (c + 1) * 128] for h in range(4)]
            pb, pbtok = psB_ring.next()
            for h in range(4):
                S.op("pe", lambda e, h=h, lf4=lf4: e.matmul(psA[0][:, h * 128:(h + 1) * 128], lf4[:, h, :], maskf[d], start=True, stop=True),
                     reads=[lf4tok, t_const], writes=[tP[0][0]])
                S.op("pe", lambda e, h=h, pb=pb: e.transpose(pb[:, h * 128:(h + 1) * 128], kTs[h], ident_b), reads=[mqtok, t_cb], writes=[pbtok])
            ecn4, ecn4tok = ecn_ring.next()
            S.op("act", lambda e, ecn4=ecn4: e.activation(
                out=ecn4[:, 0:4].unsqueeze(2), in_=psA[0][:, :].rearrange("p (h n) -> p h n", h=4)[:, :, endcol:endcol + 1], func=AF.Exp, scale=-1.0),
                reads=[tP[0][0]], writes=[ecn4tok])
            flo4, flo4tok = None, None
            if full:
                flo4, flo4tok = flo_ring.next()
                S.op("act", lambda e, flo4=flo4: e.activation(out=flo4[:].rearrange("p h n -> p (h n)"), in_=psA[0][:, :], func=AF.Exp), reads=[tP[0][0]], writes=[flo4tok])
            ktk4, ktk4tok = mktok_ring.next()
            S.op("dve", lambda e, ktk4=ktk4, pb=pb: e.tensor_copy(out=ktk4[:], in_=pb[:, 0:512]), reads=[pbtok], writes=[ktk4tok])
            if full:
                for h in range(4):
                    S.op("pe", lambda e, h=h: e.matmul(psA[1][:, h * 128:(h + 1) * 128], kTs[h], qTs[h], start=True, stop=True), reads=[mqtok], writes=[tP[1][0]])
                am4, am4tok = mam_ring.next()
                S.op("dve", lambda e, am4=am4: e.tensor_tensor(out=am4[:], in0=psA[1][:, :], in1=mask4[:, d, :], op=ALU.mult),
                     reads=[tP[1][0], t_mask4], writes=[am4tok])
                for h in range(4):
                    bk, o0 = 2 + h // 2, (h % 2) * 256
                    for half in range(2):
                        hc = slice(half * 128, (half + 1) * 128)
                        oc = slice(o0 + half * 128, o0 + (half + 1) * 128)
                        S.op("pe", lambda e, h=h, bk=bk, oc=oc, hc=hc, am4=am4, vx4=vx4: e.matmul(psA[bk][:, oc], vx4[:, h, hc], am4[:, h * 128:(h + 1) * 128], start=True, stop=False),
                             reads=[vx4tok, am4tok], writes=[tP[bk][0]])
                        S.op("pe", lambda e, h=h, bk=bk, oc=oc, hc=hc: e.matmul(psA[bk][:, oc], Sb4[d][:, h, hc], qTs[h], start=False, stop=True),
                             reads=[t_Sb4[d], mqtok], writes=[tP[bk][0]])
            for h in range(4):
                bk, o0 = 4 + h // 2, (h % 2) * 256
                S.op("pe", lambda e, h=h, bk=bk, o0=o0, ktk4=ktk4, vx4=vx4: e.matmul(psA[bk][:, o0:o0 + 256], ktk4[:, h * 128:(h + 1) * 128], vx4[:, h, :], start=True, stop=True),
                     reads=[ktk4tok, vx4tok], writes=[tP[bk][0]])
            pec, pectok = prev_ec[d]
            for h in range(4):
                bk, o0 = 4 + h // 2, (h % 2) * 256
                S.op("dve", lambda e, h=h, bk=bk, o0=o0, pec=pec: e.scalar_tensor_tensor(
                    out=T4[d][:, h, :], in0=T4[d][:, h, :], scalar=pec[:, h:h + 1], in1=psA[bk][:, o0:o0 + 256], op0=ALU.mult, op1=ALU.add),
                    reads=[tP[bk][0], pectok, t_T4[d]], writes=[t_T4[d]])
            S.op("dve", lambda e, ecn4=ecn4: e.tensor_tensor(out=Sb4[d][:], in0=T4[d][:], in1=ecn4[:, 0:4].unsqueeze(2).to_broadcast([128, 4, 256]), op=ALU.mult),
                 reads=[t_T4[d], ecn4tok], writes=[t_Sb4[d]])
            prev_ec[d] = (ecn4, ecn4tok)
            if full:
                dd4, dd4tok = dd_ring.next()
                for bk in (2, 3):
                    hs2 = slice((bk - 2) * 2, (bk - 2) * 2 + 2)
                    den = psA[bk][:, :].rearrange("p (h x n) -> p h x n", h=2, x=2)[:, :, 1, :]
                    S.op("dve", lambda e, dd4=dd4, den=den, hs2=hs2, flo4=flo4: e.scalar_tensor_tensor(
                        out=dd4[:, hs2, :], in0=den, scalar=-1.0, in1=flo4[:, hs2, :], op0=ALU.mult, op1=ALU.max),
                        reads=[tP[bk][0], flo4tok], **(dict(writes=[dd4tok]) if bk == 2 else dict(wadd=[dd4tok])))
                for bk in (2, 3):
                    hs2 = slice((bk - 2) * 2, (bk - 2) * 2 + 2)
                    den = psA[bk][:, :].rearrange("p (h x n) -> p h x n", h=2, x=2)[:, :, 1, :]
                    S.op("dve", lambda e, dd4=dd4, den=den, hs2=hs2: e.tensor_tensor(out=dd4[:, hs2, :], in0=den, in1=dd4[:, hs2, :], op=ALU.max),
                         reads=[tP[bk][0], dd4tok], wadd=[dd4tok])
                S.op("dve", lambda e, dd4=dd4: e.reciprocal(out=dd4[:], in_=dd4[:]), reads=[dd4tok], writes=[dd4tok])
                tok0 = (blk - 1) * 512 + c * 128
                if not h_written[blk]:
                    for bk in (2, 3):
                        hs2 = slice((bk - 2) * 2, (bk - 2) * 2 + 2)
                        num = psA[bk][:, :].rearrange("p (h x n) -> p h x n", h=2, x=2)[:, :, 0, :]
                        S.op("dve", lambda e, num=num, hs2=hs2, dd4=dd4: e.tensor_tensor(out=HT[:, hs2, tok0:tok0 + 128], in0=num, in1=dd4[:, hs2, :], op=ALU.mult),
                             reads=[tP[bk][0], dd4tok], wadd=[t_HT])
                else:
                    ht4, ht4tok = ht_ring.next()
                    for bk in (2, 3):
                        hs2 = slice((bk - 2) * 2, (bk - 2) * 2 + 2)
                        num = psA[bk][:, :].rearrange("p (h x n) -> p h x n", h=2, x=2)[:, :, 0, :]
                        S.op("dve", lambda e, num=num, hs2=hs2, dd4=dd4, ht4=ht4: e.tensor_tensor(out=ht4[:, hs2, :], in0=num, in1=dd4[:, hs2, :], op=ALU.mult),
                             reads=[tP[bk][0], dd4tok], **(dict(writes=[ht4tok]) if bk == 2 else dict(wadd=[ht4tok])))
                    S.op("dve", lambda e, ht4=ht4: e.tensor_tensor(out=HT[:, :, tok0:tok0 + 128], in0=HT[:, :, tok0:tok0 + 128], in1=ht4[:], op=ALU.add),
                         reads=[ht4tok, t_HT], wadd=[t_HT])
        if full:
            h_written[blk] = True

    s5m = int(os.environ.get("S5MODE", 9))
    ml_block(0, 1, False)
    if s5m >= 1:
        for blk in (8, 7, 6, 5):
            ml_block(blk, 1, False)
        ml_block(0, 0, False)
    if s5m >= 2:
        for i in range(4 if s5m >= 3 else 1):
            ml_block(1 + i, 0, True)
            ml_block(4 - i, 1, True)
    S.barrier()
    if s5m >= 4:
        finalize(HT, t_HT, smo_d, t_smo, V_MNG, 4)
    dump("HTm", HT[:, 0, :], [128, NLOC], [t_HT])
    release(n_s5)
    if dbg == 6:
        S.finish(t_mix + dump_toks)
        nc.used_inputs = used_inputs
        return nc

    n_s6 = len(es)
    x1_d = dscr("x1_s", [16, 128, D], F32)
    t_x1 = Tok()
    wout_v = wout_d.rearrange("(k p) n -> p k n", p=128)
    woutb = sb("woutb", [128, 8, D], BF16)
    t_wout = Tok()
    wo_stg = Ring([sb("wostg%d" % i, [128, 8, 256], F32) for i in range(2)])
    for pc in range(4):
        st, sttok = wo_stg.next()
        S.dma("sp", st[:], wout_v[:, :, pc * 256:(pc + 1) * 256], writes=[sttok])
        S.op("dve", lambda e, st=st, pc=pc: e.tensor_copy(out=woutb[:, :, pc * 256:(pc + 1) * 256], in_=st[:]), reads=[sttok], wadd=[t_wout])
    rw_sb = sb("rw", [128, 8, 36], F32)
    t_rw = Tok()
    S.dma("sp", rw_sb[:], rw_d.rearrange("(k p) n -> p k n", p=128), writes=[t_rw])
    rb_bc = sb("rbbc", [128, 36], F32)
    S.dma("sp", rb_bc[:], rows_d[0:1, R_RB:R_RB + 36].partition_broadcast(128), wadd=[t_rw])
    mixb_ring = Ring([sb("mixb%d" % i, [128, 8 * 512], BF16) for i in range(2)])
    xt6_ring = Ring([sb("x6t%d" % i, [128, D], F32) for i in range(2)])
    x1_ring = Ring([sb("x1t%d" % i, [128, D], F32) for i in range(2)])
    tmp6 = sb("tmp6", [128, D], F32)
    t_tmp6 = Tok()
    jk6 = sb("jk6", [128, D], F32)
    t_jk6 = Tok()
    s6_ring = Ring([(sb("s6a%d" % i, [128, 1], F32), sb("s6b%d" % i, [128, 1], F32)) for i in range(2)])
    h2s_ring = Ring([sb("h2s%d" % i, [128, D], F32) for i in range(2)])
    h2Tf_ring = Ring([sb("h2Tf%d" % i, [128, 8, 128], F32) for i in range(2)])
    Sel = sb("Sel", [128, 16, 2, 32], F32)
    t_Sel = Tok()
    h2tok = sb("h2tok", [128, 16, D], BF16)
    t_h2tok = Tok()
    LG = sb("LGall", [128, 16, 36], F32)
    t_LG = Tok()
    mixb, mixbtok = None, None
    for ti in range(16):
        lb, tt = ti // 4, ti % 4
        if tt == 0:
            mixb, mixbtok = mixb_ring.next()
            S.dma("sp", mixb[:], mixT_d[lb], reads=[t_mix[lb]], writes=[mixbtok])
        xt, xttok = xt6_ring.next()
        S.dma("sp", xt[:], x_d[ti * 128:(ti + 1) * 128, :], writes=[xttok])
        x1, x1tok = x1_ring.next()
        for half in range(2):
            po, potok = psA_ring.next()
            for k in range(8):
                S.op("pe", lambda e, po=po, k=k, half=half, mixb=mixb, tt=tt: e.matmul(
                    po[:, :], mixb[:, k * 512 + tt * 128:k * 512 + (tt + 1) * 128], woutb[:, k, half * 512:(half + 1) * 512], start=(k == 0), stop=(k == 7)),
                    reads=[mixbtok, t_wout], writes=[potok])
            hs_ = slice(half * 512, (half + 1) * 512)
            S.op("dve", lambda e, po=po, hs_=hs_: e.tensor_tensor(out=tmp6[:, hs_], in0=po[:, :], in1=g12[:, hs_], op=ALU.mult),
                 reads=[potok, t_g12], **(dict(writes=[t_tmp6]) if half == 0 else dict(wadd=[t_tmp6])))
            S.op("dve", lambda e, x1=x1, xt=xt, hs_=hs_: e.tensor_tensor(out=x1[:, hs_], in0=tmp6[:, hs_], in1=xt[:, hs_], op=ALU.add),
                 reads=[t_tmp6, xttok], **(dict(writes=[x1tok]) if half == 0 else dict(wadd=[x1tok])))
        S.dma("sp", x1_d[ti], x1[:], reads=[x1tok], wadd=[t_x1])
        ss, sstok = s6_ring.next()
        S.op("act", lambda e, x1=x1, ss=ss: e.activation(out=jk6[:], in_=x1[:], func=AF.Square, accum_out=ss[0][:, 0:1]), reads=[x1tok], writes=[t_jk6, sstok])
        S.op("dve", lambda e, ss=ss: e.tensor_scalar(out=ss[1][:, 0:1], in0=ss[0][:, 0:1], scalar1=1.0 / D, scalar2=EPS, op0=ALU.mult, op1=ALU.add), reads=[sstok], writes=[sstok])
        S.op("act", lambda e, ss=ss: e.activation(out=ss[0][:, 0:1], in_=ss[1][:, 0:1], func=AF.Ln), reads=[sstok], writes=[sstok])
        S.op("act", lambda e, ss=ss: e.activation(out=ss[1][:, 0:1], in_=ss[0][:, 0:1], func=AF.Exp, scale=-0.5), reads=[sstok], writes=[sstok])
        h2s, h2stok = h2s_ring.next()
        S.op("dve", lambda e, h2s=h2s, x1=x1, ss=ss: e.tensor_scalar(out=h2s[:], in0=x1[:], scalar1=ss[1][:, 0:1], scalar2=None, op0=ALU.mult),
             reads=[x1tok, sstok], writes=[h2stok])
        h2Tf, h2Tftok = h2Tf_ring.next()
        for g in range(2):
            pT, pTtok = psA_ring.next()
            for kk in range(4):
                k = g * 4 + kk
                S.op("pe", lambda e, pT=pT, kk=kk, k=k, h2s=h2s: e.transpose(pT[:, kk * 128:(kk + 1) * 128], h2s[:, k * 128:(k + 1) * 128], ident_f),
                     reads=[h2stok, t_const], writes=[pTtok])
            for kk in range(4):
                k = g * 4 + kk
                S.op("dve", lambda e, pT=pT, kk=kk, k=k, h2Tf=h2Tf: e.tensor_scalar(
                    out=h2Tf[:, k, :], in0=pT[:, kk * 128:(kk + 1) * 128], scalar1=A2[:, k:k + 1], scalar2=A2[:, 8 + k:9 + k], op0=ALU.mult, op1=ALU.add),
                    reads=[pTtok, t_A], **(dict(writes=[h2Tftok]) if k == 0 else dict(wadd=[h2Tftok])))
        pr, prtok = psA_ring.next()
        for k in range(8):
            S.op("pe", lambda e, pr=pr, k=k, h2Tf=h2Tf: e.matmul(pr[:, 0:36], h2Tf[:, k, :], rw_sb[:, k, :], start=(k == 0), stop=(k == 7)),
                 reads=[h2Tftok, t_rw], writes=[prtok])
        S.op("dve", lambda e, pr=pr, ti=ti: e.tensor_tensor(out=LG[:, ti, :], in0=pr[:, 0:36], in1=rb_bc[:], op=ALU.add), reads=[prtok, t_rw], wadd=[t_LG])
        S.op("dve", lambda e, h2s=h2s: e.tensor_tensor(out=tmp6[:], in0=h2s[:], in1=g12[:, 2 * D:3 * D], op=ALU.mult), reads=[h2stok, t_g12, t_tmp6], writes=[t_tmp6])
        S.op("dve", lambda e, ti=ti: e.tensor_tensor(out=h2tok[:, ti, :], in0=tmp6[:], in1=g12[:, 3 * D:4 * D], op=ALU.add), reads=[t_tmp6, t_g12], wadd=[t_h2tok])
    RT = sb("RTb", [128, 16 * 80], F32)
    t_RT = Tok()

    def V(c0, n):
        return RT[:, c0:c0 + 16 * n].rearrange("p (t n) -> p t n", n=n)

    def bc(ap2, n):
        return ap2.unsqueeze(2).to_broadcast([128, 16, n])
    G = LG[:, :, 0:4]
    E4 = LG[:, :, 4:36].rearrange("p t (g i) -> p t g i", g=4)
    gmax, gs, gw = RT[:, 0:16], RT[:, 16:32], RT[:, 32:48]
    m1, m2, w1, w2, w1g, w2g = RT[:, 48:64], RT[:, 64:80], RT[:, 80:96], RT[:, 96:112], RT[:, 112:128], RT[:, 128:144]
    goh, gex = V(144, 4), V(208, 4)
    eg, eq1, eg2, eq2, tmp8 = V(272, 8), V(400, 8), V(528, 8), V(656, 8), V(784, 8)

    def rop(fn, eng="dve", extra=()):
        S.op(eng, fn, reads=[t_RT, t_LG] + list(extra), writes=[t_RT])
    S.op("dve", lambda e: e.tensor_reduce(out=gmax, in_=G, axis=AX.X, op=ALU.max), reads=[t_LG], writes=[t_RT])
    rop(lambda e: e.tensor_tensor(out=goh, in0=G, in1=bc(gmax, 4), op=ALU.is_equal))
    rop(lambda e: e.tensor_tensor(out=gex, in0=G, in1=bc(gmax, 4), op=ALU.subtract))
    rop(lambda e: e.activation(out=RT[:, 208:272], in_=RT[:, 208:272], func=AF.Exp), eng="act")
    rop(lambda e: e.tensor_reduce(out=gs, in_=gex, axis=AX.X, op=ALU.add))
    rop(lambda e: e.reciprocal(out=gw, in_=gs))
    rop(lambda e: e.tensor_tensor(out=eg, in0=E4[:, :, 0, :], in1=bc(goh[:, :, 0], 8), op=ALU.mult))
    for g in range(1, 4):
        rop(lambda e, g=g: e.tensor_tensor(out=tmp8, in0=E4[:, :, g, :], in1=bc(goh[:, :, g], 8), op=ALU.mult))
        rop(lambda e: e.tensor_tensor(out=eg, in0=eg, in1=tmp8, op=ALU.add))
    rop(lambda e: e.tensor_reduce(out=m1, in_=eg, axis=AX.X, op=ALU.max))
    rop(lambda e: e.tensor_tensor(out=eq1, in0=eg, in1=bc(m1, 8), op=ALU.is_equal))
    rop(lambda e: e.scalar_tensor_tensor(out=RT[:, 528:656], in0=RT[:, 400:528], scalar=-1e30, in1=RT[:, 272:400], op0=ALU.mult, op1=ALU.add))
    rop(lambda e: e.tensor_reduce(out=m2, in_=eg2, axis=AX.X, op=ALU.max))
    rop(lambda e: e.tensor_tensor(out=eq2, in0=eg2, in1=bc(m2, 8), op=ALU.is_equal))
    rop(lambda e: e.tensor_tensor(out=w1, in0=m2, in1=m1, op=ALU.subtract))
    rop(lambda e: e.activation(out=w1, in_=w1, func=AF.Exp), eng="act")
    rop(lambda e: e.tensor_scalar(out=w1, in0=w1, scalar1=1.0, scalar2=None, op0=ALU.add))
    rop(lambda e: e.reciprocal(out=w1, in_=w1))
    rop(lambda e: e.tensor_scalar(out=w2, in0=w1, scalar1=-1.0, scalar2=1.0, op0=ALU.mult, op1=ALU.add))
    rop(lambda e: e.tensor_tensor(out=w1g, in0=w1, in1=gw, op=ALU.mult))
    rop(lambda e: e.tensor_tensor(out=w2g, in0=w2, in1=gw, op=ALU.mult))
    for g in range(4):
        S.op("dve", lambda e, g=g: e.tensor_tensor(out=Sel[:, :, 0, g * 8:(g + 1) * 8], in0=eq1, in1=bc(goh[:, :, g], 8), op=ALU.mult), reads=[t_RT], wadd=[t_Sel])
        S.op("dve", lambda e, g=g: e.tensor_tensor(out=Sel[:, :, 1, g * 8:(g + 1) * 8], in0=eq2, in1=bc(goh[:, :, g], 8), op=ALU.mult), reads=[t_RT], wadd=[t_Sel])
    Wt3 = Wt[:].rearrange("p (t k) -> p t k", k=2)
    S.op("dve", lambda e: e.tensor_copy(out=Wt3[:, :, 0], in_=w1g), reads=[t_RT], wadd=[t_Wt])
    S.op("dve", lambda e: e.tensor_copy(out=Wt3[:, :, 1], in_=w2g), reads=[t_RT], wadd=[t_Wt])
    Wselb = sb("Wselb", [128, 16, 32], BF16)
    t_wsel = Tok()
    S.op("dve", lambda e: e.tensor_tensor(out=Wselb[:], in0=Sel[:, :, 0, :], in1=Sel[:, :, 1, :], op=ALU.add), reads=[t_Sel], writes=[t_wsel])
    stri_b = sb("strib", [128, 128], BF16)
    t_stri = Tok()
    S.op("dve", lambda e: e.tensor_tensor(out=stri_b[:], in0=mask_b[0], in1=ident_b, op=ALU.subtract), reads=[t_cb], writes=[t_stri])
    rs = sb("rsm", [128, 512], F32)
    t_rs = Tok()
    cntf, nbf, padded, pad_end, pad_start = rs[:, 0:32], rs[:, 32:64], rs[:, 64:96], rs[:, 96:128], rs[:, 128:160]
    bef, be1024, be512 = rs[:, 192:256], rs[:, 256:320], rs[:, 320:384]
    pc, pctok = psA_ring.next()
    for ti in range(16):
        S.op("pe", lambda e, ti=ti: e.matmul(pc[:, 0:32], ones_b, Wselb[:, ti, :], start=(ti == 0), stop=(ti == 15)), reads=[t_wsel, t_cb], writes=[pctok])
    S.op("dve", lambda e: e.tensor_copy(out=cntf, in_=pc[:, 0:32]), reads=[pctok], writes=[t_rs])
    S.op("dve", lambda e: e.memset(nbf, 0.0), reads=[t_rs], writes=[t_rs])
    for j in range(16):
        S.op("dve", lambda e, j=j: e.scalar_tensor_tensor(out=nbf, in0=cntf, scalar=128.0 * j, in1=nbf, op0=ALU.is_gt, op1=ALU.add), reads=[t_rs], writes=[t_rs])
    S.op("dve", lambda e: e.tensor_scalar(out=padded, in0=nbf, scalar1=128.0, scalar2=None, op0=ALU.mult), reads=[t_rs], writes=[t_rs])
    S.op("dve", lambda e: e.tensor_tensor_scan(out=pad_end, data0=ones_f[:, 0:32], data1=padded, initial=0.0, op0=ALU.mult, op1=ALU.add), reads=[t_rs, t_const], writes=[t_rs])
    S.op("dve", lambda e: e.tensor_tensor(out=pad_start, in0=pad_end, in1=padded, op=ALU.subtract), reads=[t_rs], writes=[t_rs])
    DestF = sb("DestF", [128, 32], F32)
    t_destf = Tok()
    dt_ring = Ring([sb("dtt%d" % i, [128, 64], F32) for i in range(2)])
    for ti in range(16):
        pC, pCtok = psA_ring.next()
        for t2 in range(ti):
            S.op("pe", lambda e, pC=pC, t2=t2: e.matmul(pC[:, 0:32], ones_b, Wselb[:, t2, :], start=(t2 == 0), stop=False), reads=[t_wsel, t_cb], writes=[pCtok])
        S.op("pe", lambda e, pC=pC, ti=ti: e.matmul(pC[:, 0:32], stri_b[:], Wselb[:, ti, :], start=(ti == 0), stop=True), reads=[t_wsel, t_stri], writes=[pCtok])
        dtt, dtok = dt_ring.next()
        S.op("dve", lambda e, pC=pC, dtt=dtt: e.tensor_tensor(out=dtt[:, 0:32], in0=pC[:, 0:32], in1=pad_start, op=ALU.add), reads=[pCtok, t_rs], writes=[dtok])
        for k in range(2):
            S.op("dve", lambda e, dtt=dtt, ti=ti, k=k: e.tensor_tensor(out=dtt[:, 32:64], in0=dtt[:, 0:32], in1=Sel[:, ti, k, :], op=ALU.mult), reads=[dtok, t_Sel], writes=[dtok])
            S.op("dve", lambda e, dtt=dtt, ti=ti, k=k: e.tensor_reduce(out=DestF[:, ti * 2 + k:ti * 2 + k + 1], in_=dtt[:, 32:64], axis=AX.X, op=ALU.add),
                 reads=[dtok], wadd=[t_destf])
    S.op("dve", lambda e: e.tensor_copy(out=Desti[:], in_=DestF[:]), reads=[t_destf], writes=[t_dest])
    buf_d = dscr("moebuf_s", [8192, D], BF16)
    ybuf_d = dscr("moey_s", [8192, D], F32)
    t_buf = Tok()
    for ti in range(16):
        for k in range(2):
            S.idma(buf_d[:, :], bass.IndirectOffsetOnAxis(ap=Desti[:, ti * 2 + k:ti * 2 + k + 1], axis=0), h2tok[:, ti, :], None,
                   reads=[t_h2tok, t_dest], wadd=[t_buf])
    S.op("dve", lambda e: e.memset(bef, 0.0), reads=[t_rs], writes=[t_rs])
    TH = consts[:, 782:846]
    for ex in range(32):
        S.op("dve", lambda e, ex=ex: e.scalar_tensor_tensor(out=bef, in0=TH, scalar=rs[:, 96 + ex:97 + ex], in1=bef, op0=ALU.is_ge, op1=ALU.add), reads=[t_rs, t_const], writes=[t_rs])
    S.op("dve", lambda e: e.tensor_scalar(out=bef, in0=bef, scalar1=31.0, scalar2=None, op0=ALU.min), reads=[t_rs], writes=[t_rs])
    S.op("dve", lambda e: e.tensor_scalar(out=be1024, in0=bef, scalar1=1024.0, scalar2=None, op0=ALU.mult), reads=[t_rs], writes=[t_rs])
    S.op("dve", lambda e: e.tensor_scalar(out=be512, in0=bef, scalar1=512.0, scalar2=None, op0=ALU.mult), reads=[t_rs], writes=[t_rs])
    S.op("dve", lambda e: e.tensor_scalar(out=rs[:, 384:448], in0=TH, scalar1=rs[:, 127:128], scalar2=None, op0=ALU.is_ge), reads=[t_rs, t_const], writes=[t_rs])
    S.op("dve", lambda e: e.scalar_tensor_tensor(out=be1024, in0=rs[:, 384:448], scalar=1.0e6, in1=be1024, op0=ALU.mult, op1=ALU.add), reads=[t_rs], writes=[t_rs])
    S.op("dve", lambda e: e.scalar_tensor_tensor(out=be512, in0=rs[:, 384:448], scalar=1.0e6, in1=be512, op0=ALU.mult, op1=ALU.add), reads=[t_rs], writes=[t_rs])
    idxf = sb("idxf", [128, 64 * 12], F32)
    t_idxf = Tok()
    for b in range(64):
        S.op("dve", lambda e, b=b: e.tensor_scalar(out=idxf[:, b * 12:b * 12 + 8], in0=consts[:, 770:778], scalar1=rs[:, 256 + b:257 + b], scalar2=None, op0=ALU.add),
             reads=[t_rs, t_const], wadd=[t_idxf])
        S.op("dve", lambda e, b=b: e.tensor_scalar(out=idxf[:, b * 12 + 8:b * 12 + 12], in0=consts[:, 770:774], scalar1=rs[:, 320 + b:321 + b], scalar2=None, op0=ALU.add),
             reads=[t_rs, t_const], wadd=[t_idxf])
    S.op("dve", lambda e: e.tensor_copy(out=idxi[:], in_=idxf[:]), reads=[t_idxf], writes=[t_idx])
    dump("DestF", DestF[:], [128, 32], [t_destf])
    dump("rs", rs[:], [128, 512], [t_rs])
    release(n_s6)
    if dbg == 7:
        S.finish([t_x1, t_buf, t_idx] + dump_toks)
        nc.used_inputs = used_inputs
        return nc

    n_s7 = len(es)
    bc_reg = nc.gpsimd.to_reg(NE * D - 1)
    ewi_rows = ewi_d.rearrange("e r n -> (e r) n")
    ewo_rows = ewo_d.rearrange("e r n -> (e r) n")
    wib_ring = Ring([sb("wib%d" % i, [128, 8, 2 * DEXP], BF16) for i in range(2)])
    wob_ring = Ring([sb("wob%d" % i, [128, 4, D], BF16) for i in range(2)])
    xb_ring = Ring([sb("xbr%d" % i, [128, D], BF16) for i in range(2)])
    xbT_ring = Ring([sb("xbT%d" % i, [128, 8, 128], BF16) for i in range(2)])
    sil_ring = Ring([sb("sil%d" % i, [128, 512], F32) for i in range(2)])
    hT_ring = Ring([sb("hT%d" % i, [128, 4, 128], BF16) for i in range(2)])
    ysb_ring = Ring([sb("ysb%d" % i, [128, D], F32) for i in range(2)])
    t_ybuf = Tok()
    for b in range(int(os.environ.get("BLIM", 64))):
        wib, wibtok = wib_ring.next()
        wob, wobtok = wob_ring.next()
        for k in range(8):
            S.idma(wib[:, k, :], None, ewi_rows[:, :], bass.IndirectOffsetOnAxis(ap=idxi[:, b * 12 + k:b * 12 + k + 1], axis=0),
                   reads=[t_idx], bounds_check=bc_reg, oob_is_err=False, **(dict(writes=[wibtok]) if k == 0 else dict(wadd=[wibtok])))
        for j in range(4):
            S.idma(wob[:, j, :], None, ewo_rows[:, :], bass.IndirectOffsetOnAxis(ap=idxi[:, b * 12 + 8 + j:b * 12 + 9 + j], axis=0),
                   reads=[t_idx], bounds_check=bc_reg, oob_is_err=False, **(dict(writes=[wobtok]) if j == 0 else dict(wadd=[wobtok])))
        xb, xbtok = xb_ring.next()
        S.dma("sp", xb[:], buf_d[b * 128:(b + 1) * 128, :], reads=[t_buf], writes=[xbtok])
        pb, pbtok = psB_ring.next()
        for k in range(8):
            S.op("pe", lambda e, pb=pb, xb=xb, k=k: e.transpose(pb[:, k * 128:(k + 1) * 128], xb[:, k * 128:(k + 1) * 128], ident_b), reads=[xbtok, t_cb], writes=[pbtok])
        xbT, xbTtok = xbT_ring.next()
        S.op("dve", lambda e, xbT=xbT, pb=pb: e.tensor_copy(out=xbT[:].rearrange("p k n -> p (k n)"), in_=pb[:, :]), reads=[pbtok], writes=[xbTtok])
        pg, pgtok = psA_ring.next()
        pu, putok = psA_ring.next()
        for (pp, pptok, c0) in ((pg, pgtok, 0), (pu, putok, DEXP)):
            for j in range(4):
                for k in range(8):
                    S.op("pe", lambda e, pp=pp, j=j, k=k, c0=c0, wib=wib, xbT=xbT: e.matmul(
                        pp[:, j * 128:(j + 1) * 128], wib[:, k, c0 + j * 128:c0 + (j + 1) * 128], xbT[:, k, :], start=(k == 0), stop=(k == 7)),
                        reads=[wibtok, xbTtok], writes=[pptok])
        sil, siltok = sil_ring.next()
        S.op("act", lambda e, sil=sil, pg=pg: e.activation(out=sil[:], in_=pg[:, :], func=AF.Silu), reads=[pgtok], writes=[siltok])
        hT, hTtok = hT_ring.next()
        S.op("dve", lambda e, hT=hT, sil=sil, pu=pu: e.tensor_tensor(out=hT[:].rearrange("p j n -> p (j n)"), in0=pu[:, :], in1=sil[:], op=ALU.mult),
             reads=[putok, siltok], writes=[hTtok])
        ysb, ysbtok = ysb_ring.next()
        for half in range(2):
            py, pytok = psA_ring.next()
            for j in range(4):
                S.op("pe", lambda e, py=py, j=j, hT=hT, wob=wob, half=half: e.matmul(
                    py[:, :], hT[:, j, :], wob[:, j, half * 512:(half + 1) * 512], start=(j == 0), stop=(j == 3)), reads=[hTtok, wobtok], writes=[pytok])
            S.op("dve", lambda e, py=py, ysb=ysb, half=half: e.tensor_copy(out=ysb[:, half * 512:(half + 1) * 512], in_=py[:, :]),
                 reads=[pytok], **(dict(writes=[ysbtok]) if half == 0 else dict(wadd=[ysbtok])))
        S.dma("sp", ybuf_d[b * 128:(b + 1) * 128, :], ysb[:], reads=[ysbtok], wadd=[t_ybuf])
    release(n_s7)

    fng = sb("fng", [128, D], F32)
    t_fng = Tok()
    S.dma("sp", fng[:], rows_d[0:1, R_FNG:R_FNG + D].partition_broadcast(128), writes=[t_fng])
    fj = sb("fjunk", [128, D], F32)
    t_fj = Tok()
    fs_ring = Ring([(sb("fsa%d" % i, [128, 1], F32), sb("fsb%d" % i, [128, 1], F32)) for i in range(4)])
    y1_ring = Ring([sb("y1g%d" % i, [128, D], F32) for i in range(2)])
    y2_ring = Ring([sb("y2g%d" % i, [128, D], F32) for i in range(2)])
    x1l_ring = Ring([sb("x1l%d" % i, [128, D], F32) for i in range(2)])
    t_out = Tok()
    for ti in range(16):
        y1, y1tok = y1_ring.next()
        y2, y2tok = y2_ring.next()
        S.idma(y1[:, :], None, ybuf_d[:, :], bass.IndirectOffsetOnAxis(ap=Desti[:, ti * 2:ti * 2 + 1], axis=0), reads=[t_dest, t_ybuf], writes=[y1tok])
        S.idma(y2[:, :], None, ybuf_d[:, :], bass.IndirectOffsetOnAxis(ap=Desti[:, ti * 2 + 1:ti * 2 + 2], axis=0), reads=[t_dest, t_ybuf], writes=[y2tok])
        xl, xltok = x1l_ring.next()
        S.dma("sp", xl[:], x1_d[ti], reads=[t_x1], writes=[xltok])
        S.op("dve", lambda e, y1=y1, ti=ti: e.tensor_scalar(out=y1[:], in0=y1[:], scalar1=Wt[:, ti * 2:ti * 2 + 1], scalar2=None, op0=ALU.mult), reads=[y1tok, t_Wt], writes=[y1tok])
        S.op("dve", lambda e, y1=y1, y2=y2, ti=ti: e.scalar_tensor_tensor(out=y1[:], in0=y2[:], scalar=Wt[:, ti * 2 + 1:ti * 2 + 2], in1=y1[:], op0=ALU.mult, op1=ALU.add),
             reads=[y1tok, y2tok, t_Wt], writes=[y1tok])
        S.op("dve", lambda e, y1=y1: e.tensor_tensor(out=y1[:], in0=y1[:], in1=g12[:, D:2 * D], op=ALU.mult), reads=[y1tok, t_g12], writes=[y1tok])
        S.op("dve", lambda e, y1=y1, xl=xl: e.tensor_tensor(out=xl[:], in0=y1[:], in1=xl[:], op=ALU.add), reads=[y1tok, xltok], writes=[xltok])
        fs, fstok = fs_ring.next()
        S.op("act", lambda e, xl=xl, fs=fs: e.activation(out=fj[:], in_=xl[:], func=AF.Square, accum_out=fs[0][:, 0:1]), reads=[xltok], writes=[t_fj, fstok])
        S.op("dve", lambda e, fs=fs: e.tensor_scalar(out=fs[1][:, 0:1], in0=fs[0][:, 0:1], scalar1=1.0 / D, scalar2=EPS, op0=ALU.mult, op1=ALU.add), reads=[fstok], writes=[fstok])
        S.op("act", lambda e, fs=fs: e.activation(out=fs[0][:, 0:1], in_=fs[1][:, 0:1], func=AF.Ln), reads=[fstok], writes=[fstok])
        S.op("act", lambda e, fs=fs: e.activation(out=fs[1][:, 0:1], in_=fs[0][:, 0:1], func=AF.Exp, scale=-0.5), reads=[fstok], writes=[fstok])
        S.op("dve", lambda e, xl=xl, fs=fs: e.scalar_tensor_tensor(out=xl[:], in0=xl[:], scalar=fs[1][:, 0:1], in1=fng[:], op0=ALU.mult, op1=ALU.mult),
             reads=[fstok, t_fng, xltok], writes=[xltok])
        S.dma("sp", out_d[ti * 128:(ti + 1) * 128, :], xl[:], reads=[xltok], wadd=[t_out])
    S.finish([t_out] + dump_toks)
    nc.used_inputs = used_inputs
    return nc


def _host_inputs(inp):
    f = lambda a: np.ascontiguousarray(np.asarray(a, dtype=np.float32))
    x, c, ctx, c_ctx = f(inp["x"]), f(inp["c"]), f(inp["ctx"]), f(inp["c_ctx"])
    ada_w, ada_b = f(inp["ada_w"])[0], f(inp["ada_b"])[0]
    w_in = f(inp["w_in"])[0]
    up_w, up_b = f(inp["gla_up_w"])[0], f(inp["gla_up_b"])[0]
    conv_w, conv_b = f(inp["ml_conv_w"])[0], f(inp["ml_conv_b"])[0]
    i_b, f_b = f(inp["ml_i_b"])[0], f(inp["ml_f_b"])[0]
    consts = np.zeros((128, 1024), np.float32)
    consts[0:64, 512:640] = 1.0
    consts[64:128, 640:768] = 1.0
    consts[0:64, 768] = 1.0
    consts[64:128, 769] = 1.0
    consts[:, 770:782] = (np.arange(12)[None, :] % 8) * 128 + np.arange(128)[:, None]
    consts[:, 782:846] = np.arange(64)[None, :] * 128.0
    consts[:, 0:128] = np.eye(128)
    consts[:, 128:256] = np.triu(np.ones((128, 128)))
    consts[:, 256:384] = np.tril(np.ones((128, 128)))
    consts[:, 384:512] = 1.0
    router_w = np.concatenate([f(inp["router_group_w"])[0], f(inp["router_expert_w"])[0]], axis=1)
    shared = {
        "ada_w": ada_w, "ada_b": ada_b[None, :], "w_out": f(inp["w_out"])[0], "router_w": np.ascontiguousarray(router_w),
        "e_w_in": f(inp["expert_w_in"])[0], "e_w_out": f(inp["expert_w_out"])[0], "consts": consts,
    }
    maps = []
    for core in range(8):
        b, flip = core // 2, core % 2
        xs, cs = x[b], ctx[b]
        win, uw, ub, cw, ib, fb = w_in, up_w, up_b, conv_w, i_b, f_b
        if flip:
            xs, cs = xs[::-1], cs[::-1]
            win = win.copy()
            win[:, C_LR:C_LR + 16], win[:, C_LR + 16:C_LR + 32] = w_in[:, C_LR + 16:C_LR + 32], w_in[:, C_LR:C_LR + 16]
            win[:, C_MI:C_MI + 4], win[:, C_MI + 4:C_MI + 8] = w_in[:, C_MI + 4:C_MI + 8], w_in[:, C_MI:C_MI + 4]
            win[:, C_MI + 8:C_MI + 12], win[:, C_MI + 12:C_MI + 16] = w_in[:, C_MI + 12:C_MI + 16], w_in[:, C_MI + 8:C_MI + 12]
            uw, ub, ib, fb = uw[::-1], ub[::-1], ib[::-1], fb[::-1]
            cw = cw[::-1, ::-1]
        vecs = np.zeros((128, NV), np.float32)
        vecs[:, V_N1G:V_N1G + 8] = f(inp["norm1_g"])[0].reshape(8, 128).T
        vecs[:, V_N2G:V_N2G + 8] = f(inp["norm2_g"])[0].reshape(8, 128).T
        vecs[:, V_UPB:V_UPB + 4] = ub.reshape(2, 2, 128).transpose(2, 0, 1).reshape(128, 4)
        vecs[:, V_CONVB:V_CONVB + 8] = conv_b.reshape(8, 128).T
        vecs[:, V_CONVW:V_CONVW + 72] = cw.reshape(9, 8, 128).transpose(2, 1, 0).reshape(128, 72)
        vecs[:, V_GNG:V_GNG + 4] = f(inp["gla_norm_g"])[0].reshape(4, 128).T
        vecs[:, V_MNG:V_MNG + 4] = f(inp["ml_norm_g"])[0].reshape(4, 128).T
        rows = np.zeros((1, NR), np.float32)
        rows[0, R_GATEB:R_GATEB + 8] = ib.reshape(8)
        rows[0, R_GATEB + 8:R_GATEB + 16] = fb.reshape(8)
        rows[0, R_FNG:R_FNG + 1024] = f(inp["final_norm_g"])
        rows[0, R_RB:R_RB + 4] = f(inp["router_group_b"])[0]
        rows[0, R_RB + 4:R_RB + 36] = f(inp["router_expert_b"])[0]
        rows[0, R_N2G:R_N2G + 1024] = f(inp["norm2_g"])[0]
        cvec = np.concatenate([c[b].reshape(128, 8), c_ctx.reshape(128, 8)], axis=1)
        m = dict(shared)
        m.update({
            "x": np.ascontiguousarray(xs), "ctx": np.ascontiguousarray(cs), "cvec": np.ascontiguousarray(cvec),
            "vecs": vecs, "rows": rows, "w_in": np.ascontiguousarray(win), "up_w": np.ascontiguousarray(uw),
        })
        maps.append(m)
    return maps


def kernel(**inputs):
    maps = _host_inputs(inputs)
    nc = build()
    maps = [{k: m[k] for k in nc.used_inputs} for m in maps]
    res = run_bass_kernel_spmd(nc, maps, core_ids=list(range(8)))
    out = np.zeros((4, SEQ, D), np.float32)
    for core in range(8):
        b, flip = core // 2, core % 2
        o = res.results[core]["out"]
        if flip:
            out[b, NLOC:] = o[::-1]
        else:
            out[b, :NLOC] = o
    return out
```

```python
import numpy as np
import concourse.bass as bass
import concourse.mybir as mybir
from concourse.bass_utils import run_bass_kernel_spmd

F32 = mybir.dt.float32
BF16 = mybir.dt.bfloat16
AF = mybir.ActivationFunctionType
ALU = mybir.AluOpType
AX = mybir.AxisListType

D = 1024
SEQ = 4096
NLOC = 2048
CTX = 256
INW = 3632
NE = 32
DEXP = 512
EPS = 1e-6
NBLK = 9
C_GQ, C_GK, C_GV, C_GG, C_LR, C_MQ, C_MK, C_MV, C_MO, C_MI = 0, 256, 512, 1024, 1536, 1568, 2080, 2592, 3104, 3616

V_N1G, V_N2G, V_UPB, V_CONVB, V_CONVW, V_GNG, V_MNG = 0, 8, 16, 20, 28, 100, 104
NV = 108
R_GATEB, R_FNG, R_RB = 0, 16, 16 + 1024
R_N2G = 16 + 1024 + 36
NR = 16 + 1024 + 36 + 1024
I32 = mybir.dt.int32


class Tok:
    __slots__ = ("w", "r")

    def __init__(self):
        self.w = {}
        self.r = {}


class Sched:
    NDMA = 6

    def __init__(self, nc):
        self.nc = nc
        self.eng = {"pe": nc.tensor, "dve": nc.vector, "act": nc.scalar, "pool": nc.gpsimd, "sp": nc.sync}
        self.sem = {}
        self.cnt = {}
        self.waited = {k: {} for k in self.eng}
        self._cms = []
        for k in ["pe", "dve", "act", "pool"]:
            self._mk(k)
        self.dq = {}
        self.nq = {"sp": 8, "pool": 16, "act": 2}
        for q in ["sp", "pool", "act"]:
            names = []
            for i in range(self.nq[q]):
                n = "d%s%d" % (q, i)
                self._mk(n)
                names.append(n)
            self.dq[q] = [names, 0]
        self.ninstr = 0

    def _mk(self, k):
        cm = self.nc.semaphore("s_" + k)
        self.sem[k] = cm.__enter__()
        self._cms.append(cm)
        self.cnt[k] = 0

    def _wait(self, e, key, val):
        if self.waited[e].get(key, 0) >= val:
            return
        self.eng[e].wait_ge(self.sem[key], val)
        self.waited[e][key] = val

    def _deps(self, e, reads, writes):
        for t in reads:
            for k, v in t.w.items():
                if not (k == "pe" and e == "pe"):
                    self._wait(e, k, v)
        for t in writes:
            for k, v in t.w.items():
                if not (k == "pe" and e == "pe"):
                    self._wait(e, k, v)
            for k, v in t.r.items():
                if k != e:
                    self._wait(e, k, v)

    def _mark(self, key, val, reads, writes, wadd):
        for t in reads:
            if t.r.get(key, 0) < val:
                t.r[key] = val
        for t in writes:
            t.w = {key: val}
            t.r = {}
        for t in wadd:
            t.w[key] = val

    def op(self, e, fn, reads=(), writes=(), wadd=()):
        self._deps(e, reads, writes)
        for t in wadd:
            for k, v in t.r.items():
                if k != e:
                    self._wait(e, k, v)
        ins = fn(self.eng[e])
        self.cnt[e] += 1
        ins.then_inc(self.sem[e], 1)
        self._mark(e, self.cnt[e], reads, writes, wadd)
        self.ninstr += 1
        return ins

    def dma(self, q, out, in_, reads=(), writes=(), wadd=(), **kw):
        names, idx = self.dq[q]
        key = names[idx % len(names)]
        self.dq[q][1] = idx + 1
        self._wait(q, key, self.cnt[key])
        self._deps(q, reads, writes)
        for t in wadd:
            for k, v in t.r.items():
                self._wait(q, k, v)
        ins = self.eng[q].dma_start(out=out, in_=in_, **kw)
        self.cnt[key] += 16
        ins.then_inc(self.sem[key], 16)
        self._mark(key, self.cnt[key], reads, writes, wadd)
        self.ninstr += 1
        return ins

    def idma(self, out, out_off, in_, in_off, reads=(), writes=(), wadd=(), **kw):
        q = "pool"
        names, idx = self.dq[q]
        key = names[idx % len(names)]
        self.dq[q][1] = idx + 1
        self._wait(q, key, self.cnt[key])
        self._deps(q, reads, writes)
        for t in wadd:
            for k, v in t.r.items():
                self._wait(q, k, v)
        ins = self.eng[q].indirect_dma_start(out=out, out_offset=out_off, in_=in_, in_offset=in_off, **kw)
        self.cnt[key] += 16
        ins.then_inc(self.sem[key], 16)
        self._mark(key, self.cnt[key], reads, writes, wadd)
        self.ninstr += 1
        return ins

    def barrier(self):
        import os
        if os.environ.get('NOBAR'):
            return
        for e in self.eng:
            if e == 'pool' and os.environ.get('NOPOOLBAR'):
                continue
            for k in self.sem:
                if self.cnt[k] > 0 and k != e:
                    self._wait(e, k, self.cnt[k])

    def finish(self, toks, e="sp"):
        for t in toks:
            for k, v in t.w.items():
                self._wait(e, k, v)


class Ring:
    def __init__(self, items):
        self.items = items
        self.toks = [Tok() for _ in items]
        self.i = 0

    def next(self):
        j = self.i % len(self.items)
        self.i += 1
        return self.items[j], self.toks[j]


def build(dbg=0):
    import os
    dbg = int(os.environ.get("KSTOP", dbg))
    nc = bass.Bass("TRN2", target_bir_lowering=False)
    import os
    scratch_kind = "ExternalOutput" if (dbg or os.environ.get("SCR_EXT")) else "Internal"

    used_inputs = []

    def din(name, shape, dt=F32, need=0):
        if dbg and dbg < need:
            return None
        used_inputs.append(name)
        return nc.dram_tensor(name, list(shape), dt, kind="ExternalInput").ap()

    dump_toks = []

    def dump(name, ap, shape, toks, dt=F32):
        if not dbg:
            return
        dd = nc.dram_tensor("dbg_" + name, list(shape), dt, kind="ExternalOutput").ap()
        t = Tok()
        S.dma("sp", dd, ap, reads=toks, writes=[t])
        dump_toks.append(t)

    def dscr(name, shape, dt):
        return nc.dram_tensor(name, list(shape), dt, kind=scratch_kind).ap()

    x_d = din("x", [SEQ, D])
    ctx_d = din("ctx", [CTX, D])
    cvec_d = din("cvec", [128, 16])
    adaw_d = din("ada_w", [D, 6 * D])
    adab_d = din("ada_b", [1, 6 * D])
    vecs_d = din("vecs", [128, NV])
    rows_d = din("rows", [1, NR])
    win_d = din("w_in", [D, INW])
    upw_d = din("up_w", [2, 16, 256])
    wout_d = din("w_out", [D, D])
    rw_d = din("router_w", [D, 36])
    ewi_d = din("e_w_in", [NE, D, 2 * DEXP], need=8)
    ewo_d = din("e_w_out", [NE, DEXP, D], need=8)
    consts_d = din("consts", [128, 1024])
    out_d = nc.dram_tensor("out", [NLOC, D], F32, kind="ExternalOutput").ap()

    xnT_d = dscr("xnT_s", [NBLK, 128, 8 * 512], BF16)
    gqk_d = dscr("gqk_s", [NBLK, 128, 4 * 512], BF16)
    lr_d = dscr("lr_s", [NBLK, 16, 2 * 512], F32)
    sgg_d = dscr("sgg_s", [4, 128, 4 * 512], BF16)
    smo_d = dscr("smo_s", [4, 128, 4 * 512], BF16)
    mpre_d = dscr("mpre_s", [NBLK, 128, 8 * 512], BF16)
    gv_d = dscr("gv_s", [NBLK, 128, 4 * 512], BF16)
    mv_d = dscr("mv_s", [NBLK, 128, 4 * 512], BF16)
    gates_d = dscr("gates_s", [NBLK, 128, 4 * 16], F32)
    mqk_d = dscr("mqk_s", [NBLK, 128, 8 * 512], BF16)

    S = Sched(nc)
    es = []
    uid = [0]

    def sb(name, shape, dt):
        uid[0] += 1
        cm = nc.sbuf_tensor("sb%d_%s" % (uid[0], name), list(shape), dt)
        t = cm.__enter__()
        es.append(cm)
        return t

    def ps(name, shape, dt):
        uid[0] += 1
        cm = nc.psum_tensor("ps%d_%s" % (uid[0], name), list(shape), dt)
        t = cm.__enter__()
        es.append(cm)
        return t

    def release(n0):
        S.barrier()
        while len(es) > n0:
            es.pop().__exit__(None, None, None)

    consts = sb("consts", [128, 1024], F32)
    vecs = sb("vecs", [128, NV], F32)
    t_const = Tok()
    S.dma("sp", consts[:], consts_d[:, :], writes=[t_const])
    S.dma("sp", vecs[:], vecs_d[:, :], wadd=[t_const])
    ident_f = consts[:, 0:128]
    ones_f = consts[:, 384:512]
    cb = sb("constsb", [128, 512], BF16)
    t_cb = Tok()
    S.op("dve", lambda e: e.tensor_copy(out=cb[:], in_=consts[:, 0:512]), reads=[t_const], writes=[t_cb])
    ident_b = cb[:, 0:128]
    mask_b = [cb[:, 128:256], cb[:, 256:384]]
    ones_b = cb[:, 384:512]

    psA = [ps("psA%d" % i, [128, 512], F32) for i in range(6)]
    psB = [ps("psB%d" % i, [128, 1024], BF16) for i in range(2)]
    psA_ring = Ring(psA)
    psB_ring = Ring(psB)

    t_mod = Tok()
    A1 = sb("A1", [128, 4 * 8], F32)
    A2 = sb("A2", [128, 2 * 8], F32)
    t_A = Tok()
    g12 = sb("g12", [128, 4 * D], F32)
    t_g12 = Tok()
    idxi = sb("idxi", [128, 64 * 12], I32)
    Desti = sb("Desti", [128, 32], I32)
    Wt = sb("Wt", [128, 32], F32)
    t_idx, t_dest, t_Wt = Tok(), Tok(), Tok()

    n_keep = len(es)
    modx = sb("modx", [1, 6 * D], F32)
    modc = sb("modc", [1, 2 * D], F32)
    cvec = sb("cvec", [128, 16], F32)
    scv = sb("scv", [128, 16], F32)
    adab = sb("adab", [1, 6 * D], F32)
    t_cv, t_scv, t_adab = Tok(), Tok(), Tok()
    S.dma("sp", cvec[:], cvec_d[:, :], writes=[t_cv])
    S.dma("sp", adab[:], adab_d[:, :], writes=[t_adab])
    S.op("act", lambda e: e.activation(out=scv[:], in_=cvec[:], func=AF.Silu), reads=[t_cv], writes=[t_scv])
    adaw_v = adaw_d.rearrange("(p k) n -> p k n", k=8)
    wst = [sb("adaw%d" % i, [128, 8, 512], F32) for i in range(2)]
    wst_ring = Ring(wst)
    S.op("dve", lambda e: e.memset(modx[:], 0.0), writes=[t_mod])
    for blk in range(12):
        wt, wtok = wst_ring.next()
        S.dma("sp", wt[:], adaw_v[:, :, blk * 512:(blk + 1) * 512], writes=[wtok])
        for which in range(2):
            if which == 1 and blk >= 4:
                continue
            pt, ptok = psA_ring.next()
            for k in range(8):
                S.op("pe", lambda e, k=k, pt=pt, wt=wt, which=which: e.matmul(
                    pt[0:1, :], scv[:, which * 8 + k:which * 8 + k + 1], wt[:, k, :], start=(k == 0), stop=(k == 7)),
                    reads=[t_scv, wtok], writes=[ptok])
            dst = modx if which == 0 else modc
            S.op("dve", lambda e, pt=pt, dst=dst, blk=blk: e.tensor_tensor(
                out=dst[0:1, blk * 512:(blk + 1) * 512], in0=pt[0:1, :], in1=adab[0:1, blk * 512:(blk + 1) * 512], op=ALU.add),
                reads=[ptok, t_adab], wadd=[t_mod])
    colps, coltok = psA_ring.next()
    specs = [(modx, 1 * D), (modx, 0 * D), (modc, 1 * D), (modc, 0 * D), (modx, 4 * D), (modx, 3 * D)]
    first = True
    for si, (src, off) in enumerate(specs):
        for k in range(8):
            S.op("pe", lambda e, src=src, off=off, k=k, si=si: e.matmul(
                colps[:, si * 8 + k:si * 8 + k + 1], src[0:1, off + k * 128:off + (k + 1) * 128], ones_f[0:1, 0:1], start=True, stop=True),
                reads=[t_mod, t_const], writes=[coltok] if first else (), wadd=() if first else [coltok])
            first = False
    for (dst, c0, g0, s_sc, s_sh) in [(A1, 0, V_N1G, 0, 1), (A1, 16, V_N1G, 2, 3), (A2, 0, V_N2G, 4, 5)]:
        S.op("dve", lambda e, dst=dst, c0=c0, g0=g0, s_sc=s_sc: e.scalar_tensor_tensor(
            out=dst[:, c0:c0 + 8], in0=colps[:, s_sc * 8:s_sc * 8 + 8], scalar=1.0, in1=vecs[:, g0:g0 + 8], op0=ALU.add, op1=ALU.mult),
            reads=[coltok, t_const], wadd=[t_A])
        S.op("dve", lambda e, dst=dst, c0=c0, s_sh=s_sh: e.tensor_copy(out=dst[:, c0 + 8:c0 + 16], in_=colps[:, s_sh * 8:s_sh * 8 + 8]),
             reads=[coltok], wadd=[t_A])
    for gi, off in enumerate([2 * D, 5 * D]):
        for hf in range(2):
            pt, ptok = psA_ring.next()
            S.op("pe", lambda e, pt=pt, off=off, hf=hf: e.matmul(pt[:, :], ones_f[0:1, :], modx[0:1, off + hf * 512:off + (hf + 1) * 512], start=True, stop=True),
                 reads=[t_mod, t_const], writes=[ptok])
            S.op("act", lambda e, pt=pt, gi=gi, hf=hf: e.copy(out=g12[:, gi * D + hf * 512:gi * D + (hf + 1) * 512], in_=pt[:, :]),
                 reads=[ptok], wadd=[t_g12])
    n2gbc = sb("n2gbc", [128, D], F32)
    t_n2g = Tok()
    S.dma("sp", n2gbc[:], rows_d[0:1, R_N2G:R_N2G + D].partition_broadcast(128), writes=[t_n2g])
    for gi, off in ((2, 4 * D), (3, 3 * D)):
        for hf in range(2):
            pt, ptok = psA_ring.next()
            S.op("pe", lambda e, pt=pt, off=off, hf=hf: e.matmul(pt[:, :], ones_f[0:1, :], modx[0:1, off + hf * 512:off + (hf + 1) * 512], start=True, stop=True),
                 reads=[t_mod, t_const], writes=[ptok])
            dst = g12[:, gi * D + hf * 512:gi * D + (hf + 1) * 512]
            if gi == 2:
                S.op("dve", lambda e, pt=pt, dst=dst, hf=hf: e.scalar_tensor_tensor(out=dst, in0=pt[:, :], scalar=1.0, in1=n2gbc[:, hf * 512:(hf + 1) * 512], op0=ALU.add, op1=ALU.mult),
                     reads=[ptok, t_n2g], wadd=[t_g12])
            else:
                S.op("dve", lambda e, pt=pt, dst=dst: e.tensor_copy(out=dst, in_=pt[:, :]), reads=[ptok], wadd=[t_g12])
    dump("g12", g12[:, 0:2 * D], [128, 2 * D], [t_g12])
    dump("A1", A1[:], [128, 32], [t_A])
    dump("modx", modx[:], [1, 6 * D], [t_mod])
    if dbg == 1:
        S.finish(dump_toks)
        nc.used_inputs = used_inputs
        return nc
    release(n_keep)

    xt_ring = Ring([sb("xt%d" % i, [128, D], F32) for i in range(2)])
    xs_ring = Ring([sb("xs%d" % i, [128, D], BF16) for i in range(2)])
    junk = sb("junk", [128, D], F32)
    xsf_ring = Ring([sb("xsf%d" % i, [128, D], F32) for i in range(2)])
    t_junk = Tok()
    ss_ring = Ring([(sb("ssa%d" % i, [128, 1], F32), sb("ssb%d" % i, [128, 1], F32)) for i in range(4)])
    xnb_ring = Ring([sb("xnb%d" % i, [128, 8, 512], BF16) for i in range(2)])
    t_xnT = [Tok() for _ in range(NBLK)]
    import os
    for blk in range(int(os.environ.get('KLIM', NBLK))):
        ntile = 2 if blk == 0 else 4
        xnb, xnbtok = xnb_ring.next()
        xfirst = [True]

        def xw(tok=xnbtok, xfirst=xfirst):
            if xfirst[0]:
                xfirst[0] = False
                return dict(writes=[tok])
            return dict(wadd=[tok])
        for ti in range(ntile):
            if blk == 0:
                src = ctx_d[ti * 128:(ti + 1) * 128, :]
                acol = 16
            else:
                r0 = (blk - 1) * 512 + ti * 128
                src = x_d[r0:r0 + 128, :]
                acol = 0
            xt, xttok = xt_ring.next()
            S.dma("sp", xt[:], src, writes=[xttok])
            ss, sstok = ss_ring.next()
            S.op("act", lambda e, xt=xt, ss=ss: e.activation(out=junk[:], in_=xt[:], func=AF.Square, accum_out=ss[0][:, 0:1]),
                 reads=[xttok], writes=[t_junk, sstok])
            S.op("dve", lambda e, ss=ss: e.tensor_scalar(out=ss[1][:, 0:1], in0=ss[0][:, 0:1], scalar1=1.0 / D, scalar2=EPS, op0=ALU.mult, op1=ALU.add),
                 reads=[sstok], writes=[sstok])
            S.op("act", lambda e, ss=ss: e.activation(out=ss[0][:, 0:1], in_=ss[1][:, 0:1], func=AF.Ln), reads=[sstok], writes=[sstok])
            S.op("act", lambda e, ss=ss: e.activation(out=ss[1][:, 0:1], in_=ss[0][:, 0:1], func=AF.Exp, scale=-0.5), reads=[sstok], writes=[sstok])
            xs, xstok = xs_ring.next()
            S.op("dve", lambda e, xs=xs, xt=xt, ss=ss: e.tensor_scalar(out=xs[:], in0=xt[:], scalar1=ss[1][:, 0:1], scalar2=None, op0=ALU.mult),
                 reads=[xttok, sstok], writes=[xstok])
            pb, pbtok = psB_ring.next()
            for k in range(8):
                S.op("pe", lambda e, pb=pb, xs=xs, k=k: e.transpose(pb[:, k * 128:(k + 1) * 128], xs[:, k * 128:(k + 1) * 128], ident_b),
                     reads=[xstok, t_cb], writes=[pbtok])
            for k in range(8):
                eng = "dve"
                if eng == "act":
                    S.op("act", lambda e, pb=pb, xnb=xnb, k=k, ti=ti, acol=acol: e.activation(
                        out=xnb[:, k, ti * 128:(ti + 1) * 128], in_=pb[:, k * 128:(k + 1) * 128], func=AF.Identity,
                        scale=A1[:, acol + k:acol + k + 1], bias=A1[:, acol + 8 + k:acol + 8 + k + 1]),
                        reads=[pbtok, t_A], **xw())
                else:
                    S.op("dve", lambda e, pb=pb, xnb=xnb, k=k, ti=ti, acol=acol: e.tensor_scalar(
                        out=xnb[:, k, ti * 128:(ti + 1) * 128], in0=pb[:, k * 128:(k + 1) * 128],
                        scalar1=A1[:, acol + k:acol + k + 1], scalar2=A1[:, acol + 8 + k:acol + 8 + k + 1], op0=ALU.mult, op1=ALU.add),
                        reads=[pbtok, t_A], **xw())
        S.dma("sp", xnT_d[blk], xnb[:].rearrange("p k n -> p (k n)"), reads=[xnbtok], writes=[t_xnT[blk]])
    release(n_keep)

    final_toks = list(t_xnT)
    if dbg == 2:
        S.finish(final_toks + dump_toks)
        nc.used_inputs = used_inputs
        return nc

    n_s2 = len(es)
    win_v = win_d.rearrange("(k p) n -> p k n", p=128)
    Wb = sb("Wb", [128, 8, INW], BF16)
    t_Wb = Tok()
    wstg = Ring([sb("wstg%d" % i, [128, 8, 227], F32) for i in range(2)])
    for pc in range(16):
        st, sttok = wstg.next()
        S.dma("sp", st[:], win_v[:, :, pc * 227:(pc + 1) * 227], writes=[sttok])
        S.op("dve", lambda e, st=st, pc=pc: e.tensor_copy(out=Wb[:, :, pc * 227:(pc + 1) * 227], in_=st[:]),
             reads=[sttok], **(dict(writes=[t_Wb]) if pc == 0 else dict(wadd=[t_Wb])))
    xin_ring = Ring([sb("xin%d" % i, [128, 8, 512], BF16) for i in range(2)])
    stg = {}
    for nm, shp, dt in [("gqk", [128, 4 * 512], BF16), ("sgg", [128, 4 * 512], BF16), ("smo", [128, 4 * 512], BF16),
                        ("mpre", [128, 8 * 512], BF16), ("gv", [128, 4 * 512], BF16), ("mv", [128, 4 * 512], BF16),
                        ("lr", [16, 2 * 512], F32), ("gates", [128, 64], F32)]:
        stg[nm] = Ring([sb("st_%s%d" % (nm, i), shp, dt) for i in range(2)])
    sig_ring = Ring([sb("sigt%d" % i, [128, 512], F32) for i in range(2)])
    t_gqk = [Tok() for _ in range(NBLK)]
    t_lr = [Tok() for _ in range(NBLK)]
    t_sgg = [Tok() for _ in range(4)]
    t_smo = [Tok() for _ in range(4)]
    t_mpre = [Tok() for _ in range(NBLK)]
    t_gv = [Tok() for _ in range(NBLK)]
    t_mv = [Tok() for _ in range(NBLK)]
    t_gates = [Tok() for _ in range(NBLK)]

    class Acc:
        def __init__(self, tok):
            self.tok = tok
            self.first = True

        def kw(self):
            if self.first:
                self.first = False
                return dict(writes=[self.tok])
            return dict(wadd=[self.tok])

    for blk in range(int(os.environ.get('KLIM2', NBLK))):
        N = 256 if blk == 0 else 512
        is_ctx, is_loc, is_far = blk == 0, 1 <= blk <= 4, blk >= 5
        xin, xintok = xin_ring.next()
        S.dma("sp", xin[:].rearrange("p k n -> p (k n)"), xnT_d[blk], reads=[t_xnT[blk]], writes=[xintok])
        cur = {nm: stg[nm].next() for nm in stg}
        acc = {nm: Acc(cur[nm][1]) for nm in stg}

        def cm_tile(col0, M, N=N, xin=xin, xintok=xintok):
            pt, ptok = psA_ring.next()
            for k in range(8):
                S.op("pe", lambda e, k=k, pt=pt: e.matmul(pt[0:M, 0:N], Wb[:, k, col0:col0 + M], xin[:, k, 0:N], start=(k == 0), stop=(k == 7)),
                     reads=[t_Wb, xintok], writes=[ptok])
            return pt, ptok

        def evac(nm, dst, pt, ptok, M, scale=None, N=N):
            if scale is None:
                S.op("dve", lambda e: e.tensor_copy(out=dst, in_=pt[0:M, 0:N]), reads=[ptok], **acc[nm].kw())
            else:
                S.op("dve", lambda e: e.tensor_scalar(out=dst, in0=pt[0:M, 0:N], scalar1=scale, scalar2=None, op0=ALU.mult),
                     reads=[ptok], **acc[nm].kw())

        def evac_sig(nm, dst, pt, ptok, silu, N=N):
            if silu:
                S.op("act", lambda e: e.activation(out=dst, in_=pt[:, 0:N], func=AF.Silu), reads=[ptok], **acc[nm].kw())
                return
            sg, sgtok = sig_ring.next()
            S.op("act", lambda e: e.activation(out=sg[:, 0:N], in_=pt[:, 0:N], func=AF.Exp, scale=-1.0), reads=[ptok], writes=[sgtok])
            S.op("dve", lambda e: e.tensor_scalar(out=sg[:, 0:N], in0=sg[:, 0:N], scalar1=1.0, scalar2=None, op0=ALU.add), reads=[sgtok], writes=[sgtok])
            S.op("dve", lambda e: e.reciprocal(out=sg[:, 0:N], in_=sg[:, 0:N]), reads=[sgtok], writes=[sgtok])
            if silu:
                S.op("dve", lambda e: e.tensor_tensor(out=dst, in0=pt[:, 0:N], in1=sg[:, 0:N], op=ALU.mult), reads=[ptok, sgtok], **acc[nm].kw())
            else:
                S.op("dve", lambda e: e.tensor_copy(out=dst, in_=sg[:, 0:N]), reads=[sgtok], **acc[nm].kw())

        gq_st, lr_st, sgg_st, smo_st = cur["gqk"][0], cur["lr"][0], cur["sgg"][0], cur["smo"][0]
        mp_st, gv_st, mv_st, ga_st = cur["mpre"][0], cur["gv"][0], cur["mv"][0], cur["gates"][0]
        for j in range(4):
            if j < 2 and not is_loc:
                continue
            col0 = C_GQ + j * 128 if j < 2 else C_GK + (j - 2) * 128
            pt, ptok = cm_tile(col0, 128)
            evac("gqk", gq_st[:, j * 512:j * 512 + N], pt, ptok, 128, scale=(0.125 if j < 2 else None))
        for dd in range(2):
            if is_far and dd == 0:
                continue
            pt, ptok = cm_tile(C_LR + dd * 16, 16)
            evac("lr", lr_st[0:16, dd * 512:dd * 512 + N], pt, ptok, 16)
        for j in range(8):
            if j < 4 and not (is_loc or blk == 5):
                continue
            col0 = C_MQ + j * 128 if j < 4 else C_MK + (j - 4) * 128
            pt, ptok = cm_tile(col0, 128)
            evac("mpre", mp_st[:, j * 512:j * 512 + N], pt, ptok, 128)
        if is_loc:
            for j in range(4):
                pt, ptok = cm_tile(C_GG + j * 128, 128)
                evac_sig("sgg", sgg_st[:, j * 512:(j + 1) * 512], pt, ptok, True)
            for j in range(4):
                pt, ptok = cm_tile(C_MO + j * 128, 128)
                evac_sig("smo", smo_st[:, j * 512:(j + 1) * 512], pt, ptok, False)
        for c in range(N // 128):
            for nm, col0, ncol, st_ in [("gv", C_GV, 512, gv_st), ("mv", C_MV, 512, mv_st), ("gates", C_MI, 16, ga_st)]:
                pt, ptok = psA_ring.next()
                for k in range(8):
                    S.op("pe", lambda e, k=k, pt=pt, c=c, col0=col0, ncol=ncol: e.matmul(
                        pt[:, 0:ncol], xin[:, k, c * 128:(c + 1) * 128], Wb[:, k, col0:col0 + ncol], start=(k == 0), stop=(k == 7)),
                        reads=[t_Wb, xintok], writes=[ptok])
                S.op("dve", lambda e, pt=pt, st_=st_, c=c, ncol=ncol: e.tensor_copy(out=st_[:, c * ncol:(c + 1) * ncol], in_=pt[:, 0:ncol]),
                     reads=[ptok], **acc[nm].kw())
        S.dma("sp", gqk_d[blk], gq_st[:], reads=[cur["gqk"][1]], writes=[t_gqk[blk]])
        S.dma("sp", lr_d[blk], lr_st[:], reads=[cur["lr"][1]], writes=[t_lr[blk]])
        S.dma("sp", mpre_d[blk], mp_st[:], reads=[cur["mpre"][1]], writes=[t_mpre[blk]])
        S.dma("sp", gv_d[blk], gv_st[:], reads=[cur["gv"][1]], writes=[t_gv[blk]])
        S.dma("sp", mv_d[blk], mv_st[:], reads=[cur["mv"][1]], writes=[t_mv[blk]])
        S.dma("sp", gates_d[blk], ga_st[:], reads=[cur["gates"][1]], writes=[t_gates[blk]])
        if is_loc:
            S.dma("sp", sgg_d[blk - 1], sgg_st[:], reads=[cur["sgg"][1]], writes=[t_sgg[blk - 1]])
            S.dma("sp", smo_d[blk - 1], smo_st[:], reads=[cur["smo"][1]], writes=[t_smo[blk - 1]])
    release(n_s2)
    if dbg == 3:
        S.finish(t_gqk + t_lr + t_sgg + t_smo + t_mpre + t_gv + t_mv + t_gates + dump_toks)
        nc.used_inputs = used_inputs
        return nc

    n_s3 = len(es)
    t_mqk = [Tok() for _ in range(NBLK)]
    Pbuf = sb("convP", [128, 66 * 64], BF16)
    accb = sb("convacc", [128, 64 * 64], F32)
    eb = sb("conve", [128, 64 * 64], F32)
    outb = sb("convout", [128, 64 * 64], BF16)
    tP, tacc, teb, toutb = Tok(), Tok(), Tok(), Tok()

    def conv_tile(j, R, Wd, srcs, dsts, taps_i):
        n_el = R * Wd
        S.op("dve", lambda e: e.memset(Pbuf[:, 0:Wd], 0.0), writes=[tP])
        S.op("dve", lambda e: e.memset(Pbuf[:, Wd + n_el:2 * Wd + n_el], 0.0), wadd=[tP])
        for (ap, tok, off, n) in srcs:
            S.dma("sp", Pbuf[:, Wd + off:Wd + off + n], ap, reads=[tok], wadd=[tP])
        wcol = lambda i, jj: vecs[:, V_CONVW + j * 9 + i * 3 + jj:V_CONVW + j * 9 + i * 3 + jj + 1]
        bcol = vecs[:, V_CONVB + j:V_CONVB + j + 1]
        S.op("dve", lambda e: e.tensor_scalar(out=accb[:, 0:n_el], in0=Pbuf[:, Wd:Wd + n_el], scalar1=wcol(1, 1), scalar2=bcol, op0=ALU.mult, op1=ALU.add),
             reads=[tP, t_const], writes=[tacc])
        P3 = Pbuf[:, 0:(R + 2) * Wd].rearrange("p (r c) -> p r c", c=Wd)
        A3 = accb[:, 0:n_el].rearrange("p (r c) -> p r c", c=Wd)
        for i in taps_i:
            for jj in range(3):
                if i == 1 and jj == 1:
                    continue
                oc0, oc1 = (1, Wd) if jj == 0 else ((0, Wd) if jj == 1 else (0, Wd - 1))
                ic0 = oc0 + jj - 1
                S.op("dve", lambda e, i=i, jj=jj, oc0=oc0, oc1=oc1, ic0=ic0: e.scalar_tensor_tensor(
                    out=A3[:, :, oc0:oc1], in0=P3[:, i:i + R, ic0:ic0 + (oc1 - oc0)], scalar=wcol(i, jj), in1=A3[:, :, oc0:oc1], op0=ALU.mult, op1=ALU.add),
                    reads=[tP, t_const, tacc], writes=[tacc])
        S.op("act", lambda e: e.activation(out=eb[:, 0:n_el], in_=accb[:, 0:n_el], func=AF.Exp, scale=-1.0), reads=[tacc], writes=[teb])
        S.op("dve", lambda e: e.tensor_scalar(out=eb[:, 0:n_el], in0=eb[:, 0:n_el], scalar1=1.0, scalar2=None, op0=ALU.add), reads=[teb], writes=[teb])
        S.op("dve", lambda e: e.reciprocal(out=eb[:, 0:n_el], in_=eb[:, 0:n_el]), reads=[teb], writes=[teb])
        sc = 1.0 if j < 4 else 128.0 ** -0.5
        S.op("dve", lambda e: e.scalar_tensor_tensor(out=outb[:, 0:n_el], in0=accb[:, 0:n_el], scalar=sc, in1=eb[:, 0:n_el], op0=ALU.mult, op1=ALU.mult),
             reads=[tacc, teb], writes=[toutb])
        for (ap, tok, off, n) in dsts:
            S.dma("sp", ap, outb[:, off:off + n], reads=[toutb], wadd=[tok])

    Ppad = sb("convPp", [128, 2 + 66 * 66], BF16)
    cstg = sb("convstg", [128, 64 * 64], BF16)
    accp = sb("convaccp", [128, 64 * 66], F32)
    ebp = sb("convebp", [128, 64 * 66], F32)
    dw_ring = Ring([sb("convdw%d" % i, [128, 9, 128], BF16) for i in range(2)])
    tPp, tcstg, taccp, tebp = Tok(), Tok(), Tok(), Tok()
    last_R = [None]

    def conv_tile_pe(j, R, srcs, dsts):
        n_el, npad = R * 64, R * 66
        first = True
        for (ap, tok, off, n) in srcs:
            S.dma("sp", cstg[:, off:off + n], ap, reads=[tok], **(dict(writes=[tcstg]) if first else dict(wadd=[tcstg])))
            first = False
        if last_R[0] != R:
            S.op("dve", lambda e: e.memset(Ppad[:], 0.0), writes=[tPp])
            last_R[0] = R
        P3 = Ppad[:, 1:1 + (R + 2) * 66].rearrange("p (r c) -> p r c", c=66)
        S.op("dve", lambda e: e.tensor_copy(out=P3[:, 1:R + 1, 1:65], in_=cstg[:, 0:n_el].rearrange("p (r c) -> p r c", c=64)),
             reads=[tcstg], writes=[tPp])
        dw, dwtok = dw_ring.next()
        for t in range(9):
            S.op("dve", lambda e, t=t: e.tensor_scalar(out=dw[:, t, :], in0=ident_b, scalar1=vecs[:, V_CONVW + j * 9 + t:V_CONVW + j * 9 + t + 1], scalar2=None, op0=ALU.mult),
                 reads=[t_cb, t_const], **(dict(writes=[dwtok]) if t == 0 else dict(wadd=[dwtok])))
        bcol = vecs[:, V_CONVB + j:V_CONVB + j + 1]
        firstc = True
        for q0 in range(0, npad, 512):
            N = min(512, npad - q0)
            pt, ptok = psA_ring.next()
            for t in range(9):
                i, jj = t // 3, t % 3
                o = q0 + i * 66 + jj
                S.op("pe", lambda e, pt=pt, t=t, o=o, N=N: e.matmul(pt[:, 0:N], dw[:, t, :], Ppad[:, o:o + N], start=(t == 0), stop=(t == 8)),
                     reads=[dwtok, tPp], writes=[ptok])
            S.op("dve", lambda e, pt=pt, q0=q0, N=N: e.tensor_scalar(out=accp[:, q0:q0 + N], in0=pt[:, 0:N], scalar1=bcol, scalar2=None, op0=ALU.add),
                 reads=[ptok, t_const], **(dict(writes=[taccp]) if firstc else dict(wadd=[taccp])))
            firstc = False
        S.op("act", lambda e: e.activation(out=ebp[:, 0:npad], in_=accp[:, 0:npad], func=AF.Silu), reads=[taccp], writes=[tebp])
        sc = 1.0 if j < 4 else 128.0 ** -0.5
        E3 = ebp[:, 0:npad].rearrange("p (r c) -> p r c", c=66)[:, :, 1:65]
        S.op("dve", lambda e: e.tensor_scalar(out=outb[:, 0:n_el].rearrange("p (r c) -> p r c", c=64), in0=E3, scalar1=sc, scalar2=None, op0=ALU.mult),
             reads=[tebp], writes=[toutb])
        for (ap, tok, off, n) in dsts:
            S.dma("sp", ap, outb[:, off:off + n], reads=[toutb], wadd=[tok])

    for j in range(8):
        isq = j < 4
        nb = 4 if isq else 8
        R = 33 if isq else 64
        srcs = [(mpre_d[1 + b][:, j * 512:(j + 1) * 512], t_mpre[1 + b], b * 512, 512) for b in range(nb)]
        if isq:
            srcs.append((mpre_d[5][:, j * 512:j * 512 + 64], t_mpre[5], 2048, 64))
        dsts = [(mqk_d[1 + b][:, j * 512:(j + 1) * 512], t_mqk[1 + b], b * 512, 512) for b in range(nb)]
        conv_tile_pe(j, R, srcs, dsts)
    for j in range(4, 8):
        conv_tile(j, 1, 256, [(mpre_d[0][:, j * 512:j * 512 + 256], t_mpre[0], 0, 256)],
                  [(mqk_d[0][:, j * 512:j * 512 + 256], t_mqk[0], 0, 256)], (1,))
    release(n_s3)
    if dbg == 4:
        S.finish(t_mqk + dump_toks)
        nc.used_inputs = used_inputs
        return nc

    mixT_d = dscr("mixT_s", [4, 128, 8 * 512], BF16)
    t_mix = [Tok() for _ in range(4)]

    def finalize(OT, t_OT, gate_d, t_gate, gain_col0, koff):
        sq_ring = Ring([sb("fsq%d" % i, [128, 512], BF16) for i in range(2)])
        ms_ring = Ring([sb("fms%d" % i, [128, 512], F32) for i in range(2)])
        y_ring = Ring([sb("fy%d" % i, [128, 512], F32) for i in range(2)])
        gate_ring = Ring([sb("fgate%d" % i, [128, 4 * 512], BF16) for i in range(2)])
        mst_ring = Ring([sb("fmst%d" % i, [128, 4 * 512], BF16) for i in range(2)])
        for lb in range(4):
            gt, gttok = gate_ring.next()
            S.dma("sp", gt[:], gate_d[lb], reads=[t_gate[lb]], writes=[gttok])
            mst, msttok = mst_ring.next()
            for h in range(4):
                O = OT[:, h, lb * 512:(lb + 1) * 512]
                sq, sqtok = sq_ring.next()
                S.op("dve", lambda e, sq=sq, O=O: e.tensor_tensor(out=sq[:], in0=O, in1=O, op=ALU.mult), reads=[t_OT], writes=[sqtok])
                pt, ptok = psA_ring.next()
                S.op("pe", lambda e, pt=pt, sq=sq: e.matmul(pt[:, :], ones_b, sq[:], start=True, stop=True), reads=[sqtok, t_cb], writes=[ptok])
                ms, mstok = ms_ring.next()
                S.op("dve", lambda e, ms=ms, pt=pt: e.tensor_scalar(out=ms[:], in0=pt[:, :], scalar1=1.0 / 128, scalar2=EPS, op0=ALU.mult, op1=ALU.add),
                     reads=[ptok], writes=[mstok])
                S.op("act", lambda e, ms=ms: e.activation(out=ms[:], in_=ms[:], func=AF.Ln), reads=[mstok], writes=[mstok])
                S.op("act", lambda e, ms=ms: e.activation(out=ms[:], in_=ms[:], func=AF.Exp, scale=-0.5), reads=[mstok], writes=[mstok])
                y, ytok = y_ring.next()
                S.op("dve", lambda e, y=y, O=O, ms=ms, h=h: e.scalar_tensor_tensor(
                    out=y[:], in0=O, scalar=vecs[:, gain_col0 + h:gain_col0 + h + 1], in1=ms[:], op0=ALU.mult, op1=ALU.mult),
                    reads=[t_OT, mstok, t_const], writes=[ytok])
                S.op("dve", lambda e, y=y, gt=gt, mst=mst, h=h: e.tensor_tensor(
                    out=mst[:, h * 512:(h + 1) * 512], in0=y[:], in1=gt[:, h * 512:(h + 1) * 512], op=ALU.mult),
                    reads=[ytok, gttok], **(dict(writes=[msttok]) if h == 0 else dict(wadd=[msttok])))
            S.dma("sp", mixT_d[lb][:, koff * 512:(koff + 4) * 512], mst[:], reads=[msttok], wadd=[t_mix[lb]])

    n_s4 = len(es)
    OT = sb("OT", [128, 4, NLOC], F32)
    t_OT = Tok()
    upw_sb = sb("upw", [128, 2, 256], F32)
    t_upw = Tok()
    S.op("dve", lambda e: e.memset(upw_sb[:], 0.0), writes=[t_upw])
    S.dma("sp", upw_sb[0:16], upw_d.rearrange("d r n -> r d n"), writes=[t_upw])
    bmask = consts[:, 512:768]
    m2x = sb("m2x", [128, 2, 256], BF16)
    t_m2x = Tok()
    for d_ in range(2):
        for hh in range(2):
            S.op("dve", lambda e, d_=d_, hh=hh: e.tensor_copy(out=m2x[:, d_, hh * 128:(hh + 1) * 128], in_=mask_b[d_]),
                 reads=[t_cb], wadd=[t_m2x])
    Tst = [[sb("T%d%d" % (p, d_), [128, 256], F32) for d_ in range(2)] for p in range(2)]
    Sbt = [[sb("Sb%d%d" % (p, d_), [128, 256], BF16) for d_ in range(2)] for p in range(2)]
    ecol = [[sb("ec%d%d" % (p, d_), [128, 1], F32) for d_ in range(2)] for p in range(2)]
    t_T = [[Tok() for _ in range(2)] for _ in range(2)]
    t_Sb = [[Tok() for _ in range(2)] for _ in range(2)]
    t_ec = [[Tok() for _ in range(2)] for _ in range(2)]
    for p in range(2):
        for d_ in range(2):
            S.op("dve", lambda e, p=p, d_=d_: e.memset(Tst[p][d_][:], 0.0), writes=[t_T[p][d_]])
            S.op("dve", lambda e, p=p, d_=d_: e.memset(Sbt[p][d_][:], 0.0), writes=[t_Sb[p][d_]])
            S.op("dve", lambda e, p=p, d_=d_: e.memset(ecol[p][d_][:], 1.0), writes=[t_ec[p][d_]])
    gq_ring = Ring([sb("gqkb%d" % i, [128, 4 * 512], BF16) for i in range(2)])
    lr_ring = Ring([sb("lrb%d" % i, [128, 2 * 512], F32) for i in range(2)])
    for i_ in range(2):
        S.op("dve", lambda e, i_=i_: e.memset(lr_ring.items[i_][:], 0.0), writes=[lr_ring.toks[i_]])
    gv_ring = Ring([sb("gvb%d" % i, [128, 4 * 512], BF16) for i in range(2)])
    L_ring = Ring([sb("gL%d" % i, [128, 512], F32) for i in range(2)])
    C_ring = Ring([sb("gC%d" % i, [128, 512], F32) for i in range(2)])
    C2_ring = Ring([sb("gC2%d" % i, [128, 512], F32) for i in range(2)])
    eb_ring = Ring([sb("geb%d" % i, [128, 512], F32) for i in range(4)])
    enb_ring = Ring([sb("genb%d" % i, [128, 512], F32) for i in range(2)])
    qt_ring = Ring([sb("gqt%d" % i, [128, 512], BF16) for i in range(4)])
    kt_ring = Ring([sb("gkt%d" % i, [128, 512], BF16) for i in range(4)])
    kth_ring = Ring([sb("gkth%d" % i, [128, 512], BF16) for i in range(8)])
    ktok_ring = Ring([sb("gktok%d" % i, [128, 128], BF16) for i in range(4)])
    attm_ring = Ring([sb("gattm%d" % i, [128, 256], BF16) for i in range(4)])
    o_written = [False] * NBLK

    def gla_block(blk, d, full):
        N = 256 if blk == 0 else 512
        nch = N // 128
        gq, gqtok = gq_ring.next()
        S.dma("sp", gq[:], gqk_d[blk], reads=[t_gqk[blk]], writes=[gqtok])
        lrb, lrtok = lr_ring.next()
        S.dma("sp", lrb[0:16], lr_d[blk], reads=[t_lr[blk]], writes=[lrtok])
        gvb, gvtok = gv_ring.next()
        S.dma("sp", gvb[:], gv_d[blk], reads=[t_gv[blk]], writes=[gvtok])
        prep = []
        for p in range(2):
            pt, ptok = psA_ring.next()
            S.op("pe", lambda e, pt=pt, p=p: e.matmul(pt[:, 0:N], upw_sb[:, d, p * 128:(p + 1) * 128], lrb[:, d * 512:d * 512 + N], start=True, stop=True),
                 reads=[t_upw, lrtok], writes=[ptok])
            L, Ltok = L_ring.next()
            S.op("dve", lambda e, pt=pt, L=L, p=p: e.tensor_scalar(out=L[:, 0:N], in0=pt[:, 0:N], scalar1=vecs[:, V_UPB + d * 2 + p:V_UPB + d * 2 + p + 1], scalar2=None, op0=ALU.add),
                 reads=[ptok, t_const], writes=[Ltok])
            S.op("act", lambda e, L=L: e.activation(out=L[:, 0:N], in_=L[:, 0:N], func=AF.Exp, scale=-1.0), reads=[Ltok], writes=[Ltok])
            S.op("dve", lambda e, L=L: e.tensor_scalar(out=L[:, 0:N], in0=L[:, 0:N], scalar1=1.0, scalar2=None, op0=ALU.add), reads=[Ltok], writes=[Ltok])
            S.op("act", lambda e, L=L: e.activation(out=L[:, 0:N], in_=L[:, 0:N], func=AF.Ln), reads=[Ltok], writes=[Ltok])
            Cm, Ctok = C_ring.next()
            for c in range(nch):
                S.op("dve", lambda e, Cm=Cm, L=L, c=c: e.tensor_tensor_scan(
                    out=Cm[:, c * 128:(c + 1) * 128], data0=ones_f[:, 0:128], data1=L[:, c * 128:(c + 1) * 128], initial=0.0, op0=ALU.mult, op1=ALU.add),
                    reads=[Ltok, t_const], **(dict(writes=[Ctok]) if c == 0 else dict(wadd=[Ctok])))
            if d == 1:
                C2, C2tok = C2_ring.next()
                for c in range(nch):
                    S.op("dve", lambda e, Cm=Cm, C2=C2, c=c: e.tensor_scalar(
                        out=C2[:, c * 128:(c + 1) * 128], in0=Cm[:, c * 128:(c + 1) * 128], scalar1=-1.0, scalar2=Cm[:, c * 128 + 127:c * 128 + 128], op0=ALU.mult, op1=ALU.add),
                        reads=[Ctok], **(dict(writes=[C2tok]) if c == 0 else dict(wadd=[C2tok])))
                S.op("dve", lambda e, C2=C2, L=L: e.tensor_tensor(out=C2[:, 0:N], in0=C2[:, 0:N], in1=L[:, 0:N], op=ALU.add), reads=[C2tok, Ltok], writes=[C2tok])
                Cm, Ctok = C2, C2tok
            eb_, ebtok = eb_ring.next()
            S.op("act", lambda e, eb_=eb_, Cm=Cm: e.activation(out=eb_[:, 0:N], in_=Cm[:, 0:N], func=AF.Exp, scale=-1.0 / 16), reads=[Ctok], writes=[ebtok])
            enb, enbtok = enb_ring.next()
            S.op("act", lambda e, enb=enb, Cm=Cm: e.activation(out=enb[:, 0:N], in_=Cm[:, 0:N], func=AF.Exp, scale=1.0 / 16), reads=[Ctok], writes=[enbtok])
            kt, kttok = kt_ring.next()
            S.op("dve", lambda e, kt=kt, enb=enb, p=p: e.tensor_tensor(out=kt[:, 0:N], in0=gq[:, (2 + p) * 512:(2 + p) * 512 + N], in1=enb[:, 0:N], op=ALU.mult),
                 reads=[gqtok, enbtok], writes=[kttok])
            qt, qttok = None, None
            kth = [None, None]
            if full:
                for h in range(2):
                    kh, khtok = kth_ring.next()
                    S.op("dve", lambda e, kh=kh, enb=enb, p=p, h=h: e.scalar_tensor_tensor(
                        out=kh[:, 0:N], in0=gq[:, (2 + p) * 512:(2 + p) * 512 + N], scalar=consts[:, 768 + h:769 + h], in1=enb[:, 0:N], op0=ALU.mult, op1=ALU.mult),
                        reads=[gqtok, enbtok, t_const], writes=[khtok])
                    kth[h] = (kh, khtok)
                qt, qttok = qt_ring.next()
                S.op("dve", lambda e, qt=qt, eb_=eb_, p=p: e.tensor_tensor(out=qt[:, 0:N], in0=gq[:, p * 512:p * 512 + N], in1=eb_[:, 0:N], op=ALU.mult),
                     reads=[gqtok, ebtok], writes=[qttok])
            prep.append((eb_, ebtok, kt, kttok, qt, qttok, kth))
        order = list(range(nch)) if d == 0 else list(range(nch - 1, -1, -1))
        for c in order:
            cs = slice(c * 128, (c + 1) * 128)
            ecc = c * 128 + (127 if d == 0 else 0)
            st = [dict() for _ in range(2)]
            for p in range(2):
                eb_, ebtok, kt, kttok, qt, qttok, kth = prep[p]
                pb, pbtok = psB_ring.next()
                S.op("pe", lambda e, pb=pb, kt=kt: e.transpose(pb[:, 0:128], kt[:, cs], ident_b), reads=[kttok, t_cb], writes=[pbtok])
                st[p]["pb"] = (pb, pbtok)
                if full:
                    pa, patok = psA_ring.next()
                    for h in range(2):
                        S.op("pe", lambda e, pa=pa, h=h, kth=kth, qt=qt: e.matmul(pa[:, h * 128:(h + 1) * 128], kth[h][0][:, cs], qt[:, cs], start=True, stop=True),
                             reads=[kth[h][1], qttok], writes=[patok])
                    st[p]["pa"] = (pa, patok)
            for p in range(2):
                pb, pbtok = st[p]["pb"]
                ktk, ktktok = ktok_ring.next()
                S.op("dve", lambda e, ktk=ktk, pb=pb: e.tensor_copy(out=ktk[:], in_=pb[:, 0:128]), reads=[pbtok], writes=[ktktok])
                st[p]["ktk"] = (ktk, ktktok)
                if full:
                    pa, patok = st[p]["pa"]
                    am, amtok = attm_ring.next()
                    S.op("dve", lambda e, am=am, pa=pa: e.tensor_tensor(out=am[:], in0=pa[:, 0:256], in1=m2x[:, d, :], op=ALU.mult),
                         reads=[patok, t_m2x], writes=[amtok])
                    st[p]["am"] = (am, amtok)
            for p in range(2):
                ktk, ktktok = st[p]["ktk"]
                pd, pdtok = psA_ring.next()
                S.op("pe", lambda e, pd=pd, ktk=ktk, p=p: e.matmul(pd[:, 0:256], ktk[:], gvb[:, c * 512 + p * 256:c * 512 + (p + 1) * 256], start=True, stop=True),
                     reads=[ktktok, gvtok], writes=[pdtok])
                st[p]["pd"] = (pd, pdtok)
            if full:
                for p in range(2):
                    eb_, ebtok, kt, kttok, qt, qttok, kth = prep[p]
                    am, amtok = st[p]["am"]
                    po, potok = psA_ring.next()
                    for h in range(2):
                        hd = p * 2 + h
                        S.op("pe", lambda e, po=po, h=h, hd=hd, am=am: e.matmul(
                            po[:, h * 128:(h + 1) * 128], gvb[:, c * 512 + hd * 128:c * 512 + (hd + 1) * 128], am[:, h * 128:(h + 1) * 128], start=True, stop=False),
                            reads=[gvtok, amtok], writes=[potok])
                        S.op("pe", lambda e, po=po, h=h, qt=qt, p=p: e.matmul(
                            po[:, h * 128:(h + 1) * 128], Sbt[p][d][:, h * 128:(h + 1) * 128], qt[:, cs], start=False, stop=True),
                            reads=[t_Sb[p][d], qttok], writes=[potok])
                    st[p]["po"] = (po, potok)
            for p in range(2):
                eb_, ebtok = prep[p][0], prep[p][1]
                pd, pdtok = st[p]["pd"]
                S.op("dve", lambda e, pd=pd, p=p: e.scalar_tensor_tensor(
                    out=Tst[p][d][:], in0=Tst[p][d][:], scalar=ecol[p][d][:, 0:1], in1=pd[:, 0:256], op0=ALU.mult, op1=ALU.add),
                    reads=[pdtok, t_ec[p][d], t_T[p][d]], writes=[t_T[p][d]])
                S.op("dve", lambda e, p=p, eb_=eb_: e.scalar_tensor_tensor(
                    out=Sbt[p][d][:], in0=Tst[p][d][:], scalar=eb_[:, ecc:ecc + 1], in1=bmask, op0=ALU.mult, op1=ALU.mult),
                    reads=[t_T[p][d], ebtok, t_const], writes=[t_Sb[p][d]])
                S.op("dve", lambda e, p=p, eb_=eb_: e.tensor_copy(out=ecol[p][d][:], in_=eb_[:, ecc:ecc + 1]),
                     reads=[ebtok], writes=[t_ec[p][d]])
            if full:
                for p in range(2):
                    po, potok = st[p]["po"]
                    tok0 = (blk - 1) * 512 + c * 128
                    Odst = OT[:, p * 2:p * 2 + 2, tok0:tok0 + 128]
                    po3 = po[:, 0:256].rearrange("p (h t) -> p h t", h=2)
                    if not o_written[blk]:
                        S.op("dve", lambda e, Odst=Odst, po3=po3: e.tensor_copy(out=Odst, in_=po3), reads=[potok], wadd=[t_OT])
                    else:
                        S.op("dve", lambda e, Odst=Odst, po3=po3: e.tensor_tensor(out=Odst, in0=po3, in1=Odst, op=ALU.add), reads=[potok, t_OT], wadd=[t_OT])
        if full:
            o_written[blk] = True

    s4m = int(os.environ.get("S4MODE", 9))
    if s4m == 10:
        gla_block(8, 1, False)
    elif s4m == 11:
        gla_block(0, 0, False)
    else:
        gla_block(0, 1, False)
    if 10 > s4m >= 1:
        for blk in (8, 7, 6, 5):
            gla_block(blk, 1, False)
        gla_block(0, 0, False)
    if 10 > s4m >= 2:
        for i in range(4 if s4m >= 3 else 1):
            gla_block(1 + i, 0, True)
            gla_block(4 - i, 1, True)
    if 10 > s4m >= 4:
        finalize(OT, t_OT, sgg_d, t_sgg, V_GNG, 0)
    dump("OTg", OT[:, 0, :], [128, NLOC], [t_OT])
    release(n_s4)
    if dbg == 5:
        S.finish(t_mix + dump_toks)
        nc.used_inputs = used_inputs
        return nc

    n_s5 = len(es)
    HT = sb("HT", [128, 4, NLOC], F32)
    t_HT = Tok()
    gateb4 = sb("gateb4", [128, 64], F32)
    t_gb4 = Tok()
    for c in range(4):
        S.dma("sp", gateb4[:, c * 16:(c + 1) * 16], rows_d[0:1, R_GATEB:R_GATEB + 16].partition_broadcast(128), wadd=[t_gb4])
    maskf = [consts[:, 128:256], consts[:, 256:384]]
    T4 = [sb("T4_%d" % d_, [128, 4, 256], F32) for d_ in range(2)]
    Sb4 = [sb("Sb4_%d" % d_, [128, 4, 256], BF16) for d_ in range(2)]
    t_T4 = [Tok() for _ in range(2)]
    t_Sb4 = [Tok() for _ in range(2)]
    ec_one = sb("econe", [128, 4], F32)
    t_econe = Tok()
    S.op("dve", lambda e: e.memset(ec_one[:], 1.0), writes=[t_econe])
    prev_ec = [(ec_one, t_econe), (ec_one, t_econe)]
    for d_ in range(2):
        S.op("dve", lambda e, d_=d_: e.memset(T4[d_][:], 0.0), writes=[t_T4[d_]])
        S.op("dve", lambda e, d_=d_: e.memset(Sb4[d_][:], 0.0), writes=[t_Sb4[d_]])
    mq_ring = Ring([sb("mqkb%d" % i, [128, 8 * 512], BF16) for i in range(2)])
    mvb_ring = Ring([sb("mvb%d" % i, [128, 4 * 512], BF16) for i in range(2)])
    ga_ring = Ring([sb("gab%d" % i, [128, 64], F32) for i in range(2)])
    gbb_ring = Ring([sb("gbb%d" % i, [128, 64], F32) for i in range(2)])
    Lf_ring = Ring([sb("Lf%d" % i, [128, 16], F32) for i in range(2)])
    es_ring = Ring([sb("es%d" % i, [128, 16], F32) for i in range(2)])
    lfbc_ring = Ring([sb("lfbc%d" % i, [128, 4, 128], F32) for i in range(2)])
    flo_ring = Ring([sb("flo%d" % i, [128, 4, 128], F32) for i in range(2)])
    ecn_ring = Ring([sb("ecn%d" % i, [128, 4], F32) for i in range(12)])
    vext_ring = Ring([sb("vext%d" % i, [128, 4, 256], BF16) for i in range(2)])
    mktok_ring = Ring([sb("mktok%d" % i, [128, 512], BF16) for i in range(2)])
    mam_ring = Ring([sb("mam%d" % i, [128, 512], BF16) for i in range(2)])
    mask4 = sb("mask4", [128, 2, 512], BF16)
    t_mask4 = Tok()
    for d_ in range(2):
        for hh in range(4):
            S.op("dve", lambda e, d_=d_, hh=hh: e.tensor_copy(out=mask4[:, d_, hh * 128:(hh + 1) * 128], in_=mask_b[d_]), reads=[t_cb], wadd=[t_mask4])
    tP = [[t_] * 4 for t_ in [Tok() for _ in range(6)]]
    dd_ring = Ring([sb("mdd%d" % i, [128, 4, 128], F32) for i in range(2)])
    ht_ring = Ring([sb("mht%d" % i, [128, 4, 128], F32) for i in range(2)])
    h_written = [False] * NBLK

    def ml_block(blk, d, full):
        N = 256 if blk == 0 else 512
        nch = N // 128
        mq, mqtok = mq_ring.next()
        S.dma("sp", mq[:], mqk_d[blk], reads=[t_mqk[blk]], writes=[mqtok])
        mvb, mvtok = mvb_ring.next()
        S.dma("sp", mvb[:], mv_d[blk], reads=[t_mv[blk]], writes=[mvtok])
        ga, gatok = ga_ring.next()
        S.dma("sp", ga[:], gates_d[blk], reads=[t_gates[blk]], writes=[gatok])
        gbb, gbtok = gbb_ring.next()
        S.op("dve", lambda e: e.tensor_tensor(out=gbb[:], in0=ga[:], in1=gateb4[:], op=ALU.add), reads=[gatok, t_gb4], writes=[gbtok])
        gb3 = gbb[:].rearrange("p (c g) -> p c g", g=16)
        Lf, Lftok = Lf_ring.next()
        Lf3 = Lf[:].rearrange("p (c h) -> p c h", h=4)
        S.op("act", lambda e: e.activation(out=Lf3[:, 0:nch, :], in_=gb3[:, 0:nch, 8 + d * 4:12 + d * 4], func=AF.Exp, scale=-1.0), reads=[gbtok], writes=[Lftok])
        S.op("dve", lambda e: e.tensor_scalar(out=Lf[:, 0:nch * 4], in0=Lf[:, 0:nch * 4], scalar1=1.0, scalar2=None, op0=ALU.add), reads=[Lftok], writes=[Lftok])
        S.op("act", lambda e: e.activation(out=Lf[:, 0:nch * 4], in_=Lf[:, 0:nch * 4], func=AF.Ln), reads=[Lftok], writes=[Lftok])
        pt, ptok = psA[0], tP[0][0]
        S.op("pe", lambda e: e.matmul(pt[:, 0:nch * 4], maskf[d], Lf[:, 0:nch * 4], start=True, stop=True), reads=[Lftok, t_const], writes=[ptok])
        es_, estok = es_ring.next()
        es3 = es_[:].rearrange("p (c h) -> p c h", h=4)
        pt3 = pt[:, 0:16].rearrange("p (c h) -> p c h", h=4)
        S.op("dve", lambda e: e.tensor_tensor(out=es3[:, 0:nch, :], in0=pt3[:, 0:nch, :], in1=gb3[:, 0:nch, d * 4:d * 4 + 4], op=ALU.add),
             reads=[ptok, gbtok], writes=[estok])
        S.op("act", lambda e: e.activation(out=es_[:, 0:nch * 4], in_=es_[:, 0:nch * 4], func=AF.Exp), reads=[estok], writes=[estok])
        order = list(range(nch)) if d == 0 else list(range(nch - 1, -1, -1))
        endcol = 127 if d == 0 else 0
        for c in order:
            lf4, lf4tok = lfbc_ring.next()
            S.op("dve", lambda e, lf4=lf4: e.tensor_copy(out=lf4[:], in_=Lf[:, c * 4:c * 4 + 4].unsqueeze(2).to_broadcast([128, 4, 128])),
                 reads=[Lftok], writes=[lf4tok])
            vx4, vx4tok = vext_ring.next()
            es_bc = es_[:, c * 4:c * 4 + 4].unsqueeze(2).to_broadcast([128, 4, 128])
            S.op("dve", lambda e, vx4=vx4, es_bc=es_bc: e.tensor_tensor(
                out=vx4[:, :, 0:128], in0=mvb[:, c * 512:(c + 1) * 512].rearrange("p (h n) -> p h n", h=4), in1=es_bc, op=ALU.mult),
                reads=[mvtok, estok], writes=[vx4tok])
            S.op("dve", lambda e, vx4=vx4, es_bc=es_bc: e.tensor_copy(out=vx4[:, :, 128:256], in_=es_bc), reads=[estok], wadd=[vx4tok])
            kTs = [mq[:, (4 + h) * 512 + c * 128:(4 + h) * 512 + (c + 1) * 128] for h in range(4)]
            qTs = [mq[:, h * 512 + c * 128:h * 512 + (c + 1) * 128] for h in range(4)]
            pb, pbtok = psB_ring.next()
            for h in range(4):
                S.op("pe", lambda e, h=h, lf4=lf4: e.matmul(psA[0][:, h * 128:(h + 1) * 128], lf4[:, h, :], maskf[d], start=True, stop=True),
                     reads=[lf4tok, t_const], writes=[tP[0][0]])
                S.op("pe", lambda e, h=h, pb=pb: e.transpose(pb[:, h * 128:(h + 1) * 128], kTs[h], ident_b), reads=[mqtok, t_cb], writes=[pbtok])
            ecn4, ecn4tok = ecn_ring.next()
            S.op("act", lambda e, ecn4=ecn4: e.activation(
                out=ecn4[:, 0:4].unsqueeze(2), in_=psA[0][:, :].rearrange("p (h n) -> p h n", h=4)[:, :, endcol:endcol + 1], func=AF.Exp, scale=-1.0),
                reads=[tP[0][0]], writes=[ecn4tok])
            flo4, flo4tok = None, None
            if full:
                flo4, flo4tok = flo_ring.next()
                S.op("act", lambda e, flo4=flo4: e.activation(out=flo4[:].rearrange("p h n -> p (h n)"), in_=psA[0][:, :], func=AF.Exp), reads=[tP[0][0]], writes=[flo4tok])
            ktk4, ktk4tok = mktok_ring.next()
            S.op("dve", lambda e, ktk4=ktk4, pb=pb: e.tensor_copy(out=ktk4[:], in_=pb[:, 0:512]), reads=[pbtok], writes=[ktk4tok])
            if full:
                for h in range(4):
                    S.op("pe", lambda e, h=h: e.matmul(psA[1][:, h * 128:(h + 1) * 128], kTs[h], qTs[h], start=True, stop=True), reads=[mqtok], writes=[tP[1][0]])
                am4, am4tok = mam_ring.next()
                S.op("dve", lambda e, am4=am4: e.tensor_tensor(out=am4[:], in0=psA[1][:, :], in1=mask4[:, d, :], op=ALU.mult),
                     reads=[tP[1][0], t_mask4], writes=[am4tok])
                for h in range(4):
                    bk, o0 = 2 + h // 2, (h % 2) * 256
                    for half in range(2):
                        hc = slice(half * 128, (half + 1) * 128)
                        oc = slice(o0 + half * 128, o0 + (half + 1) * 128)
                        S.op("pe", lambda e, h=h, bk=bk, oc=oc, hc=hc, am4=am4, vx4=vx4: e.matmul(psA[bk][:, oc], vx4[:, h, hc], am4[:, h * 128:(h + 1) * 128], start=True, stop=False),
                             reads=[vx4tok, am4tok], writes=[tP[bk][0]])
                        S.op("pe", lambda e, h=h, bk=bk, oc=oc, hc=hc: e.matmul(psA[bk][:, oc], Sb4[d][:, h, hc], qTs[h], start=False, stop=True),
                             reads=[t_Sb4[d], mqtok], writes=[tP[bk][0]])
            for h in range(4):
                bk, o0 = 4 + h // 2, (h % 2) * 256
                S.op("pe", lambda e, h=h, bk=bk, o0=o0, ktk4=ktk4, vx4=vx4: e.matmul(psA[bk][:, o0:o0 + 256], ktk4[:, h * 128:(h + 1) * 128], vx4[:, h, :], start=True, stop=True),
                     reads=[ktk4tok, vx4tok], writes=[tP[bk][0]])
            pec, pectok = prev_ec[d]
            for h in range(4):
                bk, o0 = 4 + h // 2, (h % 2) * 256
                S.op("dve", lambda e, h=h, bk=bk, o0=o0, pec=pec: e.scalar_tensor_tensor(
                    out=T4[d][:, h, :], in0=T4[d][:, h, :], scalar=pec[:, h:h + 1], in1=psA[bk][:, o0:o0 + 256], op0=ALU.mult, op1=ALU.add),
                    reads=[tP[bk][0], pectok, t_T4[d]], writes=[t_T4[d]])
            S.op("dve", lambda e, ecn4=ecn4: e.tensor_tensor(out=Sb4[d][:], in0=T4[d][:], in1=ecn4[:, 0:4].unsqueeze(2).to_broadcast([128, 4, 256]), op=ALU.mult),
                 reads=[t_T4[d], ecn4tok], writes=[t_Sb4[d]])
            prev_ec[d] = (ecn4, ecn4tok)
            if full:
                dd4, dd4tok = dd_ring.next()
                for bk in (2, 3):
                    hs2 = slice((bk - 2) * 2, (bk - 2) * 2 + 2)
                    den = psA[bk][:, :].rearrange("p (h x n) -> p h x n", h=2, x=2)[:, :, 1, :]
                    S.op("dve", lambda e, dd4=dd4, den=den, hs2=hs2, flo4=flo4: e.scalar_tensor_tensor(
                        out=dd4[:, hs2, :], in0=den, scalar=-1.0, in1=flo4[:, hs2, :], op0=ALU.mult, op1=ALU.max),
                        reads=[tP[bk][0], flo4tok], **(dict(writes=[dd4tok]) if bk == 2 else dict(wadd=[dd4tok])))
                for bk in (2, 3):
                    hs2 = slice((bk - 2) * 2, (bk - 2) * 2 + 2)
                    den = psA[bk][:, :].rearrange("p (h x n) -> p h x n", h=2, x=2)[:, :, 1, :]
                    S.op("dve", lambda e, dd4=dd4, den=den, hs2=hs2: e.tensor_tensor(out=dd4[:, hs2, :], in0=den, in1=dd4[:, hs2, :], op=ALU.max),
                         reads=[tP[bk][0], dd4tok], wadd=[dd4tok])
                S.op("dve", lambda e, dd4=dd4: e.reciprocal(out=dd4[:], in_=dd4[:]), reads=[dd4tok], writes=[dd4tok])
                tok0 = (blk - 1) * 512 + c * 128
                if not h_written[blk]:
                    for bk in (2, 3):
                        hs2 = slice((bk - 2) * 2, (bk - 2) * 2 + 2)
                        num = psA[bk][:, :].rearrange("p (h x n) -> p h x n", h=2, x=2)[:, :, 0, :]
                        S.op("dve", lambda e, num=num, hs2=hs2, dd4=dd4: e.tensor_tensor(out=HT[:, hs2, tok0:tok0 + 128], in0=num, in1=dd4[:, hs2, :], op=ALU.mult),
                             reads=[tP[bk][0], dd4tok], wadd=[t_HT])
                else:
                    ht4, ht4tok = ht_ring.next()
                    for bk in (2, 3):
                        hs2 = slice((bk - 2) * 2, (bk - 2) * 2 + 2)
                        num = psA[bk][:, :].rearrange("p (h x n) -> p h x n", h=2, x=2)[:, :, 0, :]
                        S.op("dve", lambda e, num=num, hs2=hs2, dd4=dd4, ht4=ht4: e.tensor_tensor(out=ht4[:, hs2, :], in0=num, in1=dd4[:, hs2, :], op=ALU.mult),
                             reads=[tP[bk][0], dd4tok], **(dict(writes=[ht4tok]) if bk == 2 else dict(wadd=[ht4tok])))
                    S.op("dve", lambda e, ht4=ht4: e.tensor_tensor(out=HT[:, :, tok0:tok0 + 128], in0=HT[:, :, tok0:tok0 + 128], in1=ht4[:], op=ALU.add),
                         reads=[ht4tok, t_HT], wadd=[t_HT])
        if full:
            h_written[blk] = True

    s5m = int(os.environ.get("S5MODE", 9))
    ml_block(0, 1, False)
    if s5m >= 1:
        for blk in (8, 7, 6, 5):
            ml_block(blk, 1, False)
        ml_block(0, 0, False)
    if s5m >= 2:
        for i in range(4 if s5m >= 3 else 1):
            ml_block(1 + i, 0, True)
            ml_block(4 - i, 1, True)
    S.barrier()
    if s5m >= 4:
        finalize(HT, t_HT, smo_d, t_smo, V_MNG, 4)
    dump("HTm", HT[:, 0, :], [128, NLOC], [t_HT])
    release(n_s5)
    if dbg == 6:
        S.finish(t_mix + dump_toks)
        nc.used_inputs = used_inputs
        return nc

    n_s6 = len(es)
    x1_d = dscr("x1_s", [16, 128, D], F32)
    t_x1 = Tok()
    wout_v = wout_d.rearrange("(k p) n -> p k n", p=128)
    woutb = sb("woutb", [128, 8, D], BF16)
    t_wout = Tok()
    wo_stg = Ring([sb("wostg%d" % i, [128, 8, 256], F32) for i in range(2)])
    for pc in range(4):
        st, sttok = wo_stg.next()
        S.dma("sp", st[:], wout_v[:, :, pc * 256:(pc + 1) * 256], writes=[sttok])
        S.op("dve", lambda e, st=st, pc=pc: e.tensor_copy(out=woutb[:, :, pc * 256:(pc + 1) * 256], in_=st[:]), reads=[sttok], wadd=[t_wout])
    rw_sb = sb("rw", [128, 8, 36], F32)
    t_rw = Tok()
    S.dma("sp", rw_sb[:], rw_d.rearrange("(k p) n -> p k n", p=128), writes=[t_rw])
    rb_bc = sb("rbbc", [128, 36], F32)
    S.dma("sp", rb_bc[:], rows_d[0:1, R_RB:R_RB + 36].partition_broadcast(128), wadd=[t_rw])
    mixb_ring = Ring([sb("mixb%d" % i, [128, 8 * 512], BF16) for i in range(2)])
    xt6_ring = Ring([sb("x6t%d" % i, [128, D], F32) for i in range(2)])
    x1_ring = Ring([sb("x1t%d" % i, [128, D], F32) for i in range(2)])
    tmp6 = sb("tmp6", [128, D], F32)
    t_tmp6 = Tok()
    jk6 = sb("jk6", [128, D], F32)
    t_jk6 = Tok()
    s6_ring = Ring([(sb("s6a%d" % i, [128, 1], F32), sb("s6b%d" % i, [128, 1], F32)) for i in range(2)])
    h2s_ring = Ring([sb("h2s%d" % i, [128, D], F32) for i in range(2)])
    h2Tf_ring = Ring([sb("h2Tf%d" % i, [128, 8, 128], F32) for i in range(2)])
    Sel = sb("Sel", [128, 16, 2, 32], F32)
    t_Sel = Tok()
    h2tok = sb("h2tok", [128, 16, D], BF16)
    t_h2tok = Tok()
    LG = sb("LGall", [128, 16, 36], F32)
    t_LG = Tok()
    mixb, mixbtok = None, None
    for ti in range(16):
        lb, tt = ti // 4, ti % 4
        if tt == 0:
            mixb, mixbtok = mixb_ring.next()
            S.dma("sp", mixb[:], mixT_d[lb], reads=[t_mix[lb]], writes=[mixbtok])
        xt, xttok = xt6_ring.next()
        S.dma("sp", xt[:], x_d[ti * 128:(ti + 1) * 128, :], writes=[xttok])
        x1, x1tok = x1_ring.next()
        for half in range(2):
            po, potok = psA_ring.next()
            for k in range(8):
                S.op("pe", lambda e, po=po, k=k, half=half, mixb=mixb, tt=tt: e.matmul(
                    po[:, :], mixb[:, k * 512 + tt * 128:k * 512 + (tt + 1) * 128], woutb[:, k, half * 512:(half + 1) * 512], start=(k == 0), stop=(k == 7)),
                    reads=[mixbtok, t_wout], writes=[potok])
            hs_ = slice(half * 512, (half + 1) * 512)
            S.op("dve", lambda e, po=po, hs_=hs_: e.tensor_tensor(out=tmp6[:, hs_], in0=po[:, :], in1=g12[:, hs_], op=ALU.mult),
                 reads=[potok, t_g12], **(dict(writes=[t_tmp6]) if half == 0 else dict(wadd=[t_tmp6])))
            S.op("dve", lambda e, x1=x1, xt=xt, hs_=hs_: e.tensor_tensor(out=x1[:, hs_], in0=tmp6[:, hs_], in1=xt[:, hs_], op=ALU.add),
                 reads=[t_tmp6, xttok], **(dict(writes=[x1tok]) if half == 0 else dict(wadd=[x1tok])))
        S.dma("sp", x1_d[ti], x1[:], reads=[x1tok], wadd=[t_x1])
        ss, sstok = s6_ring.next()
        S.op("act", lambda e, x1=x1, ss=ss: e.activation(out=jk6[:], in_=x1[:], func=AF.Square, accum_out=ss[0][:, 0:1]), reads=[x1tok], writes=[t_jk6, sstok])
        S.op("dve", lambda e, ss=ss: e.tensor_scalar(out=ss[1][:, 0:1], in0=ss[0][:, 0:1], scalar1=1.0 / D, scalar2=EPS, op0=ALU.mult, op1=ALU.add), reads=[sstok], writes=[sstok])
        S.op("act", lambda e, ss=ss: e.activation(out=ss[0][:, 0:1], in_=ss[1][:, 0:1], func=AF.Ln), reads=[sstok], writes=[sstok])
        S.op("act", lambda e, ss=ss: e.activation(out=ss[1][:, 0:1], in_=ss[0][:, 0:1], func=AF.Exp, scale=-0.5), reads=[sstok], writes=[sstok])
        h2s, h2stok = h2s_ring.next()
        S.op("dve", lambda e, h2s=h2s, x1=x1, ss=ss: e.tensor_scalar(out=h2s[:], in0=x1[:], scalar1=ss[1][:, 0:1], scalar2=None, op0=ALU.mult),
             reads=[x1tok, sstok], writes=[h2stok])
        h2Tf, h2Tftok = h2Tf_ring.next()
        for g in range(2):
            pT, pTtok = psA_ring.next()
            for kk in range(4):
                k = g * 4 + kk
                S.op("pe", lambda e, pT=pT, kk=kk, k=k, h2s=h2s: e.transpose(pT[:, kk * 128:(kk + 1) * 128], h2s[:, k * 128:(k + 1) * 128], ident_f),
                     reads=[h2stok, t_const], writes=[pTtok])
            for kk in range(4):
                k = g * 4 + kk
                S.op("dve", lambda e, pT=pT, kk=kk, k=k, h2Tf=h2Tf: e.tensor_scalar(
                    out=h2Tf[:, k, :], in0=pT[:, kk * 128:(kk + 1) * 128], scalar1=A2[:, k:k + 1], scalar2=A2[:, 8 + k:9 + k], op0=ALU.mult, op1=ALU.add),
                    reads=[pTtok, t_A], **(dict(writes=[h2Tftok]) if k == 0 else dict(wadd=[h2Tftok])))
        pr, prtok = psA_ring.next()
        for k in range(8):
            S.op("pe", lambda e, pr=pr, k=k, h2Tf=h2Tf: e.matmul(pr[:, 0:36], h2Tf[:, k, :], rw_sb[:, k, :], start=(k == 0), stop=(k == 7)),
                 reads=[h2Tftok, t_rw], writes=[prtok])
        S.op("dve", lambda e, pr=pr, ti=ti: e.tensor_tensor(out=LG[:, ti, :], in0=pr[:, 0:36], in1=rb_bc[:], op=ALU.add), reads=[prtok, t_rw], wadd=[t_LG])
        S.op("dve", lambda e, h2s=h2s: e.tensor_tensor(out=tmp6[:], in0=h2s[:], in1=g12[:, 2 * D:3 * D], op=ALU.mult), reads=[h2stok, t_g12, t_tmp6], writes=[t_tmp6])
        S.op("dve", lambda e, ti=ti: e.tensor_tensor(out=h2tok[:, ti, :], in0=tmp6[:], in1=g12[:, 3 * D:4 * D], op=ALU.add), reads=[t_tmp6, t_g12], wadd=[t_h2tok])
    RT = sb("RTb", [128, 16 * 80], F32)
    t_RT = Tok()

    def V(c0, n):
        return RT[:, c0:c0 + 16 * n].rearrange("p (t n) -> p t n", n=n)

    def bc(ap2, n):
        return ap2.unsqueeze(2).to_broadcast([128, 16, n])
    G = LG[:, :, 0:4]
    E4 = LG[:, :, 4:36].rearrange("p t (g i) -> p t g i", g=4)
    gmax, gs, gw = RT[:, 0:16], RT[:, 16:32], RT[:, 32:48]
    m1, m2, w1, w2, w1g, w2g = RT[:, 48:64], RT[:, 64:80], RT[:, 80:96], RT[:, 96:112], RT[:, 112:128], RT[:, 128:144]
    goh, gex = V(144, 4), V(208, 4)
    eg, eq1, eg2, eq2, tmp8 = V(272, 8), V(400, 8), V(528, 8), V(656, 8), V(784, 8)

    def rop(fn, eng="dve", extra=()):
        S.op(eng, fn, reads=[t_RT, t_LG] + list(extra), writes=[t_RT])
    S.op("dve", lambda e: e.tensor_reduce(out=gmax, in_=G, axis=AX.X, op=ALU.max), reads=[t_LG], writes=[t_RT])
    rop(lambda e: e.tensor_tensor(out=goh, in0=G, in1=bc(gmax, 4), op=ALU.is_equal))
    rop(lambda e: e.tensor_tensor(out=gex, in0=G, in1=bc(gmax, 4), op=ALU.subtract))
    rop(lambda e: e.activation(out=RT[:, 208:272], in_=RT[:, 208:272], func=AF.Exp), eng="act")
    rop(lambda e: e.tensor_reduce(out=gs, in_=gex, axis=AX.X, op=ALU.add))
    rop(lambda e: e.reciprocal(out=gw, in_=gs))
    rop(lambda e: e.tensor_tensor(out=eg, in0=E4[:, :, 0, :], in1=bc(goh[:, :, 0], 8), op=ALU.mult))
    for g in range(1, 4):
        rop(lambda e, g=g: e.tensor_tensor(out=tmp8, in0=E4[:, :, g, :], in1=bc(goh[:, :, g], 8), op=ALU.mult))
        rop(lambda e: e.tensor_tensor(out=eg, in0=eg, in1=tmp8, op=ALU.add))
    rop(lambda e: e.tensor_reduce(out=m1, in_=eg, axis=AX.X, op=ALU.max))
    rop(lambda e: e.tensor_tensor(out=eq1, in0=eg, in1=bc(m1, 8), op=ALU.is_equal))
    rop(lambda e: e.scalar_tensor_tensor(out=RT[:, 528:656], in0=RT[:, 400:528], scalar=-1e30, in1=RT[:, 272:400], op0=ALU.mult, op1=ALU.add))
    rop(lambda e: e.tensor_reduce(out=m2, in_=eg2, axis=AX.X, op=ALU.max))
    rop(lambda e: e.tensor_tensor(out=eq2, in0=eg2, in1=bc(m2, 8), op=ALU.is_equal))
    rop(lambda e: e.tensor_tensor(out=w1, in0=m2, in1=m1, op=ALU.subtract))
    rop(lambda e: e.activation(out=w1, in_=w1, func=AF.Exp), eng="act")
    rop(lambda e: e.tensor_scalar(out=w1, in0=w1, scalar1=1.0, scalar2=None, op0=ALU.add))
    rop(lambda e: e.reciprocal(out=w1, in_=w1))
    rop(lambda e: e.tensor_scalar(out=w2, in0=w1, scalar1=-1.0, scalar2=1.0, op0=ALU.mult, op1=ALU.add))
    rop(lambda e: e.tensor_tensor(out=w1g, in0=w1, in1=gw, op=ALU.mult))
    rop(lambda e: e.tensor_tensor(out=w2g, in0=w2, in1=gw, op=ALU.mult))
    for g in range(4):
        S.op("dve", lambda e, g=g: e.tensor_tensor(out=Sel[:, :, 0, g * 8:(g + 1) * 8], in0=eq1, in1=bc(goh[:, :, g], 8), op=ALU.mult), reads=[t_RT], wadd=[t_Sel])
        S.op("dve", lambda e, g=g: e.tensor_tensor(out=Sel[:, :, 1, g * 8:(g + 1) * 8], in0=eq2, in1=bc(goh[:, :, g], 8), op=ALU.mult), reads=[t_RT], wadd=[t_Sel])
    Wt3 = Wt[:].rearrange("p (t k) -> p t k", k=2)
    S.op("dve", lambda e: e.tensor_copy(out=Wt3[:, :, 0], in_=w1g), reads=[t_RT], wadd=[t_Wt])
    S.op("dve", lambda e: e.tensor_copy(out=Wt3[:, :, 1], in_=w2g), reads=[t_RT], wadd=[t_Wt])
    Wselb = sb("Wselb", [128, 16, 32], BF16)
    t_wsel = Tok()
    S.op("dve", lambda e: e.tensor_tensor(out=Wselb[:], in0=Sel[:, :, 0, :], in1=Sel[:, :, 1, :], op=ALU.add), reads=[t_Sel], writes=[t_wsel])
    stri_b = sb("strib", [128, 128], BF16)
    t_stri = Tok()
    S.op("dve", lambda e: e.tensor_tensor(out=stri_b[:], in0=mask_b[0], in1=ident_b, op=ALU.subtract), reads=[t_cb], writes=[t_stri])
    rs = sb("rsm", [128, 512], F32)
    t_rs = Tok()
    cntf, nbf, padded, pad_end, pad_start = rs[:, 0:32], rs[:, 32:64], rs[:, 64:96], rs[:, 96:128], rs[:, 128:160]
    bef, be1024, be512 = rs[:, 192:256], rs[:, 256:320], rs[:, 320:384]
    pc, pctok = psA_ring.next()
    for ti in range(16):
        S.op("pe", lambda e, ti=ti: e.matmul(pc[:, 0:32], ones_b, Wselb[:, ti, :], start=(ti == 0), stop=(ti == 15)), reads=[t_wsel, t_cb], writes=[pctok])
    S.op("dve", lambda e: e.tensor_copy(out=cntf, in_=pc[:, 0:32]), reads=[pctok], writes=[t_rs])
    S.op("dve", lambda e: e.memset(nbf, 0.0), reads=[t_rs], writes=[t_rs])
    for j in range(16):
        S.op("dve", lambda e, j=j: e.scalar_tensor_tensor(out=nbf, in0=cntf, scalar=128.0 * j, in1=nbf, op0=ALU.is_gt, op1=ALU.add), reads=[t_rs], writes=[t_rs])
    S.op("dve", lambda e: e.tensor_scalar(out=padded, in0=nbf, scalar1=128.0, scalar2=None, op0=ALU.mult), reads=[t_rs], writes=[t_rs])
    S.op("dve", lambda e: e.tensor_tensor_scan(out=pad_end, data0=ones_f[:, 0:32], data1=padded, initial=0.0, op0=ALU.mult, op1=ALU.add), reads=[t_rs, t_const], writes=[t_rs])
    S.op("dve", lambda e: e.tensor_tensor(out=pad_start, in0=pad_end, in1=padded, op=ALU.subtract), reads=[t_rs], writes=[t_rs])
    DestF = sb("DestF", [128, 32], F32)
    t_destf = Tok()
    dt_ring = Ring([sb("dtt%d" % i, [128, 64], F32) for i in range(2)])
    for ti in range(16):
        pC, pCtok = psA_ring.next()
        for t2 in range(ti):
            S.op("pe", lambda e, pC=pC, t2=t2: e.matmul(pC[:, 0:32], ones_b, Wselb[:, t2, :], start=(t2 == 0), stop=False), reads=[t_wsel, t_cb], writes=[pCtok])
        S.op("pe", lambda e, pC=pC, ti=ti: e.matmul(pC[:, 0:32], stri_b[:], Wselb[:, ti, :], start=(ti == 0), stop=True), reads=[t_wsel, t_stri], writes=[pCtok])
        dtt, dtok = dt_ring.next()
        S.op("dve", lambda e, pC=pC, dtt=dtt: e.tensor_tensor(out=dtt[:, 0:32], in0=pC[:, 0:32], in1=pad_start, op=ALU.add), reads=[pCtok, t_rs], writes=[dtok])
        for k in range(2):
            S.op("dve", lambda e, dtt=dtt, ti=ti, k=k: e.tensor_tensor(out=dtt[:, 32:64], in0=dtt[:, 0:32], in1=Sel[:, ti, k, :], op=ALU.mult), reads=[dtok, t_Sel], writes=[dtok])
            S.op("dve", lambda e, dtt=dtt, ti=ti, k=k: e.tensor_reduce(out=DestF[:, ti * 2 + k:ti * 2 + k + 1], in_=dtt[:, 32:64], axis=AX.X, op=ALU.add),
                 reads=[dtok], wadd=[t_destf])
    S.op("dve", lambda e: e.tensor_copy(out=Desti[:], in_=DestF[:]), reads=[t_destf], writes=[t_dest])
    buf_d = dscr("moebuf_s", [8192, D], BF16)
    ybuf_d = dscr("moey_s", [8192, D], F32)
    t_buf = Tok()
    for ti in range(16):
        for k in range(2):
            S.idma(buf_d[:, :], bass.IndirectOffsetOnAxis(ap=Desti[:, ti * 2 + k:ti * 2 + k + 1], axis=0), h2tok[:, ti, :], None,
                   reads=[t_h2tok, t_dest], wadd=[t_buf])
    S.op("dve", lambda e: e.memset(bef, 0.0), reads=[t_rs], writes=[t_rs])
    TH = consts[:, 782:846]
    for ex in range(32):
        S.op("dve", lambda e, ex=ex: e.scalar_tensor_tensor(out=bef, in0=TH, scalar=rs[:, 96 + ex:97 + ex], in1=bef, op0=ALU.is_ge, op1=ALU.add), reads=[t_rs, t_const], writes=[t_rs])
    S.op("dve", lambda e: e.tensor_scalar(out=bef, in0=bef, scalar1=31.0, scalar2=None, op0=ALU.min), reads=[t_rs], writes=[t_rs])
    S.op("dve", lambda e: e.tensor_scalar(out=be1024, in0=bef, scalar1=1024.0, scalar2=None, op0=ALU.mult), reads=[t_rs], writes=[t_rs])
    S.op("dve", lambda e: e.tensor_scalar(out=be512, in0=bef, scalar1=512.0, scalar2=None, op0=ALU.mult), reads=[t_rs], writes=[t_rs])
    S.op("dve", lambda e: e.tensor_scalar(out=rs[:, 384:448], in0=TH, scalar1=rs[:, 127:128], scalar2=None, op0=ALU.is_ge), reads=[t_rs, t_const], writes=[t_rs])
    S.op("dve", lambda e: e.scalar_tensor_tensor(out=be1024, in0=rs[:, 384:448], scalar=1.0e6, in1=be1024, op0=ALU.mult, op1=ALU.add), reads=[t_rs], writes=[t_rs])
    S.op("dve", lambda e: e.scalar_tensor_tensor(out=be512, in0=rs[:, 384:448], scalar=1.0e6, in1=be512, op0=ALU.mult, op1=ALU.add), reads=[t_rs], writes=[t_rs])
    idxf = sb("idxf", [128, 64 * 12], F32)
    t_idxf = Tok()
    for b in range(64):
        S.op("dve", lambda e, b=b: e.tensor_scalar(out=idxf[:, b * 12:b * 12 + 8], in0=consts[:, 770:778], scalar1=rs[:, 256 + b:257 + b], scalar2=None, op0=ALU.add),
             reads=[t_rs, t_const], wadd=[t_idxf])
        S.op("dve", lambda e, b=b: e.tensor_scalar(out=idxf[:, b * 12 + 8:b * 12 + 12], in0=consts[:, 770:774], scalar1=rs[:, 320 + b:321 + b], scalar2=None, op0=ALU.add),
             reads=[t_rs, t_const], wadd=[t_idxf])
    S.op("dve", lambda e: e.tensor_copy(out=idxi[:], in_=idxf[:]), reads=[t_idxf], writes=[t_idx])
    dump("DestF", DestF[:], [128, 32], [t_destf])
    dump("rs", rs[:], [128, 512], [t_rs])
    release(n_s6)
    if dbg == 7:
        S.finish([t_x1, t_buf, t_idx] + dump_toks)
        nc.used_inputs = used_inputs
        return nc

    n_s7 = len(es)
    bc_reg = nc.gpsimd.to_reg(NE * D - 1)
    ewi_rows = ewi_d.rearrange("e r n -> (e r) n")
    ewo_rows = ewo_d.rearrange("e r n -> (e r) n")
    wib_ring = Ring([sb("wib%d" % i, [128, 8, 2 * DEXP], BF16) for i in range(2)])
    wob_ring = Ring([sb("wob%d" % i, [128, 4, D], BF16) for i in range(2)])
    xb_ring = Ring([sb("xbr%d" % i, [128, D], BF16) for i in range(2)])
    xbT_ring = Ring([sb("xbT%d" % i, [128, 8, 128], BF16) for i in range(2)])
    sil_ring = Ring([sb("sil%d" % i, [128, 512], F32) for i in range(2)])
    hT_ring = Ring([sb("hT%d" % i, [128, 4, 128], BF16) for i in range(2)])
    ysb_ring = Ring([sb("ysb%d" % i, [128, D], F32) for i in range(2)])
    t_ybuf = Tok()
    for b in range(int(os.environ.get("BLIM", 64))):
        wib, wibtok = wib_ring.next()
        wob, wobtok = wob_ring.next()
        for k in range(8):
            S.idma(wib[:, k, :], None, ewi_rows[:, :], bass.IndirectOffsetOnAxis(ap=idxi[:, b * 12 + k:b * 12 + k + 1], axis=0),
                   reads=[t_idx], bounds_check=bc_reg, oob_is_err=False, **(dict(writes=[wibtok]) if k == 0 else dict(wadd=[wibtok])))
        for j in range(4):
            S.idma(wob[:, j, :], None, ewo_rows[:, :], bass.IndirectOffsetOnAxis(ap=idxi[:, b * 12 + 8 + j:b * 12 + 9 + j], axis=0),
                   reads=[t_idx], bounds_check=bc_reg, oob_is_err=False, **(dict(writes=[wobtok]) if j == 0 else dict(wadd=[wobtok])))
        xb, xbtok = xb_ring.next()
        S.dma("sp", xb[:], buf_d[b * 128:(b + 1) * 128, :], reads=[t_buf], writes=[xbtok])
        pb, pbtok = psB_ring.next()
        for k in range(8):
            S.op("pe", lambda e, pb=pb, xb=xb, k=k: e.transpose(pb[:, k * 128:(k + 1) * 128], xb[:, k * 128:(k + 1) * 128], ident_b), reads=[xbtok, t_cb], writes=[pbtok])
        xbT, xbTtok = xbT_ring.next()
        S.op("dve", lambda e, xbT=xbT, pb=pb: e.tensor_copy(out=xbT[:].rearrange("p k n -> p (k n)"), in_=pb[:, :]), reads=[pbtok], writes=[xbTtok])
        pg, pgtok = psA_ring.next()
        pu, putok = psA_ring.next()
        for (pp, pptok, c0) in ((pg, pgtok, 0), (pu, putok, DEXP)):
            for j in range(4):
                for k in range(8):
                    S.op("pe", lambda e, pp=pp, j=j, k=k, c0=c0, wib=wib, xbT=xbT: e.matmul(
                        pp[:, j * 128:(j + 1) * 128], wib[:, k, c0 + j * 128:c0 + (j + 1) * 128], xbT[:, k, :], start=(k == 0), stop=(k == 7)),
                        reads=[wibtok, xbTtok], writes=[pptok])
        sil, siltok = sil_ring.next()
        S.op("act", lambda e, sil=sil, pg=pg: e.activation(out=sil[:], in_=pg[:, :], func=AF.Silu), reads=[pgtok], writes=[siltok])
        hT, hTtok = hT_ring.next()
        S.op("dve", lambda e, hT=hT, sil=sil, pu=pu: e.tensor_tensor(out=hT[:].rearrange("p j n -> p (j n)"), in0=pu[:, :], in1=sil[:], op=ALU.mult),
             reads=[putok, siltok], writes=[hTtok])
        ysb, ysbtok = ysb_ring.next()
        for half in range(2):
            py, pytok = psA_ring.next()
            for j in range(4):
                S.op("pe", lambda e, py=py, j=j, hT=hT, wob=wob, half=half: e.matmul(
                    py[:, :], hT[:, j, :], wob[:, j, half * 512:(half + 1) * 512], start=(j == 0), stop=(j == 3)), reads=[hTtok, wobtok], writes=[pytok])
            S.op("dve", lambda e, py=py, ysb=ysb, half=half: e.tensor_copy(out=ysb[:, half * 512:(half + 1) * 512], in_=py[:, :]),
                 reads=[pytok], **(dict(writes=[ysbtok]) if half == 0 else dict(wadd=[ysbtok])))
        S.dma("sp", ybuf_d[b * 128:(b + 1) * 128, :], ysb[:], reads=[ysbtok], wadd=[t_ybuf])
    release(n_s7)

    fng = sb("fng", [128, D], F32)
    t_fng = Tok()
    S.dma("sp", fng[:], rows_d[0:1, R_FNG:R_FNG + D].partition_broadcast(128), writes=[t_fng])
    fj = sb("fjunk", [128, D], F32)
    t_fj = Tok()
    fs_ring = Ring([(sb("fsa%d" % i, [128, 1], F32), sb("fsb%d" % i, [128, 1], F32)) for i in range(4)])
    y1_ring = Ring([sb("y1g%d" % i, [128, D], F32) for i in range(2)])
    y2_ring = Ring([sb("y2g%d" % i, [128, D], F32) for i in range(2)])
    x1l_ring = Ring([sb("x1l%d" % i, [128, D], F32) for i in range(2)])
    t_out = Tok()
    for ti in range(16):
        y1, y1tok = y1_ring.next()
        y2, y2tok = y2_ring.next()
        S.idma(y1[:, :], None, ybuf_d[:, :], bass.IndirectOffsetOnAxis(ap=Desti[:, ti * 2:ti * 2 + 1], axis=0), reads=[t_dest, t_ybuf], writes=[y1tok])
        S.idma(y2[:, :], None, ybuf_d[:, :], bass.IndirectOffsetOnAxis(ap=Desti[:, ti * 2 + 1:ti * 2 + 2], axis=0), reads=[t_dest, t_ybuf], writes=[y2tok])
        xl, xltok = x1l_ring.next()
        S.dma("sp", xl[:], x1_d[ti], reads=[t_x1], writes=[xltok])
        S.op("dve", lambda e, y1=y1, ti=ti: e.tensor_scalar(out=y1[:], in0=y1[:], scalar1=Wt[:, ti * 2:ti * 2 + 1], scalar2=None, op0=ALU.mult), reads=[y1tok, t_Wt], writes=[y1tok])
        S.op("dve", lambda e, y1=y1, y2=y2, ti=ti: e.scalar_tensor_tensor(out=y1[:], in0=y2[:], scalar=Wt[:, ti * 2 + 1:ti * 2 + 2], in1=y1[:], op0=ALU.mult, op1=ALU.add),
             reads=[y1tok, y2tok, t_Wt], writes=[y1tok])
        S.op("dve", lambda e, y1=y1: e.tensor_tensor(out=y1[:], in0=y1[:], in1=g12[:, D:2 * D], op=ALU.mult), reads=[y1tok, t_g12], writes=[y1tok])
        S.op("dve", lambda e, y1=y1, xl=xl: e.tensor_tensor(out=xl[:], in0=y1[:], in1=xl[:], op=ALU.add), reads=[y1tok, xltok], writes=[xltok])
        fs, fstok = fs_ring.next()
        S.op("act", lambda e, xl=xl, fs=fs: e.activation(out=fj[:], in_=xl[:], func=AF.Square, accum_out=fs[0][:, 0:1]), reads=[xltok], writes=[t_fj, fstok])
        S.op("dve", lambda e, fs=fs: e.tensor_scalar(out=fs[1][:, 0:1], in0=fs[0][:, 0:1], scalar1=1.0 / D, scalar2=EPS, op0=ALU.mult, op1=ALU.add), reads=[fstok], writes=[fstok])
        S.op("act", lambda e, fs=fs: e.activation(out=fs[0][:, 0:1], in_=fs[1][:, 0:1], func=AF.Ln), reads=[fstok], writes=[fstok])
        S.op("act", lambda e, fs=fs: e.activation(out=fs[1][:, 0:1], in_=fs[0][:, 0:1], func=AF.Exp, scale=-0.5), reads=[fstok], writes=[fstok])
        S.op("dve", lambda e, xl=xl, fs=fs: e.scalar_tensor_tensor(out=xl[:], in0=xl[:], scalar=fs[1][:, 0:1], in1=fng[:], op0=ALU.mult, op1=ALU.mult),
             reads=[fstok, t_fng, xltok], writes=[xltok])
        S.dma("sp", out_d[ti * 128:(ti + 1) * 128, :], xl[:], reads=[xltok], wadd=[t_out])
    S.finish([t_out] + dump_toks)
    nc.used_inputs = used_inputs
    return nc


def _host_inputs(inp):
    f = lambda a: np.ascontiguousarray(np.asarray(a, dtype=np.float32))
    x, c, ctx, c_ctx = f(inp["x"]), f(inp["c"]), f(inp["ctx"]), f(inp["c_ctx"])
    ada_w, ada_b = f(inp["ada_w"])[0], f(inp["ada_b"])[0]
    w_in = f(inp["w_in"])[0]
    up_w, up_b = f(inp["gla_up_w"])[0], f(inp["gla_up_b"])[0]
    conv_w, conv_b = f(inp["ml_conv_w"])[0], f(inp["ml_conv_b"])[0]
    i_b, f_b = f(inp["ml_i_b"])[0], f(inp["ml_f_b"])[0]
    consts = np.zeros((128, 1024), np.float32)
    consts[0:64, 512:640] = 1.0
    consts[64:128, 640:768] = 1.0
    consts[0:64, 768] = 1.0
    consts[64:128, 769] = 1.0
    consts[:, 770:782] = (np.arange(12)[None, :] % 8) * 128 + np.arange(128)[:, None]
    consts[:, 782:846] = np.arange(64)[None, :] * 128.0
    consts[:, 0:128] = np.eye(128)
    consts[:, 128:256] = np.triu(np.ones((128, 128)))
    consts[:, 256:384] = np.tril(np.ones((128, 128)))
    consts[:, 384:512] = 1.0
    router_w = np.concatenate([f(inp["router_group_w"])[0], f(inp["router_expert_w"])[0]], axis=1)
    shared = {
        "ada_w": ada_w, "ada_b": ada_b[None, :], "w_out": f(inp["w_out"])[0], "router_w": np.ascontiguousarray(router_w),
        "e_w_in": f(inp["expert_w_in"])[0], "e_w_out": f(inp["expert_w_out"])[0], "consts": consts,
    }
    maps = []
    for core in range(8):
        b, flip = core // 2, core % 2
        xs, cs = x[b], ctx[b]
        win, uw, ub, cw, ib, fb = w_in, up_w, up_b, conv_w, i_b, f_b
        if flip:
            xs, cs = xs[::-1], cs[::-1]
            win = win.copy()
            win[:, C_LR:C_LR + 16], win[:, C_LR + 16:C_LR + 32] = w_in[:, C_LR + 16:C_LR + 32], w_in[:, C_LR:C_LR + 16]
            win[:, C_MI:C_MI + 4], win[:, C_MI + 4:C_MI + 8] = w_in[:, C_MI + 4:C_MI + 8], w_in[:, C_MI:C_MI + 4]
            win[:, C_MI + 8:C_MI + 12], win[:, C_MI + 12:C_MI + 16] = w_in[:, C_MI + 12:C_MI + 16], w_in[:, C_MI + 8:C_MI + 12]
            uw, ub, ib, fb = uw[::-1], ub[::-1], ib[::-1], fb[::-1]
            cw = cw[::-1, ::-1]
        vecs = np.zeros((128, NV), np.float32)
        vecs[:, V_N1G:V_N1G + 8] = f(inp["norm1_g"])[0].reshape(8, 128).T
        vecs[:, V_N2G:V_N2G + 8] = f(inp["norm2_g"])[0].reshape(8, 128).T
        vecs[:, V_UPB:V_UPB + 4] = ub.reshape(2, 2, 128).transpose(2, 0, 1).reshape(128, 4)
        vecs[:, V_CONVB:V_CONVB + 8] = conv_b.reshape(8, 128).T
        vecs[:, V_CONVW:V_CONVW + 72] = cw.reshape(9, 8, 128).transpose(2, 1, 0).reshape(128, 72)
        vecs[:, V_GNG:V_GNG + 4] = f(inp["gla_norm_g"])[0].reshape(4, 128).T
        vecs[:, V_MNG:V_MNG + 4] = f(inp["ml_norm_g"])[0].reshape(4, 128).T
        rows = np.zeros((1, NR), np.float32)
        rows[0, R_GATEB:R_GATEB + 8] = ib.reshape(8)
        rows[0, R_GATEB + 8:R_GATEB + 16] = fb.reshape(8)
        rows[0, R_FNG:R_FNG + 1024] = f(inp["final_norm_g"])
        rows[0, R_RB:R_RB + 4] = f(inp["router_group_b"])[0]
        rows[0, R_RB + 4:R_RB + 36] = f(inp["router_expert_b"])[0]
        rows[0, R_N2G:R_N2G + 1024] = f(inp["norm2_g"])[0]
        cvec = np.concatenate([c[b].reshape(128, 8), c_ctx.reshape(128, 8)], axis=1)
        m = dict(shared)
        m.update({
            "x": np.ascontiguousarray(xs), "ctx": np.ascontiguousarray(cs), "cvec": np.ascontiguousarray(cvec),
            "vecs": vecs, "rows": rows, "w_in": np.ascontiguousarray(win), "up_w": np.ascontiguousarray(uw),
        })
        maps.append(m)
    return maps


def kernel(**inputs):
    maps = _host_inputs(inputs)
    nc = build()
    maps = [{k: m[k] for k in nc.used_inputs} for m in maps]
    res = run_bass_kernel_spmd(nc, maps, core_ids=list(range(8)))
    out = np.zeros((4, SEQ, D), np.float32)
    for core in range(8):
        b, flip = core // 2, core % 2
        o = res.results[core]["out"]
        if flip:
            out[b, NLOC:] = o[::-1]
        else:
            out[b, :NLOC] = o
    return out
```

```python
import numpy as np
import concourse.bass as bass
import concourse.mybir as mybir
from concourse.bass_utils import run_bass_kernel_spmd

F32 = mybir.dt.float32
BF16 = mybir.dt.bfloat16
AF = mybir.ActivationFunctionType
ALU = mybir.AluOpType
AX = mybir.AxisListType

D = 1024
SEQ = 4096
NLOC = 2048
CTX = 256
INW = 3632
NE = 32
DEXP = 512
EPS = 1e-6
NBLK = 9
C_GQ, C_GK, C_GV, C_GG, C_LR, C_MQ, C_MK, C_MV, C_MO, C_MI = 0, 256, 512, 1024, 1536, 1568, 2080, 2592, 3104, 3616

V_N1G, V_N2G, V_UPB, V_CONVB, V_CONVW, V_GNG, V_MNG = 0, 8, 16, 20, 28, 100, 104
NV = 108
R_GATEB, R_FNG, R_RB = 0, 16, 16 + 1024
R_N2G = 16 + 1024 + 36
NR = 16 + 1024 + 36 + 1024
I32 = mybir.dt.int32


class Tok:
    __slots__ = ("w", "r")

    def __init__(self):
        self.w = {}
        self.r = {}


class Sched:
    NDMA = 6

    def __init__(self, nc):
        self.nc = nc
        self.eng = {"pe": nc.tensor, "dve": nc.vector, "act": nc.scalar, "pool": nc.gpsimd, "sp": nc.sync}
        self.sem = {}
        self.cnt = {}
        self.waited = {k: {} for k in self.eng}
        self._cms = []
        for k in ["pe", "dve", "act", "pool"]:
            self._mk(k)
        self.dq = {}
        self.nq = {"sp": 8, "pool": 16, "act": 2}
        for q in ["sp", "pool", "act"]:
            names = []
            for i in range(self.nq[q]):
                n = "d%s%d" % (q, i)
                self._mk(n)
                names.append(n)
            self.dq[q] = [names, 0]
        self.ninstr = 0

    def _mk(self, k):
        cm = self.nc.semaphore("s_" + k)
        self.sem[k] = cm.__enter__()
        self._cms.append(cm)
        self.cnt[k] = 0

    def _wait(self, e, key, val):
        if self.waited[e].get(key, 0) >= val:
            return
        self.eng[e].wait_ge(self.sem[key], val)
        self.waited[e][key] = val

    def _deps(self, e, reads, writes):
        for t in reads:
            for k, v in t.w.items():
                if not (k == "pe" and e == "pe"):
                    self._wait(e, k, v)
        for t in writes:
            for k, v in t.w.items():
                if not (k == "pe" and e == "pe"):
                    self._wait(e, k, v)
            for k, v in t.r.items():
                if k != e:
                    self._wait(e, k, v)

    def _mark(self, key, val, reads, writes, wadd):
        for t in reads:
            if t.r.get(key, 0) < val:
                t.r[key] = val
        for t in writes:
            t.w = {key: val}
            t.r = {}
        for t in wadd:
            t.w[key] = val

    def op(self, e, fn, reads=(), writes=(), wadd=()):
        self._deps(e, reads, writes)
        for t in wadd:
            for k, v in t.r.items():
                if k != e:
                    self._wait(e, k, v)
        ins = fn(self.eng[e])
        self.cnt[e] += 1
        ins.then_inc(self.sem[e], 1)
        self._mark(e, self.cnt[e], reads, writes, wadd)
        self.ninstr += 1
        return ins

    def dma(self, q, out, in_, reads=(), writes=(), wadd=(), **kw):
        names, idx = self.dq[q]
        key = names[idx % len(names)]
        self.dq[q][1] = idx + 1
        self._wait(q, key, self.cnt[key])
        self._deps(q, reads, writes)
        for t in wadd:
            for k, v in t.r.items():
                self._wait(q, k, v)
        ins = self.eng[q].dma_start(out=out, in_=in_, **kw)
        self.cnt[key] += 16
        ins.then_inc(self.sem[key], 16)
        self._mark(key, self.cnt[key], reads, writes, wadd)
        self.ninstr += 1
        return ins

    def idma(self, out, out_off, in_, in_off, reads=(), writes=(), wadd=(), **kw):
        q = "pool"
        names, idx = self.dq[q]
        key = names[idx % len(names)]
        self.dq[q][1] = idx + 1
        self._wait(q, key, self.cnt[key])
        self._deps(q, reads, writes)
        for t in wadd:
            for k, v in t.r.items():
                self._wait(q, k, v)
        ins = self.eng[q].indirect_dma_start(out=out, out_offset=out_off, in_=in_, in_offset=in_off, **kw)
        self.cnt[key] += 16
        ins.then_inc(self.sem[key], 16)
        self._mark(key, self.cnt[key], reads, writes, wadd)
        self.ninstr += 1
        return ins

    def barrier(self):
        import os
        if os.environ.get('NOBAR'):
            return
        for e in self.eng:
            if e == 'pool' and os.environ.get('NOPOOLBAR'):
                continue
            for k in self.sem:
                if self.cnt[k] > 0 and k != e:
                    self._wait(e, k, self.cnt[k])

    def finish(self, toks, e="sp"):
        for t in toks:
            for k, v in t.w.items():
                self._wait(e, k, v)


class Ring:
    def __init__(self, items):
        self.items = items
        self.toks = [Tok() for _ in items]
        self.i = 0

    def next(self):
        j = self.i % len(self.items)
        self.i += 1
        return self.items[j], self.toks[j]


def build(dbg=0):
    import os
    dbg = int(os.environ.get("KSTOP", dbg))
    nc = bass.Bass("TRN2", target_bir_lowering=False)
    import os
    scratch_kind = "ExternalOutput" if (dbg or os.environ.get("SCR_EXT")) else "Internal"

    used_inputs = []

    def din(name, shape, dt=F32, need=0):
        if dbg and dbg < need:
            return None
        used_inputs.append(name)
        return nc.dram_tensor(name, list(shape), dt, kind="ExternalInput").ap()

    dump_toks = []

    def dump(name, ap, shape, toks, dt=F32):
        if not dbg:
            return
        dd = nc.dram_tensor("dbg_" + name, list(shape), dt, kind="ExternalOutput").ap()
        t = Tok()
        S.dma("sp", dd, ap, reads=toks, writes=[t])
        dump_toks.append(t)

    def dscr(name, shape, dt):
        return nc.dram_tensor(name, list(shape), dt, kind=scratch_kind).ap()

    x_d = din("x", [SEQ, D])
    ctx_d = din("ctx", [CTX, D])
    cvec_d = din("cvec", [128, 16])
    adaw_d = din("ada_w", [D, 6 * D])
    adab_d = din("ada_b", [1, 6 * D])
    vecs_d = din("vecs", [128, NV])
    rows_d = din("rows", [1, NR])
    win_d = din("w_in", [D, INW])
    upw_d = din("up_w", [2, 16, 256])
    wout_d = din("w_out", [D, D])
    rw_d = din("router_w", [D, 36])
    ewi_d = din("e_w_in", [NE, D, 2 * DEXP], need=8)
    ewo_d = din("e_w_out", [NE, DEXP, D], need=8)
    consts_d = din("consts", [128, 1024])
    out_d = nc.dram_tensor("out", [NLOC, D], F32, kind="ExternalOutput").ap()

    xnT_d = dscr("xnT_s", [NBLK, 128, 8 * 512], BF16)
    gqk_d = dscr("gqk_s", [NBLK, 128, 4 * 512], BF16)
    lr_d = dscr("lr_s", [NBLK, 16, 2 * 512], F32)
    sgg_d = dscr("sgg_s", [4, 128, 4 * 512], BF16)
    smo_d = dscr("smo_s", [4, 128, 4 * 512], BF16)
    mpre_d = dscr("mpre_s", [NBLK, 128, 8 * 512], BF16)
    gv_d = dscr("gv_s", [NBLK, 128, 4 * 512], BF16)
    mv_d = dscr("mv_s", [NBLK, 128, 4 * 512], BF16)
    gates_d = dscr("gates_s", [NBLK, 128, 4 * 16], F32)
    mqk_d = dscr("mqk_s", [NBLK, 128, 8 * 512], BF16)

    S = Sched(nc)
    es = []
    uid = [0]

    def sb(name, shape, dt):
        uid[0] += 1
        cm = nc.sbuf_tensor("sb%d_%s" % (uid[0], name), list(shape), dt)
        t = cm.__enter__()
        es.append(cm)
        return t

    def ps(name, shape, dt):
        uid[0] += 1
        cm = nc.psum_tensor("ps%d_%s" % (uid[0], name), list(shape), dt)
        t = cm.__enter__()
        es.append(cm)
        return t

    def release(n0):
        S.barrier()
        while len(es) > n0:
            es.pop().__exit__(None, None, None)

    consts = sb("consts", [128, 1024], F32)
    vecs = sb("vecs", [128, NV], F32)
    t_const = Tok()
    S.dma("sp", consts[:], consts_d[:, :], writes=[t_const])
    S.dma("sp", vecs[:], vecs_d[:, :], wadd=[t_const])
    ident_f = consts[:, 0:128]
    ones_f = consts[:, 384:512]
    cb = sb("constsb", [128, 512], BF16)
    t_cb = Tok()
    S.op("dve", lambda e: e.tensor_copy(out=cb[:], in_=consts[:, 0:512]), reads=[t_const], writes=[t_cb])
    ident_b = cb[:, 0:128]
    mask_b = [cb[:, 128:256], cb[:, 256:384]]
    ones_b = cb[:, 384:512]

    psA = [ps("psA%d" % i, [128, 512], F32) for i in range(6)]
    psB = [ps("psB%d" % i, [128, 1024], BF16) for i in range(2)]
    psA_ring = Ring(psA)
    psB_ring = Ring(psB)

    t_mod = Tok()
    A1 = sb("A1", [128, 4 * 8], F32)
    A2 = sb("A2", [128, 2 * 8], F32)
    t_A = Tok()
    g12 = sb("g12", [128, 4 * D], F32)
    t_g12 = Tok()
    idxi = sb("idxi", [128, 64 * 12], I32)
    Desti = sb("Desti", [128, 32], I32)
    Wt = sb("Wt", [128, 32], F32)
    t_idx, t_dest, t_Wt = Tok(), Tok(), Tok()

    n_keep = len(es)
    modx = sb("modx", [1, 6 * D], F32)
    modc = sb("modc", [1, 2 * D], F32)
    cvec = sb("cvec", [128, 16], F32)
    scv = sb("scv", [128, 16], F32)
    adab = sb("adab", [1, 6 * D], F32)
    t_cv, t_scv, t_adab = Tok(), Tok(), Tok()
    S.dma("sp", cvec[:], cvec_d[:, :], writes=[t_cv])
    S.dma("sp", adab[:], adab_d[:, :], writes=[t_adab])
    S.op("act", lambda e: e.activation(out=scv[:], in_=cvec[:], func=AF.Silu), reads=[t_cv], writes=[t_scv])
    adaw_v = adaw_d.rearrange("(p k) n -> p k n", k=8)
    wst = [sb("adaw%d" % i, [128, 8, 512], F32) for i in range(2)]
    wst_ring = Ring(wst)
    S.op("dve", lambda e: e.memset(modx[:], 0.0), writes=[t_mod])
    for blk in range(12):
        wt, wtok = wst_ring.next()
        S.dma("sp", wt[:], adaw_v[:, :, blk * 512:(blk + 1) * 512], writes=[wtok])
        for which in range(2):
            if which == 1 and blk >= 4:
                continue
            pt, ptok = psA_ring.next()
            for k in range(8):
                S.op("pe", lambda e, k=k, pt=pt, wt=wt, which=which: e.matmul(
                    pt[0:1, :], scv[:, which * 8 + k:which * 8 + k + 1], wt[:, k, :], start=(k == 0), stop=(k == 7)),
                    reads=[t_scv, wtok], writes=[ptok])
            dst = modx if which == 0 else modc
            S.op("dve", lambda e, pt=pt, dst=dst, blk=blk: e.tensor_tensor(
                out=dst[0:1, blk * 512:(blk + 1) * 512], in0=pt[0:1, :], in1=adab[0:1, blk * 512:(blk + 1) * 512], op=ALU.add),
                reads=[ptok, t_adab], wadd=[t_mod])
    colps, coltok = psA_ring.next()
    specs = [(modx, 1 * D), (modx, 0 * D), (modc, 1 * D), (modc, 0 * D), (modx, 4 * D), (modx, 3 * D)]
    first = True
    for si, (src, off) in enumerate(specs):
        for k in range(8):
            S.op("pe", lambda e, src=src, off=off, k=k, si=si: e.matmul(
                colps[:, si * 8 + k:si * 8 + k + 1], src[0:1, off + k * 128:off + (k + 1) * 128], ones_f[0:1, 0:1], start=True, stop=True),
                reads=[t_mod, t_const], writes=[coltok] if first else (), wadd=() if first else [coltok])
            first = False
    for (dst, c0, g0, s_sc, s_sh) in [(A1, 0, V_N1G, 0, 1), (A1, 16, V_N1G, 2, 3), (A2, 0, V_N2G, 4, 5)]:
        S.op("dve", lambda e, dst=dst, c0=c0, g0=g0, s_sc=s_sc: e.scalar_tensor_tensor(
            out=dst[:, c0:c0 + 8], in0=colps[:, s_sc * 8:s_sc * 8 + 8], scalar=1.0, in1=vecs[:, g0:g0 + 8], op0=ALU.add, op1=ALU.mult),
            reads=[coltok, t_const], wadd=[t_A])
        S.op("dve", lambda e, dst=dst, c0=c0, s_sh=s_sh: e.tensor_copy(out=dst[:, c0 + 8:c0 + 16], in_=colps[:, s_sh * 8:s_sh * 8 + 8]),
             reads=[coltok], wadd=[t_A])
    for gi, off in enumerate([2 * D, 5 * D]):
        for hf in range(2):
            pt, ptok = psA_ring.next()
            S.op("pe", lambda e, pt=pt, off=off, hf=hf: e.matmul(pt[:, :], ones_f[0:1, :], modx[0:1, off + hf * 512:off + (hf + 1) * 512], start=True, stop=True),
                 reads=[t_mod, t_const], writes=[ptok])
            S.op("act", lambda e, pt=pt, gi=gi, hf=hf: e.copy(out=g12[:, gi * D + hf * 512:gi * D + (hf + 1) * 512], in_=pt[:, :]),
                 reads=[ptok], wadd=[t_g12])
    n2gbc = sb("n2gbc", [128, D], F32)
    t_n2g = Tok()
    S.dma("sp", n2gbc[:], rows_d[0:1, R_N2G:R_N2G + D].partition_broadcast(128), writes=[t_n2g])
    for gi, off in ((2, 4 * D), (3, 3 * D)):
        for hf in range(2):
            pt, ptok = psA_ring.next()
            S.op("pe", lambda e, pt=pt, off=off, hf=hf: e.matmul(pt[:, :], ones_f[0:1, :], modx[0:1, off + hf * 512:off + (hf + 1) * 512], start=True, stop=True),
                 reads=[t_mod, t_const], writes=[ptok])
            dst = g12[:, gi * D + hf * 512:gi * D + (hf + 1) * 512]
            if gi == 2:
                S.op("dve", lambda e, pt=pt, dst=dst, hf=hf: e.scalar_tensor_tensor(out=dst, in0=pt[:, :], scalar=1.0, in1=n2gbc[:, hf * 512:(hf + 1) * 512], op0=ALU.add, op1=ALU.mult),
                     reads=[ptok, t_n2g], wadd=[t_g12])
            else:
                S.op("dve", lambda e, pt=pt, dst=dst: e.tensor_copy(out=dst, in_=pt[:, :]), reads=[ptok], wadd=[t_g12])
    dump("g12", g12[:, 0:2 * D], [128, 2 * D], [t_g12])
    dump("A1", A1[:], [128, 32], [t_A])
    dump("modx", modx[:], [1, 6 * D], [t_mod])
    if dbg == 1:
        S.finish(dump_toks)
        nc.used_inputs = used_inputs
        return nc
    release(n_keep)

    xt_ring = Ring([sb("xt%d" % i, [128, D], F32) for i in range(2)])
    xs_ring = Ring([sb("xs%d" % i, [128, D], BF16) for i in range(2)])
    junk = sb("junk", [128, D], F32)
    xsf_ring = Ring([sb("xsf%d" % i, [128, D], F32) for i in range(2)])
    t_junk = Tok()
    ss_ring = Ring([(sb("ssa%d" % i, [128, 1], F32), sb("ssb%d" % i, [128, 1], F32)) for i in range(4)])
    xnb_ring = Ring([sb("xnb%d" % i, [128, 8, 512], BF16) for i in range(2)])
    t_xnT = [Tok() for _ in range(NBLK)]
    import os
    for blk in range(int(os.environ.get('KLIM', NBLK))):
        ntile = 2 if blk == 0 else 4
        xnb, xnbtok = xnb_ring.next()
        xfirst = [True]

        def xw(tok=xnbtok, xfirst=xfirst):
            if xfirst[0]:
                xfirst[0] = False
                return dict(writes=[tok])
            return dict(wadd=[tok])
        for ti in range(ntile):
            if blk == 0:
                src = ctx_d[ti * 128:(ti + 1) * 128, :]
                acol = 16
            else:
                r0 = (blk - 1) * 512 + ti * 128
                src = x_d[r0:r0 + 128, :]
                acol = 0
            xt, xttok = xt_ring.next()
            S.dma("sp", xt[:], src, writes=[xttok])
            ss, sstok = ss_ring.next()
            S.op("act", lambda e, xt=xt, ss=ss: e.activation(out=junk[:], in_=xt[:], func=AF.Square, accum_out=ss[0][:, 0:1]),
                 reads=[xttok], writes=[t_junk, sstok])
            S.op("dve", lambda e, ss=ss: e.tensor_scalar(out=ss[1][:, 0:1], in0=ss[0][:, 0:1], scalar1=1.0 / D, scalar2=EPS, op0=ALU.mult, op1=ALU.add),
                 reads=[sstok], writes=[sstok])
            S.op("act", lambda e, ss=ss: e.activation(out=ss[0][:, 0:1], in_=ss[1][:, 0:1], func=AF.Ln), reads=[sstok], writes=[sstok])
            S.op("act", lambda e, ss=ss: e.activation(out=ss[1][:, 0:1], in_=ss[0][:, 0:1], func=AF.Exp, scale=-0.5), reads=[sstok], writes=[sstok])
            xs, xstok = xs_ring.next()
            S.op("dve", lambda e, xs=xs, xt=xt, ss=ss: e.tensor_scalar(out=xs[:], in0=xt[:], scalar1=ss[1][:, 0:1], scalar2=None, op0=ALU.mult),
                 reads=[xttok, sstok], writes=[xstok])
            pb, pbtok = psB_ring.next()
            for k in range(8):
                S.op("pe", lambda e, pb=pb, xs=xs, k=k: e.transpose(pb[:, k * 128:(k + 1) * 128], xs[:, k * 128:(k + 1) * 128], ident_b),
                     reads=[xstok, t_cb], writes=[pbtok])
            for k in range(8):
                eng = "dve"
                if eng == "act":
                    S.op("act", lambda e, pb=pb, xnb=xnb, k=k, ti=ti, acol=acol: e.activation(
                        out=xnb[:, k, ti * 128:(ti + 1) * 128], in_=pb[:, k * 128:(k + 1) * 128], func=AF.Identity,
                        scale=A1[:, acol + k:acol + k + 1], bias=A1[:, acol + 8 + k:acol + 8 + k + 1]),
                        reads=[pbtok, t_A], **xw())
                else:
                    S.op("dve", lambda e, pb=pb, xnb=xnb, k=k, ti=ti, acol=acol: e.tensor_scalar(
                        out=xnb[:, k, ti * 128:(ti + 1) * 128], in0=pb[:, k * 128:(k + 1) * 128],
                        scalar1=A1[:, acol + k:acol + k + 1], scalar2=A1[:, acol + 8 + k:acol + 8 + k + 1], op0=ALU.mult, op1=ALU.add),
                        reads=[pbtok, t_A], **xw())
        S.dma("sp", xnT_d[blk], xnb[:].rearrange("p k n -> p (k n)"), reads=[xnbtok], writes=[t_xnT[blk]])
    release(n_keep)

    final_toks = list(t_xnT)
    if dbg == 2:
        S.finish(final_toks + dump_toks)
        nc.used_inputs = used_inputs
        return nc

    n_s2 = len(es)
    win_v = win_d.rearrange("(k p) n -> p k n", p=128)
    Wb = sb("Wb", [128, 8, INW], BF16)
    t_Wb = Tok()
    wstg = Ring([sb("wstg%d" % i, [128, 8, 227], F32) for i in range(2)])
    for pc in range(16):
        st, sttok = wstg.next()
        S.dma("sp", st[:], win_v[:, :, pc * 227:(pc + 1) * 227], writes=[sttok])
        S.op("dve", lambda e, st=st, pc=pc: e.tensor_copy(out=Wb[:, :, pc * 227:(pc + 1) * 227], in_=st[:]),
             reads=[sttok], **(dict(writes=[t_Wb]) if pc == 0 else dict(wadd=[t_Wb])))
    xin_ring = Ring([sb("xin%d" % i, [128, 8, 512], BF16) for i in range(2)])
    stg = {}
    for nm, shp, dt in [("gqk", [128, 4 * 512], BF16), ("sgg", [128, 4 * 512], BF16), ("smo", [128, 4 * 512], BF16),
                        ("mpre", [128, 8 * 512], BF16), ("gv", [128, 4 * 512], BF16), ("mv", [128, 4 * 512], BF16),
                        ("lr", [16, 2 * 512], F32), ("gates", [128, 64], F32)]:
        stg[nm] = Ring([sb("st_%s%d" % (nm, i), shp, dt) for i in range(2)])
    sig_ring = Ring([sb("sigt%d" % i, [128, 512], F32) for i in range(2)])
    t_gqk = [Tok() for _ in range(NBLK)]
    t_lr = [Tok() for _ in range(NBLK)]
    t_sgg = [Tok() for _ in range(4)]
    t_smo = [Tok() for _ in range(4)]
    t_mpre = [Tok() for _ in range(NBLK)]
    t_gv = [Tok() for _ in range(NBLK)]
    t_mv = [Tok() for _ in range(NBLK)]
    t_gates = [Tok() for _ in range(NBLK)]

    class Acc:
        def __init__(self, tok):
            self.tok = tok
            self.first = True

        def kw(self):
            if self.first:
                self.first = False
                return dict(writes=[self.tok])
            return dict(wadd=[self.tok])

    for blk in range(int(os.environ.get('KLIM2', NBLK))):
        N = 256 if blk == 0 else 512
        is_ctx, is_loc, is_far = blk == 0, 1 <= blk <= 4, blk >= 5
        xin, xintok = xin_ring.next()
        S.dma("sp", xin[:].rearrange("p k n -> p (k n)"), xnT_d[blk], reads=[t_xnT[blk]], writes=[xintok])
        cur = {nm: stg[nm].next() for nm in stg}
        acc = {nm: Acc(cur[nm][1]) for nm in stg}

        def cm_tile(col0, M, N=N, xin=xin, xintok=xintok):
            pt, ptok = psA_ring.next()
            for k in range(8):
                S.op("pe", lambda e, k=k, pt=pt: e.matmul(pt[0:M, 0:N], Wb[:, k, col0:col0 + M], xin[:, k, 0:N], start=(k == 0), stop=(k == 7)),
                     reads=[t_Wb, xintok], writes=[ptok])
            return pt, ptok

        def evac(nm, dst, pt, ptok, M, scale=None, N=N):
            if scale is None:
                S.op("dve", lambda e: e.tensor_copy(out=dst, in_=pt[0:M, 0:N]), reads=[ptok], **acc[nm].kw())
            else:
                S.op("dve", lambda e: e.tensor_scalar(out=dst, in0=pt[0:M, 0:N], scalar1=scale, scalar2=None, op0=ALU.mult),
                     reads=[ptok], **acc[nm].kw())

        def evac_sig(nm, dst, pt, ptok, silu, N=N):
            if silu:
                S.op("act", lambda e: e.activation(out=dst, in_=pt[:, 0:N], func=AF.Silu), reads=[ptok], **acc[nm].kw())
                return
            sg, sgtok = sig_ring.next()
            S.op("act", lambda e: e.activation(out=sg[:, 0:N], in_=pt[:, 0:N], func=AF.Exp, scale=-1.0), reads=[ptok], writes=[sgtok])
            S.op("dve", lambda e: e.tensor_scalar(out=sg[:, 0:N], in0=sg[:, 0:N], scalar1=1.0, scalar2=None, op0=ALU.add), reads=[sgtok], writes=[sgtok])
            S.op("dve", lambda e: e.reciprocal(out=sg[:, 0:N], in_=sg[:, 0:N]), reads=[sgtok], writes=[sgtok])
            if silu:
                S.op("dve", lambda e: e.tensor_tensor(out=dst, in0=pt[:, 0:N], in1=sg[:, 0:N], op=ALU.mult), reads=[ptok, sgtok], **acc[nm].kw())
            else:
                S.op("dve", lambda e: e.tensor_copy(out=dst, in_=sg[:, 0:N]), reads=[sgtok], **acc[nm].kw())

        gq_st, lr_st, sgg_st, smo_st = cur["gqk"][0], cur["lr"][0], cur["sgg"][0], cur["smo"][0]
        mp_st, gv_st, mv_st, ga_st = cur["mpre"][0], cur["gv"][0], cur["mv"][0], cur["gates"][0]
        for j in range(4):
            if j < 2 and not is_loc:
                continue
            col0 = C_GQ + j * 128 if j < 2 else C_GK + (j - 2) * 128
            pt, ptok = cm_tile(col0, 128)
            evac("gqk", gq_st[:, j * 512:j * 512 + N], pt, ptok, 128, scale=(0.125 if j < 2 else None))
        for dd in range(2):
            if is_far and dd == 0:
                continue
            pt, ptok = cm_tile(C_LR + dd * 16, 16)
            evac("lr", lr_st[0:16, dd * 512:dd * 512 + N], pt, ptok, 16)
        for j in range(8):
            if j < 4 and not (is_loc or blk == 5):
                continue
            col0 = C_MQ + j * 128 if j < 4 else C_MK + (j - 4) * 128
            pt, ptok = cm_tile(col0, 128)
            evac("mpre", mp_st[:, j * 512:j * 512 + N], pt, ptok, 128)
        if is_loc:
            for j in range(4):
                pt, ptok = cm_tile(C_GG + j * 128, 128)
                evac_sig("sgg", sgg_st[:, j * 512:(j + 1) * 512], pt, ptok, True)
            for j in range(4):
                pt, ptok = cm_tile(C_MO + j * 128, 128)
                evac_sig("smo", smo_st[:, j * 512:(j + 1) * 512], pt, ptok, False)
        for c in range(N // 128):
            for nm, col0, ncol, st_ in [("gv", C_GV, 512, gv_st), ("mv", C_MV, 512, mv_st), ("gates", C_MI, 16, ga_st)]:
                pt, ptok = psA_ring.next()
                for k in range(8):
                    S.op("pe", lambda e, k=k, pt=pt, c=c, col0=col0, ncol=ncol: e.matmul(
                        pt[:, 0:ncol], xin[:, k, c * 128:(c + 1) * 128], Wb[:, k, col0:col0 + ncol], start=(k == 0), stop=(k == 7)),
                        reads=[t_Wb, xintok], writes=[ptok])
                S.op("dve", lambda e, pt=pt, st_=st_, c=c, ncol=ncol: e.tensor_copy(out=st_[:, c * ncol:(c + 1) * ncol], in_=pt[:, 0:ncol]),
                     reads=[ptok], **acc[nm].kw())
        S.dma("sp", gqk_d[blk], gq_st[:], reads=[cur["gqk"][1]], writes=[t_gqk[blk]])
        S.dma("sp", lr_d[blk], lr_st[:], reads=[cur["lr"][1]], writes=[t_lr[blk]])
        S.dma("sp", mpre_d[blk], mp_st[:], reads=[cur["mpre"][1]], writes=[t_mpre[blk]])
        S.dma("sp", gv_d[blk], gv_st[:], reads=[cur["gv"][1]], writes=[t_gv[blk]])
        S.dma("sp", mv_d[blk], mv_st[:], reads=[cur["mv"][1]], writes=[t_mv[blk]])
        S.dma("sp", gates_d[blk], ga_st[:], reads=[cur["gates"][1]], writes=[t_gates[blk]])
        if is_loc:
            S.dma("sp", sgg_d[blk - 1], sgg_st[:], reads=[cur["sgg"][1]], writes=[t_sgg[blk - 1]])
            S.dma("sp", smo_d[blk - 1], smo_st[:], reads=[cur["smo"][1]], writes=[t_smo[blk - 1]])
    release(n_s2)
    if dbg == 3:
        S.finish(t_gqk + t_lr + t_sgg + t_smo + t_mpre + t_gv + t_mv + t_gates + dump_toks)
        nc.used_inputs = used_inputs
        return nc

    n_s3 = len(es)
    t_mqk = [Tok() for _ in range(NBLK)]
    Pbuf = sb("convP", [128, 66 * 64], BF16)
    accb = sb("convacc", [128, 64 * 64], F32)
    eb = sb("conve", [128, 64 * 64], F32)
    outb = sb("convout", [128, 64 * 64], BF16)
    tP, tacc, teb, toutb = Tok(), Tok(), Tok(), Tok()

    def conv_tile(j, R, Wd, srcs, dsts, taps_i):
        n_el = R * Wd
        S.op("dve", lambda e: e.memset(Pbuf[:, 0:Wd], 0.0), writes=[tP])
        S.op("dve", lambda e: e.memset(Pbuf[:, Wd + n_el:2 * Wd + n_el], 0.0), wadd=[tP])
        for (ap, tok, off, n) in srcs:
            S.dma("sp", Pbuf[:, Wd + off:Wd + off + n], ap, reads=[tok], wadd=[tP])
        wcol = lambda i, jj: vecs[:, V_CONVW + j * 9 + i * 3 + jj:V_CONVW + j * 9 + i * 3 + jj + 1]
        bcol = vecs[:, V_CONVB + j:V_CONVB + j + 1]
        S.op("dve", lambda e: e.tensor_scalar(out=accb[:, 0:n_el], in0=Pbuf[:, Wd:Wd + n_el], scalar1=wcol(1, 1), scalar2=bcol, op0=ALU.mult, op1=ALU.add),
             reads=[tP, t_const], writes=[tacc])
        P3 = Pbuf[:, 0:(R + 2) * Wd].rearrange("p (r c) -> p r c", c=Wd)
        A3 = accb[:, 0:n_el].rearrange("p (r c) -> p r c", c=Wd)
        for i in taps_i:
            for jj in range(3):
                if i == 1 and jj == 1:
                    continue
                oc0, oc1 = (1, Wd) if jj == 0 else ((0, Wd) if jj == 1 else (0, Wd - 1))
                ic0 = oc0 + jj - 1
                S.op("dve", lambda e, i=i, jj=jj, oc0=oc0, oc1=oc1, ic0=ic0: e.scalar_tensor_tensor(
                    out=A3[:, :, oc0:oc1], in0=P3[:, i:i + R, ic0:ic0 + (oc1 - oc0)], scalar=wcol(i, jj), in1=A3[:, :, oc0:oc1], op0=ALU.mult, op1=ALU.add),
                    reads=[tP, t_const, tacc], writes=[tacc])
        S.op("act", lambda e: e.activation(out=eb[:, 0:n_el], in_=accb[:, 0:n_el], func=AF.Exp, scale=-1.0), reads=[tacc], writes=[teb])
        S.op("dve", lambda e: e.tensor_scalar(out=eb[:, 0:n_el], in0=eb[:, 0:n_el], scalar1=1.0, scalar2=None, op0=ALU.add), reads=[teb], writes=[teb])
        S.op("dve", lambda e: e.reciprocal(out=eb[:, 0:n_el], in_=eb[:, 0:n_el]), reads=[teb], writes=[teb])
        sc = 1.0 if j < 4 else 128.0 ** -0.5
        S.op("dve", lambda e: e.scalar_tensor_tensor(out=outb[:, 0:n_el], in0=accb[:, 0:n_el], scalar=sc, in1=eb[:, 0:n_el], op0=ALU.mult, op1=ALU.mult),
             reads=[tacc, teb], writes=[toutb])
        for (ap, tok, off, n) in dsts:
            S.dma("sp", ap, outb[:, off:off + n], reads=[toutb], wadd=[tok])

    Ppad = sb("convPp", [128, 2 + 66 * 66], BF16)
    cstg = sb("convstg", [128, 64 * 64], BF16)
    accp = sb("convaccp", [128, 64 * 66], F32)
    ebp = sb("convebp", [128, 64 * 66], F32)
    dw_ring = Ring([sb("convdw%d" % i, [128, 9, 128], BF16) for i in range(2)])
    tPp, tcstg, taccp, tebp = Tok(), Tok(), Tok(), Tok()
    last_R = [None]

    def conv_tile_pe(j, R, srcs, dsts):
        n_el, npad = R * 64, R * 66
        first = True
        for (ap, tok, off, n) in srcs:
            S.dma("sp", cstg[:, off:off + n], ap, reads=[tok], **(dict(writes=[tcstg]) if first else dict(wadd=[tcstg])))
            first = False
        if last_R[0] != R:
            S.op("dve", lambda e: e.memset(Ppad[:], 0.0), writes=[tPp])
            last_R[0] = R
        P3 = Ppad[:, 1:1 + (R + 2) * 66].rearrange("p (r c) -> p r c", c=66)
        S.op("dve", lambda e: e.tensor_copy(out=P3[:, 1:R + 1, 1:65], in_=cstg[:, 0:n_el].rearrange("p (r c) -> p r c", c=64)),
             reads=[tcstg], writes=[tPp])
        dw, dwtok = dw_ring.next()
        for t in range(9):
            S.op("dve", lambda e, t=t: e.tensor_scalar(out=dw[:, t, :], in0=ident_b, scalar1=vecs[:, V_CONVW + j * 9 + t:V_CONVW + j * 9 + t + 1], scalar2=None, op0=ALU.mult),
                 reads=[t_cb, t_const], **(dict(writes=[dwtok]) if t == 0 else dict(wadd=[dwtok])))
        bcol = vecs[:, V_CONVB + j:V_CONVB + j + 1]
        firstc = True
        for q0 in range(0, npad, 512):
            N = min(512, npad - q0)
            pt, ptok = psA_ring.next()
            for t in range(9):
                i, jj = t // 3, t % 3
                o = q0 + i * 66 + jj
                S.op("pe", lambda e, pt=pt, t=t, o=o, N=N: e.matmul(pt[:, 0:N], dw[:, t, :], Ppad[:, o:o + N], start=(t == 0), stop=(t == 8)),
                     reads=[dwtok, tPp], writes=[ptok])
            S.op("dve", lambda e, pt=pt, q0=q0, N=N: e.tensor_scalar(out=accp[:, q0:q0 + N], in0=pt[:, 0:N], scalar1=bcol, scalar2=None, op0=ALU.add),
                 reads=[ptok, t_const], **(dict(writes=[taccp]) if firstc else dict(wadd=[taccp])))
            firstc = False
        S.op("act", lambda e: e.activation(out=ebp[:, 0:npad], in_=accp[:, 0:npad], func=AF.Silu), reads=[taccp], writes=[tebp])
        sc = 1.0 if j < 4 else 128.0 ** -0.5
        E3 = ebp[:, 0:npad].rearrange("p (r c) -> p r c", c=66)[:, :, 1:65]
        S.op("dve", lambda e: e.tensor_scalar(out=outb[:, 0:n_el].rearrange("p (r c) -> p r c", c=64), in0=E3, scalar1=sc, scalar2=None, op0=ALU.mult),
             reads=[tebp], writes=[toutb])
        for (ap, tok, off, n) in dsts:
            S.dma("sp", ap, outb[:, off:off + n], reads=[toutb], wadd=[tok])

    for j in range(8):
        isq = j < 4
        nb = 4 if isq else 8
        R = 33 if isq else 64
        srcs = [(mpre_d[1 + b][:, j * 512:(j + 1) * 512], t_mpre[1 + b], b * 512, 512) for b in range(nb)]
        if isq:
            srcs.append((mpre_d[5][:, j * 512:j * 512 + 64], t_mpre[5], 2048, 64))
        dsts = [(mqk_d[1 + b][:, j * 512:(j + 1) * 512], t_mqk[1 + b], b * 512, 512) for b in range(nb)]
        conv_tile_pe(j, R, srcs, dsts)
    for j in range(4, 8):
        conv_tile(j, 1, 256, [(mpre_d[0][:, j * 512:j * 512 + 256], t_mpre[0], 0, 256)],
                  [(mqk_d[0][:, j * 512:j * 512 + 256], t_mqk[0], 0, 256)], (1,))
    release(n_s3)
    if dbg == 4:
        S.finish(t_mqk + dump_toks)
        nc.used_inputs = used_inputs
        return nc

    mixT_d = dscr("mixT_s", [4, 128, 8 * 512], BF16)
    t_mix = [Tok() for _ in range(4)]

    def finalize(OT, t_OT, gate_d, t_gate, gain_col0, koff):
        sq_ring = Ring([sb("fsq%d" % i, [128, 512], BF16) for i in range(2)])
        ms_ring = Ring([sb("fms%d" % i, [128, 512], F32) for i in range(2)])
        y_ring = Ring([sb("fy%d" % i, [128, 512], F32) for i in range(2)])
        gate_ring = Ring([sb("fgate%d" % i, [128, 4 * 512], BF16) for i in range(2)])
        mst_ring = Ring([sb("fmst%d" % i, [128, 4 * 512], BF16) for i in range(2)])
        for lb in range(4):
            gt, gttok = gate_ring.next()
            S.dma("sp", gt[:], gate_d[lb], reads=[t_gate[lb]], writes=[gttok])
            mst, msttok = mst_ring.next()
            for h in range(4):
                O = OT[:, h, lb * 512:(lb + 1) * 512]
                sq, sqtok = sq_ring.next()
                S.op("dve", lambda e, sq=sq, O=O: e.tensor_tensor(out=sq[:], in0=O, in1=O, op=ALU.mult), reads=[t_OT], writes=[sqtok])
                pt, ptok = psA_ring.next()
                S.op("pe", lambda e, pt=pt, sq=sq: e.matmul(pt[:, :], ones_b, sq[:], start=True, stop=True), reads=[sqtok, t_cb], writes=[ptok])
                ms, mstok = ms_ring.next()
                S.op("dve", lambda e, ms=ms, pt=pt: e.tensor_scalar(out=ms[:], in0=pt[:, :], scalar1=1.0 / 128, scalar2=EPS, op0=ALU.mult, op1=ALU.add),
                     reads=[ptok], writes=[mstok])
                S.op("act", lambda e, ms=ms: e.activation(out=ms[:], in_=ms[:], func=AF.Ln), reads=[mstok], writes=[mstok])
                S.op("act", lambda e, ms=ms: e.activation(out=ms[:], in_=ms[:], func=AF.Exp, scale=-0.5), reads=[mstok], writes=[mstok])
                y, ytok = y_ring.next()
                S.op("dve", lambda e, y=y, O=O, ms=ms, h=h: e.scalar_tensor_tensor(
                    out=y[:], in0=O, scalar=vecs[:, gain_col0 + h:gain_col0 + h + 1], in1=ms[:], op0=ALU.mult, op1=ALU.mult),
                    reads=[t_OT, mstok, t_const], writes=[ytok])
                S.op("dve", lambda e, y=y, gt=gt, mst=mst, h=h: e.tensor_tensor(
                    out=mst[:, h * 512:(h + 1) * 512], in0=y[:], in1=gt[:, h * 512:(h + 1) * 512], op=ALU.mult),
                    reads=[ytok, gttok], **(dict(writes=[msttok]) if h == 0 else dict(wadd=[msttok])))
            S.dma("sp", mixT_d[lb][:, koff * 512:(koff + 4) * 512], mst[:], reads=[msttok], wadd=[t_mix[lb]])

    n_s4 = len(es)
    OT = sb("OT", [128, 4, NLOC], F32)
    t_OT = Tok()
    upw_sb = sb("upw", [128, 2, 256], F32)
    t_upw = Tok()
    S.op("dve", lambda e: e.memset(upw_sb[:], 0.0), writes=[t_upw])
    S.dma("sp", upw_sb[0:16], upw_d.rearrange("d r n -> r d n"), writes=[t_upw])
    bmask = consts[:, 512:768]
    nub = sb("nub", [128, 4], F32)
    t_nub = Tok()
    S.op("dve", lambda e: e.tensor_scalar(out=nub[:], in0=vecs[:, V_UPB:V_UPB + 4], scalar1=-1.0, scalar2=None, op0=ALU.mult), reads=[t_const], writes=[t_nub])
    m2x = sb("m2x", [128, 2, 256], BF16)
    t_m2x = Tok()
    for d_ in range(2):
        for hh in range(2):
            S.op("dve", lambda e, d_=d_, hh=hh: e.tensor_copy(out=m2x[:, d_, hh * 128:(hh + 1) * 128], in_=mask_b[d_]),
                 reads=[t_cb], wadd=[t_m2x])
    Tst = [[sb("T%d%d" % (p, d_), [128, 256], F32) for d_ in range(2)] for p in range(2)]
    Sbt = [[sb("Sb%d%d" % (p, d_), [128, 256], BF16) for d_ in range(2)] for p in range(2)]
    ecol = [[sb("ec%d%d" % (p, d_), [128, 1], F32) for d_ in range(2)] for p in range(2)]
    t_T = [[Tok() for _ in range(2)] for _ in range(2)]
    t_Sb = [[Tok() for _ in range(2)] for _ in range(2)]
    t_ec = [[Tok() for _ in range(2)] for _ in range(2)]
    for p in range(2):
        for d_ in range(2):
            S.op("dve", lambda e, p=p, d_=d_: e.memset(Tst[p][d_][:], 0.0), writes=[t_T[p][d_]])
            S.op("dve", lambda e, p=p, d_=d_: e.memset(Sbt[p][d_][:], 0.0), writes=[t_Sb[p][d_]])
            S.op("dve", lambda e, p=p, d_=d_: e.memset(ecol[p][d_][:], 1.0), writes=[t_ec[p][d_]])
    gq_ring = Ring([sb("gqkb%d" % i, [128, 4 * 512], BF16) for i in range(2)])
    lr_ring = Ring([sb("lrb%d" % i, [128, 2 * 512], F32) for i in range(2)])
    for i_ in range(2):
        S.op("dve", lambda e, i_=i_: e.memset(lr_ring.items[i_][:], 0.0), writes=[lr_ring.toks[i_]])
    gv_ring = Ring([sb("gvb%d" % i, [128, 4 * 512], BF16) for i in range(2)])
    L_ring = Ring([sb("gL%d" % i, [128, 512], F32) for i in range(2)])
    C_ring = Ring([sb("gC%d" % i, [128, 512], F32) for i in range(2)])
    C2_ring = Ring([sb("gC2%d" % i, [128, 512], F32) for i in range(2)])
    eb_ring = Ring([sb("geb%d" % i, [128, 512], F32) for i in range(4)])
    enb_ring = Ring([sb("genb%d" % i, [128, 512], F32) for i in range(2)])
    qt_ring = Ring([sb("gqt%d" % i, [128, 512], BF16) for i in range(4)])
    kt_ring = Ring([sb("gkt%d" % i, [128, 512], BF16) for i in range(4)])
    kth_ring = Ring([sb("gkth%d" % i, [128, 512], BF16) for i in range(8)])
    ktok_ring = Ring([sb("gktok%d" % i, [128, 128], BF16) for i in range(4)])
    attm_ring = Ring([sb("gattm%d" % i, [128, 256], BF16) for i in range(4)])
    o_written = [False] * NBLK

    def gla_block(blk, d, full):
        N = 256 if blk == 0 else 512
        nch = N // 128
        gq, gqtok = gq_ring.next()
        S.dma("sp", gq[:], gqk_d[blk], reads=[t_gqk[blk]], writes=[gqtok])
        lrb, lrtok = lr_ring.next()
        S.dma("sp", lrb[0:16], lr_d[blk], reads=[t_lr[blk]], writes=[lrtok])
        gvb, gvtok = gv_ring.next()
        S.dma("sp", gvb[:], gv_d[blk], reads=[t_gv[blk]], writes=[gvtok])
        prep = []
        for p in range(2):
            pt, ptok = psA_ring.next()
            S.op("pe", lambda e, pt=pt, p=p: e.matmul(pt[:, 0:N], upw_sb[:, d, p * 128:(p + 1) * 128], lrb[:, d * 512:d * 512 + N], start=True, stop=True),
                 reads=[t_upw, lrtok], writes=[ptok])
            L, Ltok = L_ring.next()
            S.op("act", lambda e, pt=pt, L=L, p=p: e.activation(out=L[:, 0:N], in_=pt[:, 0:N], func=AF.Exp, scale=-1.0, bias=nub[:, d * 2 + p:d * 2 + p + 1]),
                 reads=[ptok, t_nub], writes=[Ltok])
            S.op("act", lambda e, L=L: e.activation(out=L[:, 0:N], in_=L[:, 0:N], func=AF.Ln, bias=ones_f[:, 0:1]), reads=[Ltok, t_const], writes=[Ltok])
            Cm, Ctok = C_ring.next()
            for c in range(nch):
                S.op("dve", lambda e, Cm=Cm, L=L, c=c: e.tensor_tensor_scan(
                    out=Cm[:, c * 128:(c + 1) * 128], data0=ones_f[:, 0:128], data1=L[:, c * 128:(c + 1) * 128], initial=0.0, op0=ALU.mult, op1=ALU.add),
                    reads=[Ltok, t_const], **(dict(writes=[Ctok]) if c == 0 else dict(wadd=[Ctok])))
            if d == 1:
                C2, C2tok = C2_ring.next()
                for c in range(nch):
                    S.op("dve", lambda e, Cm=Cm, C2=C2, c=c: e.tensor_scalar(
                        out=C2[:, c * 128:(c + 1) * 128], in0=Cm[:, c * 128:(c + 1) * 128], scalar1=-1.0, scalar2=Cm[:, c * 128 + 127:c * 128 + 128], op0=ALU.mult, op1=ALU.add),
                        reads=[Ctok], **(dict(writes=[C2tok]) if c == 0 else dict(wadd=[C2tok])))
                S.op("dve", lambda e, C2=C2, L=L: e.tensor_tensor(out=C2[:, 0:N], in0=C2[:, 0:N], in1=L[:, 0:N], op=ALU.add), reads=[C2tok, Ltok], writes=[C2tok])
                Cm, Ctok = C2, C2tok
            eb_, ebtok = eb_ring.next()
            S.op("act", lambda e, eb_=eb_, Cm=Cm: e.activation(out=eb_[:, 0:N], in_=Cm[:, 0:N], func=AF.Exp, scale=-1.0 / 16), reads=[Ctok], writes=[ebtok])
            enb, enbtok = enb_ring.next()
            S.op("act", lambda e, enb=enb, Cm=Cm: e.activation(out=enb[:, 0:N], in_=Cm[:, 0:N], func=AF.Exp, scale=1.0 / 16), reads=[Ctok], writes=[enbtok])
            kt, kttok = kt_ring.next()
            S.op("dve", lambda e, kt=kt, enb=enb, p=p: e.tensor_tensor(out=kt[:, 0:N], in0=gq[:, (2 + p) * 512:(2 + p) * 512 + N], in1=enb[:, 0:N], op=ALU.mult),
                 reads=[gqtok, enbtok], writes=[kttok])
            qt, qttok = None, None
            kth = [None, None]
            if full:
                for h in range(2):
                    kh, khtok = kth_ring.next()
                    S.op("dve", lambda e, kh=kh, enb=enb, p=p, h=h: e.scalar_tensor_tensor(
                        out=kh[:, 0:N], in0=gq[:, (2 + p) * 512:(2 + p) * 512 + N], scalar=consts[:, 768 + h:769 + h], in1=enb[:, 0:N], op0=ALU.mult, op1=ALU.mult),
                        reads=[gqtok, enbtok, t_const], writes=[khtok])
                    kth[h] = (kh, khtok)
                qt, qttok = qt_ring.next()
                S.op("dve", lambda e, qt=qt, eb_=eb_, p=p: e.tensor_tensor(out=qt[:, 0:N], in0=gq[:, p * 512:p * 512 + N], in1=eb_[:, 0:N], op=ALU.mult),
                     reads=[gqtok, ebtok], writes=[qttok])
            prep.append((eb_, ebtok, kt, kttok, qt, qttok, kth))
        order = list(range(nch)) if d == 0 else list(range(nch - 1, -1, -1))
        for c in order:
            cs = slice(c * 128, (c + 1) * 128)
            ecc = c * 128 + (127 if d == 0 else 0)
            st = [dict() for _ in range(2)]
            for p in range(2):
                eb_, ebtok, kt, kttok, qt, qttok, kth = prep[p]
                pb, pbtok = psB_ring.next()
                S.op("pe", lambda e, pb=pb, kt=kt: e.transpose(pb[:, 0:128], kt[:, cs], ident_b), reads=[kttok, t_cb], writes=[pbtok])
                st[p]["pb"] = (pb, pbtok)
                if full:
                    pa, patok = psA_ring.next()
                    for h in range(2):
                        S.op("pe", lambda e, pa=pa, h=h, kth=kth, qt=qt: e.matmul(pa[:, h * 128:(h + 1) * 128], kth[h][0][:, cs], qt[:, cs], start=True, stop=True),
                             reads=[kth[h][1], qttok], writes=[patok])
                    st[p]["pa"] = (pa, patok)
            for p in range(2):
                pb, pbtok = st[p]["pb"]
                ktk, ktktok = ktok_ring.next()
                S.op("dve", lambda e, ktk=ktk, pb=pb: e.tensor_copy(out=ktk[:], in_=pb[:, 0:128]), reads=[pbtok], writes=[ktktok])
                st[p]["ktk"] = (ktk, ktktok)
                if full:
                    pa, patok = st[p]["pa"]
                    am, amtok = attm_ring.next()
                    S.op("dve", lambda e, am=am, pa=pa: e.tensor_tensor(out=am[:], in0=pa[:, 0:256], in1=m2x[:, d, :], op=ALU.mult),
                         reads=[patok, t_m2x], writes=[amtok])
                    st[p]["am"] = (am, amtok)
            for p in range(2):
                ktk, ktktok = st[p]["ktk"]
                pd, pdtok = psA_ring.next()
                S.op("pe", lambda e, pd=pd, ktk=ktk, p=p: e.matmul(pd[:, 0:256], ktk[:], gvb[:, c * 512 + p * 256:c * 512 + (p + 1) * 256], start=True, stop=True),
                     reads=[ktktok, gvtok], writes=[pdtok])
                st[p]["pd"] = (pd, pdtok)
            if full:
                for p in range(2):
                    eb_, ebtok, kt, kttok, qt, qttok, kth = prep[p]
                    am, amtok = st[p]["am"]
                    po, potok = psA_ring.next()
                    for h in range(2):
                        hd = p * 2 + h
                        S.op("pe", lambda e, po=po, h=h, hd=hd, am=am: e.matmul(
                            po[:, h * 128:(h + 1) * 128], gvb[:, c * 512 + hd * 128:c * 512 + (hd + 1) * 128], am[:, h * 128:(h + 1) * 128], start=True, stop=False),
                            reads=[gvtok, amtok], writes=[potok])
                        S.op("pe", lambda e, po=po, h=h, qt=qt, p=p: e.matmul(
                            po[:, h * 128:(h + 1) * 128], Sbt[p][d][:, h * 128:(h + 1) * 128], qt[:, cs], start=False, stop=True),
                            reads=[t_Sb[p][d], qttok], writes=[potok])
                    st[p]["po"] = (po, potok)
            for p in range(2):
                eb_, ebtok = prep[p][0], prep[p][1]
                pd, pdtok = st[p]["pd"]
                S.op("dve", lambda e, pd=pd, p=p: e.scalar_tensor_tensor(
                    out=Tst[p][d][:], in0=Tst[p][d][:], scalar=ecol[p][d][:, 0:1], in1=pd[:, 0:256], op0=ALU.mult, op1=ALU.add),
                    reads=[pdtok, t_ec[p][d], t_T[p][d]], writes=[t_T[p][d]])
                S.op("dve", lambda e, p=p, eb_=eb_: e.scalar_tensor_tensor(
                    out=Sbt[p][d][:], in0=Tst[p][d][:], scalar=eb_[:, ecc:ecc + 1], in1=bmask, op0=ALU.mult, op1=ALU.mult),
                    reads=[t_T[p][d], ebtok, t_const], writes=[t_Sb[p][d]])
                S.op("dve", lambda e, p=p, eb_=eb_: e.tensor_copy(out=ecol[p][d][:], in_=eb_[:, ecc:ecc + 1]),
                     reads=[ebtok], writes=[t_ec[p][d]])
            if full:
                for p in range(2):
                    po, potok = st[p]["po"]
                    tok0 = (blk - 1) * 512 + c * 128
                    Odst = OT[:, p * 2:p * 2 + 2, tok0:tok0 + 128]
                    po3 = po[:, 0:256].rearrange("p (h t) -> p h t", h=2)
                    if not o_written[blk]:
                        S.op("dve", lambda e, Odst=Odst, po3=po3: e.tensor_copy(out=Odst, in_=po3), reads=[potok], wadd=[t_OT])
                    else:
                        S.op("dve", lambda e, Odst=Odst, po3=po3: e.tensor_tensor(out=Odst, in0=po3, in1=Odst, op=ALU.add), reads=[potok, t_OT], wadd=[t_OT])
        if full:
            o_written[blk] = True

    s4m = int(os.environ.get("S4MODE", 9))
    if s4m == 10:
        gla_block(8, 1, False)
    elif s4m == 11:
        gla_block(0, 0, False)
    else:
        gla_block(0, 1, False)
    if 10 > s4m >= 1:
        for blk in (8, 7, 6, 5):
            gla_block(blk, 1, False)
        gla_block(0, 0, False)
    if 10 > s4m >= 2:
        for i in range(4 if s4m >= 3 else 1):
            gla_block(1 + i, 0, True)
            gla_block(4 - i, 1, True)
    if 10 > s4m >= 4:
        finalize(OT, t_OT, sgg_d, t_sgg, V_GNG, 0)
    dump("OTg", OT[:, 0, :], [128, NLOC], [t_OT])
    release(n_s4)
    if dbg == 5:
        S.finish(t_mix + dump_toks)
        nc.used_inputs = used_inputs
        return nc

    n_s5 = len(es)
    HT = sb("HT", [128, 4, NLOC], F32)
    t_HT = Tok()
    gateb4 = sb("gateb4", [128, 64], F32)
    t_gb4 = Tok()
    for c in range(4):
        S.dma("sp", gateb4[:, c * 16:(c + 1) * 16], rows_d[0:1, R_GATEB:R_GATEB + 16].partition_broadcast(128), wadd=[t_gb4])
    maskf = [consts[:, 128:256], consts[:, 256:384]]
    T4 = [sb("T4_%d" % d_, [128, 4, 256], F32) for d_ in range(2)]
    Sb4 = [sb("Sb4_%d" % d_, [128, 4, 256], BF16) for d_ in range(2)]
    t_T4 = [Tok() for _ in range(2)]
    t_Sb4 = [Tok() for _ in range(2)]
    ec_one = sb("econe", [128, 4], F32)
    t_econe = Tok()
    S.op("dve", lambda e: e.memset(ec_one[:], 1.0), writes=[t_econe])
    prev_ec = [(ec_one, t_econe), (ec_one, t_econe)]
    for d_ in range(2):
        S.op("dve", lambda e, d_=d_: e.memset(T4[d_][:], 0.0), writes=[t_T4[d_]])
        S.op("dve", lambda e, d_=d_: e.memset(Sb4[d_][:], 0.0), writes=[t_Sb4[d_]])
    mq_ring = Ring([sb("mqkb%d" % i, [128, 8 * 512], BF16) for i in range(2)])
    mvb_ring = Ring([sb("mvb%d" % i, [128, 4 * 512], BF16) for i in range(2)])
    ga_ring = Ring([sb("gab%d" % i, [128, 64], F32) for i in range(2)])
    gbb_ring = Ring([sb("gbb%d" % i, [128, 64], F32) for i in range(2)])
    Lf_ring = Ring([sb("Lf%d" % i, [128, 16], F32) for i in range(2)])
    es_ring = Ring([sb("es%d" % i, [128, 16], F32) for i in range(2)])
    lfbc_ring = Ring([sb("lfbc%d" % i, [128, 4, 128], F32) for i in range(2)])
    flo_ring = Ring([sb("flo%d" % i, [128, 4, 128], F32) for i in range(2)])
    ecn_ring = Ring([sb("ecn%d" % i, [128, 4], F32) for i in range(12)])
    vext_ring = Ring([sb("vext%d" % i, [128, 4, 256], BF16) for i in range(2)])
    mktok_ring = Ring([sb("mktok%d" % i, [128, 512], BF16) for i in range(2)])
    mam_ring = Ring([sb("mam%d" % i, [128, 512], BF16) for i in range(2)])
    mask4 = sb("mask4", [128, 2, 512], BF16)
    t_mask4 = Tok()
    for d_ in range(2):
        for hh in range(4):
            S.op("dve", lambda e, d_=d_, hh=hh: e.tensor_copy(out=mask4[:, d_, hh * 128:(hh + 1) * 128], in_=mask_b[d_]), reads=[t_cb], wadd=[t_mask4])
    tP = [[t_] * 4 for t_ in [Tok() for _ in range(6)]]
    dd_ring = Ring([sb("mdd%d" % i, [128, 4, 128], F32) for i in range(2)])
    ht_ring = Ring([sb("mht%d" % i, [128, 4, 128], F32) for i in range(2)])
    h_written = [False] * NBLK

    def ml_block(blk, d, full):
        N = 256 if blk == 0 else 512
        nch = N // 128
        mq, mqtok = mq_ring.next()
        S.dma("sp", mq[:], mqk_d[blk], reads=[t_mqk[blk]], writes=[mqtok])
        mvb, mvtok = mvb_ring.next()
        S.dma("sp", mvb[:], mv_d[blk], reads=[t_mv[blk]], writes=[mvtok])
        ga, gatok = ga_ring.next()
        S.dma("sp", ga[:], gates_d[blk], reads=[t_gates[blk]], writes=[gatok])
        gbb, gbtok = gbb_ring.next()
        S.op("dve", lambda e: e.tensor_tensor(out=gbb[:], in0=ga[:], in1=gateb4[:], op=ALU.add), reads=[gatok, t_gb4], writes=[gbtok])
        gb3 = gbb[:].rearrange("p (c g) -> p c g", g=16)
        Lf, Lftok = Lf_ring.next()
        Lf3 = Lf[:].rearrange("p (c h) -> p c h", h=4)
        S.op("act", lambda e: e.activation(out=Lf3[:, 0:nch, :], in_=gb3[:, 0:nch, 8 + d * 4:12 + d * 4], func=AF.Exp, scale=-1.0), reads=[gbtok], writes=[Lftok])
        S.op("dve", lambda e: e.tensor_scalar(out=Lf[:, 0:nch * 4], in0=Lf[:, 0:nch * 4], scalar1=1.0, scalar2=None, op0=ALU.add), reads=[Lftok], writes=[Lftok])
        S.op("act", lambda e: e.activation(out=Lf[:, 0:nch * 4], in_=Lf[:, 0:nch * 4], func=AF.Ln), reads=[Lftok], writes=[Lftok])
        pt, ptok = psA[0], tP[0][0]
        S.op("pe", lambda e: e.matmul(pt[:, 0:nch * 4], maskf[d], Lf[:, 0:nch * 4], start=True, stop=True), reads=[Lftok, t_const], writes=[ptok])
        es_, estok = es_ring.next()
        es3 = es_[:].rearrange("p (c h) -> p c h", h=4)
        pt3 = pt[:, 0:16].rearrange("p (c h) -> p c h", h=4)
        S.op("dve", lambda e: e.tensor_tensor(out=es3[:, 0:nch, :], in0=pt3[:, 0:nch, :], in1=gb3[:, 0:nch, d * 4:d * 4 + 4], op=ALU.add),
             reads=[ptok, gbtok], writes=[estok])
        S.op("act", lambda e: e.activation(out=es_[:, 0:nch * 4], in_=es_[:, 0:nch * 4], func=AF.Exp), reads=[estok], writes=[estok])
        order = list(range(nch)) if d == 0 else list(range(nch - 1, -1, -1))
        endcol = 127 if d == 0 else 0
        for c in order:
            lf4, lf4tok = lfbc_ring.next()
            S.op("dve", lambda e, lf4=lf4: e.tensor_copy(out=lf4[:], in_=Lf[:, c * 4:c * 4 + 4].unsqueeze(2).to_broadcast([128, 4, 128])),
                 reads=[Lftok], writes=[lf4tok])
            vx4, vx4tok = vext_ring.next()
            es_bc = es_[:, c * 4:c * 4 + 4].unsqueeze(2).to_broadcast([128, 4, 128])
            S.op("dve", lambda e, vx4=vx4, es_bc=es_bc: e.tensor_tensor(
                out=vx4[:, :, 0:128], in0=mvb[:, c * 512:(c + 1) * 512].rearrange("p (h n) -> p h n", h=4), in1=es_bc, op=ALU.mult),
                reads=[mvtok, estok], writes=[vx4tok])
            S.op("dve", lambda e, vx4=vx4, es_bc=es_bc: e.tensor_copy(out=vx4[:, :, 128:256], in_=es_bc), reads=[estok], wadd=[vx4tok])
            kTs = [mq[:, (4 + h) * 512 + c * 128:(4 + h) * 512 + (c + 1) * 128] for h in range(4)]
            qTs = [mq[:, h * 512 + c * 128:h * 512 + (c + 1) * 128] for h in range(4)]
            pb, pbtok = psB_ring.next()
            for h in range(4):
                S.op("pe", lambda e, h=h, lf4=lf4: e.matmul(psA[0][:, h * 128:(h + 1) * 128], lf4[:, h, :], maskf[d], start=True, stop=True),
                     reads=[lf4tok, t_const], writes=[tP[0][0]])
                S.op("pe", lambda e, h=h, pb=pb: e.transpose(pb[:, h * 128:(h + 1) * 128], kTs[h], ident_b), reads=[mqtok, t_cb], writes=[pbtok])
            ecn4, ecn4tok = ecn_ring.next()
            S.op("act", lambda e, ecn4=ecn4: e.activation(
                out=ecn4[:, 0:4].unsqueeze(2), in_=psA[0][:, :].rearrange("p (h n) -> p h n", h=4)[:, :, endcol:endcol + 1], func=AF.Exp, scale=-1.0),
                reads=[tP[0][0]], writes=[ecn4tok])
            flo4, flo4tok = None, None
            if full:
                flo4, flo4tok = flo_ring.next()
                S.op("act", lambda e, flo4=flo4: e.activation(out=flo4[:].rearrange("p h n -> p (h n)"), in_=psA[0][:, :], func=AF.Exp), reads=[tP[0][0]], writes=[flo4tok])
            ktk4, ktk4tok = mktok_ring.next()
            S.op("dve", lambda e, ktk4=ktk4, pb=pb: e.tensor_copy(out=ktk4[:], in_=pb[:, 0:512]), reads=[pbtok], writes=[ktk4tok])
            if full:
                for h in range(4):
                    S.op("pe", lambda e, h=h: e.matmul(psA[1][:, h * 128:(h + 1) * 128], kTs[h], qTs[h], start=True, stop=True), reads=[mqtok], writes=[tP[1][0]])
                am4, am4tok = mam_ring.next()
                S.op("dve", lambda e, am4=am4: e.tensor_tensor(out=am4[:], in0=psA[1][:, :], in1=mask4[:, d, :], op=ALU.mult),
                     reads=[tP[1][0], t_mask4], writes=[am4tok])
                for h in range(4):
                    bk, o0 = 2 + h // 2, (h % 2) * 256
                    for half in range(2):
                        hc = slice(half * 128, (half + 1) * 128)
                        oc = slice(o0 + half * 128, o0 + (half + 1) * 128)
                        S.op("pe", lambda e, h=h, bk=bk, oc=oc, hc=hc, am4=am4, vx4=vx4: e.matmul(psA[bk][:, oc], vx4[:, h, hc], am4[:, h * 128:(h + 1) * 128], start=True, stop=False),
                             reads=[vx4tok, am4tok], writes=[tP[bk][0]])
                        S.op("pe", lambda e, h=h, bk=bk, oc=oc, hc=hc: e.matmul(psA[bk][:, oc], Sb4[d][:, h, hc], qTs[h], start=False, stop=True),
                             reads=[t_Sb4[d], mqtok], writes=[tP[bk][0]])
            for h in range(4):
                bk, o0 = 4 + h // 2, (h % 2) * 256
                S.op("pe", lambda e, h=h, bk=bk, o0=o0, ktk4=ktk4, vx4=vx4: e.matmul(psA[bk][:, o0:o0 + 256], ktk4[:, h * 128:(h + 1) * 128], vx4[:, h, :], start=True, stop=True),
                     reads=[ktk4tok, vx4tok], writes=[tP[bk][0]])
            pec, pectok = prev_ec[d]
            for h in range(4):
                bk, o0 = 4 + h // 2, (h % 2) * 256
                S.op("dve", lambda e, h=h, bk=bk, o0=o0, pec=pec: e.scalar_tensor_tensor(
                    out=T4[d][:, h, :], in0=T4[d][:, h, :], scalar=pec[:, h:h + 1], in1=psA[bk][:, o0:o0 + 256], op0=ALU.mult, op1=ALU.add),
                    reads=[tP[bk][0], pectok, t_T4[d]], writes=[t_T4[d]])
            S.op("dve", lambda e, ecn4=ecn4: e.tensor_tensor(out=Sb4[d][:], in0=T4[d][:], in1=ecn4[:, 0:4].unsqueeze(2).to_broadcast([128, 4, 256]), op=ALU.mult),
                 reads=[t_T4[d], ecn4tok], writes=[t_Sb4[d]])
            prev_ec[d] = (ecn4, ecn4tok)
            if full:
                dd4, dd4tok = dd_ring.next()
                for bk in (2, 3):
                    hs2 = slice((bk - 2) * 2, (bk - 2) * 2 + 2)
                    den = psA[bk][:, :].rearrange("p (h x n) -> p h x n", h=2, x=2)[:, :, 1, :]
                    S.op("dve", lambda e, dd4=dd4, den=den, hs2=hs2, flo4=flo4: e.scalar_tensor_tensor(
                        out=dd4[:, hs2, :], in0=den, scalar=-1.0, in1=flo4[:, hs2, :], op0=ALU.mult, op1=ALU.max),
                        reads=[tP[bk][0], flo4tok], **(dict(writes=[dd4tok]) if bk == 2 else dict(wadd=[dd4tok])))
                for bk in (2, 3):
                    hs2 = slice((bk - 2) * 2, (bk - 2) * 2 + 2)
                    den = psA[bk][:, :].rearrange("p (h x n) -> p h x n", h=2, x=2)[:, :, 1, :]
                    S.op("dve", lambda e, dd4=dd4, den=den, hs2=hs2: e.tensor_tensor(out=dd4[:, hs2, :], in0=den, in1=dd4[:, hs2, :], op=ALU.max),
                         reads=[tP[bk][0], dd4tok], wadd=[dd4tok])
                S.op("dve", lambda e, dd4=dd4: e.reciprocal(out=dd4[:], in_=dd4[:]), reads=[dd4tok], writes=[dd4tok])
                tok0 = (blk - 1) * 512 + c * 128
                if not h_written[blk]:
                    for bk in (2, 3):
                        hs2 = slice((bk - 2) * 2, (bk - 2) * 2 + 2)
                        num = psA[bk][:, :].rearrange("p (h x n) -> p h x n", h=2, x=2)[:, :, 0, :]
                        S.op("dve", lambda e, num=num, hs2=hs2, dd4=dd4: e.tensor_tensor(out=HT[:, hs2, tok0:tok0 + 128], in0=num, in1=dd4[:, hs2, :], op=ALU.mult),
                             reads=[tP[bk][0], dd4tok], wadd=[t_HT])
                else:
                    ht4, ht4tok = ht_ring.next()
                    for bk in (2, 3):
                        hs2 = slice((bk - 2) * 2, (bk - 2) * 2 + 2)
                        num = psA[bk][:, :].rearrange("p (h x n) -> p h x n", h=2, x=2)[:, :, 0, :]
                        S.op("dve", lambda e, num=num, hs2=hs2, dd4=dd4, ht4=ht4: e.tensor_tensor(out=ht4[:, hs2, :], in0=num, in1=dd4[:, hs2, :], op=ALU.mult),
                             reads=[tP[bk][0], dd4tok], **(dict(writes=[ht4tok]) if bk == 2 else dict(wadd=[ht4tok])))
                    S.op("dve", lambda e, ht4=ht4: e.tensor_tensor(out=HT[:, :, tok0:tok0 + 128], in0=HT[:, :, tok0:tok0 + 128], in1=ht4[:], op=ALU.add),
                         reads=[ht4tok, t_HT], wadd=[t_HT])
        if full:
            h_written[blk] = True

    s5m = int(os.environ.get("S5MODE", 9))
    ml_block(0, 1, False)
    if s5m >= 1:
        for blk in (8, 7, 6, 5):
            ml_block(blk, 1, False)
        ml_block(0, 0, False)
    if s5m >= 2:
        for i in range(4 if s5m >= 3 else 1):
            ml_block(1 + i, 0, True)
            ml_block(4 - i, 1, True)
    S.barrier()
    if s5m >= 4:
        finalize(HT, t_HT, smo_d, t_smo, V_MNG, 4)
    dump("HTm", HT[:, 0, :], [128, NLOC], [t_HT])
    release(n_s5)
    if dbg == 6:
        S.finish(t_mix + dump_toks)
        nc.used_inputs = used_inputs
        return nc

    n_s6 = len(es)
    x1_d = dscr("x1_s", [16, 128, D], F32)
    t_x1 = Tok()
    wout_v = wout_d.rearrange("(k p) n -> p k n", p=128)
    woutb = sb("woutb", [128, 8, D], BF16)
    t_wout = Tok()
    wo_stg = Ring([sb("wostg%d" % i, [128, 8, 256], F32) for i in range(2)])
    for pc in range(4):
        st, sttok = wo_stg.next()
        S.dma("sp", st[:], wout_v[:, :, pc * 256:(pc + 1) * 256], writes=[sttok])
        S.op("dve", lambda e, st=st, pc=pc: e.tensor_copy(out=woutb[:, :, pc * 256:(pc + 1) * 256], in_=st[:]), reads=[sttok], wadd=[t_wout])
    rw_sb = sb("rw", [128, 8, 36], F32)
    t_rw = Tok()
    S.dma("sp", rw_sb[:], rw_d.rearrange("(k p) n -> p k n", p=128), writes=[t_rw])
    rb_bc = sb("rbbc", [128, 36], F32)
    S.dma("sp", rb_bc[:], rows_d[0:1, R_RB:R_RB + 36].partition_broadcast(128), wadd=[t_rw])
    mixb_ring = Ring([sb("mixb%d" % i, [128, 8 * 512], BF16) for i in range(2)])
    xt6_ring = Ring([sb("x6t%d" % i, [128, D], F32) for i in range(2)])
    x1_ring = Ring([sb("x1t%d" % i, [128, D], F32) for i in range(2)])
    tmp6 = sb("tmp6", [128, D], F32)
    t_tmp6 = Tok()
    jk6 = sb("jk6", [128, D], F32)
    t_jk6 = Tok()
    s6_ring = Ring([(sb("s6a%d" % i, [128, 1], F32), sb("s6b%d" % i, [128, 1], F32)) for i in range(2)])
    h2s_ring = Ring([sb("h2s%d" % i, [128, D], F32) for i in range(2)])
    h2Tf_ring = Ring([sb("h2Tf%d" % i, [128, 8, 128], F32) for i in range(2)])
    Sel = sb("Sel", [128, 16, 2, 32], F32)
    t_Sel = Tok()
    h2tok = sb("h2tok", [128, 16, D], BF16)
    t_h2tok = Tok()
    LG = sb("LGall", [128, 16, 36], F32)
    t_LG = Tok()
    mixb, mixbtok = None, None
    for ti in range(16):
        lb, tt = ti // 4, ti % 4
        if tt == 0:
            mixb, mixbtok = mixb_ring.next()
            S.dma("sp", mixb[:], mixT_d[lb], reads=[t_mix[lb]], writes=[mixbtok])
        xt, xttok = xt6_ring.next()
        S.dma("sp", xt[:], x_d[ti * 128:(ti + 1) * 128, :], writes=[xttok])
        x1, x1tok = x1_ring.next()
        for half in range(2):
            po, potok = psA_ring.next()
            for k in range(8):
                S.op("pe", lambda e, po=po, k=k, half=half, mixb=mixb, tt=tt: e.matmul(
                    po[:, :], mixb[:, k * 512 + tt * 128:k * 512 + (tt + 1) * 128], woutb[:, k, half * 512:(half + 1) * 512], start=(k == 0), stop=(k == 7)),
                    reads=[mixbtok, t_wout], writes=[potok])
            hs_ = slice(half * 512, (half + 1) * 512)
            S.op("dve", lambda e, po=po, hs_=hs_: e.tensor_tensor(out=tmp6[:, hs_], in0=po[:, :], in1=g12[:, hs_], op=ALU.mult),
                 reads=[potok, t_g12], **(dict(writes=[t_tmp6]) if half == 0 else dict(wadd=[t_tmp6])))
            S.op("dve", lambda e, x1=x1, xt=xt, hs_=hs_: e.tensor_tensor(out=x1[:, hs_], in0=tmp6[:, hs_], in1=xt[:, hs_], op=ALU.add),
                 reads=[t_tmp6, xttok], **(dict(writes=[x1tok]) if half == 0 else dict(wadd=[x1tok])))
        S.dma("sp", x1_d[ti], x1[:], reads=[x1tok], wadd=[t_x1])
        ss, sstok = s6_ring.next()
        S.op("act", lambda e, x1=x1, ss=ss: e.activation(out=jk6[:], in_=x1[:], func=AF.Square, accum_out=ss[0][:, 0:1]), reads=[x1tok], writes=[t_jk6, sstok])
        S.op("dve", lambda e, ss=ss: e.tensor_scalar(out=ss[1][:, 0:1], in0=ss[0][:, 0:1], scalar1=1.0 / D, scalar2=EPS, op0=ALU.mult, op1=ALU.add), reads=[sstok], writes=[sstok])
        S.op("act", lambda e, ss=ss: e.activation(out=ss[0][:, 0:1], in_=ss[1][:, 0:1], func=AF.Ln), reads=[sstok], writes=[sstok])
        S.op("act", lambda e, ss=ss: e.activation(out=ss[1][:, 0:1], in_=ss[0][:, 0:1], func=AF.Exp, scale=-0.5), reads=[sstok], writes=[sstok])
        h2s, h2stok = h2s_ring.next()
        S.op("dve", lambda e, h2s=h2s, x1=x1, ss=ss: e.tensor_scalar(out=h2s[:], in0=x1[:], scalar1=ss[1][:, 0:1], scalar2=None, op0=ALU.mult),
             reads=[x1tok, sstok], writes=[h2stok])
        h2Tf, h2Tftok = h2Tf_ring.next()
        for g in range(2):
            pT, pTtok = psA_ring.next()
            for kk in range(4):
                k = g * 4 + kk
                S.op("pe", lambda e, pT=pT, kk=kk, k=k, h2s=h2s: e.transpose(pT[:, kk * 128:(kk + 1) * 128], h2s[:, k * 128:(k + 1) * 128], ident_f),
                     reads=[h2stok, t_const], writes=[pTtok])
            for kk in range(4):
                k = g * 4 + kk
                S.op("dve", lambda e, pT=pT, kk=kk, k=k, h2Tf=h2Tf: e.tensor_scalar(
                    out=h2Tf[:, k, :], in0=pT[:, kk * 128:(kk + 1) * 128], scalar1=A2[:, k:k + 1], scalar2=A2[:, 8 + k:9 + k], op0=ALU.mult, op1=ALU.add),
                    reads=[pTtok, t_A], **(dict(writes=[h2Tftok]) if k == 0 else dict(wadd=[h2Tftok])))
        pr, prtok = psA_ring.next()
        for k in range(8):
            S.op("pe", lambda e, pr=pr, k=k, h2Tf=h2Tf: e.matmul(pr[:, 0:36], h2Tf[:, k, :], rw_sb[:, k, :], start=(k == 0), stop=(k == 7)),
                 reads=[h2Tftok, t_rw], writes=[prtok])
        S.op("dve", lambda e, pr=pr, ti=ti: e.tensor_tensor(out=LG[:, ti, :], in0=pr[:, 0:36], in1=rb_bc[:], op=ALU.add), reads=[prtok, t_rw], wadd=[t_LG])
        S.op("dve", lambda e, h2s=h2s: e.tensor_tensor(out=tmp6[:], in0=h2s[:], in1=g12[:, 2 * D:3 * D], op=ALU.mult), reads=[h2stok, t_g12, t_tmp6], writes=[t_tmp6])
        S.op("dve", lambda e, ti=ti: e.tensor_tensor(out=h2tok[:, ti, :], in0=tmp6[:], in1=g12[:, 3 * D:4 * D], op=ALU.add), reads=[t_tmp6, t_g12], wadd=[t_h2tok])
    RT = sb("RTb", [128, 16 * 80], F32)
    t_RT = Tok()

    def V(c0, n):
        return RT[:, c0:c0 + 16 * n].rearrange("p (t n) -> p t n", n=n)

    def bc(ap2, n):
        return ap2.unsqueeze(2).to_broadcast([128, 16, n])
    G = LG[:, :, 0:4]
    E4 = LG[:, :, 4:36].rearrange("p t (g i) -> p t g i", g=4)
    gmax, gs, gw = RT[:, 0:16], RT[:, 16:32], RT[:, 32:48]
    m1, m2, w1, w2, w1g, w2g = RT[:, 48:64], RT[:, 64:80], RT[:, 80:96], RT[:, 96:112], RT[:, 112:128], RT[:, 128:144]
    goh, gex = V(144, 4), V(208, 4)
    eg, eq1, eg2, eq2, tmp8 = V(272, 8), V(400, 8), V(528, 8), V(656, 8), V(784, 8)

    def rop(fn, eng="dve", extra=()):
        S.op(eng, fn, reads=[t_RT, t_LG] + list(extra), writes=[t_RT])
    S.op("dve", lambda e: e.tensor_reduce(out=gmax, in_=G, axis=AX.X, op=ALU.max), reads=[t_LG], writes=[t_RT])
    rop(lambda e: e.tensor_tensor(out=goh, in0=G, in1=bc(gmax, 4), op=ALU.is_equal))
    rop(lambda e: e.tensor_tensor(out=gex, in0=G, in1=bc(gmax, 4), op=ALU.subtract))
    rop(lambda e: e.activation(out=RT[:, 208:272], in_=RT[:, 208:272], func=AF.Exp), eng="act")
    rop(lambda e: e.tensor_reduce(out=gs, in_=gex, axis=AX.X, op=ALU.add))
    rop(lambda e: e.reciprocal(out=gw, in_=gs))
    rop(lambda e: e.tensor_tensor(out=eg, in0=E4[:, :, 0, :], in1=bc(goh[:, :, 0], 8), op=ALU.mult))
    for g in range(1, 4):
        rop(lambda e, g=g: e.tensor_tensor(out=tmp8, in0=E4[:, :, g, :], in1=bc(goh[:, :, g], 8), op=ALU.mult))
        rop(lambda e: e.tensor_tensor(out=eg, in0=eg, in1=tmp8, op=ALU.add))
    rop(lambda e: e.tensor_reduce(out=m1, in_=eg, axis=AX.X, op=ALU.max))
    rop(lambda e: e.tensor_tensor(out=eq1, in0=eg, in1=bc(m1, 8), op=ALU.is_equal))
    rop(lambda e: e.scalar_tensor_tensor(out=RT[:, 528:656], in0=RT[:, 400:528], scalar=-1e30, in1=RT[:, 272:400], op0=ALU.mult, op1=ALU.add))
    rop(lambda e: e.tensor_reduce(out=m2, in_=eg2, axis=AX.X, op=ALU.max))
    rop(lambda e: e.tensor_tensor(out=eq2, in0=eg2, in1=bc(m2, 8), op=ALU.is_equal))
    rop(lambda e: e.tensor_tensor(out=w1, in0=m2, in1=m1, op=ALU.subtract))
    rop(lambda e: e.activation(out=w1, in_=w1, func=AF.Exp), eng="act")
    rop(lambda e: e.tensor_scalar(out=w1, in0=w1, scalar1=1.0, scalar2=None, op0=ALU.add))
    rop(lambda e: e.reciprocal(out=w1, in_=w1))
    rop(lambda e: e.tensor_scalar(out=w2, in0=w1, scalar1=-1.0, scalar2=1.0, op0=ALU.mult, op1=ALU.add))
    rop(lambda e: e.tensor_tensor(out=w1g, in0=w1, in1=gw, op=ALU.mult))
    rop(lambda e: e.tensor_tensor(out=w2g, in0=w2, in1=gw, op=ALU.mult))
    for g in range(4):
        S.op("dve", lambda e, g=g: e.tensor_tensor(out=Sel[:, :, 0, g * 8:(g + 1) * 8], in0=eq1, in1=bc(goh[:, :, g], 8), op=ALU.mult), reads=[t_RT], wadd=[t_Sel])
        S.op("dve", lambda e, g=g: e.tensor_tensor(out=Sel[:, :, 1, g * 8:(g + 1) * 8], in0=eq2, in1=bc(goh[:, :, g], 8), op=ALU.mult), reads=[t_RT], wadd=[t_Sel])
    Wt3 = Wt[:].rearrange("p (t k) -> p t k", k=2)
    S.op("dve", lambda e: e.tensor_copy(out=Wt3[:, :, 0], in_=w1g), reads=[t_RT], wadd=[t_Wt])
    S.op("dve", lambda e: e.tensor_copy(out=Wt3[:, :, 1], in_=w2g), reads=[t_RT], wadd=[t_Wt])
    Wselb = sb("Wselb", [128, 16, 32], BF16)
    t_wsel = Tok()
    S.op("dve", lambda e: e.tensor_tensor(out=Wselb[:], in0=Sel[:, :, 0, :], in1=Sel[:, :, 1, :], op=ALU.add), reads=[t_Sel], writes=[t_wsel])
    stri_b = sb("strib", [128, 128], BF16)
    t_stri = Tok()
    S.op("dve", lambda e: e.tensor_tensor(out=stri_b[:], in0=mask_b[0], in1=ident_b, op=ALU.subtract), reads=[t_cb], writes=[t_stri])
    rs = sb("rsm", [128, 512], F32)
    t_rs = Tok()
    cntf, nbf, padded, pad_end, pad_start = rs[:, 0:32], rs[:, 32:64], rs[:, 64:96], rs[:, 96:128], rs[:, 128:160]
    bef, be1024, be512 = rs[:, 192:256], rs[:, 256:320], rs[:, 320:384]
    pc, pctok = psA_ring.next()
    for ti in range(16):
        S.op("pe", lambda e, ti=ti: e.matmul(pc[:, 0:32], ones_b, Wselb[:, ti, :], start=(ti == 0), stop=(ti == 15)), reads=[t_wsel, t_cb], writes=[pctok])
    S.op("dve", lambda e: e.tensor_copy(out=cntf, in_=pc[:, 0:32]), reads=[pctok], writes=[t_rs])
    S.op("dve", lambda e: e.memset(nbf, 0.0), reads=[t_rs], writes=[t_rs])
    for j in range(16):
        S.op("dve", lambda e, j=j: e.scalar_tensor_tensor(out=nbf, in0=cntf, scalar=128.0 * j, in1=nbf, op0=ALU.is_gt, op1=ALU.add), reads=[t_rs], writes=[t_rs])
    S.op("dve", lambda e: e.tensor_scalar(out=padded, in0=nbf, scalar1=128.0, scalar2=None, op0=ALU.mult), reads=[t_rs], writes=[t_rs])
    S.op("dve", lambda e: e.tensor_tensor_scan(out=pad_end, data0=ones_f[:, 0:32], data1=padded, initial=0.0, op0=ALU.mult, op1=ALU.add), reads=[t_rs, t_const], writes=[t_rs])
    S.op("dve", lambda e: e.tensor_tensor(out=pad_start, in0=pad_end, in1=padded, op=ALU.subtract), reads=[t_rs], writes=[t_rs])
    DestF = sb("DestF", [128, 32], F32)
    t_destf = Tok()
    dt_ring = Ring([sb("dtt%d" % i, [128, 64], F32) for i in range(2)])
    for ti in range(16):
        pC, pCtok = psA_ring.next()
        for t2 in range(ti):
            S.op("pe", lambda e, pC=pC, t2=t2: e.matmul(pC[:, 0:32], ones_b, Wselb[:, t2, :], start=(t2 == 0), stop=False), reads=[t_wsel, t_cb], writes=[pCtok])
        S.op("pe", lambda e, pC=pC, ti=ti: e.matmul(pC[:, 0:32], stri_b[:], Wselb[:, ti, :], start=(ti == 0), stop=True), reads=[t_wsel, t_stri], writes=[pCtok])
        dtt, dtok = dt_ring.next()
        S.op("dve", lambda e, pC=pC, dtt=dtt: e.tensor_tensor(out=dtt[:, 0:32], in0=pC[:, 0:32], in1=pad_start, op=ALU.add), reads=[pCtok, t_rs], writes=[dtok])
        for k in range(2):
            S.op("dve", lambda e, dtt=dtt, ti=ti, k=k: e.tensor_tensor(out=dtt[:, 32:64], in0=dtt[:, 0:32], in1=Sel[:, ti, k, :], op=ALU.mult), reads=[dtok, t_Sel], writes=[dtok])
            S.op("dve", lambda e, dtt=dtt, ti=ti, k=k: e.tensor_reduce(out=DestF[:, ti * 2 + k:ti * 2 + k + 1], in_=dtt[:, 32:64], axis=AX.X, op=ALU.add),
                 reads=[dtok], wadd=[t_destf])
    S.op("dve", lambda e: e.tensor_copy(out=Desti[:], in_=DestF[:]), reads=[t_destf], writes=[t_dest])
    buf_d = dscr("moebuf_s", [8192, D], BF16)
    ybuf_d = dscr("moey_s", [8192, D], F32)
    t_buf = Tok()
    for ti in range(16):
        for k in range(2):
            S.idma(buf_d[:, :], bass.IndirectOffsetOnAxis(ap=Desti[:, ti * 2 + k:ti * 2 + k + 1], axis=0), h2tok[:, ti, :], None,
                   reads=[t_h2tok, t_dest], wadd=[t_buf])
    S.op("dve", lambda e: e.memset(bef, 0.0), reads=[t_rs], writes=[t_rs])
    TH = consts[:, 782:846]
    for ex in range(32):
        S.op("dve", lambda e, ex=ex: e.scalar_tensor_tensor(out=bef, in0=TH, scalar=rs[:, 96 + ex:97 + ex], in1=bef, op0=ALU.is_ge, op1=ALU.add), reads=[t_rs, t_const], writes=[t_rs])
    S.op("dve", lambda e: e.tensor_scalar(out=bef, in0=bef, scalar1=31.0, scalar2=None, op0=ALU.min), reads=[t_rs], writes=[t_rs])
    S.op("dve", lambda e: e.tensor_scalar(out=be1024, in0=bef, scalar1=1024.0, scalar2=None, op0=ALU.mult), reads=[t_rs], writes=[t_rs])
    S.op("dve", lambda e: e.tensor_scalar(out=be512, in0=bef, scalar1=512.0, scalar2=None, op0=ALU.mult), reads=[t_rs], writes=[t_rs])
    S.op("dve", lambda e: e.tensor_scalar(out=rs[:, 384:448], in0=TH, scalar1=rs[:, 127:128], scalar2=None, op0=ALU.is_ge), reads=[t_rs, t_const], writes=[t_rs])
    S.op("dve", lambda e: e.scalar_tensor_tensor(out=be1024, in0=rs[:, 384:448], scalar=1.0e6, in1=be1024, op0=ALU.mult, op1=ALU.add), reads=[t_rs], writes=[t_rs])
    S.op("dve", lambda e: e.scalar_tensor_tensor(out=be512, in0=rs[:, 384:448], scalar=1.0e6, in1=be512, op0=ALU.mult, op1=ALU.add), reads=[t_rs], writes=[t_rs])
    idxf = sb("idxf", [128, 64 * 12], F32)
    t_idxf = Tok()
    for b in range(64):
        S.op("dve", lambda e, b=b: e.tensor_scalar(out=idxf[:, b * 12:b * 12 + 8], in0=consts[:, 770:778], scalar1=rs[:, 256 + b:257 + b], scalar2=None, op0=ALU.add),
             reads=[t_rs, t_const], wadd=[t_idxf])
        S.op("dve", lambda e, b=b: e.tensor_scalar(out=idxf[:, b * 12 + 8:b * 12 + 12], in0=consts[:, 770:774], scalar1=rs[:, 320 + b:321 + b], scalar2=None, op0=ALU.add),
             reads=[t_rs, t_const], wadd=[t_idxf])
    S.op("dve", lambda e: e.tensor_copy(out=idxi[:], in_=idxf[:]), reads=[t_idxf], writes=[t_idx])
    dump("DestF", DestF[:], [128, 32], [t_destf])
    dump("rs", rs[:], [128, 512], [t_rs])
    release(n_s6)
    if dbg == 7:
        S.finish([t_x1, t_buf, t_idx] + dump_toks)
        nc.used_inputs = used_inputs
        return nc

    n_s7 = len(es)
    bc_reg = nc.gpsimd.to_reg(NE * D - 1)
    ewi_rows = ewi_d.rearrange("e r n -> (e r) n")
    ewo_rows = ewo_d.rearrange("e r n -> (e r) n")
    wib_ring = Ring([sb("wib%d" % i, [128, 8, 2 * DEXP], BF16) for i in range(2)])
    wob_ring = Ring([sb("wob%d" % i, [128, 4, D], BF16) for i in range(2)])
    xb_ring = Ring([sb("xbr%d" % i, [128, D], BF16) for i in range(2)])
    xbT_ring = Ring([sb("xbT%d" % i, [128, 8, 128], BF16) for i in range(2)])
    sil_ring = Ring([sb("sil%d" % i, [128, 512], F32) for i in range(2)])
    hT_ring = Ring([sb("hT%d" % i, [128, 4, 128], BF16) for i in range(2)])
    ysb_ring = Ring([sb("ysb%d" % i, [128, D], F32) for i in range(2)])
    t_ybuf = Tok()
    for b in range(int(os.environ.get("BLIM", 64))):
        wib, wibtok = wib_ring.next()
        wob, wobtok = wob_ring.next()
        for k in range(8):
            S.idma(wib[:, k, :], None, ewi_rows[:, :], bass.IndirectOffsetOnAxis(ap=idxi[:, b * 12 + k:b * 12 + k + 1], axis=0),
                   reads=[t_idx], bounds_check=bc_reg, oob_is_err=False, **(dict(writes=[wibtok]) if k == 0 else dict(wadd=[wibtok])))
        for j in range(4):
            S.idma(wob[:, j, :], None, ewo_rows[:, :], bass.IndirectOffsetOnAxis(ap=idxi[:, b * 12 + 8 + j:b * 12 + 9 + j], axis=0),
                   reads=[t_idx], bounds_check=bc_reg, oob_is_err=False, **(dict(writes=[wobtok]) if j == 0 else dict(wadd=[wobtok])))
        xb, xbtok = xb_ring.next()
        S.dma("sp", xb[:], buf_d[b * 128:(b + 1) * 128, :], reads=[t_buf], writes=[xbtok])
        pb, pbtok = psB_ring.next()
        for k in range(8):
            S.op("pe", lambda e, pb=pb, xb=xb, k=k: e.transpose(pb[:, k * 128:(k + 1) * 128], xb[:, k * 128:(k + 1) * 128], ident_b), reads=[xbtok, t_cb], writes=[pbtok])
        xbT, xbTtok = xbT_ring.next()
        S.op("dve", lambda e, xbT=xbT, pb=pb: e.tensor_copy(out=xbT[:].rearrange("p k n -> p (k n)"), in_=pb[:, :]), reads=[pbtok], writes=[xbTtok])
        pg, pgtok = psA_ring.next()
        pu, putok = psA_ring.next()
        for (pp, pptok, c0) in ((pg, pgtok, 0), (pu, putok, DEXP)):
            for j in range(4):
                for k in range(8):
                    S.op("pe", lambda e, pp=pp, j=j, k=k, c0=c0, wib=wib, xbT=xbT: e.matmul(
                        pp[:, j * 128:(j + 1) * 128], wib[:, k, c0 + j * 128:c0 + (j + 1) * 128], xbT[:, k, :], start=(k == 0), stop=(k == 7)),
                        reads=[wibtok, xbTtok], writes=[pptok])
        sil, siltok = sil_ring.next()
        S.op("act", lambda e, sil=sil, pg=pg: e.activation(out=sil[:], in_=pg[:, :], func=AF.Silu), reads=[pgtok], writes=[siltok])
        hT, hTtok = hT_ring.next()
        S.op("dve", lambda e, hT=hT, sil=sil, pu=pu: e.tensor_tensor(out=hT[:].rearrange("p j n -> p (j n)"), in0=pu[:, :], in1=sil[:], op=ALU.mult),
             reads=[putok, siltok], writes=[hTtok])
        ysb, ysbtok = ysb_ring.next()
        for half in range(2):
            py, pytok = psA_ring.next()
            for j in range(4):
                S.op("pe", lambda e, py=py, j=j, hT=hT, wob=wob, half=half: e.matmul(
                    py[:, :], hT[:, j, :], wob[:, j, half * 512:(half + 1) * 512], start=(j == 0), stop=(j == 3)), reads=[hTtok, wobtok], writes=[pytok])
            S.op("dve", lambda e, py=py, ysb=ysb, half=half: e.tensor_copy(out=ysb[:, half * 512:(half + 1) * 512], in_=py[:, :]),
                 reads=[pytok], **(dict(writes=[ysbtok]) if half == 0 else dict(wadd=[ysbtok])))
        S.dma("sp", ybuf_d[b * 128:(b + 1) * 128, :], ysb[:], reads=[ysbtok], wadd=[t_ybuf])
    release(n_s7)

    fng = sb("fng", [128, D], F32)
    t_fng = Tok()
    S.dma("sp", fng[:], rows_d[0:1, R_FNG:R_FNG + D].partition_broadcast(128), writes=[t_fng])
    fj = sb("fjunk", [128, D], F32)
    t_fj = Tok()
    fs_ring = Ring([(sb("fsa%d" % i, [128, 1], F32), sb("fsb%d" % i, [128, 1], F32)) for i in range(4)])
    y1_ring = Ring([sb("y1g%d" % i, [128, D], F32) for i in range(2)])
    y2_ring = Ring([sb("y2g%d" % i, [128, D], F32) for i in range(2)])
    x1l_ring = Ring([sb("x1l%d" % i, [128, D], F32) for i in range(2)])
    t_out = Tok()
    for ti in range(16):
        y1, y1tok = y1_ring.next()
        y2, y2tok = y2_ring.next()
        S.idma(y1[:, :], None, ybuf_d[:, :], bass.IndirectOffsetOnAxis(ap=Desti[:, ti * 2:ti * 2 + 1], axis=0), reads=[t_dest, t_ybuf], writes=[y1tok])
        S.idma(y2[:, :], None, ybuf_d[:, :], bass.IndirectOffsetOnAxis(ap=Desti[:, ti * 2 + 1:ti * 2 + 2], axis=0), reads=[t_dest, t_ybuf], writes=[y2tok])
        xl, xltok = x1l_ring.next()
        S.dma("sp", xl[:], x1_d[ti], reads=[t_x1], writes=[xltok])
        S.op("dve", lambda e, y1=y1, ti=ti: e.tensor_scalar(out=y1[:], in0=y1[:], scalar1=Wt[:, ti * 2:ti * 2 + 1], scalar2=None, op0=ALU.mult), reads=[y1tok, t_Wt], writes=[y1tok])
        S.op("dve", lambda e, y1=y1, y2=y2, ti=ti: e.scalar_tensor_tensor(out=y1[:], in0=y2[:], scalar=Wt[:, ti * 2 + 1:ti * 2 + 2], in1=y1[:], op0=ALU.mult, op1=ALU.add),
             reads=[y1tok, y2tok, t_Wt], writes=[y1tok])
        S.op("dve", lambda e, y1=y1: e.tensor_tensor(out=y1[:], in0=y1[:], in1=g12[:, D:2 * D], op=ALU.mult), reads=[y1tok, t_g12], writes=[y1tok])
        S.op("dve", lambda e, y1=y1, xl=xl: e.tensor_tensor(out=xl[:], in0=y1[:], in1=xl[:], op=ALU.add), reads=[y1tok, xltok], writes=[xltok])
        fs, fstok = fs_ring.next()
        S.op("act", lambda e, xl=xl, fs=fs: e.activation(out=fj[:], in_=xl[:], func=AF.Square, accum_out=fs[0][:, 0:1]), reads=[xltok], writes=[t_fj, fstok])
        S.op("dve", lambda e, fs=fs: e.tensor_scalar(out=fs[1][:, 0:1], in0=fs[0][:, 0:1], scalar1=1.0 / D, scalar2=EPS, op0=ALU.mult, op1=ALU.add), reads=[fstok], writes=[fstok])
        S.op("act", lambda e, fs=fs: e.activation(out=fs[0][:, 0:1], in_=fs[1][:, 0:1], func=AF.Ln), reads=[fstok], writes=[fstok])
        S.op("act", lambda e, fs=fs: e.activation(out=fs[1][:, 0:1], in_=fs[0][:, 0:1], func=AF.Exp, scale=-0.5), reads=[fstok], writes=[fstok])
        S.op("dve", lambda e, xl=xl, fs=fs: e.scalar_tensor_tensor(out=xl[:], in0=xl[:], scalar=fs[1][:, 0:1], in1=fng[:], op0=ALU.mult, op1=ALU.mult),
             reads=[fstok, t_fng, xltok], writes=[xltok])
        S.dma("sp", out_d[ti * 128:(ti + 1) * 128, :], xl[:], reads=[xltok], wadd=[t_out])
    S.finish([t_out] + dump_toks)
    nc.used_inputs = used_inputs
    return nc


def _host_inputs(inp):
    f = lambda a: np.ascontiguousarray(np.asarray(a, dtype=np.float32))
    x, c, ctx, c_ctx = f(inp["x"]), f(inp["c"]), f(inp["ctx"]), f(inp["c_ctx"])
    ada_w, ada_b = f(inp["ada_w"])[0], f(inp["ada_b"])[0]
    w_in = f(inp["w_in"])[0]
    up_w, up_b = f(inp["gla_up_w"])[0], f(inp["gla_up_b"])[0]
    conv_w, conv_b = f(inp["ml_conv_w"])[0], f(inp["ml_conv_b"])[0]
    i_b, f_b = f(inp["ml_i_b"])[0], f(inp["ml_f_b"])[0]
    consts = np.zeros((128, 1024), np.float32)
    consts[0:64, 512:640] = 1.0
    consts[64:128, 640:768] = 1.0
    consts[0:64, 768] = 1.0
    consts[64:128, 769] = 1.0
    consts[:, 770:782] = (np.arange(12)[None, :] % 8) * 128 + np.arange(128)[:, None]
    consts[:, 782:846] = np.arange(64)[None, :] * 128.0
    consts[:, 0:128] = np.eye(128)
    consts[:, 128:256] = np.triu(np.ones((128, 128)))
    consts[:, 256:384] = np.tril(np.ones((128, 128)))
    consts[:, 384:512] = 1.0
    router_w = np.concatenate([f(inp["router_group_w"])[0], f(inp["router_expert_w"])[0]], axis=1)
    shared = {
        "ada_w": ada_w, "ada_b": ada_b[None, :], "w_out": f(inp["w_out"])[0], "router_w": np.ascontiguousarray(router_w),
        "e_w_in": f(inp["expert_w_in"])[0], "e_w_out": f(inp["expert_w_out"])[0], "consts": consts,
    }
    maps = []
    for core in range(8):
        b, flip = core // 2, core % 2
        xs, cs = x[b], ctx[b]
        win, uw, ub, cw, ib, fb = w_in, up_w, up_b, conv_w, i_b, f_b
        if flip:
            xs, cs = xs[::-1], cs[::-1]
            win = win.copy()
            win[:, C_LR:C_LR + 16], win[:, C_LR + 16:C_LR + 32] = w_in[:, C_LR + 16:C_LR + 32], w_in[:, C_LR:C_LR + 16]
            win[:, C_MI:C_MI + 4], win[:, C_MI + 4:C_MI + 8] = w_in[:, C_MI + 4:C_MI + 8], w_in[:, C_MI:C_MI + 4]
            win[:, C_MI + 8:C_MI + 12], win[:, C_MI + 12:C_MI + 16] = w_in[:, C_MI + 12:C_MI + 16], w_in[:, C_MI + 8:C_MI + 12]
            uw, ub, ib, fb = uw[::-1], ub[::-1], ib[::-1], fb[::-1]
            cw = cw[::-1, ::-1]
        vecs = np.zeros((128, NV), np.float32)
        vecs[:, V_N1G:V_N1G + 8] = f(inp["norm1_g"])[0].reshape(8, 128).T
        vecs[:, V_N2G:V_N2G + 8] = f(inp["norm2_g"])[0].reshape(8, 128).T
        vecs[:, V_UPB:V_UPB + 4] = ub.reshape(2, 2, 128).transpose(2, 0, 1).reshape(128, 4)
        vecs[:, V_CONVB:V_CONVB + 8] = conv_b.reshape(8, 128).T
        vecs[:, V_CONVW:V_CONVW + 72] = cw.reshape(9, 8, 128).transpose(2, 1, 0).reshape(128, 72)
        vecs[:, V_GNG:V_GNG + 4] = f(inp["gla_norm_g"])[0].reshape(4, 128).T
        vecs[:, V_MNG:V_MNG + 4] = f(inp["ml_norm_g"])[0].reshape(4, 128).T
        rows = np.zeros((1, NR), np.float32)
        rows[0, R_GATEB:R_GATEB + 8] = ib.reshape(8)
        rows[0, R_GATEB + 8:R_GATEB + 16] = fb.reshape(8)
        rows[0, R_FNG:R_FNG + 1024] = f(inp["final_norm_g"])
        rows[0, R_RB:R_RB + 4] = f(inp["router_group_b"])[0]
        rows[0, R_RB + 4:R_RB + 36] = f(inp["router_expert_b"])[0]
        rows[0, R_N2G:R_N2G + 1024] = f(inp["norm2_g"])[0]
        cvec = np.concatenate([c[b].reshape(128, 8), c_ctx.reshape(128, 8)], axis=1)
        m = dict(shared)
        m.update({
            "x": np.ascontiguousarray(xs), "ctx": np.ascontiguousarray(cs), "cvec": np.ascontiguousarray(cvec),
            "vecs": vecs, "rows": rows, "w_in": np.ascontiguousarray(win), "up_w": np.ascontiguousarray(uw),
        })
        maps.append(m)
    return maps


def kernel(**inputs):
    maps = _host_inputs(inputs)
    nc = build()
    maps = [{k: m[k] for k in nc.used_inputs} for m in maps]
    res = run_bass_kernel_spmd(nc, maps, core_ids=list(range(8)))
    out = np.zeros((4, SEQ, D), np.float32)
    for core in range(8):
        b, flip = core // 2, core % 2
        o = res.results[core]["out"]
        if flip:
            out[b, NLOC:] = o[::-1]
        else:
            out[b, :NLOC] = o
    return out
```
